# Optimizing a Trainium2 kernel written in Bass

```python
import math
import jax, jax.numpy as jnp
from jax import lax
import numpy as np

D_MODEL = 2048
BATCH = 8
SEQ = 4096
DEPTH = 4

N_MIXERS = 4
N_A = (DEPTH + 3) // 4
N_B = (DEPTH + 2) // 4
N_C = (DEPTH + 1) // 4
N_D = DEPTH // 4
EPS = 1e-6

ATT_HEADS = 16
ATT_HEAD_DIM = 128
Q_LORA = 512
KV_LORA = 256
IDX_HEADS = 16
IDX_DIM = 64
TOPK_MAX = 256
Q_BLOCK = 128
A_IN = Q_LORA + KV_LORA + IDX_DIM + IDX_HEADS
ATT_SCALE = ATT_HEAD_DIM ** -0.5
REL_BUCKETS = 32
REL_MAX_DIST = 128
HG_HEADS = 16
HG_DK = D_MODEL // HG_HEADS
HG_DV = D_MODEL // HG_HEADS
HG_CHUNK = 64
POOL_WINDOWS = (2, 4, 8, 16)
POOL_GROUP = D_MODEL // len(POOL_WINDOWS)
CONV_WIDTH = 31
D_FF = 5632
FFN_CONV = 3

kernel_name = 'hybrid_dsa_hgrn2_pool_conformer_trunk'


def rmsnorm(x, g):
    xf = x.astype(jnp.float32)
    y = xf * lax.rsqrt(jnp.mean(xf * xf, axis=-1, keepdims=True) + EPS)
    return (y * g.astype(jnp.float32)).astype(x.dtype)


def layernorm(x, g, b):
    xf = x.astype(jnp.float32)
    mu = jnp.mean(xf, axis=-1, keepdims=True)
    xc = xf - mu
    y = xc * lax.rsqrt(jnp.mean(xc * xc, axis=-1, keepdims=True) + EPS)
    return (y * g.astype(jnp.float32) + b.astype(jnp.float32)).astype(x.dtype)


def causal_depthwise_conv(x, w, b):
    width = w.shape[0]
    y = lax.conv_general_dilated(
        x, w[:, None, :].astype(x.dtype), window_strides=(1,),
        padding=[(width - 1, 0)], dimension_numbers=('NWC', 'WIO', 'NWC'),
        feature_group_count=x.shape[-1])
    return y + b.astype(x.dtype)


def t5_bucket(dist):
    max_exact = REL_BUCKETS // 2
    n = jnp.maximum(dist, 0)
    nf = jnp.maximum(n, 1).astype(jnp.float32)
    large = max_exact + (jnp.log(nf / max_exact) / math.log(REL_MAX_DIST / max_exact)
                         * (REL_BUCKETS - max_exact)).astype(jnp.int32)
    large = jnp.minimum(large, REL_BUCKETS - 1)
    return jnp.where(n < max_exact, n, large)


def dsa_mixer(h, rel_bias, w_in, g_q, g_kv, w_uq, w_qidx, w_uk, w_uv, w_o):
    bsz, seq, _ = h.shape
    topk = min(TOPK_MAX, seq // 4)
    nb = seq // Q_BLOCK
    c_q, c_kv, k_idx, w_idx = jnp.split(
        h @ w_in, [Q_LORA, Q_LORA + KV_LORA, Q_LORA + KV_LORA + IDX_DIM], axis=-1)
    c_q = rmsnorm(c_q, g_q)
    c_kv = rmsnorm(c_kv, g_kv)
    q = (c_q @ w_uq).reshape(bsz, seq, ATT_HEADS, ATT_HEAD_DIM)
    q_lat = jnp.einsum('bshd,hdc->bshc', q, w_uk)
    q_idx = (c_q @ w_qidx).reshape(bsz, seq, IDX_HEADS, IDX_DIM)
    w_idx = w_idx * (IDX_HEADS ** -0.5)
    key_pos = jnp.arange(seq, dtype=jnp.int32)

    def to_blocks(t):
        return t.reshape((bsz, nb, Q_BLOCK) + t.shape[2:]).swapaxes(0, 1)

    def block(args):
        qb_lat, qb_idx, wb_idx, start = args
        q_pos = start + jnp.arange(Q_BLOCK, dtype=jnp.int32)
        causal = key_pos[None, :] <= q_pos[:, None]
        idx_logits = jnp.einsum('bqhd,bsd->bqhs', qb_idx, k_idx) * (IDX_DIM ** -0.5)
        score = jnp.einsum('bqhs,bqh->bqs', jax.nn.relu(idx_logits), wb_idx)
        score = jnp.where(causal[None], score.astype(jnp.float32), -jnp.inf)
        _, sel = lax.top_k(score, topk)
        valid = sel <= q_pos[None, :, None]
        kv_sel = jax.vmap(lambda c, i: c[i])(c_kv, sel)
        logits = jnp.einsum('bqhc,bqkc->bqhk', qb_lat, kv_sel).astype(jnp.float32) * ATT_SCALE
        bias = rel_bias[t5_bucket(q_pos[None, :, None] - sel)]
        logits = logits + jnp.moveaxis(bias, -1, 2).astype(jnp.float32)
        logits = jnp.where(valid[:, :, None, :], logits, -jnp.inf)
        p = jax.nn.softmax(logits, axis=-1).astype(kv_sel.dtype)
        return jnp.einsum('bqhk,bqkc->bqhc', p, kv_sel)

    starts = jnp.arange(nb, dtype=jnp.int32) * Q_BLOCK
    o_lat = lax.map(block, (to_blocks(q_lat), to_blocks(q_idx), to_blocks(w_idx), starts))
    o_lat = o_lat.swapaxes(0, 1).reshape(bsz, seq, ATT_HEADS, KV_LORA)
    o = jnp.einsum('bshc,hcd->bshd', o_lat, w_uv).reshape(bsz, seq, ATT_HEADS * ATT_HEAD_DIM)
    return o @ w_o


def hgrn2_mixer(h, w_in, lower_bound, g_norm, w_o):
    bsz, seq, _ = h.shape
    nc = seq // HG_CHUNK
    q, f, i_in, g = jnp.split(h @ w_in, 4, axis=-1)
    lb = lower_bound.astype(jnp.float32)
    f_gate = lb + (1.0 - lb) * jax.nn.sigmoid(f.astype(jnp.float32))
    log_f = jnp.log(f_gate)
    k = 1.0 - f_gate
    q = jax.nn.silu(q.astype(jnp.float32))
    v = i_in.astype(jnp.float32)

    def to_chunks(t, d):
        return t.reshape(bsz, nc, HG_CHUNK, HG_HEADS, d).transpose(1, 0, 3, 2, 4)

    mask = jnp.tril(jnp.ones((HG_CHUNK, HG_CHUNK), dtype=bool))[:, :, None]

    def step(state, inp):
        qc, kc, vc, gc = inp
        a = jnp.cumsum(gc, axis=2)
        diff = a[:, :, :, None, :] - a[:, :, None, :, :]
        decay = jnp.exp(jnp.where(mask, diff, -jnp.inf))
        scores = jnp.einsum('bhtc,bhsc,bhtsc->bhts', qc, kc, decay)
        a_last = a[:, :, -1:, :]
        out = (jnp.einsum('bhts,bhsv->bhtv', scores, vc)
               + jnp.einsum('bhtc,bhcv->bhtv', qc * jnp.exp(a), state))
        new_state = (jnp.exp(a_last[:, :, 0, :, None]) * state
                     + jnp.einsum('bhsc,bhsv->bhcv', kc * jnp.exp(a_last - a), vc))
        return new_state, out

    state0 = jnp.zeros((bsz, HG_HEADS, HG_DK, HG_DV), jnp.float32)
    _, o = lax.scan(step, state0, (to_chunks(q, HG_DK), to_chunks(k, HG_DK),
                                   to_chunks(v, HG_DV), to_chunks(log_f, HG_DK)))
    o = o.transpose(1, 0, 3, 2, 4).reshape(bsz, seq, HG_HEADS, HG_DV)
    o = o * lax.rsqrt(jnp.mean(o * o, axis=-1, keepdims=True) + EPS)
    o = o.reshape(bsz, seq, HG_HEADS * HG_DV) * g_norm.astype(jnp.float32)
    o = (o * jax.nn.silu(g.astype(jnp.float32))).astype(h.dtype)
    return o @ w_o


def pool_mixer(h, w_group, scale):
    bsz, seq, d = h.shape
    hf = h.astype(jnp.float32)
    cs = jnp.cumsum(hf, axis=1)
    cs_pad = jnp.concatenate([jnp.zeros((bsz, 1, d), jnp.float32), cs], axis=1)
    pos = jnp.arange(seq, dtype=jnp.int32)
    pooled = []
    for gi, win in enumerate(POOL_WINDOWS):
        sl = slice(gi * POOL_GROUP, (gi + 1) * POOL_GROUP)
        lo = jnp.maximum(pos + 1 - win, 0)
        window_sum = cs[:, :, sl] - cs_pad[:, lo, sl]
        count = jnp.minimum(pos + 1, win).astype(jnp.float32)
        pooled.append(window_sum / count[None, :, None])
    diff = jnp.concatenate(pooled, axis=-1) - hf
    y = jnp.einsum('bsgc,gcd->bsgd', diff.reshape(bsz, seq, len(POOL_WINDOWS), POOL_GROUP),
                   w_group.astype(jnp.float32)).reshape(bsz, seq, d)
    return (y * scale.astype(jnp.float32)).astype(h.dtype)


def conformer_conv(h, w_pw1, b_pw1, w_dw, b_dw, ln_g, ln_b, w_pw2, b_pw2):
    a, gate = jnp.split(h @ w_pw1 + b_pw1, 2, axis=-1)
    u = a * jax.nn.sigmoid(gate)
    u = causal_depthwise_conv(u, w_dw, b_dw)
    u = jax.nn.silu(layernorm(u, ln_g, ln_b))
    return u @ w_pw2 + b_pw2


def conv_ffn(h, w_up, w_conv, b_conv, w_down):
    u = causal_depthwise_conv(h @ w_up, w_conv, b_conv)
    a, b = jnp.split(u, 2, axis=-1)
    return (jax.nn.silu(a) * b) @ w_down


def setup_inputs(seed: int = 0) -> dict:
    key = jax.random.key(seed)
    ks = iter(jax.random.split(key, 64))
    D = D_MODEL

    def nrm(shape, scale):
        return jax.random.normal(next(ks), shape, jnp.float32) * scale

    def gain(shape):
        return 1.0 + nrm(shape, 0.02)

    return {
        'x': nrm((BATCH, SEQ, D), 1.0),
        'rel_bias': nrm((REL_BUCKETS, ATT_HEADS), 0.2),
        'a_w_in': nrm((N_A, D, A_IN), D ** -0.5),
        'a_g_q': gain((N_A, Q_LORA)),
        'a_g_kv': gain((N_A, KV_LORA)),
        'a_w_uq': nrm((N_A, Q_LORA, ATT_HEADS * ATT_HEAD_DIM), Q_LORA ** -0.5),
        'a_w_qidx': nrm((N_A, Q_LORA, IDX_HEADS * IDX_DIM), Q_LORA ** -0.5),
        'a_w_uk': nrm((N_A, ATT_HEADS, ATT_HEAD_DIM, KV_LORA), KV_LORA ** -0.5),
        'a_w_uv': nrm((N_A, ATT_HEADS, KV_LORA, ATT_HEAD_DIM), KV_LORA ** -0.5),
        'a_w_o': nrm((N_A, ATT_HEADS * ATT_HEAD_DIM, D), (ATT_HEADS * ATT_HEAD_DIM) ** -0.5),
        'b_w_in': nrm((N_B, D, 4 * D), D ** -0.5),
        'b_lower_bounds': nrm((DEPTH, HG_HEADS * HG_DK), 0.1),
        'b_g_norm': gain((N_B, HG_HEADS * HG_DV)),
        'b_w_o': nrm((N_B, HG_HEADS * HG_DV, D), D ** -0.5),
        'c_w_group': nrm((N_C, len(POOL_WINDOWS), POOL_GROUP, POOL_GROUP), POOL_GROUP ** -0.5),
        'c_scale': gain((N_C, D)),
        'd_w_pw1': nrm((N_D, D, 2 * D), D ** -0.5),
        'd_b_pw1': nrm((N_D, 2 * D), 0.02),
        'd_w_dw': nrm((N_D, CONV_WIDTH, D), CONV_WIDTH ** -0.5),
        'd_b_dw': nrm((N_D, D), 0.02),
        'd_ln_g': gain((N_D, D)),
        'd_ln_b': nrm((N_D, D), 0.02),
        'd_w_pw2': nrm((N_D, D, D), D ** -0.5),
        'd_b_pw2': nrm((N_D, D), 0.02),
        'norm_mix': gain((DEPTH, D)),
        'norm_ffn': gain((DEPTH, D)),
        'ffn_w_up': nrm((DEPTH, D, 2 * D_FF), D ** -0.5),
        'ffn_w_conv': nrm((DEPTH, FFN_CONV, 2 * D_FF), FFN_CONV ** -0.5),
        'ffn_b_conv': nrm((DEPTH, 2 * D_FF), 0.02),
        'ffn_w_down': nrm((DEPTH, D_FF, D), D_FF ** -0.5),
        'final_norm': gain((D,)),
    }


def reference(x, rel_bias, a_w_in, a_g_q, a_g_kv, a_w_uq, a_w_qidx, a_w_uk, a_w_uv, a_w_o,
              b_w_in, b_lower_bounds, b_g_norm, b_w_o, c_w_group, c_scale,
              d_w_pw1, d_b_pw1, d_w_dw, d_b_dw, d_ln_g, d_ln_b, d_w_pw2, d_b_pw2,
              norm_mix, norm_ffn, ffn_w_up, ffn_w_conv, ffn_b_conv, ffn_w_down, final_norm):
    lb_soft = jax.nn.softmax(b_lower_bounds.astype(jnp.float32), axis=0)
    lb_all = jnp.cumsum(lb_soft, axis=0) - lb_soft[0]
    h = x
    for i in range(DEPTH):
        j = i // N_MIXERS
        kind = i % N_MIXERS
        u = rmsnorm(h, norm_mix[i])
        if kind == 0:
            m = dsa_mixer(u, rel_bias, a_w_in[j], a_g_q[j], a_g_kv[j], a_w_uq[j], a_w_qidx[j],
                          a_w_uk[j], a_w_uv[j], a_w_o[j])
        elif kind == 1:
            m = hgrn2_mixer(u, b_w_in[j], lb_all[i], b_g_norm[j], b_w_o[j])
        elif kind == 2:
            m = pool_mixer(u, c_w_group[j], c_scale[j])
        else:
            m = conformer_conv(u, d_w_pw1[j], d_b_pw1[j], d_w_dw[j], d_b_dw[j], d_ln_g[j],
                               d_ln_b[j], d_w_pw2[j], d_b_pw2[j])
        h = h + m.astype(h.dtype)
        f = conv_ffn(rmsnorm(h, norm_ffn[i]), ffn_w_up[i], ffn_w_conv[i], ffn_b_conv[i], ffn_w_down[i])
        h = h + f.astype(h.dtype)
    return rmsnorm(h, final_norm)
```

```python
from contextlib import ExitStack
import math
import numpy as np
import concourse.bass as bass
import concourse.mybir as mybir
from concourse.bass_utils import run_bass_kernel_spmd

F32 = mybir.dt.float32
BF16 = mybir.dt.bfloat16
AF = mybir.ActivationFunctionType
ALU = mybir.AluOpType
AX = mybir.AxisListType

D = 2048
S = 4096
DC = D // 128
TT = 512
NT = S // TT
DFF = 5632
FC = DFF // 128
EPS = 1e-6
DEPTH = 4
NDMA_SEMS = 8


class Buf:
    __slots__ = ("name", "w", "wold", "r", "open", "excl")

    def __init__(self, name):
        self.name = name
        self.excl = False
        self.w = {}
        self.wold = {}
        self.r = {}
        self.open = False


def _mx(d, k, v):
    if d.get(k, 0) < v:
        d[k] = v


class Prog:
    STREAMS = ("pe", "act", "dve", "pool", "sp")

    def __init__(self, nc, stack):
        self.nc = nc
        self.ops = {s: [] for s in self.STREAMS}
        self.sems = {}
        self.known = {s: {} for s in self.STREAMS}
        self.cnt = {}
        for s in ("pe", "act", "dve", "pool"):
            self.sems[s] = stack.enter_context(nc.semaphore("c_" + s))
            self.cnt[s] = 0
        self.dma_sems = {}
        self.dma_n = {}
        for q in ("sp", "act", "pool"):
            self.dma_sems[q] = []
            for i in range(NDMA_SEMS):
                k = "d_%s%d" % (q, i)
                self.sems[k] = stack.enter_context(nc.semaphore(k))
                self.cnt[k] = 0
                self.dma_sems[q].append(k)
            self.dma_n[q] = 0
        self.nbuf = 0

    def buf(self, name=None):
        self.nbuf += 1
        return Buf(name or "b%d" % self.nbuf)

    def bufs(self, n, name=None):
        return [self.buf() for _ in range(n)]

    def pbuf(self):
        b = self.buf()
        b.excl = True
        return b

    def pbufs(self, n):
        return [self.pbuf() for _ in range(n)]

    def _deps(self, stream, reads, writes, pwrites, own_key):
        need = {}

        def add(k, v, same_ok):
            if k == own_key and same_ok:
                return
            _mx(need, k, v)

        for b in reads:
            for k, v in b.w.items():
                add(k, v, False)
            for k, v in b.wold.items():
                add(k, v, False)
        for b in writes:
            for dd in (b.w, b.wold, b.r):
                for k, v in dd.items():
                    add(k, v, True)
        for b in pwrites:
            if not b.open:
                for k, v in b.w.items():
                    _mx(b.wold, k, v)
                b.w = {}
                b.open = True
            for dd in (b.wold, b.r):
                for k, v in dd.items():
                    add(k, v, True)
        kn = self.known[stream]
        waits = []
        for k, v in need.items():
            if kn.get(k, 0) < v:
                kn[k] = v
                waits.append((k, v))
        return waits

    def _commit(self, tok, reads, writes, pwrites):
        k, v = tok
        for b in reads:
            _mx(b.r, k, v)
            b.open = False
        for b in writes:
            b.w = {k: v}
            b.wold = {}
            b.r = {}
            b.open = False
        for b in pwrites:
            _mx(b.w, k, v)

    def op(self, stream, fn, reads=(), writes=(), pwrites=()):
        if stream != "pe" and any(b.excl for b in reads):
            writes = list(writes) + [b for b in reads if b.excl]
            reads = [b for b in reads if not b.excl]
        waits = self._deps(stream, reads, writes, pwrites, stream)
        self.cnt[stream] += 1
        tok = (stream, self.cnt[stream])
        self.ops[stream].append((waits, fn, (stream, 1)))
        self._commit(tok, reads, writes, pwrites)
        return tok

    def dma(self, q, fn, reads=(), writes=(), pwrites=()):
        i = self.dma_n[q]
        self.dma_n[q] += 1
        key = self.dma_sems[q][i % NDMA_SEMS]
        waits = self._deps(q, reads, writes, pwrites, None)
        prev = self.cnt[key]
        kn = self.known[q]
        if prev > 0 and kn.get(key, 0) < prev:
            kn[key] = prev
            waits.append((key, prev))
        self.cnt[key] = prev + 16
        tok = (key, prev + 16)
        self.ops[q].append((waits, fn, (key, 16)))
        self._commit(tok, reads, writes, pwrites)
        return tok

    def barrier(self):
        cur = dict(self.cnt)
        for s in self.STREAMS:
            waits = []
            for k, v in cur.items():
                if v > 0 and self.known[s].get(k, 0) < v:
                    self.known[s][k] = v
                    waits.append((k, v))
            if waits:
                self.ops[s].append((waits, None, None))

    def emit(self):
        nc = self.nc
        sems = self.sems

        def run(stream):
            def body(eng):
                for waits, fn, inc in self.ops[stream]:
                    for k, v in waits:
                        eng.wait_ge(sems[k], v)
                    if fn is not None:
                        fn(eng).then_inc(sems[inc[0]], inc[1])
            return body

        with nc.Block() as block:
            block.tensor(run("pe"))
            block.scalar(run("act"))
            block.vector(run("dve"))
            block.gpsimd(run("pool"))
            block.sync(run("sp"))


class VecReg:
    def __init__(self):
        self.off = {}
        self.n = 0

    def add(self, name, ncols):
        self.off[name] = self.n
        self.n += ncols


def vec_registry():
    R = VecReg()
    for i in range(DEPTH):
        R.add("norm_mix%d" % i, DC)
        R.add("norm_ffn%d" % i, DC)
        R.add("ffn_b_conv%d" % i, 2 * FC)
        for k in range(3):
            R.add("ffn_w_conv%d_%d" % (i, k), 2 * FC)
    R.add("final_norm", DC)
    R.add("c_scale", DC)
    R.add("d_b_pw1", 2 * DC)
    for k in range(31):
        R.add("d_w_dw%d" % k, DC)
    R.add("d_b_dw", DC)
    R.add("d_ln_g", DC)
    R.add("d_ln_b", DC)
    R.add("d_b_pw2", DC)
    R.add("b_g_norm", DC)
    for i in range(DEPTH):
        R.add("b_lb%d" % i, DC)
    return R


def _cols(v):
    v = np.ascontiguousarray(v, dtype=np.float32).reshape(-1, 128)
    return v.T


def pack_vecs(inp):
    R = vec_registry()
    out = np.zeros((128, R.n), np.float32)

    def put(name, v):
        c = _cols(v)
        out[:, R.off[name]:R.off[name] + c.shape[1]] = c

    for i in range(DEPTH):
        put("norm_mix%d" % i, inp["norm_mix"][i])
        put("norm_ffn%d" % i, inp["norm_ffn"][i])
        put("ffn_b_conv%d" % i, inp["ffn_b_conv"][i])
        for k in range(3):
            put("ffn_w_conv%d_%d" % (i, k), inp["ffn_w_conv"][i, k])
        put("b_lb%d" % i, inp["b_lower_bounds"][i])
    put("final_norm", inp["final_norm"])
    put("c_scale", inp["c_scale"][0])
    put("d_b_pw1", inp["d_b_pw1"][0])
    for k in range(31):
        put("d_w_dw%d" % k, inp["d_w_dw"][0, k])
    put("d_b_dw", inp["d_b_dw"][0])
    put("d_ln_g", inp["d_ln_g"][0])
    put("d_ln_b", inp["d_ln_b"][0])
    put("d_b_pw2", inp["d_b_pw2"][0])
    put("b_g_norm", inp["b_g_norm"][0])
    return out


class K:
    pass


class NCProxy:
    def __init__(self, nc):
        self._nc = nc
        self.tag = 0

    def sbuf_tensor(self, name, *a, **kw):
        return self._nc.sbuf_tensor("%s_%d" % (name, self.tag), *a, **kw)

    def psum_tensor(self, name, *a, **kw):
        return self._nc.psum_tensor("%s_%d" % (name, self.tag), *a, **kw)

    def __getattr__(self, n):
        return getattr(self._nc, n)


def fm(ap2d, c0, nck, t0, nt):
    return ap2d[c0 * 128:(c0 + nck) * 128, t0:t0 + nt].rearrange("(c p) t -> p c t", p=128)


def phase_in(k):
    nc, P = k.nc, k.P
    with ExitStack() as st:
        xin = [st.enter_context(nc.sbuf_tensor("pi_x%d" % i, [128, D], F32)) for i in range(2)]
        xin_b = P.bufs(2)
        stg = [st.enter_context(nc.sbuf_tensor("pi_s%d" % i, [128, DC, TT], F32)) for i in range(2)]
        stg_b = P.bufs(2)
        ps = [st.enter_context(nc.psum_tensor("pi_p%d" % i, [128, 512], F32)) for i in range(4)]
        ps_b = P.pbufs(4)
        n = 0
        for tt in range(NT):
            sg, sgb = stg[tt % 2], stg_b[tt % 2]
            for sub in range(4):
                si = tt * 4 + sub
                xt, xb = xin[si % 2], xin_b[si % 2]
                P.dma("sp", lambda e, xt=xt, si=si: e.dma_start(out=xt[:], in_=k.x[si * 128:(si + 1) * 128, :]),
                      writes=[xb])
                for cg in range(4):
                    pt, pb = ps[n % 4], ps_b[n % 4]
                    for ci in range(4):
                        c = cg * 4 + ci
                        P.op("pe", lambda e, pt=pt, xt=xt, c=c, ci=ci: e.transpose(
                            out=pt[:, ci * 128:(ci + 1) * 128], in_=xt[:, c * 128:(c + 1) * 128],
                            identity=k.ident[:]), reads=[xb, k.ident_b], pwrites=[pb])
                    eng = "act" if n % 2 == 0 else "dve"
                    if eng == "act":
                        P.op("act", lambda e, pt=pt, sg=sg, cg=cg, sub=sub: e.activation(
                            out=sg[:, cg * 4:(cg + 1) * 4, sub * 128:(sub + 1) * 128],
                            in_=pt[:].rearrange("p (c s) -> p c s", c=4), func=AF.Copy),
                            reads=[pb], pwrites=[sgb])
                    else:
                        P.op("dve", lambda e, pt=pt, sg=sg, cg=cg, sub=sub: e.tensor_copy(
                            out=sg[:, cg * 4:(cg + 1) * 4, sub * 128:(sub + 1) * 128],
                            in_=pt[:].rearrange("p (c s) -> p c s", c=4)),
                            reads=[pb], pwrites=[sgb])
                    n += 1
            P.dma("sp", lambda e, sg=sg, tt=tt: e.dma_start(out=fm(k.hT, 0, DC, tt * TT, TT), in_=sg[:]),
                  reads=[sgb], writes=[k.hT_b[tt]])
    P.barrier()


def norm_tile(k, st_tiles, src_h, src_hb, gcol, out_tile, out_b, tag):
    nc, P = k.nc, k.P
    sq, sq_b, pss, pss_b, rb, rb_b = st_tiles
    for c in range(DC):
        P.op("act", lambda e, c=c: e.activation(out=sq[:, c, :], in_=src_h[:, c, :], func=AF.Square),
             reads=[src_hb], pwrites=[sq_b])
    for c in range(DC):
        P.op("pe", lambda e, c=c: e.matmul(pss[:], lhsT=k.ones_bf[:], rhs=sq[:, c, :],
                                           start=(c == 0), stop=(c == DC - 1)),
             reads=[sq_b, k.ones_b], pwrites=[pss_b])
    P.op("act", lambda e: e.activation(out=rb[:], in_=pss[:], func=AF.Sqrt, bias=k.eps_t[:, 0:1], scale=1.0 / D),
         reads=[pss_b, k.eps_b], writes=[rb_b])
    P.op("dve", lambda e: e.reciprocal(out=rb[:], in_=rb[:]), reads=[rb_b], writes=[rb_b])
    for c in range(DC):
        P.op("dve", lambda e, c=c: e.scalar_tensor_tensor(
            out=out_tile[:, c, :], in0=src_h[:, c, :], scalar=k.vecs[:, gcol + c:gcol + c + 1],
            in1=rb[:], op0=ALU.mult, op1=ALU.mult),
            reads=[src_hb, rb_b, k.vecs_b], pwrites=[out_b])


def phase_norm(k, gname):
    nc, P = k.nc, k.P
    gcol = k.R.off[gname]
    with ExitStack() as st:
        hin = [st.enter_context(nc.sbuf_tensor("pn_h%d" % i, [128, DC, TT], F32)) for i in range(2)]
        hin_b = P.bufs(2)
        sq = st.enter_context(nc.sbuf_tensor("pn_sq", [128, DC, TT], BF16))
        rb = st.enter_context(nc.sbuf_tensor("pn_rb", [128, TT], F32))
        xo = [st.enter_context(nc.sbuf_tensor("pn_o%d" % i, [128, DC, TT], BF16)) for i in range(2)]
        xo_b = P.bufs(2)
        pss = st.enter_context(nc.psum_tensor("pn_ps", [128, TT], F32))
        tiles = (sq, P.buf(), pss, P.pbuf(), rb, P.buf())
        for tt in range(NT):
            h, hb = hin[tt % 2], hin_b[tt % 2]
            o, ob = xo[tt % 2], xo_b[tt % 2]
            P.dma("sp", lambda e, h=h, tt=tt: e.dma_start(out=h[:], in_=fm(k.hT, 0, DC, tt * TT, TT)),
                  reads=[k.hT_b[tt]], writes=[hb])
            norm_tile(k, tiles, h, hb, gcol, o, ob, "pn")
            P.dma("sp", lambda e, o=o, tt=tt: e.dma_start(out=fm(k.xnT, 0, DC, tt * TT, TT), in_=o[:]),
                  reads=[ob], writes=[k.xnT_b[tt]])
    P.barrier()


def phase_out(k):
    nc, P = k.nc, k.P
    gcol = k.R.off["final_norm"]
    with ExitStack() as st:
        hin = [st.enter_context(nc.sbuf_tensor("po_h%d" % i, [128, DC, TT], F32)) for i in range(2)]
        hin_b = P.bufs(2)
        sq = st.enter_context(nc.sbuf_tensor("po_sq", [128, DC, TT], BF16))
        rb = st.enter_context(nc.sbuf_tensor("po_rb", [128, TT], F32))
        xo = st.enter_context(nc.sbuf_tensor("po_o", [128, DC, TT], F32))
        xo_b = P.buf()
        og = [st.enter_context(nc.sbuf_tensor("po_g%d" % i, [128, D], F32)) for i in range(2)]
        og_b = P.bufs(2)
        pss = st.enter_context(nc.psum_tensor("po_ps", [128, TT], F32))
        ps = [st.enter_context(nc.psum_tensor("po_p%d" % i, [128, 512], F32)) for i in range(4)]
        ps_b = P.pbufs(4)
        tiles = (sq, P.buf(), pss, P.pbuf(), rb, P.buf())
        n = 0
        toks = []
        for tt in range(NT):
            h, hb = hin[tt % 2], hin_b[tt % 2]
            P.dma("sp", lambda e, h=h, tt=tt: e.dma_start(out=h[:], in_=fm(k.hT, 0, DC, tt * TT, TT)),
                  reads=[k.hT_b[tt]], writes=[hb])
            if k.raw_out:
                src, srcb = h, hb
            else:
                norm_tile(k, tiles, h, hb, gcol, xo, xo_b, "po")
                src, srcb = xo, xo_b
            for sub in range(4):
                si = tt * 4 + sub
                o, ob = og[si % 2], og_b[si % 2]
                for cg in range(4):
                    pt, pb = ps[n % 4], ps_b[n % 4]
                    for ci in range(4):
                        c = cg * 4 + ci
                        P.op("pe", lambda e, pt=pt, src=src, c=c, ci=ci, sub=sub: e.transpose(
                            out=pt[:, ci * 128:(ci + 1) * 128], in_=src[:, c, sub * 128:(sub + 1) * 128],
                            identity=k.ident[:]), reads=[srcb, k.ident_b], pwrites=[pb])
                    if n % 2 == 0:
                        P.op("act", lambda e, pt=pt, o=o, cg=cg: e.activation(
                            out=o[:, cg * 512:(cg + 1) * 512], in_=pt[:], func=AF.Copy),
                            reads=[pb], pwrites=[ob])
                    else:
                        P.op("dve", lambda e, pt=pt, o=o, cg=cg: e.tensor_copy(
                            out=o[:, cg * 512:(cg + 1) * 512], in_=pt[:]),
                            reads=[pb], pwrites=[ob])
                    n += 1
                toks.append(P.dma("sp", lambda e, o=o, si=si: e.dma_start(
                    out=k.out[si * 128:(si + 1) * 128, :], in_=o[:]), reads=[ob], writes=[k.out_b]))
    P.barrier()


def phase_ffn(k, L):
    nc, P = k.nc, k.P
    R = k.R
    wup = k.w["ffn_w_up"][L]
    wdn = k.w["ffn_w_down"][L]
    bcol = R.off["ffn_b_conv%d" % L]
    wcol = [R.off["ffn_w_conv%d_%d" % (L, t)] for t in range(3)]
    with ExitStack() as st:
        xn = st.enter_context(nc.sbuf_tensor("ff_xn", [128, DC, TT], BF16))
        xn_b = P.buf()
        g = st.enter_context(nc.sbuf_tensor("ff_g", [128, FC, TT], BF16))
        g_b = P.buf()
        sup = [st.enter_context(nc.sbuf_tensor("ff_su%d" % i, [128, 2, DC, 128], F32)) for i in range(2)]
        sup_b = P.bufs(2)
        wub = [st.enter_context(nc.sbuf_tensor("ff_wu%d" % i, [128, 2, DC, 128], BF16)) for i in range(2)]
        wub_b = P.bufs(2)
        sdn = [st.enter_context(nc.sbuf_tensor("ff_sd%d" % i, [128, 22, 128], F32)) for i in range(2)]
        sdn_b = P.bufs(2)
        wdb = [st.enter_context(nc.sbuf_tensor("ff_wd%d" % i, [128, FC, 128], BF16)) for i in range(2)]
        wdb_b = P.bufs(2)
        ub = [st.enter_context(nc.sbuf_tensor("ff_ub%d" % i, [128, 2, TT + 2], F32)) for i in range(2)]
        ub_b = P.bufs(2)
        acc = [st.enter_context(nc.sbuf_tensor("ff_ac%d" % i, [128, 2, TT], F32)) for i in range(2)]
        acc_b = P.bufs(2)
        sil = [st.enter_context(nc.sbuf_tensor("ff_si%d" % i, [128, TT], F32)) for i in range(2)]
        sil_b = P.bufs(2)
        carry = st.enter_context(nc.sbuf_tensor("ff_cy", [128, 2 * FC, 2], F32))
        carry_b = P.buf()
        hres = [st.enter_context(nc.sbuf_tensor("ff_hr%d" % i, [128, TT], F32)) for i in range(2)]
        hres_b = P.bufs(2)
        pu = [st.enter_context(nc.psum_tensor("ff_pu%d" % i, [128, 2, TT], F32)) for i in range(2)]
        pu_b = P.pbufs(2)
        pd = [st.enter_context(nc.psum_tensor("ff_pd%d" % i, [128, TT], F32)) for i in range(2)]
        pd_b = P.pbufs(2)

        P.op("pool", lambda e: e.memset(carry[:], 0.0), writes=[carry_b])
        nu = 0
        nd = 0
        for tt in range(NT):
            P.dma("sp", lambda e, tt=tt: e.dma_start(out=xn[:], in_=fm(k.xnT, 0, DC, tt * TT, TT)),
                  reads=[k.xnT_b[tt]], writes=[xn_b])
            for j in range(FC):
                s_, sb_ = sup[nu % 2], sup_b[nu % 2]
                w_, wb_ = wub[nu % 2], wub_b[nu % 2]
                u_, ubb = ub[nu % 2], ub_b[nu % 2]
                a_, ab_ = acc[nu % 2], acc_b[nu % 2]
                si_, sib = sil[nu % 2], sil_b[nu % 2]
                p_, pb_ = pu[nu % 2], pu_b[nu % 2]
                for half in range(2):
                    n0 = half * DFF + j * 128
                    P.dma("sp", lambda e, s_=s_, half=half, n0=n0: e.dma_start(
                        out=s_[:, half, :, :], in_=wup[:, n0:n0 + 128].rearrange("(c p) n -> p c n", p=128)),
                        pwrites=[sb_])
                P.op("pool", lambda e, s_=s_, w_=w_: e.tensor_copy(out=w_[:], in_=s_[:]),
                     reads=[sb_], writes=[wb_])
                for half in range(2):
                    for c in range(DC):
                        P.op("pe", lambda e, p_=p_, w_=w_, half=half, c=c: e.matmul(
                            p_[:, half, :], lhsT=w_[:, half, c, :], rhs=xn[:, c, :],
                            start=(c == 0), stop=(c == DC - 1)),
                            reads=[wb_, xn_b], pwrites=[pb_])
                P.op("act", lambda e, u_=u_, j=j: e.activation(
                    out=u_[:, :, 0:2], in_=carry[:, 2 * j:2 * j + 2, :], func=AF.Copy),
                    reads=[carry_b], pwrites=[ubb])
                P.op("act", lambda e, u_=u_, p_=p_: e.activation(
                    out=u_[:, :, 2:TT + 2], in_=p_[:], func=AF.Copy), reads=[pb_], pwrites=[ubb])
                P.op("act", lambda e, u_=u_, j=j: e.activation(
                    out=carry[:, 2 * j:2 * j + 2, :], in_=u_[:, :, TT:TT + 2], func=AF.Copy),
                    reads=[ubb], pwrites=[carry_b])
                for half in range(2):
                    col = half * FC + j
                    eng = "dve"
                    P.op(eng, lambda e, a_=a_, u_=u_, half=half, col=col: e.tensor_scalar(
                        out=a_[:, half, :], in0=u_[:, half, 2:TT + 2],
                        scalar1=k.vecs[:, wcol[2] + col:wcol[2] + col + 1],
                        scalar2=k.vecs[:, bcol + col:bcol + col + 1], op0=ALU.mult, op1=ALU.add),
                        reads=[ubb, k.vecs_b], pwrites=[ab_])
                for tap in (1, 0):
                    for half in range(2):
                        col = half * FC + j
                        P.op("dve", lambda e, a_=a_, u_=u_, half=half, col=col, tap=tap: e.scalar_tensor_tensor(
                            out=a_[:, half, :], in0=u_[:, half, tap:tap + TT],
                            scalar=k.vecs[:, wcol[tap] + col:wcol[tap] + col + 1],
                            in1=a_[:, half, :], op0=ALU.mult, op1=ALU.add),
                            reads=[ubb, k.vecs_b, ab_], pwrites=[ab_])
                P.op("act", lambda e, si_=si_, a_=a_: e.activation(out=si_[:], in_=a_[:, 0, :], func=AF.Silu),
                     reads=[ab_], writes=[sib])
                P.op("pool", lambda e, si_=si_, a_=a_, j=j: e.tensor_tensor(
                    out=g[:, j, :], in0=si_[:], in1=a_[:, 1, :], op=ALU.mult),
                    reads=[sib, ab_], pwrites=[g_b])
                nu += 1
            for dc in range(DC):
                w_, wb_ = wdb[nd % 2], wdb_b[nd % 2]
                p_, pb_ = pd[nd % 2], pd_b[nd % 2]
                hr, hrb = hres[nd % 2], hres_b[nd % 2]
                for hf in range(2):
                    s_, sb_ = sdn[(2 * nd + hf) % 2], sdn_b[(2 * nd + hf) % 2]
                    P.dma("sp", lambda e, s_=s_, hf=hf, dc=dc: e.dma_start(
                        out=s_[:], in_=wdn[hf * 22 * 128:(hf + 1) * 22 * 128, dc * 128:(dc + 1) * 128].rearrange(
                            "(c p) n -> p c n", p=128)), writes=[sb_])
                    P.op("pool", lambda e, s_=s_, w_=w_, hf=hf: e.tensor_copy(
                        out=w_[:, hf * 22:(hf + 1) * 22, :], in_=s_[:]), reads=[sb_], pwrites=[wb_])
                P.dma("sp", lambda e, hr=hr, dc=dc, tt=tt: e.dma_start(
                    out=hr[:], in_=k.hT[dc * 128:(dc + 1) * 128, tt * TT:(tt + 1) * TT]),
                    reads=[k.hT_b[tt]], writes=[hrb])
                for c in range(FC):
                    P.op("pe", lambda e, p_=p_, w_=w_, c=c: e.matmul(
                        p_[:], lhsT=w_[:, c, :], rhs=g[:, c, :], start=(c == 0), stop=(c == FC - 1)),
                        reads=[wb_, g_b], pwrites=[pb_])
                P.op("dve", lambda e, hr=hr, p_=p_: e.tensor_tensor(out=hr[:], in0=p_[:], in1=hr[:], op=ALU.add),
                     reads=[pb_, hrb], writes=[hrb])
                P.dma("sp", lambda e, hr=hr, dc=dc, tt=tt: e.dma_start(
                    out=k.hT[dc * 128:(dc + 1) * 128, tt * TT:(tt + 1) * TT], in_=hr[:]),
                    reads=[hrb], pwrites=[k.hT_b[tt]])
                nd += 1
    P.barrier()


class WStream:
    def __init__(self, k, st, name, KC, ncol=128, nbuf=2):
        nc, P = k.nc, k.P
        self.k, self.KC, self.ncol, self.nbuf = k, KC, ncol, nbuf
        self.stg = [st.enter_context(nc.sbuf_tensor("%s_s%d" % (name, i), [128, KC, ncol], F32)) for i in range(nbuf)]
        self.wb = [st.enter_context(nc.sbuf_tensor("%s_w%d" % (name, i), [128, KC, ncol], BF16)) for i in range(nbuf)]
        self.stg_b = P.bufs(nbuf)
        self.wb_b = P.bufs(nbuf)
        self.n = 0

    def load(self, W2d, r0, c0):
        P = self.k.P
        i = self.n % self.nbuf
        self.n += 1
        stg, wb = self.stg[i], self.wb[i]
        KC, ncol = self.KC, self.ncol
        P.dma("sp", lambda e: e.dma_start(
            out=stg[:], in_=W2d[r0:r0 + KC * 128, c0:c0 + ncol].rearrange("(c p) n -> p c n", p=128)),
            writes=[self.stg_b[i]])
        P.op("pool", lambda e: e.tensor_copy(out=wb[:], in_=stg[:]), reads=[self.stg_b[i]], writes=[self.wb_b[i]])
        return wb, self.wb_b[i]


def residual_store(k, hr, hrb, dc, tt):
    k.P.dma("sp", lambda e: e.dma_start(
        out=k.hT[dc * 128:(dc + 1) * 128, tt * TT:(tt + 1) * TT], in_=hr[:]),
        reads=[hrb], pwrites=[k.hT_b[tt]])


def residual_load(k, hr, hrb, dc, tt):
    k.P.dma("sp", lambda e: e.dma_start(
        out=hr[:], in_=k.hT[dc * 128:(dc + 1) * 128, tt * TT:(tt + 1) * TT]),
        reads=[k.hT_b[tt]], writes=[hrb])


POOL_H = 15


def phase_mix_pool(k):
    nc, P = k.nc, k.P
    wg = k.w["c_w_group"][0]
    scol = k.R.off["c_scale"]
    H = POOL_H
    W_ = TT + H
    with ExitStack() as st:
        wres = st.enter_context(nc.sbuf_tensor("pl_w", [128, 4, 4, 512], BF16))
        wres_b = P.buf()
        stg = [st.enter_context(nc.sbuf_tensor("pl_s%d" % i, [128, 4, 512], F32)) for i in range(2)]
        stg_b = P.bufs(2)
        invc = st.enter_context(nc.sbuf_tensor("pl_ic", [128, 64], F32))
        invc_b = P.buf()
        P.dma("sp", lambda e: e.dma_start(out=invc[:], in_=k.consts["pool_invc"]), writes=[invc_b])
        for g in range(4):
            sg, sgb = stg[g % 2], stg_b[g % 2]
            P.dma("sp", lambda e, sg=sg, g=g: e.dma_start(
                out=sg[:], in_=wg[g].rearrange("(c p) n -> p c n", p=128)), writes=[sgb])
            P.op("pool", lambda e, sg=sg, g=g: e.tensor_copy(out=wres[:, g, :, :], in_=sg[:]),
                 reads=[sgb], pwrites=[wres_b])
        xn = [st.enter_context(nc.sbuf_tensor("pl_x%d" % i, [128, DC, W_], BF16)) for i in range(2)]
        xn_b = P.bufs(2)
        pp = [[st.enter_context(nc.sbuf_tensor("pl_p%d%d" % (e_, i), [128, W_], F32)) for i in range(2)] for e_ in range(2)]
        pp_b = [P.bufs(2) for _ in range(2)]
        t16 = st.enter_context(nc.sbuf_tensor("pl_t16", [128, 16], F32))
        t16_b = P.buf()
        diff = st.enter_context(nc.sbuf_tensor("pl_d", [128, DC, TT], BF16))
        diff_b = P.buf()
        hres = [st.enter_context(nc.sbuf_tensor("pl_h%d" % i, [128, TT], F32)) for i in range(2)]
        hres_b = P.bufs(2)
        ps = [st.enter_context(nc.psum_tensor("pl_ps%d" % i, [128, TT], F32)) for i in range(2)]
        ps_b = P.pbufs(2)
        nn = 0
        for tt in range(NT):
            x_, xb = xn[tt % 2], xn_b[tt % 2]
            if tt == 0:
                P.op("pool", lambda e, x_=x_: e.memset(x_[:, :, 0:H], 0.0), pwrites=[xb])
                P.dma("sp", lambda e, x_=x_: e.dma_start(out=x_[:, :, H:W_], in_=fm(k.xnT, 0, DC, 0, TT)),
                      reads=[k.xnT_b[0]], pwrites=[xb])
            else:
                P.dma("sp", lambda e, x_=x_, tt=tt: e.dma_start(out=x_[:], in_=fm(k.xnT, 0, DC, tt * TT - H, W_)),
                      reads=[k.xnT_b[tt - 1], k.xnT_b[tt]], writes=[xb])
            for c in range(DC):
                g = c // 4
                w = 2 << g
                ei = c % 2
                eng = "dve" if ei == 0 else "pool"
                cur, curb = x_[:, c, :], xb
                for stp in range(g + 1):
                    sh = 1 << stp
                    nxt, nxtb = pp[ei][stp % 2], pp_b[ei][stp % 2]
                    P.op(eng, lambda e, nxt=nxt, cur=cur, sh=sh: e.tensor_tensor(
                        out=nxt[:, sh:W_], in0=cur[:, sh:W_], in1=cur[:, 0:W_ - sh], op=ALU.add),
                        reads=[curb], writes=[nxtb])
                    cur, curb = nxt[:], nxtb
                P.op("dve", lambda e, cur=cur, c=c, w=w, x_=x_: e.scalar_tensor_tensor(
                    out=diff[:, c, :], in0=cur[:, H:W_], scalar=1.0 / w, in1=x_[:, c, H:W_],
                    op0=ALU.mult, op1=ALU.subtract), reads=[curb, xb], pwrites=[diff_b])
                if tt == 0:
                    P.op("dve", lambda e, cur=cur, g=g: e.tensor_tensor(
                        out=t16[:], in0=cur[:, H:H + 16], in1=invc[:, g * 16:(g + 1) * 16], op=ALU.mult),
                        reads=[curb, invc_b], writes=[t16_b])
                    P.op("dve", lambda e, c=c, x_=x_: e.tensor_tensor(
                        out=diff[:, c, 0:16], in0=t16[:], in1=x_[:, c, H:H + 16], op=ALU.subtract),
                        reads=[t16_b, xb], pwrites=[diff_b])
            for n in range(DC):
                g, ni = n // 4, n % 4
                p_, pb_ = ps[nn % 2], ps_b[nn % 2]
                hr, hrb = hres[nn % 2], hres_b[nn % 2]
                residual_load(k, hr, hrb, n, tt)
                for kc in range(4):
                    P.op("pe", lambda e, p_=p_, g=g, kc=kc, ni=ni: e.matmul(
                        p_[:], lhsT=wres[:, g, kc, ni * 128:(ni + 1) * 128], rhs=diff[:, g * 4 + kc, :],
                        start=(kc == 0), stop=(kc == 3)), reads=[wres_b, diff_b], pwrites=[pb_])
                P.op("dve", lambda e, p_=p_, hr=hr, n=n: e.scalar_tensor_tensor(
                    out=hr[:], in0=p_[:], scalar=k.vecs[:, scol + n:scol + n + 1], in1=hr[:],
                    op0=ALU.mult, op1=ALU.add), reads=[pb_, hrb, k.vecs_b], writes=[hrb])
                residual_store(k, hr, hrb, n, tt)
                nn += 1
    P.barrier()


CONF_W = 31
CONF_H = CONF_W - 1


def phase_mix_conf(k):
    nc, P = k.nc, k.P
    R = k.R
    w1 = k.w["d_w_pw1"][0]
    w2 = k.w["d_w_pw2"][0]
    b1 = R.off["d_b_pw1"]
    wdw = [R.off["d_w_dw%d" % t] for t in range(CONF_W)]
    bdw = R.off["d_b_dw"]
    lng, lnb = R.off["d_ln_g"], R.off["d_ln_b"]
    b2 = R.off["d_b_pw2"]
    H = CONF_H
    W_ = TT + H
    with ExitStack() as st:
        xn = st.enter_context(nc.sbuf_tensor("cf_xn", [128, DC, TT], BF16))
        xn_b = P.buf()
        ws1 = WStream(k, st, "cf_w1", DC, 128, nbuf=3)
        ws2 = WStream(k, st, "cf_w2", DC, 128, nbuf=2)
        ub = st.enter_context(nc.sbuf_tensor("cf_ub", [128, DC, W_], F32))
        ub_b = [P.buf() for _ in range(DC)]
        gate = [st.enter_context(nc.sbuf_tensor("cf_g%d" % i, [128, TT], F32)) for i in range(2)]
        gate_b = P.bufs(2)
        v = st.enter_context(nc.sbuf_tensor("cf_v", [128, DC, TT], F32))
        v_b = [P.buf() for _ in range(DC)]
        sq = st.enter_context(nc.sbuf_tensor("cf_sq", [128, DC, TT], BF16))
        sq_b = P.buf()
        ones_f = st.enter_context(nc.sbuf_tensor("cf_1f", [128, 128], F32))
        ones_fb = P.buf()
        P.op("pool", lambda e: e.memset(ones_f[:], 1.0), writes=[ones_fb])
        mean = st.enter_context(nc.sbuf_tensor("cf_mean", [128, TT], F32))
        mean_b = P.buf()
        rstd = st.enter_context(nc.sbuf_tensor("cf_rstd", [128, TT], F32))
        rstd_b = P.buf()
        tmp = [st.enter_context(nc.sbuf_tensor("cf_t%d" % i, [128, TT], F32)) for i in range(2)]
        tmp_b = P.bufs(2)
        lo = st.enter_context(nc.sbuf_tensor("cf_lo", [128, DC, TT], BF16))
        lo_b = P.buf()
        hres = [st.enter_context(nc.sbuf_tensor("cf_h%d" % i, [128, TT], F32)) for i in range(2)]
        hres_b = P.bufs(2)
        pa = [st.enter_context(nc.psum_tensor("cf_pa%d" % i, [128, TT], F32)) for i in range(2)]
        pa_b = P.pbufs(2)
        pg = [st.enter_context(nc.psum_tensor("cf_pg%d" % i, [128, TT], F32)) for i in range(2)]
        pg_b = P.pbufs(2)
        pm = st.enter_context(nc.psum_tensor("cf_pm", [128, TT], F32))
        pm_b = P.pbuf()
        pq = st.enter_context(nc.psum_tensor("cf_pq", [128, TT], F32))
        pq_b = P.pbuf()
        po = [st.enter_context(nc.psum_tensor("cf_po%d" % i, [128, TT], F32)) for i in range(2)]
        po_b = P.pbufs(2)
        nj = 0
        nd = 0
        for tt in range(NT):
            P.dma("sp", lambda e, tt=tt: e.dma_start(out=xn[:], in_=fm(k.xnT, 0, DC, tt * TT, TT)),
                  reads=[k.xnT_b[tt]], writes=[xn_b])
            for j in range(DC):
                ubj = ub_b[j]
                if tt == 0:
                    P.op("pool", lambda e, j=j: e.memset(ub[:, j, 0:H], 0.0), pwrites=[ubj])
                else:
                    P.op("pool", lambda e, j=j: e.tensor_copy(out=ub[:, j, 0:H], in_=ub[:, j, TT:W_]),
                         reads=[ubj], writes=[ubj])
                wa, wab = ws1.load(w1, 0, j * 128)
                wgt, wgb = ws1.load(w1, 0, D + j * 128)
                pa_, pab = pa[nj % 2], pa_b[nj % 2]
                pg_, pgb = pg[nj % 2], pg_b[nj % 2]
                gt, gtb = gate[nj % 2], gate_b[nj % 2]
                for c in range(DC):
                    P.op("pe", lambda e, pa_=pa_, wa=wa, c=c: e.matmul(
                        pa_[:], lhsT=wa[:, c, :], rhs=xn[:, c, :], start=(c == 0), stop=(c == DC - 1)),
                        reads=[wab, xn_b], pwrites=[pab])
                for c in range(DC):
                    P.op("pe", lambda e, pg_=pg_, wgt=wgt, c=c: e.matmul(
                        pg_[:], lhsT=wgt[:, c, :], rhs=xn[:, c, :], start=(c == 0), stop=(c == DC - 1)),
                        reads=[wgb, xn_b], pwrites=[pgb])
                P.op("act", lambda e, gt=gt, pg_=pg_, j=j: e.activation(
                    out=gt[:], in_=pg_[:], func=AF.Sigmoid, bias=k.vecs[:, b1 + DC + j:b1 + DC + j + 1]),
                    reads=[pgb, k.vecs_b], writes=[gtb])
                P.op("dve", lambda e, gt=gt, pa_=pa_, j=j: e.scalar_tensor_tensor(
                    out=ub[:, j, H:W_], in0=pa_[:], scalar=k.vecs[:, b1 + j:b1 + j + 1], in1=gt[:],
                    op0=ALU.add, op1=ALU.mult), reads=[pab, gtb, k.vecs_b], pwrites=[ubj])
                P.op("act", lambda e, j=j: e.activation(
                    out=v[:, j, :], in_=ub[:, j, H:W_], func=AF.Identity,
                    scale=k.vecs[:, wdw[CONF_W - 1] + j:wdw[CONF_W - 1] + j + 1],
                    bias=k.vecs[:, bdw + j:bdw + j + 1]), reads=[ubj, k.vecs_b], writes=[v_b[j]])
                for tap in range(CONF_W - 1):
                    P.op("dve", lambda e, j=j, tap=tap: e.scalar_tensor_tensor(
                        out=v[:, j, :], in0=ub[:, j, tap:tap + TT],
                        scalar=k.vecs[:, wdw[tap] + j:wdw[tap] + j + 1], in1=v[:, j, :],
                        op0=ALU.mult, op1=ALU.add), reads=[ubj, v_b[j], k.vecs_b], writes=[v_b[j]])
                P.op("act", lambda e, j=j: e.activation(out=sq[:, j, :], in_=v[:, j, :], func=AF.Square),
                     reads=[v_b[j]], pwrites=[sq_b])
                nj += 1
            for c in range(DC):
                P.op("pe", lambda e, c=c: e.matmul(pm[:], lhsT=ones_f[:], rhs=v[:, c, :],
                                                   start=(c == 0), stop=(c == DC - 1)),
                     reads=[ones_fb, v_b[c]], pwrites=[pm_b])
            for c in range(DC):
                P.op("pe", lambda e, c=c: e.matmul(pq[:], lhsT=k.ones_bf[:], rhs=sq[:, c, :],
                                                   start=(c == 0), stop=(c == DC - 1)),
                     reads=[k.ones_b, sq_b], pwrites=[pq_b])
            P.op("act", lambda e: e.activation(out=mean[:], in_=pm[:], func=AF.Copy, scale=1.0 / D),
                 reads=[pm_b], writes=[mean_b])
            P.op("dve", lambda e: e.tensor_tensor(out=rstd[:], in0=mean[:], in1=mean[:], op=ALU.mult),
                 reads=[mean_b], writes=[rstd_b])
            P.op("dve", lambda e: e.scalar_tensor_tensor(
                out=rstd[:], in0=pq[:], scalar=1.0 / D, in1=rstd[:], op0=ALU.mult, op1=ALU.subtract),
                reads=[pq_b, rstd_b], writes=[rstd_b])
            P.op("act", lambda e: e.activation(out=rstd[:], in_=rstd[:], func=AF.Sqrt, bias=k.eps_t[:, 0:1]),
                 reads=[rstd_b, k.eps_b], writes=[rstd_b])
            P.op("dve", lambda e: e.reciprocal(out=rstd[:], in_=rstd[:]), reads=[rstd_b], writes=[rstd_b])
            for c in range(DC):
                t_, tb = tmp[c % 2], tmp_b[c % 2]
                P.op("pool", lambda e, t_=t_, c=c: e.tensor_tensor(out=t_[:], in0=v[:, c, :], in1=mean[:], op=ALU.subtract),
                     reads=[v_b[c], mean_b], writes=[tb])
                P.op("dve", lambda e, t_=t_, c=c: e.scalar_tensor_tensor(
                    out=t_[:], in0=t_[:], scalar=k.vecs[:, lng + c:lng + c + 1], in1=rstd[:],
                    op0=ALU.mult, op1=ALU.mult), reads=[tb, rstd_b, k.vecs_b], writes=[tb])
                P.op("act", lambda e, t_=t_, c=c: e.activation(
                    out=lo[:, c, :], in_=t_[:], func=AF.Silu, bias=k.vecs[:, lnb + c:lnb + c + 1]),
                    reads=[tb, k.vecs_b], pwrites=[lo_b])
            for dc in range(DC):
                w_, wb_ = ws2.load(w2, 0, dc * 128)
                p_, pb_ = po[nd % 2], po_b[nd % 2]
                hr, hrb = hres[nd % 2], hres_b[nd % 2]
                residual_load(k, hr, hrb, dc, tt)
                for c in range(DC):
                    P.op("pe", lambda e, p_=p_, w_=w_, c=c: e.matmul(
                        p_[:], lhsT=w_[:, c, :], rhs=lo[:, c, :], start=(c == 0), stop=(c == DC - 1)),
                        reads=[wb_, lo_b], pwrites=[pb_])
                P.op("dve", lambda e, p_=p_, hr=hr, dc=dc: e.scalar_tensor_tensor(
                    out=hr[:], in0=p_[:], scalar=k.vecs[:, b2 + dc:b2 + dc + 1], in1=hr[:],
                    op0=ALU.add, op1=ALU.add), reads=[pb_, hrb, k.vecs_b], writes=[hrb])
                residual_store(k, hr, hrb, dc, tt)
                nd += 1
    P.barrier()


HG_C = 64


def phase_mix_hgrn(k, L):
    nc, P = k.nc, k.P
    R = k.R
    win = k.w["b_w_in"][0]
    wo = k.w["b_w_o"][0]
    gn = R.off["b_g_norm"]
    NCH = TT // HG_C
    with ExitStack() as st:
        def sb(name, shape, dt=F32):
            return st.enter_context(nc.sbuf_tensor("hg_" + name, shape, dt))

        xn = sb("xn", [128, DC, TT], BF16); xn_b = P.buf()
        ws = WStream(k, st, "hg_wi", DC, 128, nbuf=4)
        wso = WStream(k, st, "hg_wo", DC, 128, nbuf=2)
        lb = sb("lb", [128, DC]); oml = sb("oml", [128, DC]); lbt = sb("lbt", [128, 4, DC]); lb_b = P.buf()
        ones64 = sb("ones64", [128, HG_C]); ones64_b = P.buf()
        mask = sb("mask", [64, TT]); mask_b = P.buf()
        identb = sb("identb", [128, 128], BF16); identb_b = P.buf()
        state = sb("state", [128, 16, 128]); state_b = [P.buf() for _ in range(16)]
        snap = sb("snap", [128, NCH + 1, 128], BF16); snap_b = [P.buf() for _ in range(NCH + 1)]
        sg = sb("sg", [128, TT]); sg_b = P.buf()
        lf = sb("lf", [128, TT]); lf_b = P.buf()
        kk = sb("kk", [128, TT]); kk_b = P.buf()
        a = sb("a", [128, TT]); a_b = P.buf()
        ea = sb("ea", [128, TT]); ea_b = P.buf()
        ena = sb("ena", [128, TT]); ena_b = P.buf()
        qs = sb("qs", [128, TT]); qs_b = P.buf()
        gs = sb("gs", [128, TT]); gs_b = P.buf()
        tmp = sb("tmp", [128, TT]); tmp_b = P.buf()
        rs = sb("rs", [128, TT]); rs_b = P.buf()
        qt = sb("qt", [128, TT], BF16); qt_b = P.buf()
        kt = sb("kt", [128, TT], BF16); kt_b = P.buf()
        kh = sb("kh", [128, TT], BF16); kh_b = P.buf()
        vb = sb("vb", [128, TT], BF16); vb_b = P.buf()
        osq = sb("osq", [128, TT], BF16); osq_b = P.buf()
        scb = sb("scb", [64, TT], BF16); scb_b = P.buf()
        vtok = sb("vtok", [64, NCH, 128], BF16); vtok_b = P.buf()
        khtok = sb("khtok", [64, NCH, 128], BF16); khtok_b = P.buf()
        ob = sb("ob", [128, DC, TT], BF16); ob_b = P.buf()
        hres = [sb("hr%d" % i, [128, TT]) for i in range(2)]; hres_b = P.bufs(2)
        pq = st.enter_context(nc.psum_tensor("hg_pq", [128, TT], F32)); pq_b = P.pbuf()
        pf = st.enter_context(nc.psum_tensor("hg_pf", [128, TT], F32)); pf_b = P.pbuf()
        pi = st.enter_context(nc.psum_tensor("hg_pi", [128, TT], F32)); pi_b = P.pbuf()
        pg = st.enter_context(nc.psum_tensor("hg_pg", [128, TT], F32)); pg_b = P.pbuf()
        po = st.enter_context(nc.psum_tensor("hg_po", [128, TT], F32)); po_b = P.pbuf()
        psc = st.enter_context(nc.psum_tensor("hg_psc", [128, TT], F32)); psc_b = P.pbuf()
        pkv = st.enter_context(nc.psum_tensor("hg_pkv", [128, 4, 128], F32)); pkv_b = [P.pbuf()] * 4
        ptr = st.enter_context(nc.psum_tensor("hg_ptr", [64, NCH, 128], BF16)); ptr_b = P.pbuf()

        P.dma("sp", lambda e: e.dma_start(out=mask[:], in_=k.consts["hg_mask"]), writes=[mask_b])
        P.op("pool", lambda e: e.memset(ones64[:], 1.0), writes=[ones64_b])
        P.op("pool", lambda e: e.tensor_copy(out=identb[:], in_=k.ident[:]), reads=[k.ident_b], writes=[identb_b])
        P.op("pool", lambda e: e.memset(state[:], 0.0), writes=state_b)
        for l in range(DEPTH):
            c0 = R.off["b_lb%d" % l]
            P.op("act", lambda e, l=l, c0=c0: e.activation(out=lbt[:, l, :], in_=k.vecs[:, c0:c0 + DC], func=AF.Exp),
                 reads=[k.vecs_b], pwrites=[lb_b])
        P.op("dve", lambda e: e.tensor_tensor(out=oml[:], in0=lbt[:, 0, :], in1=lbt[:, 1, :], op=ALU.add),
             reads=[lb_b], pwrites=[lb_b])
        P.op("dve", lambda e: e.tensor_tensor(out=oml[:], in0=oml[:], in1=lbt[:, 2, :], op=ALU.add),
             reads=[lb_b], writes=[lb_b])
        P.op("dve", lambda e: e.tensor_tensor(out=oml[:], in0=oml[:], in1=lbt[:, 3, :], op=ALU.add),
             reads=[lb_b], writes=[lb_b])
        P.op("dve", lambda e: e.reciprocal(out=oml[:], in_=oml[:]), reads=[lb_b], writes=[lb_b])
        P.op("dve", lambda e: e.tensor_copy(out=lb[:], in_=lbt[:, 1, :]), reads=[lb_b], writes=[lb_b])
        for l in range(2, L + 1):
            P.op("dve", lambda e, l=l: e.tensor_tensor(out=lb[:], in0=lb[:], in1=lbt[:, l, :], op=ALU.add),
                 reads=[lb_b], writes=[lb_b])
        P.op("dve", lambda e: e.tensor_tensor(out=lb[:], in0=lb[:], in1=oml[:], op=ALU.mult),
             reads=[lb_b], writes=[lb_b])
        P.op("dve", lambda e: e.tensor_scalar(out=oml[:], in0=lb[:], scalar1=-1.0, scalar2=1.0,
                                              op0=ALU.mult, op1=ALU.add), reads=[lb_b], writes=[lb_b])
        nd = 0
        import os
        STG = int(os.environ.get("HG_STAGE", "99"))
        NTT = int(os.environ.get("HG_NTT", str(NT)))
        NH = int(os.environ.get("HG_NH", "16"))
        for tt in range(NTT):
            P.dma("sp", lambda e, tt=tt: e.dma_start(out=xn[:], in_=fm(k.xnT, 0, DC, tt * TT, TT)),
                  reads=[k.xnT_b[tt]], writes=[xn_b])
            for h in range(NH):
                sl = []
                for sec in range(4):
                    sl.append(ws.load(win, 0, sec * D + h * 128))
                for sec, (pt, ptb) in enumerate(((pq, pq_b), (pf, pf_b), (pi, pi_b), (pg, pg_b))):
                    w_, wb_ = sl[sec]
                    for c in range(DC):
                        P.op("pe", lambda e, pt=pt, w_=w_, c=c: e.matmul(
                            pt[:], lhsT=w_[:, c, :], rhs=xn[:, c, :], start=(c == 0), stop=(c == DC - 1)),
                            reads=[wb_, xn_b], pwrites=[ptb])
                if STG < 2:
                    continue
                P.op("act", lambda e: e.activation(out=sg[:], in_=pf[:], func=AF.Sigmoid), reads=[pf_b], writes=[sg_b])
                P.op("dve", lambda e, h=h: e.tensor_scalar(
                    out=sg[:], in0=sg[:], scalar1=oml[:, h:h + 1], scalar2=lb[:, h:h + 1],
                    op0=ALU.mult, op1=ALU.add), reads=[sg_b, lb_b], writes=[sg_b])
                P.op("act", lambda e: e.activation(out=lf[:], in_=sg[:], func=AF.Ln), reads=[sg_b], writes=[lf_b])
                P.op("pool", lambda e: e.tensor_scalar(out=kk[:], in0=sg[:], scalar1=-1.0, scalar2=1.0,
                                                       op0=ALU.mult, op1=ALU.add), reads=[sg_b], writes=[kk_b])
                for n in range(NCH):
                    cs = slice(n * HG_C, (n + 1) * HG_C)
                    P.op("dve", lambda e, cs=cs: e.tensor_tensor_scan(
                        out=a[:, cs], data0=ones64[:], data1=lf[:, cs], initial=0.0, op0=ALU.mult, op1=ALU.add),
                        reads=[lf_b, ones64_b], pwrites=[a_b])
                P.op("act", lambda e: e.activation(out=ea[:], in_=a[:], func=AF.Exp), reads=[a_b], writes=[ea_b])
                P.op("act", lambda e: e.activation(out=ena[:], in_=a[:], func=AF.Exp, scale=-1.0), reads=[a_b], writes=[ena_b])
                P.op("act", lambda e: e.activation(out=qs[:], in_=pq[:], func=AF.Silu), reads=[pq_b], writes=[qs_b])
                P.op("act", lambda e: e.activation(out=vb[:], in_=pi[:], func=AF.Copy), reads=[pi_b], writes=[vb_b])
                P.op("act", lambda e: e.activation(out=gs[:], in_=pg[:], func=AF.Silu), reads=[pg_b], writes=[gs_b])
                P.op("pool", lambda e: e.tensor_tensor(out=qt[:], in0=qs[:], in1=ea[:], op=ALU.mult),
                     reads=[qs_b, ea_b], writes=[qt_b])
                P.op("pool", lambda e: e.tensor_tensor(out=kt[:], in0=kk[:], in1=ena[:], op=ALU.mult),
                     reads=[kk_b, ena_b], writes=[kt_b])
                for n in range(NCH):
                    cs = slice(n * HG_C, (n + 1) * HG_C)
                    last = n * HG_C + HG_C - 1
                    P.op("dve", lambda e, cs=cs, last=last: e.tensor_scalar(
                        out=kh[:, cs], in0=kt[:, cs], scalar1=ea[:, last:last + 1], scalar2=None, op0=ALU.mult),
                        reads=[kt_b, ea_b], pwrites=[kh_b])
                if STG < 3:
                    continue
                for n in range(NCH):
                    cs = slice(n * HG_C, (n + 1) * HG_C)
                    P.op("pe", lambda e, n=n, cs=cs: e.transpose(out=ptr[:, n, :], in_=vb[:, cs], identity=identb[:]),
                         reads=[vb_b, identb_b], pwrites=[ptr_b])
                P.op("act", lambda e: e.activation(out=vtok[:], in_=ptr[:], func=AF.Copy), reads=[ptr_b], writes=[vtok_b])
                for n in range(NCH):
                    cs = slice(n * HG_C, (n + 1) * HG_C)
                    P.op("pe", lambda e, n=n, cs=cs: e.transpose(out=ptr[:, n, :], in_=kh[:, cs], identity=identb[:]),
                         reads=[kh_b, identb_b], pwrites=[ptr_b])
                P.op("act", lambda e: e.activation(out=khtok[:], in_=ptr[:], func=AF.Copy), reads=[ptr_b], writes=[khtok_b])
                if STG < 4:
                    continue
                for n in range(NCH):
                    cs = slice(n * HG_C, (n + 1) * HG_C)
                    P.op("pe", lambda e, cs=cs: e.matmul(psc[0:64, cs], lhsT=kt[:, cs], rhs=qt[:, cs], start=True, stop=True),
                         reads=[kt_b, qt_b], pwrites=[psc_b])
                SUB = int(os.environ.get("HG_SUB", "9"))
                if SUB < 1:
                    continue
                P.op("dve", lambda e: e.tensor_tensor(out=scb[:], in0=psc[0:64, :], in1=mask[:], op=ALU.mult),
                     reads=[psc_b, mask_b], writes=[scb_b])
                if SUB < 2:
                    continue
                P.op("act", lambda e, h=h: e.activation(out=snap[:, 0, :], in_=state[:, h, :], func=AF.Copy),
                     reads=[state_b[h]], writes=[snap_b[0]])
                if STG < 5:
                    continue
                for n in range(NCH):
                    last = n * HG_C + HG_C - 1
                    P.op("pe", lambda e, n=n: e.matmul(pkv[:, n % 4, :], lhsT=khtok[:, n, :], rhs=vtok[:, n, :],
                                                      start=True, stop=True),
                         reads=[khtok_b, vtok_b], writes=[pkv_b[n % 4]])
                    P.op("dve", lambda e, n=n, h=h, last=last: e.scalar_tensor_tensor(
                        out=state[:, h, :], in0=state[:, h, :], scalar=ea[:, last:last + 1], in1=pkv[:, n % 4, :],
                        op0=ALU.mult, op1=ALU.add), reads=[state_b[h], ea_b, pkv_b[n % 4]], writes=[state_b[h]])
                    P.op("act", lambda e, n=n, h=h: e.activation(out=snap[:, n + 1, :], in_=state[:, h, :], func=AF.Copy),
                         reads=[state_b[h]], writes=[snap_b[n + 1]])
                if STG < 6:
                    continue
                for n in range(NCH):
                    cs = slice(n * HG_C, (n + 1) * HG_C)
                    P.op("pe", lambda e, n=n, cs=cs: e.matmul(po[:, cs], lhsT=vtok[:, n, :], rhs=scb[:, cs],
                                                              start=True, stop=False),
                         reads=[vtok_b, scb_b], pwrites=[po_b])
                    P.op("pe", lambda e, n=n, cs=cs: e.matmul(po[:, cs], lhsT=snap[:, n, :], rhs=qt[:, cs],
                                                              start=False, stop=True),
                         reads=[snap_b[n], qt_b], pwrites=[po_b])
                if STG < 7:
                    continue
                P.op("act", lambda e: e.activation(out=osq[:], in_=po[:], func=AF.Square), reads=[po_b], writes=[osq_b])
                P.op("pe", lambda e: e.matmul(psc[:], lhsT=k.ones_bf[:], rhs=osq[:], start=True, stop=True),
                     reads=[osq_b, k.ones_b, scb_b], writes=[psc_b])
                P.op("act", lambda e: e.activation(out=rs[:], in_=psc[:], func=AF.Sqrt, bias=k.eps_t[:, 0:1], scale=1.0 / 128),
                     reads=[psc_b, k.eps_b], writes=[rs_b])
                P.op("dve", lambda e: e.reciprocal(out=rs[:], in_=rs[:]), reads=[rs_b], writes=[rs_b])
                P.op("dve", lambda e: e.tensor_tensor(out=tmp[:], in0=po[:], in1=rs[:], op=ALU.mult),
                     reads=[po_b, rs_b], writes=[tmp_b])
                P.op("dve", lambda e, h=h: e.scalar_tensor_tensor(
                    out=ob[:, h, :], in0=tmp[:], scalar=k.vecs[:, gn + h:gn + h + 1], in1=gs[:],
                    op0=ALU.mult, op1=ALU.mult), reads=[tmp_b, gs_b, k.vecs_b], pwrites=[ob_b])
            if STG < 8:
                continue
            for dc in range(DC):
                w_, wb_ = wso.load(wo, 0, dc * 128)
                p_, pb_ = (pq, pq_b) if nd % 2 == 0 else (pf, pf_b)
                hr, hrb = hres[nd % 2], hres_b[nd % 2]
                residual_load(k, hr, hrb, dc, tt)
                for c in range(DC):
                    P.op("pe", lambda e, p_=p_, w_=w_, c=c: e.matmul(
                        p_[:], lhsT=w_[:, c, :], rhs=ob[:, c, :], start=(c == 0), stop=(c == DC - 1)),
                        reads=[wb_, ob_b], pwrites=[pb_])
                P.op("dve", lambda e, p_=p_, hr=hr: e.tensor_tensor(out=hr[:], in0=p_[:], in1=hr[:], op=ALU.add),
                     reads=[pb_, hrb], writes=[hrb])
                residual_store(k, hr, hrb, dc, tt)
                nd += 1
    P.barrier()


ATT_SCALE = 128 ** -0.5
NEG = -1.0e30
MNEG = -30000.0
NSB = S // 128


def t5_bucket_np(d):
    d = np.maximum(np.asarray(d, np.int64), 0)
    nf = np.maximum(d, 1).astype(np.float32)
    large = 16 + (np.log(nf / np.float32(16)) / np.float32(math.log(128 / 16)) * np.float32(16)).astype(np.int32)
    large = np.minimum(large, 31)
    return np.where(d < 16, d, large).astype(np.int64)


def dsa_layout_inputs(inp):
    out = {}
    out["a_gq_b"] = np.ascontiguousarray(np.broadcast_to(inp["a_g_q"][0][None, :], (128, 512)), dtype=np.float32)
    out["a_gkv_b"] = np.ascontiguousarray(np.broadcast_to(inp["a_g_kv"][0][None, :], (128, 256)), dtype=np.float32)
    rb = np.asarray(inp["rel_bias"], np.float32)
    out["a_cvec"] = np.ascontiguousarray(np.broadcast_to(rb[31][None, :], (128, 16)), dtype=np.float32)
    sl = np.arange(128)[:, None, None]
    r = np.arange(5)[None, :, None]
    ql = np.arange(512)[None, None, :]
    bidx = t5_bucket_np(ql - sl + 128 - 128 * r)
    out["a_bt"] = np.ascontiguousarray(np.moveaxis(rb[bidx], -1, 0), dtype=np.float32)
    return out


DSA_LAYOUT_SHAPES = {"a_gq_b": [128, 512], "a_gkv_b": [128, 256], "a_cvec": [128, 16], "a_bt": [16, 128, 5, 512]}


def phase_dsa_a(k):
    nc, P = k.nc, k.P
    win = k.w["a_w_in"][0]
    with ExitStack() as st:
        def sb(name, shape, dt=F32):
            return st.enter_context(nc.sbuf_tensor("da_" + name, shape, dt))

        xn = sb("xn", [128, DC, TT], BF16); xn_b = P.buf()
        wst = [sb("wst%d" % i, [128, 4, 848]) for i in range(2)]; wst_b = P.bufs(2)
        wbf = sb("wbf", [128, DC, 848], BF16); wbf_b = P.buf()
        gq = sb("gq", [128, 512]); gkv = sb("gkv", [128, 256]); g_b = P.buf()
        identb = sb("identb", [128, 128], BF16); identb_b = P.buf()
        junk = sb("junk", [128, 512], BF16); junk_b = P.buf()
        ss = sb("ss", [128, 2]); ss_b = P.buf()
        cqn = sb("cqn", [128, 512], BF16); cqn_b = P.buf()
        ckvn = [sb("ckvn%d" % i, [128, 256], BF16) for i in range(2)]; ckvn_b = P.bufs(2)
        kix = sb("kix", [128, 64], BF16); kix_b = P.buf()
        widx = sb("widx", [128, NSB, 16]); widx_b = P.buf()
        cqT = [sb("cqT%d" % i, [128, 4, TT], BF16) for i in range(2)]; cqT_b = P.bufs(2)
        ckvT = [sb("ckvT%d" % i, [128, 2, TT], BF16) for i in range(2)]; ckvT_b = P.bufs(2)
        kixT = [sb("kixT%d" % i, [64, TT], BF16) for i in range(2)]; kixT_b = P.bufs(2)
        pA = [st.enter_context(nc.psum_tensor("da_pA%d" % i, [128, 512], F32)) for i in range(2)]; pA_b = P.pbufs(2)
        pB = [st.enter_context(nc.psum_tensor("da_pB%d" % i, [128, 512], F32)) for i in range(2)]; pB_b = P.pbufs(2)
        ptr = [st.enter_context(nc.psum_tensor("da_ptr%d" % i, [128, 8, 128], BF16)) for i in range(2)]; ptr_b = P.pbufs(2)

        P.dma("sp", lambda e: e.dma_start(out=gq[:], in_=k.lay["a_gq_b"]), pwrites=[g_b])
        P.dma("sp", lambda e: e.dma_start(out=gkv[:], in_=k.lay["a_gkv_b"]), pwrites=[g_b])
        P.op("pool", lambda e: e.tensor_copy(out=identb[:], in_=k.ident[:]), reads=[k.ident_b], writes=[identb_b])
        for i in range(4):
            w_, wb_ = wst[i % 2], wst_b[i % 2]
            P.dma("sp", lambda e, w_=w_, i=i: e.dma_start(
                out=w_[:], in_=win[i * 512:(i + 1) * 512, :].rearrange("(c p) n -> p c n", p=128)), writes=[wb_])
            P.op("pool", lambda e, w_=w_, i=i: e.tensor_copy(out=wbf[:, i * 4:(i + 1) * 4, :], in_=w_[:]),
                 reads=[wb_], pwrites=[wbf_b])
        n = 0
        for tt in range(NT):
            P.dma("sp", lambda e, tt=tt: e.dma_start(out=xn[:], in_=fm(k.xnT, 0, DC, tt * TT, TT)),
                  reads=[k.xnT_b[tt]], writes=[xn_b])
            cq_t, cq_tb = cqT[tt % 2], cqT_b[tt % 2]
            ckv_t, ckv_tb = ckvT[tt % 2], ckvT_b[tt % 2]
            kix_t, kix_tb = kixT[tt % 2], kixT_b[tt % 2]
            for sub in range(4):
                sblk = tt * 4 + sub
                ts_ = slice(sub * 128, (sub + 1) * 128)
                a_, ab_ = pA[n % 2], pA_b[n % 2]
                b_, bb_ = pB[n % 2], pB_b[n % 2]
                t_, tb_ = ptr[n % 2], ptr_b[n % 2]
                ck, ckb = ckvn[n % 2], ckvn_b[n % 2]
                for c in range(DC):
                    P.op("pe", lambda e, a_=a_, c=c, ts_=ts_: e.matmul(
                        a_[:], lhsT=xn[:, c, ts_], rhs=wbf[:, c, 0:512], start=(c == 0), stop=(c == DC - 1)),
                        reads=[xn_b, wbf_b], pwrites=[ab_])
                for c in range(DC):
                    P.op("pe", lambda e, b_=b_, c=c, ts_=ts_: e.matmul(
                        b_[:, 0:336], lhsT=xn[:, c, ts_], rhs=wbf[:, c, 512:848], start=(c == 0), stop=(c == DC - 1)),
                        reads=[xn_b, wbf_b], pwrites=[bb_])
                P.op("act", lambda e, a_=a_: e.activation(out=junk[:], in_=a_[:], func=AF.Square, accum_out=ss[:, 0:1]),
                     reads=[ab_], writes=[junk_b], pwrites=[ss_b])
                P.op("act", lambda e, b_=b_: e.activation(out=junk[:, 0:256], in_=b_[:, 0:256], func=AF.Square,
                                                          accum_out=ss[:, 1:2]),
                     reads=[bb_], writes=[junk_b], pwrites=[ss_b])
                P.op("act", lambda e: e.activation(out=ss[:, 0:1], in_=ss[:, 0:1], func=AF.Sqrt,
                                                   bias=k.eps_t[:, 0:1], scale=1.0 / 512), reads=[ss_b, k.eps_b], pwrites=[ss_b])
                P.op("act", lambda e: e.activation(out=ss[:, 1:2], in_=ss[:, 1:2], func=AF.Sqrt,
                                                   bias=k.eps_t[:, 0:1], scale=1.0 / 256), reads=[ss_b, k.eps_b], pwrites=[ss_b])
                P.op("dve", lambda e: e.reciprocal(out=ss[:], in_=ss[:]), reads=[ss_b], writes=[ss_b])
                P.op("dve", lambda e, a_=a_: e.scalar_tensor_tensor(
                    out=cqn[:], in0=a_[:], scalar=ss[:, 0:1], in1=gq[:], op0=ALU.mult, op1=ALU.mult),
                    reads=[ab_, ss_b, g_b], writes=[cqn_b])
                P.op("dve", lambda e, b_=b_, ck=ck: e.scalar_tensor_tensor(
                    out=ck[:], in0=b_[:, 0:256], scalar=ss[:, 1:2], in1=gkv[:], op0=ALU.mult, op1=ALU.mult),
                    reads=[bb_, ss_b, g_b], writes=[ckb])
                P.op("act", lambda e, b_=b_: e.activation(out=kix[:], in_=b_[:, 256:320], func=AF.Copy),
                     reads=[bb_], writes=[kix_b])
                P.op("act", lambda e, b_=b_, sblk=sblk: e.activation(out=widx[:, sblk, :], in_=b_[:, 320:336], func=AF.Copy),
                     reads=[bb_], pwrites=[widx_b])
                P.dma("sp", lambda e, ck=ck, sblk=sblk: e.dma_start(
                    out=k.ckv_tok[sblk * 128:(sblk + 1) * 128, :], in_=ck[:]), reads=[ckb], pwrites=[k.dsa_b])
                for j in range(4):
                    P.op("pe", lambda e, t_=t_, j=j: e.transpose(out=t_[:, j, :], in_=cqn[:, j * 128:(j + 1) * 128],
                                                                identity=identb[:]),
                         reads=[cqn_b, identb_b], pwrites=[tb_])
                for j in range(2):
                    P.op("pe", lambda e, t_=t_, j=j, ck=ck: e.transpose(out=t_[:, 4 + j, :], in_=ck[:, j * 128:(j + 1) * 128],
                                                                       identity=identb[:]),
                         reads=[ckb, identb_b], pwrites=[tb_])
                P.op("pe", lambda e, t_=t_: e.transpose(out=t_[0:64, 6, :], in_=kix[:], identity=identb[:]),
                     reads=[kix_b, identb_b], pwrites=[tb_])
                P.op("act", lambda e, t_=t_, cq_t=cq_t, ts_=ts_: e.activation(out=cq_t[:, :, ts_], in_=t_[:, 0:4, :], func=AF.Copy),
                     reads=[tb_], pwrites=[cq_tb])
                P.op("dve", lambda e, t_=t_, ckv_t=ckv_t, ts_=ts_: e.tensor_copy(out=ckv_t[:, :, ts_], in_=t_[:, 4:6, :]),
                     reads=[tb_], pwrites=[ckv_tb])
                P.op("dve", lambda e, t_=t_, kix_t=kix_t, ts_=ts_: e.tensor_copy(out=kix_t[:, ts_], in_=t_[0:64, 6, :]),
                     reads=[tb_], pwrites=[kix_tb])
                n += 1
            c0 = tt * TT
            P.dma("sp", lambda e, cq_t=cq_t, c0=c0: e.dma_start(out=fm(k.cqT, 0, 4, c0, TT), in_=cq_t[:]),
                  reads=[cq_tb], pwrites=[k.dsa_b])
            P.dma("sp", lambda e, ckv_t=ckv_t, c0=c0: e.dma_start(out=fm(k.ckvT, 0, 2, c0, TT), in_=ckv_t[:]),
                  reads=[ckv_tb], pwrites=[k.dsa_b])
            P.dma("sp", lambda e, kix_t=kix_t, c0=c0: e.dma_start(out=k.kidxT[:, c0:c0 + TT], in_=kix_t[:]),
                  reads=[kix_tb], pwrites=[k.dsa_b])
        P.dma("sp", lambda e: e.dma_start(out=k.widx, in_=widx[:]), reads=[widx_b], pwrites=[k.dsa_b])
    P.barrier()


def phase_dsa_b(k):
    nc, P = k.nc, k.P
    wq = k.w["a_w_qidx"][0]
    import os
    NQB = int(os.environ.get("DSA_NQB", str(NSB)))
    with ExitStack() as st:
        def sb(name, shape, dt=F32):
            return st.enter_context(nc.sbuf_tensor("db_" + name, shape, dt))

        kixT = sb("kixT", [64, S], BF16); kixT_b = P.buf()
        widx = sb("widx", [128, NSB, 16]); widx_b = P.buf()
        wst = sb("wst", [128, 4, 1024]); wst_b = P.buf()
        wqb = sb("wqb", [128, 4, 1024], BF16); wqb_b = P.buf()
        identb = sb("identb", [128, 128], BF16); identb_b = P.buf()
        cm = sb("cm", [128, 128]); cm30 = sb("cm30", [128, 128], BF16); cm_b = P.buf()
        neg30 = sb("neg30", [128, 3, 128], BF16); neg30_b = P.buf()
        cq = [sb("cq%d" % i, [128, 4, 128], BF16) for i in range(2)]; cq_b = P.bufs(2)
        qix = [sb("qix%d" % i, [64, 16, 128], BF16) for i in range(2)]; qix_b = P.bufs(2)
        rl = [sb("rl%d" % i, [128, 512]) for i in range(4)]; rl_b = P.bufs(4)
        acc = [sb("acc%d" % i, [128, S]) for i in range(2)]; acc_b = P.bufs(2)
        m8 = sb("m8", [128, 8]); m8_b = P.buf()
        mq = [sb("mq%d" % i, [128, S], BF16) for i in range(2)]; mq_b = P.bufs(2)
        mT = [sb("mT%d" % i, [128, NSB, 128], BF16) for i in range(2)]; mT_b = P.bufs(2)
        pqi = [st.enter_context(nc.psum_tensor("db_pqi%d" % i, [64, 4, 128], F32)) for i in range(2)]; pqi_b = P.pbufs(2)
        ps = [st.enter_context(nc.psum_tensor("db_ps%d" % i, [128, 512], F32)) for i in range(4)]; ps_b = P.pbufs(4)
        ptr = [st.enter_context(nc.psum_tensor("db_ptr%d" % i, [128, 8, 128], BF16)) for i in range(2)]; ptr_b = P.pbufs(2)

        P.dma("sp", lambda e: e.dma_start(out=kixT[:], in_=k.kidxT), reads=[k.dsa_b], writes=[kixT_b])
        P.dma("sp", lambda e: e.dma_start(out=widx[:], in_=k.widx), reads=[k.dsa_b], writes=[widx_b])
        P.dma("sp", lambda e: e.dma_start(out=wst[:], in_=wq.rearrange("(c p) n -> p c n", p=128)), writes=[wst_b])
        P.op("pool", lambda e: e.tensor_copy(out=wqb[:], in_=wst[:]), reads=[wst_b], writes=[wqb_b])
        P.op("pool", lambda e: e.tensor_copy(out=identb[:], in_=k.ident[:]), reads=[k.ident_b], writes=[identb_b])
        P.dma("sp", lambda e: e.dma_start(out=cm[:], in_=k.consts["dsa_cm"]), pwrites=[cm_b])
        P.op("pool", lambda e: e.memset(neg30[:], MNEG), writes=[neg30_b])
        P.op("dve", lambda e: e.tensor_scalar(out=cm30[:], in0=cm[:], scalar1=-1.0, scalar2=MNEG,
                                              op0=ALU.is_lt, op1=ALU.mult), reads=[cm_b], pwrites=[cm_b])
        npe = 0
        ntr = 0
        for qb in range(NQB):
            Lq = (qb + 1) * 128
            c_, cb_ = cq[qb % 2], cq_b[qb % 2]
            qx, qxb = qix[qb % 2], qix_b[qb % 2]
            ac, acb = acc[qb % 2], acc_b[qb % 2]
            m_, mb_ = mq[qb % 2], mq_b[qb % 2]
            mt, mtb = mT[qb % 2], mT_b[qb % 2]
            P.dma("sp", lambda e, c_=c_, qb=qb: e.dma_start(out=c_[:], in_=fm(k.cqT, 0, 4, qb * 128, 128)),
                  reads=[k.dsa_b], writes=[cb_])
            if qb >= 2:
                for hg in range(4):
                    pq_, pqb = pqi[hg % 2], pqi_b[hg % 2]
                    for hh in range(4):
                        h = hg * 4 + hh
                        for c in range(4):
                            P.op("pe", lambda e, pq_=pq_, hh=hh, h=h, c=c, c_=c_: e.matmul(
                                pq_[:, hh, :], lhsT=wqb[:, c, h * 64:(h + 1) * 64], rhs=c_[:, c, :],
                                start=(c == 0), stop=(c == 3)), reads=[wqb_b, cb_], pwrites=[pqb])
                    P.op("act", lambda e, pq_=pq_, qx=qx, hg=hg: e.activation(
                        out=qx[:, hg * 4:(hg + 1) * 4, :], in_=pq_[:], func=AF.Copy), reads=[pqb], pwrites=[qxb])
                nkt = (Lq + 511) // 512
                for kt in range(nkt):
                    wd = min(512, Lq - kt * 512)
                    ks = slice(kt * 512, kt * 512 + wd)
                    for h in range(16):
                        p_, pb_ = ps[npe % 4], ps_b[npe % 4]
                        r_, rb_ = rl[npe % 4], rl_b[npe % 4]
                        npe += 1
                        P.op("pe", lambda e, p_=p_, qx=qx, h=h, ks=ks, wd=wd: e.matmul(
                            p_[:, 0:wd], lhsT=qx[:, h, :], rhs=kixT[:, ks], start=True, stop=True),
                            reads=[qxb, kixT_b], writes=[pb_])
                        P.op("act", lambda e, p_=p_, r_=r_, wd=wd: e.activation(out=r_[:, 0:wd], in_=p_[:, 0:wd], func=AF.Relu),
                             reads=[pb_], writes=[rb_])
                        if h == 0:
                            P.op("dve", lambda e, r_=r_, ac=ac, ks=ks, wd=wd, qb=qb: e.tensor_scalar(
                                out=ac[:, ks], in0=r_[:, 0:wd], scalar1=widx[:, qb, 0:1], scalar2=None, op0=ALU.mult),
                                reads=[rb_, widx_b], pwrites=[acb])
                        else:
                            P.op("dve", lambda e, r_=r_, ac=ac, ks=ks, wd=wd, qb=qb, h=h: e.scalar_tensor_tensor(
                                out=ac[:, ks], in0=r_[:, 0:wd], scalar=widx[:, qb, h:h + 1], in1=ac[:, ks],
                                op0=ALU.mult, op1=ALU.add), reads=[rb_, widx_b, acb], pwrites=[acb])
                dg = slice(Lq - 128, Lq)
                P.op("dve", lambda e, ac=ac, dg=dg: e.tensor_tensor(out=ac[:, dg], in0=ac[:, dg], in1=cm[:], op=ALU.add),
                     reads=[acb, cm_b], writes=[acb])
                for rnd in range(32):
                    P.op("dve", lambda e, ac=ac, Lq=Lq: e.max(out=m8[:], in_=ac[:, 0:Lq]), reads=[acb], writes=[m8_b])
                    P.op("dve", lambda e, ac=ac, Lq=Lq: e.match_replace(
                        out=ac[:, 0:Lq], in_to_replace=m8[:], in_values=ac[:, 0:Lq], imm_value=NEG),
                        reads=[acb, m8_b], writes=[acb])
                P.op("dve", lambda e, ac=ac, m_=m_, Lq=Lq: e.tensor_scalar(
                    out=m_[:, 0:Lq], in0=ac[:, 0:Lq], scalar1=-5.0e29, scalar2=MNEG, op0=ALU.is_gt, op1=ALU.mult),
                    reads=[acb], writes=[mb_])
                P.op("pool", lambda e, m_=m_, dg=dg: e.tensor_tensor(out=m_[:, dg], in0=m_[:, dg], in1=cm30[:], op=ALU.add),
                     reads=[mb_, cm_b], writes=[mb_])
            else:
                P.op("pool", lambda e, m_=m_, Lq=Lq: e.memset(m_[:, 0:Lq], 0.0), writes=[mb_])
                dg = slice(Lq - 128, Lq)
                P.op("pool", lambda e, m_=m_, dg=dg: e.tensor_copy(out=m_[:, dg], in_=cm30[:]), reads=[mb_, cm_b], writes=[mb_])
            for b0 in range(0, qb + 1, 8):
                nb = min(8, qb + 1 - b0)
                t_, tb_ = ptr[ntr % 2], ptr_b[ntr % 2]
                ntr += 1
                for j in range(nb):
                    P.op("pe", lambda e, t_=t_, j=j, m_=m_, b0=b0: e.transpose(
                        out=t_[:, j, :], in_=m_[:, (b0 + j) * 128:(b0 + j + 1) * 128], identity=identb[:]),
                        reads=[mb_, identb_b], pwrites=[tb_])
                P.op("act", lambda e, t_=t_, mt=mt, b0=b0, nb=nb: e.activation(
                    out=mt[:, b0:b0 + nb, :], in_=t_[:, 0:nb, :], func=AF.Copy), reads=[tb_], pwrites=[mtb])
            P.dma("sp", lambda e, mt=mt, qb=qb: e.dma_start(
                out=k.maskT[:, 0:qb + 1, qb * 128:(qb + 1) * 128], in_=mt[:, 0:qb + 1, :]),
                reads=[mtb], pwrites=[k.mask_b])
            nfill = 3 - (qb % 4)
            if nfill > 0:
                P.dma("sp", lambda e, qb=qb, nfill=nfill: e.dma_start(
                    out=k.maskT[:, qb + 1:qb + 1 + nfill, qb * 128:(qb + 1) * 128], in_=neg30[:, 0:nfill, :]),
                    reads=[neg30_b], pwrites=[k.mask_b])
    P.barrier()


def phase_dsa_c(k):
    nc, P = k.nc, k.P
    wuq = k.w["a_w_uq"][0]
    wuk = k.w["a_w_uk"][0]
    wuv = k.w["a_w_uv"][0]
    wo = k.w["a_w_o"][0]
    import os
    NG = int(os.environ.get("DSA_NG", str(NT)))
    with ExitStack() as st:
        def sb(name, shape, dt=F32):
            return st.enter_context(nc.sbuf_tensor("dc_" + name, shape, dt))

        ckvT = sb("ckvT", [128, 2, S], BF16); ckvT_b = P.buf()
        ckvk = sb("ckvk", [128, NSB, 256], BF16); ckvk_b = P.buf()
        wst = [sb("wst%d" % i, [128, 2048]) for i in range(2)]; wst_b = P.bufs(2)
        wuqb = sb("wuqb", [128, 4, 2048], BF16); wuqb_b = P.buf()
        wukb = sb("wukb", [128, 16, 256], BF16); wukb_b = P.buf()
        wuvb = sb("wuvb", [128, 16, 2, 128], BF16); wuvb_b = P.buf()
        cvec = sb("cvec", [128, 16]); cvec_b = P.buf()
        cq = sb("cq", [128, 4, TT], BF16); cq_b = P.buf()
        mk = sb("mk", [128, NSB, TT], BF16); mk_b = P.buf()
        bt = [sb("bt%d" % i, [128, 5, TT]) for i in range(2)]; bt_b = P.bufs(2)
        qT = sb("qT", [128, TT], BF16); qT_b = P.buf()
        ql = [sb("ql%d" % i, [128, 2, TT], BF16) for i in range(2)]; ql_b = P.bufs(2)
        tmp = [sb("tmp%d" % i, [128, TT]) for i in range(2)]; tmp_b = P.bufs(2)
        pT = [sb("pT%d" % i, [128, TT], BF16) for i in range(2)]; pT_b = P.bufs(2)
        rden = sb("rden", [128, TT]); rden_b = P.buf()
        oln = sb("oln", [128, 2, TT], BF16); oln_b = P.buf()
        oT = sb("oT", [128, 16, TT], BF16); oT_b = P.buf()
        wso = WStream(k, st, "dc_wo", DC, 128, nbuf=2)
        hres = [sb("hr%d" % i, [128, TT]) for i in range(2)]; hres_b = P.bufs(2)
        pm = [st.enter_context(nc.psum_tensor("dc_pm%d" % i, [128, TT], F32)) for i in range(2)]; pm_b = P.pbufs(2)
        pl = [st.enter_context(nc.psum_tensor("dc_pl%d" % i, [128, TT], F32)) for i in range(2)]; pl_b = P.pbufs(2)
        po = [st.enter_context(nc.psum_tensor("dc_po%d" % i, [128, TT], F32)) for i in range(2)]; po_b = P.pbufs(2)
        pden = st.enter_context(nc.psum_tensor("dc_pden", [128, TT], F32)); pden_b = P.pbuf()

        P.dma("sp", lambda e: e.dma_start(out=ckvT[:], in_=fm(k.ckvT, 0, 2, 0, S)), reads=[k.dsa_b], writes=[ckvT_b])
        P.dma("sp", lambda e: e.dma_start(out=ckvk[:], in_=k.ckv_tok.rearrange("(b p) c -> p b c", p=128)),
              reads=[k.dsa_b], writes=[ckvk_b])
        P.dma("sp", lambda e: e.dma_start(out=cvec[:], in_=k.lay["a_cvec"]), writes=[cvec_b])
        nw = 0
        for c in range(4):
            w_, wb_ = wst[nw % 2], wst_b[nw % 2]; nw += 1
            P.dma("sp", lambda e, w_=w_, c=c: e.dma_start(out=w_[:], in_=wuq[c * 128:(c + 1) * 128, :]), writes=[wb_])
            P.op("pool", lambda e, w_=w_, c=c: e.tensor_copy(out=wuqb[:, c, :], in_=w_[:]), reads=[wb_], pwrites=[wuqb_b])
        for hg in range(2):
            w_, wb_ = wst[nw % 2], wst_b[nw % 2]; nw += 1
            P.dma("sp", lambda e, w_=w_, hg=hg: e.dma_start(
                out=w_[:].rearrange("p (h c) -> p h c", h=8), in_=wuk[hg * 8:(hg + 1) * 8].rearrange("h d c -> d h c")),
                writes=[wb_])
            P.op("pool", lambda e, w_=w_, hg=hg: e.tensor_copy(
                out=wukb[:, hg * 8:(hg + 1) * 8, :], in_=w_[:].rearrange("p (h c) -> p h c", h=8)),
                reads=[wb_], pwrites=[wukb_b])
        for hg in range(2):
            w_, wb_ = wst[nw % 2], wst_b[nw % 2]; nw += 1
            P.dma("sp", lambda e, w_=w_, hg=hg: e.dma_start(
                out=w_[:].rearrange("p (h a d) -> p h a d", h=8, a=2),
                in_=wuv[hg * 8:(hg + 1) * 8].rearrange("h (a p) d -> p h a d", p=128)), writes=[wb_])
            P.op("pool", lambda e, w_=w_, hg=hg: e.tensor_copy(
                out=wuvb[:, hg * 8:(hg + 1) * 8, :, :], in_=w_[:].rearrange("p (h a d) -> p h a d", h=8, a=2)),
                reads=[wb_], pwrites=[wuvb_b])
        nl = 0
        nd = 0
        nh = 0
        for g in range(NG):
            q0 = g * TT
            nsb = 4 * g + 4
            P.dma("sp", lambda e, q0=q0: e.dma_start(out=cq[:], in_=fm(k.cqT, 0, 4, q0, TT)), reads=[k.dsa_b], writes=[cq_b])
            P.dma("sp", lambda e, q0=q0, nsb=nsb: e.dma_start(out=mk[:, 0:nsb, :], in_=k.maskT[:, 0:nsb, q0:q0 + TT]),
                  reads=[k.mask_b], writes=[mk_b])
            for h in range(16):
                b_, bb_ = bt[nh % 2], bt_b[nh % 2]
                q_, qb_ = ql[nh % 2], ql_b[nh % 2]
                nh += 1
                P.dma("sp", lambda e, b_=b_, h=h: e.dma_start(out=b_[:], in_=k.lay["a_bt"][h]), writes=[bb_])
                pq_, pqb = pm[0], pm_b[0]
                for c in range(4):
                    P.op("pe", lambda e, pq_=pq_, c=c, h=h: e.matmul(
                        pq_[:], lhsT=wuqb[:, c, h * 128:(h + 1) * 128], rhs=cq[:, c, :], start=(c == 0), stop=(c == 3)),
                        reads=[wuqb_b, cq_b], pwrites=[pqb])
                P.op("act", lambda e, pq_=pq_: e.activation(out=qT[:], in_=pq_[:], func=AF.Copy), reads=[pqb], writes=[qT_b])
                for cc in range(2):
                    pc_, pcb = pm[1], pm_b[1]
                    P.op("pe", lambda e, pc_=pc_, cc=cc, h=h: e.matmul(
                        pc_[:], lhsT=wukb[:, h, cc * 128:(cc + 1) * 128], rhs=qT[:], start=True, stop=True),
                        reads=[wukb_b, qT_b], writes=[pcb])
                    P.op("act", lambda e, pc_=pc_, q_=q_, cc=cc: e.activation(out=q_[:, cc, :], in_=pc_[:], func=AF.Copy),
                         reads=[pcb], pwrites=[qb_])
                for sbk in range(nsb):
                    ss_ = slice(sbk * 128, (sbk + 1) * 128)
                    l_, lb_ = pl[nl % 2], pl_b[nl % 2]
                    t_, tb_ = tmp[nl % 2], tmp_b[nl % 2]
                    p_, pb_ = pT[nl % 2], pT_b[nl % 2]
                    nl += 1
                    for cc in range(2):
                        P.op("pe", lambda e, l_=l_, cc=cc, ss_=ss_, q_=q_: e.matmul(
                            l_[:], lhsT=ckvT[:, cc, ss_], rhs=q_[:, cc, :], start=(cc == 0), stop=(cc == 1)),
                            reads=[ckvT_b, qb_], pwrites=[lb_])
                    P.op("dve", lambda e, l_=l_, t_=t_, sbk=sbk: e.scalar_tensor_tensor(
                        out=t_[:], in0=l_[:], scalar=ATT_SCALE, in1=mk[:, sbk, :], op0=ALU.mult, op1=ALU.add),
                        reads=[lb_, mk_b], writes=[tb_])
                    r = sbk - (4 * g - 1)
                    if r >= 0:
                        P.op("pool", lambda e, t_=t_, b_=b_, r=r: e.tensor_tensor(out=t_[:], in0=t_[:], in1=b_[:, r, :], op=ALU.add),
                             reads=[tb_, bb_], writes=[tb_])
                        P.op("act", lambda e, t_=t_, p_=p_: e.activation(out=p_[:], in_=t_[:], func=AF.Exp),
                             reads=[tb_], writes=[pb_])
                    else:
                        P.op("act", lambda e, t_=t_, p_=p_, h=h: e.activation(
                            out=p_[:], in_=t_[:], func=AF.Exp, bias=cvec[:, h:h + 1]), reads=[tb_, cvec_b], writes=[pb_])
                    for cc in range(2):
                        P.op("pe", lambda e, cc=cc, sbk=sbk, p_=p_, nsb=nsb: e.matmul(
                            po[cc][:], lhsT=ckvk[:, sbk, cc * 128:(cc + 1) * 128], rhs=p_[:],
                            start=(sbk == 0), stop=(sbk == nsb - 1)), reads=[ckvk_b, pb_], pwrites=[po_b[cc]])
                    P.op("pe", lambda e, sbk=sbk, p_=p_, nsb=nsb: e.matmul(
                        pden[:], lhsT=k.ones_bf[:], rhs=p_[:], start=(sbk == 0), stop=(sbk == nsb - 1)),
                        reads=[k.ones_b, pb_], pwrites=[pden_b])
                P.op("dve", lambda e: e.reciprocal(out=rden[:], in_=pden[:]), reads=[pden_b], writes=[rden_b])
                for cc in range(2):
                    P.op("dve", lambda e, cc=cc: e.tensor_tensor(out=oln[:, cc, :], in0=po[cc][:], in1=rden[:], op=ALU.mult),
                         reads=[po_b[cc], rden_b], pwrites=[oln_b])
                pq_, pqb = pm[0], pm_b[0]
                for cc in range(2):
                    P.op("pe", lambda e, pq_=pq_, cc=cc, h=h: e.matmul(
                        pq_[:], lhsT=wuvb[:, h, cc, :], rhs=oln[:, cc, :], start=(cc == 0), stop=(cc == 1)),
                        reads=[wuvb_b, oln_b], pwrites=[pqb])
                P.op("act", lambda e, pq_=pq_, h=h: e.activation(out=oT[:, h, :], in_=pq_[:], func=AF.Copy),
                     reads=[pqb], pwrites=[oT_b])
            for dc in range(DC):
                w_, wb_ = wso.load(wo, 0, dc * 128)
                p_, pb_ = pm[nd % 2], pm_b[nd % 2]
                hr, hrb = hres[nd % 2], hres_b[nd % 2]
                residual_load(k, hr, hrb, dc, g)
                for c in range(DC):
                    P.op("pe", lambda e, p_=p_, w_=w_, c=c: e.matmul(
                        p_[:], lhsT=w_[:, c, :], rhs=oT[:, c, :], start=(c == 0), stop=(c == DC - 1)),
                        reads=[wb_, oT_b], pwrites=[pb_])
                P.op("dve", lambda e, p_=p_, hr=hr: e.tensor_tensor(out=hr[:], in0=p_[:], in1=hr[:], op=ALU.add),
                     reads=[pb_, hrb], writes=[hrb])
                residual_store(k, hr, hrb, dc, g)
                nd += 1
    P.barrier()


WEIGHT_SHAPES = {
    "a_w_in": [1, 2048, 848], "a_w_uq": [1, 512, 2048], "a_w_qidx": [1, 512, 1024],
    "a_w_uk": [1, 16, 128, 256], "a_w_uv": [1, 16, 256, 128], "a_w_o": [1, 2048, 2048],
    "b_w_in": [1, 2048, 8192], "b_w_o": [1, 2048, 2048],
    "c_w_group": [1, 4, 512, 512],
    "d_w_pw1": [1, 2048, 4096], "d_w_pw2": [1, 2048, 2048],
    "ffn_w_up": [4, 2048, 11264], "ffn_w_down": [4, 5632, 2048],
}

PLAN_FULL = ["in"] + sum([["norm_mix%d" % i, "mix%d" % i, "norm_ffn%d" % i, "ffn%d" % i] for i in range(DEPTH)], []) + ["out"]


def plan_weights(plan):
    ws = set()
    for p in plan:
        if p.startswith("ffn"):
            ws.update(["ffn_w_up", "ffn_w_down"])
        if p == "mix0":
            ws.update(["a_w_in", "a_w_uq", "a_w_qidx", "a_w_uk", "a_w_uv", "a_w_o"])
        if p == "mix1":
            ws.update(["b_w_in", "b_w_o"])
        if p == "mix2":
            ws.update(["c_w_group"])
        if p == "mix3":
            ws.update(["d_w_pw1", "d_w_pw2"])
    return sorted(ws)


def build_nc(plan=PLAN_FULL, raw_out=False):
    nc = bass.Bass("TRN2", target_bir_lowering=False)
    k = K()
    k.nc = nc
    k.raw_out = raw_out
    k.R = vec_registry()
    k.x = nc.dram_tensor("x", [S, D], F32, kind="ExternalInput").ap()
    k.out = nc.dram_tensor("out", [S, D], F32, kind="ExternalOutput").ap()
    vecs_d = nc.dram_tensor("vecs", [128, k.R.n], F32, kind="ExternalInput").ap()
    ident_d = nc.dram_tensor("ident", [128, 128], F32, kind="ExternalInput").ap()
    k.consts = {}
    for name, arr in make_consts().items():
        k.consts[name] = nc.dram_tensor("c_" + name, list(arr.shape), F32, kind="ExternalInput").ap()
    k.w = {}
    for name in plan_weights(plan):
        k.w[name] = nc.dram_tensor(name, WEIGHT_SHAPES[name], F32, kind="ExternalInput").ap()
    k.lay = {}
    if "mix0" in plan:
        for name, shp in DSA_LAYOUT_SHAPES.items():
            k.lay[name] = nc.dram_tensor(name, shp, F32, kind="ExternalInput").ap()
        k.cqT = nc.dram_tensor("s_cqT", [512, S], BF16).ap()
        k.ckvT = nc.dram_tensor("s_ckvT", [256, S], BF16).ap()
        k.ckv_tok = nc.dram_tensor("s_ckvtok", [S, 256], BF16).ap()
        k.kidxT = nc.dram_tensor("s_kidxT", [64, S], BF16).ap()
        k.widx = nc.dram_tensor("s_widx", [128, NSB, 16], F32).ap()
        k.maskT = nc.dram_tensor("s_maskT", [128, NSB, S], BF16).ap()
    k.hT = nc.dram_tensor("hT", [D, S], F32).ap()
    k.xnT = nc.dram_tensor("xnT", [D, S], BF16).ap()
    with ExitStack() as st:
        P = Prog(nc, st)
        k.P = P
        k.hT_b = P.bufs(NT)
        k.xnT_b = P.bufs(NT)
        k.out_b = P.buf()
        k.dsa_b = P.buf()
        k.mask_b = P.buf()
        k.vecs = st.enter_context(nc.sbuf_tensor("vecs_t", [128, k.R.n], F32))
        k.vecs_b = P.buf()
        k.ident = st.enter_context(nc.sbuf_tensor("ident_t", [128, 128], F32))
        k.ident_b = P.buf()
        k.ones_bf = st.enter_context(nc.sbuf_tensor("ones_bf", [128, 128], BF16))
        k.ones_b = P.buf()
        k.eps_t = st.enter_context(nc.sbuf_tensor("eps_t", [128, 1], F32))
        k.eps_b = P.buf()
        P.dma("sp", lambda e: e.dma_start(out=k.vecs[:], in_=vecs_d), writes=[k.vecs_b])
        P.dma("sp", lambda e: e.dma_start(out=k.ident[:], in_=ident_d), writes=[k.ident_b])
        P.op("pool", lambda e: e.memset(k.ones_bf[:], 1.0), writes=[k.ones_b])
        P.op("pool", lambda e: e.memset(k.eps_t[:], EPS), writes=[k.eps_b])
        k.nc = NCProxy(nc)
        for p in plan:
            k.nc.tag += 1
            if p == "in":
                phase_in(k)
            elif p == "out":
                phase_out(k)
            elif p.startswith("norm_"):
                phase_norm(k, p)
            elif p.startswith("ffn"):
                phase_ffn(k, int(p[3:]))
            elif p == "mix0":
                phase_dsa_a(k)
                phase_dsa_b(k)
                phase_dsa_c(k)
            elif p == "mix1":
                phase_mix_hgrn(k, 1)
            elif p == "mix2":
                phase_mix_pool(k)
            elif p == "mix3":
                phase_mix_conf(k)
            else:
                raise NotImplementedError(p)
        P.barrier()
        P.emit()
    return nc


def make_consts():
    c = {}
    invc = np.zeros((128, 64), np.float32)
    for g, w in enumerate((2, 4, 8, 16)):
        for t in range(16):
            invc[:, g * 16 + t] = 1.0 / min(t + 1, w)
    c["pool_invc"] = invc
    hm = np.zeros((64, TT), np.float32)
    for n in range(TT // 64):
        hm[:, n * 64:(n + 1) * 64] = np.triu(np.ones((64, 64), np.float32))
    c["hg_mask"] = hm
    cm = np.zeros((128, 128), np.float32)
    cm[np.triu_indices(128, 1)] = -1.0e30
    c["dsa_cm"] = cm
    return c


def make_in_maps(inp, plan, n_cores=8, xs=None):
    vecs = pack_vecs(inp)
    consts = make_consts()
    ident = np.eye(128, dtype=np.float32)
    wnames = plan_weights(plan)
    lay = dsa_layout_inputs(inp) if "mix0" in plan else {}
    maps = []
    for c in range(n_cores):
        m = {"x": np.ascontiguousarray(inp["x"][c] if xs is None else xs[c]), "vecs": vecs, "ident": ident}
        for w in wnames:
            m[w] = np.ascontiguousarray(inp[w], dtype=np.float32)
        for cn, arr in consts.items():
            m["c_" + cn] = arr
        m.update(lay)
        maps.append(m)
    return maps


def kernel(**inputs):
    inp = {k_: np.asarray(v) for k_, v in inputs.items()}
    nc = build_nc(PLAN_FULL)
    maps = make_in_maps(inp, PLAN_FULL, 8)
    res = run_bass_kernel_spmd(nc, maps, core_ids=list(range(8)))
    return np.stack([np.asarray(r["out"]) for r in res.results], axis=0).astype(np.float32)
```

```python
from contextlib import ExitStack
import math
import numpy as np
import concourse.bass as bass
import concourse.mybir as mybir
from concourse.bass_utils import run_bass_kernel_spmd

F32 = mybir.dt.float32
BF16 = mybir.dt.bfloat16
AF = mybir.ActivationFunctionType
ALU = mybir.AluOpType
AX = mybir.AxisListType

D = 2048
S = 4096
DC = D // 128
TT = 512
NT = S // TT
DFF = 5632
FC = DFF // 128
EPS = 1e-6
DEPTH = 4
NDMA_SEMS = 8


class Buf:
    __slots__ = ("name", "w", "wold", "r", "open", "excl")

    def __init__(self, name):
        self.name = name
        self.excl = False
        self.w = {}
        self.wold = {}
        self.r = {}
        self.open = False


def _mx(d, k, v):
    if d.get(k, 0) < v:
        d[k] = v


class Prog:
    STREAMS = ("pe", "act", "dve", "pool", "sp")

    def __init__(self, nc, stack):
        self.nc = nc
        self.ops = {s: [] for s in self.STREAMS}
        self.sems = {}
        self.known = {s: {} for s in self.STREAMS}
        self.cnt = {}
        for s in ("pe", "act", "dve", "pool"):
            self.sems[s] = stack.enter_context(nc.semaphore("c_" + s))
            self.cnt[s] = 0
        self.dma_sems = {}
        self.dma_n = {}
        for q in ("sp", "act", "pool"):
            self.dma_sems[q] = []
            for i in range(NDMA_SEMS):
                k = "d_%s%d" % (q, i)
                self.sems[k] = stack.enter_context(nc.semaphore(k))
                self.cnt[k] = 0
                self.dma_sems[q].append(k)
            self.dma_n[q] = 0
        self.nbuf = 0

    def buf(self, name=None):
        self.nbuf += 1
        return Buf(name or "b%d" % self.nbuf)

    def bufs(self, n, name=None):
        return [self.buf() for _ in range(n)]

    def pbuf(self):
        b = self.buf()
        b.excl = True
        return b

    def pbufs(self, n):
        return [self.pbuf() for _ in range(n)]

    def _deps(self, stream, reads, writes, pwrites, own_key):
        need = {}

        def add(k, v, same_ok):
            if k == own_key and same_ok:
                return
            _mx(need, k, v)

        for b in reads:
            for k, v in b.w.items():
                add(k, v, False)
            for k, v in b.wold.items():
                add(k, v, False)
        for b in writes:
            for dd in (b.w, b.wold, b.r):
                for k, v in dd.items():
                    add(k, v, True)
        for b in pwrites:
            if not b.open:
                for k, v in b.w.items():
                    _mx(b.wold, k, v)
                b.w = {}
                b.open = True
            for dd in (b.wold, b.r):
                for k, v in dd.items():
                    add(k, v, True)
        kn = self.known[stream]
        waits = []
        for k, v in need.items():
            if kn.get(k, 0) < v:
                kn[k] = v
                waits.append((k, v))
        return waits

    def _commit(self, tok, reads, writes, pwrites):
        k, v = tok
        for b in reads:
            _mx(b.r, k, v)
            b.open = False
        for b in writes:
            b.w = {k: v}
            b.wold = {}
            b.r = {}
            b.open = False
        for b in pwrites:
            _mx(b.w, k, v)

    def op(self, stream, fn, reads=(), writes=(), pwrites=()):
        if stream != "pe" and any(b.excl for b in reads):
            writes = list(writes) + [b for b in reads if b.excl]
            reads = [b for b in reads if not b.excl]
        waits = self._deps(stream, reads, writes, pwrites, stream)
        self.cnt[stream] += 1
        tok = (stream, self.cnt[stream])
        self.ops[stream].append((waits, fn, (stream, 1)))
        self._commit(tok, reads, writes, pwrites)
        return tok

    def dma(self, q, fn, reads=(), writes=(), pwrites=()):
        i = self.dma_n[q]
        self.dma_n[q] += 1
        key = self.dma_sems[q][i % NDMA_SEMS]
        waits = self._deps(q, reads, writes, pwrites, None)
        prev = self.cnt[key]
        kn = self.known[q]
        if prev > 0 and kn.get(key, 0) < prev:
            kn[key] = prev
            waits.append((key, prev))
        self.cnt[key] = prev + 16
        tok = (key, prev + 16)
        self.ops[q].append((waits, fn, (key, 16)))
        self._commit(tok, reads, writes, pwrites)
        return tok

    def barrier(self):
        cur = dict(self.cnt)
        for s in self.STREAMS:
            waits = []
            for k, v in cur.items():
                if v > 0 and self.known[s].get(k, 0) < v:
                    self.known[s][k] = v
                    waits.append((k, v))
            if waits:
                self.ops[s].append((waits, None, None))

    def emit(self):
        nc = self.nc
        sems = self.sems

        def run(stream):
            def body(eng):
                for waits, fn, inc in self.ops[stream]:
                    for k, v in waits:
                        eng.wait_ge(sems[k], v)
                    if fn is not None:
                        fn(eng).then_inc(sems[inc[0]], inc[1])
            return body

        with nc.Block() as block:
            block.tensor(run("pe"))
            block.scalar(run("act"))
            block.vector(run("dve"))
            block.gpsimd(run("pool"))
            block.sync(run("sp"))


class VecReg:
    def __init__(self):
        self.off = {}
        self.n = 0

    def add(self, name, ncols):
        self.off[name] = self.n
        self.n += ncols


def vec_registry():
    R = VecReg()
    for i in range(DEPTH):
        R.add("norm_mix%d" % i, DC)
        R.add("norm_ffn%d" % i, DC)
        R.add("ffn_b_conv%d" % i, 2 * FC)
        for k in range(3):
            R.add("ffn_w_conv%d_%d" % (i, k), 2 * FC)
    R.add("final_norm", DC)
    R.add("c_scale", DC)
    R.add("d_b_pw1", 2 * DC)
    for k in range(31):
        R.add("d_w_dw%d" % k, DC)
    R.add("d_b_dw", DC)
    R.add("d_ln_g", DC)
    R.add("d_ln_b", DC)
    R.add("d_b_pw2", DC)
    R.add("b_g_norm", DC)
    for i in range(DEPTH):
        R.add("b_lb%d" % i, DC)
    return R


def _cols(v):
    v = np.ascontiguousarray(v, dtype=np.float32).reshape(-1, 128)
    return v.T


def pack_vecs(inp):
    R = vec_registry()
    out = np.zeros((128, R.n), np.float32)

    def put(name, v):
        c = _cols(v)
        out[:, R.off[name]:R.off[name] + c.shape[1]] = c

    for i in range(DEPTH):
        put("norm_mix%d" % i, inp["norm_mix"][i])
        put("norm_ffn%d" % i, inp["norm_ffn"][i])
        put("ffn_b_conv%d" % i, inp["ffn_b_conv"][i])
        for k in range(3):
            put("ffn_w_conv%d_%d" % (i, k), inp["ffn_w_conv"][i, k])
        put("b_lb%d" % i, inp["b_lower_bounds"][i])
    put("final_norm", inp["final_norm"])
    put("c_scale", inp["c_scale"][0])
    put("d_b_pw1", inp["d_b_pw1"][0])
    for k in range(31):
        put("d_w_dw%d" % k, inp["d_w_dw"][0, k])
    put("d_b_dw", inp["d_b_dw"][0])
    put("d_ln_g", inp["d_ln_g"][0])
    put("d_ln_b", inp["d_ln_b"][0])
    put("d_b_pw2", inp["d_b_pw2"][0])
    put("b_g_norm", inp["b_g_norm"][0])
    return out


class K:
    pass


class NCProxy:
    def __init__(self, nc):
        self._nc = nc
        self.tag = 0

    def sbuf_tensor(self, name, *a, **kw):
        return self._nc.sbuf_tensor("%s_%d" % (name, self.tag), *a, **kw)

    def psum_tensor(self, name, *a, **kw):
        return self._nc.psum_tensor("%s_%d" % (name, self.tag), *a, **kw)

    def __getattr__(self, n):
        return getattr(self._nc, n)


def fm(ap2d, c0, nck, t0, nt):
    return ap2d[c0 * 128:(c0 + nck) * 128, t0:t0 + nt].rearrange("(c p) t -> p c t", p=128)


def phase_in(k):
    nc, P = k.nc, k.P
    with ExitStack() as st:
        xin = [st.enter_context(nc.sbuf_tensor("pi_x%d" % i, [128, D], F32)) for i in range(2)]
        xin_b = P.bufs(2)
        stg = [st.enter_context(nc.sbuf_tensor("pi_s%d" % i, [128, DC, TT], F32)) for i in range(2)]
        stg_b = P.bufs(2)
        ps = [st.enter_context(nc.psum_tensor("pi_p%d" % i, [128, 512], F32)) for i in range(4)]
        ps_b = P.pbufs(4)
        n = 0
        for tt in range(NT):
            sg, sgb = stg[tt % 2], stg_b[tt % 2]
            for sub in range(4):
                si = tt * 4 + sub
                xt, xb = xin[si % 2], xin_b[si % 2]
                P.dma("sp", lambda e, xt=xt, si=si: e.dma_start(out=xt[:], in_=k.x[si * 128:(si + 1) * 128, :]),
                      writes=[xb])
                for cg in range(4):
                    pt, pb = ps[n % 4], ps_b[n % 4]
                    for ci in range(4):
                        c = cg * 4 + ci
                        P.op("pe", lambda e, pt=pt, xt=xt, c=c, ci=ci: e.transpose(
                            out=pt[:, ci * 128:(ci + 1) * 128], in_=xt[:, c * 128:(c + 1) * 128],
                            identity=k.ident[:]), reads=[xb, k.ident_b], pwrites=[pb])
                    eng = "act" if n % 2 == 0 else "dve"
                    if eng == "act":
                        P.op("act", lambda e, pt=pt, sg=sg, cg=cg, sub=sub: e.activation(
                            out=sg[:, cg * 4:(cg + 1) * 4, sub * 128:(sub + 1) * 128],
                            in_=pt[:].rearrange("p (c s) -> p c s", c=4), func=AF.Copy),
                            reads=[pb], pwrites=[sgb])
                    else:
                        P.op("dve", lambda e, pt=pt, sg=sg, cg=cg, sub=sub: e.tensor_copy(
                            out=sg[:, cg * 4:(cg + 1) * 4, sub * 128:(sub + 1) * 128],
                            in_=pt[:].rearrange("p (c s) -> p c s", c=4)),
                            reads=[pb], pwrites=[sgb])
                    n += 1
            P.dma("sp", lambda e, sg=sg, tt=tt: e.dma_start(out=fm(k.hT, 0, DC, tt * TT, TT), in_=sg[:]),
                  reads=[sgb], writes=[k.hT_b[tt]])
    P.barrier()


def norm_tile(k, st_tiles, src_h, src_hb, gcol, out_tile, out_b, tag):
    nc, P = k.nc, k.P
    sq, sq_b, pss, pss_b, rb, rb_b = st_tiles
    for c in range(DC):
        P.op("act", lambda e, c=c: e.activation(out=sq[:, c, :], in_=src_h[:, c, :], func=AF.Square),
             reads=[src_hb], pwrites=[sq_b])
    for c in range(DC):
        P.op("pe", lambda e, c=c: e.matmul(pss[:], lhsT=k.ones_bf[:], rhs=sq[:, c, :],
                                           start=(c == 0), stop=(c == DC - 1)),
             reads=[sq_b, k.ones_b], pwrites=[pss_b])
    P.op("act", lambda e: e.activation(out=rb[:], in_=pss[:], func=AF.Sqrt, bias=k.eps_t[:, 0:1], scale=1.0 / D),
         reads=[pss_b, k.eps_b], writes=[rb_b])
    P.op("dve", lambda e: e.reciprocal(out=rb[:], in_=rb[:]), reads=[rb_b], writes=[rb_b])
    for c in range(DC):
        P.op("dve", lambda e, c=c: e.scalar_tensor_tensor(
            out=out_tile[:, c, :], in0=src_h[:, c, :], scalar=k.vecs[:, gcol + c:gcol + c + 1],
            in1=rb[:], op0=ALU.mult, op1=ALU.mult),
            reads=[src_hb, rb_b, k.vecs_b], pwrites=[out_b])


def phase_norm(k, gname):
    nc, P = k.nc, k.P
    gcol = k.R.off[gname]
    with ExitStack() as st:
        hin = [st.enter_context(nc.sbuf_tensor("pn_h%d" % i, [128, DC, TT], F32)) for i in range(2)]
        hin_b = P.bufs(2)
        sq = st.enter_context(nc.sbuf_tensor("pn_sq", [128, DC, TT], BF16))
        rb = st.enter_context(nc.sbuf_tensor("pn_rb", [128, TT], F32))
        xo = [st.enter_context(nc.sbuf_tensor("pn_o%d" % i, [128, DC, TT], BF16)) for i in range(2)]
        xo_b = P.bufs(2)
        pss = st.enter_context(nc.psum_tensor("pn_ps", [128, TT], F32))
        tiles = (sq, P.buf(), pss, P.pbuf(), rb, P.buf())
        for tt in range(NT):
            h, hb = hin[tt % 2], hin_b[tt % 2]
            o, ob = xo[tt % 2], xo_b[tt % 2]
            P.dma("sp", lambda e, h=h, tt=tt: e.dma_start(out=h[:], in_=fm(k.hT, 0, DC, tt * TT, TT)),
                  reads=[k.hT_b[tt]], writes=[hb])
            norm_tile(k, tiles, h, hb, gcol, o, ob, "pn")
            P.dma("sp", lambda e, o=o, tt=tt: e.dma_start(out=fm(k.xnT, 0, DC, tt * TT, TT), in_=o[:]),
                  reads=[ob], writes=[k.xnT_b[tt]])
    P.barrier()


def phase_out(k):
    nc, P = k.nc, k.P
    gcol = k.R.off["final_norm"]
    with ExitStack() as st:
        hin = [st.enter_context(nc.sbuf_tensor("po_h%d" % i, [128, DC, TT], F32)) for i in range(2)]
        hin_b = P.bufs(2)
        sq = st.enter_context(nc.sbuf_tensor("po_sq", [128, DC, TT], BF16))
        rb = st.enter_context(nc.sbuf_tensor("po_rb", [128, TT], F32))
        xo = st.enter_context(nc.sbuf_tensor("po_o", [128, DC, TT], F32))
        xo_b = P.buf()
        og = [st.enter_context(nc.sbuf_tensor("po_g%d" % i, [128, D], F32)) for i in range(2)]
        og_b = P.bufs(2)
        pss = st.enter_context(nc.psum_tensor("po_ps", [128, TT], F32))
        ps = [st.enter_context(nc.psum_tensor("po_p%d" % i, [128, 512], F32)) for i in range(4)]
        ps_b = P.pbufs(4)
        tiles = (sq, P.buf(), pss, P.pbuf(), rb, P.buf())
        n = 0
        toks = []
        for tt in range(NT):
            h, hb = hin[tt % 2], hin_b[tt % 2]
            P.dma("sp", lambda e, h=h, tt=tt: e.dma_start(out=h[:], in_=fm(k.hT, 0, DC, tt * TT, TT)),
                  reads=[k.hT_b[tt]], writes=[hb])
            if k.raw_out:
                src, srcb = h, hb
            else:
                norm_tile(k, tiles, h, hb, gcol, xo, xo_b, "po")
                src, srcb = xo, xo_b
            for sub in range(4):
                si = tt * 4 + sub
                o, ob = og[si % 2], og_b[si % 2]
                for cg in range(4):
                    pt, pb = ps[n % 4], ps_b[n % 4]
                    for ci in range(4):
                        c = cg * 4 + ci
                        P.op("pe", lambda e, pt=pt, src=src, c=c, ci=ci, sub=sub: e.transpose(
                            out=pt[:, ci * 128:(ci + 1) * 128], in_=src[:, c, sub * 128:(sub + 1) * 128],
                            identity=k.ident[:]), reads=[srcb, k.ident_b], pwrites=[pb])
                    if n % 2 == 0:
                        P.op("act", lambda e, pt=pt, o=o, cg=cg: e.activation(
                            out=o[:, cg * 512:(cg + 1) * 512], in_=pt[:], func=AF.Copy),
                            reads=[pb], pwrites=[ob])
                    else:
                        P.op("dve", lambda e, pt=pt, o=o, cg=cg: e.tensor_copy(
                            out=o[:, cg * 512:(cg + 1) * 512], in_=pt[:]),
                            reads=[pb], pwrites=[ob])
                    n += 1
                toks.append(P.dma("sp", lambda e, o=o, si=si: e.dma_start(
                    out=k.out[si * 128:(si + 1) * 128, :], in_=o[:]), reads=[ob], writes=[k.out_b]))
    P.barrier()


def phase_ffn(k, L):
    phase_ffn_up(k, L)
    phase_ffn_down(k, L)


def phase_ffn_up(k, L):
    nc, P = k.nc, k.P
    R = k.R
    wup = k.w["ffn_w_up"][L]
    bcol = R.off["ffn_b_conv%d" % L]
    wcol = [R.off["ffn_w_conv%d_%d" % (L, t)] for t in range(3)]
    with ExitStack() as st:
        xn = st.enter_context(nc.sbuf_tensor("fu_xn", [128, DC, S], BF16))
        xn_b = P.bufs(NT)
        sup = st.enter_context(nc.sbuf_tensor("fu_su", [128, 2, DC, 128], F32))
        sup_b = P.buf()
        wub = [st.enter_context(nc.sbuf_tensor("fu_wu%d" % i, [128, 2, DC, 128], BF16)) for i in range(2)]
        wub_b = P.bufs(2)
        ub = [st.enter_context(nc.sbuf_tensor("fu_ub%d" % i, [128, 2, TT + 2], F32)) for i in range(2)]
        ub_b = P.bufs(2)
        acc = [st.enter_context(nc.sbuf_tensor("fu_ac%d" % i, [128, 2, TT], F32)) for i in range(2)]
        acc_b = P.bufs(2)
        sil = [st.enter_context(nc.sbuf_tensor("fu_si%d" % i, [128, TT], F32)) for i in range(2)]
        sil_b = P.bufs(2)
        grow = [st.enter_context(nc.sbuf_tensor("fu_g%d" % i, [128, S], BF16)) for i in range(2)]
        grow_b = P.bufs(2)
        pu = [st.enter_context(nc.psum_tensor("fu_pu%d" % i, [128, 2, TT], F32)) for i in range(2)]
        pu_b = P.pbufs(2)
        for tt in range(NT):
            P.dma("sp", lambda e, tt=tt: e.dma_start(out=xn[:, :, tt * TT:(tt + 1) * TT], in_=fm(k.xnT, 0, DC, tt * TT, TT)),
                  reads=[k.xnT_b[tt]], writes=[xn_b[tt]])
        nu = 0
        for j in range(FC):
            w_, wb_ = wub[j % 2], wub_b[j % 2]
            gr, grb = grow[j % 2], grow_b[j % 2]
            for half in range(2):
                n0 = half * DFF + j * 128
                P.dma("sp", lambda e, half=half, n0=n0: e.dma_start(
                    out=sup[:, half, :, :], in_=wup[:, n0:n0 + 128].rearrange("(c p) n -> p c n", p=128)),
                    pwrites=[sup_b])
            P.op("pool", lambda e, w_=w_: e.tensor_copy(out=w_[:], in_=sup[:]), reads=[sup_b], writes=[wb_])
            for tt in range(NT):
                ts_ = slice(tt * TT, (tt + 1) * TT)
                u_, ubb = ub[nu % 2], ub_b[nu % 2]
                up_, upb = ub[(nu + 1) % 2], ub_b[(nu + 1) % 2]
                a_, ab_ = acc[nu % 2], acc_b[nu % 2]
                si_, sib = sil[nu % 2], sil_b[nu % 2]
                p_, pb_ = pu[nu % 2], pu_b[nu % 2]
                for half in range(2):
                    for c in range(DC):
                        P.op("pe", lambda e, p_=p_, w_=w_, half=half, c=c, ts_=ts_: e.matmul(
                            p_[:, half, :], lhsT=w_[:, half, c, :], rhs=xn[:, c, ts_],
                            start=(c == 0), stop=(c == DC - 1)),
                            reads=[wb_, xn_b[tt]], pwrites=[pb_])
                if tt == 0:
                    P.op("pool", lambda e, u_=u_: e.memset(u_[:, :, 0:2], 0.0), pwrites=[ubb])
                else:
                    P.op("pool", lambda e, u_=u_, up_=up_: e.tensor_copy(out=u_[:, :, 0:2], in_=up_[:, :, TT:TT + 2]),
                         reads=[upb], pwrites=[ubb])
                P.op("act", lambda e, u_=u_, p_=p_: e.activation(
                    out=u_[:, :, 2:TT + 2], in_=p_[:], func=AF.Copy), reads=[pb_], pwrites=[ubb])
                for half in range(2):
                    col = half * FC + j
                    P.op("dve", lambda e, a_=a_, u_=u_, half=half, col=col: e.tensor_scalar(
                        out=a_[:, half, :], in0=u_[:, half, 2:TT + 2],
                        scalar1=k.vecs[:, wcol[2] + col:wcol[2] + col + 1],
                        scalar2=k.vecs[:, bcol + col:bcol + col + 1], op0=ALU.mult, op1=ALU.add),
                        reads=[ubb, k.vecs_b], pwrites=[ab_])
                for tap in (1, 0):
                    for half in range(2):
                        col = half * FC + j
                        P.op("dve", lambda e, a_=a_, u_=u_, half=half, col=col, tap=tap: e.scalar_tensor_tensor(
                            out=a_[:, half, :], in0=u_[:, half, tap:tap + TT],
                            scalar=k.vecs[:, wcol[tap] + col:wcol[tap] + col + 1],
                            in1=a_[:, half, :], op0=ALU.mult, op1=ALU.add),
                            reads=[ubb, k.vecs_b, ab_], pwrites=[ab_])
                P.op("act", lambda e, si_=si_, a_=a_: e.activation(out=si_[:], in_=a_[:, 0, :], func=AF.Silu),
                     reads=[ab_], writes=[sib])
                P.op("pool", lambda e, si_=si_, a_=a_, gr=gr, ts_=ts_: e.tensor_tensor(
                    out=gr[:, ts_], in0=si_[:], in1=a_[:, 1, :], op=ALU.mult),
                    reads=[sib, ab_], pwrites=[grb])
                nu += 1
            P.dma("sp", lambda e, gr=gr, j=j: e.dma_start(out=k.gT[j * 128:(j + 1) * 128, :], in_=gr[:]),
                  reads=[grb], pwrites=[k.gT_b])
    P.barrier()


FD_T = 1024


def phase_ffn_down(k, L):
    nc, P = k.nc, k.P
    wdn = k.w["ffn_w_down"][L]
    NTD = S // FD_T
    with ExitStack() as st:
        g = st.enter_context(nc.sbuf_tensor("fd_g", [128, FC, FD_T], BF16))
        g_b = P.bufs(4)
        sdn = [st.enter_context(nc.sbuf_tensor("fd_sd%d" % i, [128, 22, 256], F32)) for i in range(2)]
        sdn_b = P.bufs(2)
        wdb = [st.enter_context(nc.sbuf_tensor("fd_wd%d" % i, [128, FC, 256], BF16)) for i in range(2)]
        wdb_b = P.bufs(2)
        hres = [st.enter_context(nc.sbuf_tensor("fd_hr%d" % i, [128, TT], F32)) for i in range(4)]
        hres_b = P.bufs(4)
        pd = [st.enter_context(nc.psum_tensor("fd_pd%d" % i, [128, TT], F32)) for i in range(4)]
        pd_b = P.pbufs(4)
        nd = 0
        nw = 0
        for t2 in range(NTD):
            t0 = t2 * FD_T
            for q4 in range(4):
                P.dma("sp", lambda e, q4=q4, t0=t0: e.dma_start(
                    out=g[:, q4 * 11:(q4 + 1) * 11, :], in_=fm(k.gT, q4 * 11, 11, t0, FD_T)),
                    reads=[k.gT_b], writes=[g_b[q4]])
            for dp in range(DC // 2):
                w_, wb_ = wdb[nw % 2], wdb_b[nw % 2]
                nw += 1
                for hf in range(2):
                    s_, sb_ = sdn[hf], sdn_b[hf]
                    P.dma("sp", lambda e, s_=s_, hf=hf, dp=dp: e.dma_start(
                        out=s_[:], in_=wdn[hf * 22 * 128:(hf + 1) * 22 * 128, dp * 256:(dp + 1) * 256].rearrange(
                            "(c p) n -> p c n", p=128)), writes=[sb_])
                    P.op("pool", lambda e, s_=s_, w_=w_, hf=hf: e.tensor_copy(
                        out=w_[:, hf * 22:(hf + 1) * 22, :], in_=s_[:]), reads=[sb_], pwrites=[wb_])
                for di in range(2):
                    dc = dp * 2 + di
                    for th in range(FD_T // TT):
                        tt = (t0 // TT) + th
                        p_, pb_ = pd[nd % 4], pd_b[nd % 4]
                        hr, hrb = hres[nd % 4], hres_b[nd % 4]
                        nd += 1
                        residual_load(k, hr, hrb, dc, tt)
                        for c in range(FC):
                            P.op("pe", lambda e, p_=p_, w_=w_, c=c, di=di, th=th: e.matmul(
                                p_[:], lhsT=w_[:, c, di * 128:(di + 1) * 128], rhs=g[:, c, th * TT:(th + 1) * TT],
                                start=(c == 0), stop=(c == FC - 1)),
                                reads=[wb_, g_b[c // 11]], pwrites=[pb_])
                        P.op("dve", lambda e, hr=hr, p_=p_: e.tensor_tensor(out=hr[:], in0=p_[:], in1=hr[:], op=ALU.add),
                             reads=[pb_, hrb], writes=[hrb])
                        residual_store(k, hr, hrb, dc, tt)
    P.barrier()


class WStream:
    def __init__(self, k, st, name, KC, ncol=128, nbuf=2):
        nc, P = k.nc, k.P
        self.k, self.KC, self.ncol, self.nbuf = k, KC, ncol, nbuf
        self.stg = [st.enter_context(nc.sbuf_tensor("%s_s%d" % (name, i), [128, KC, ncol], F32)) for i in range(nbuf)]
        self.wb = [st.enter_context(nc.sbuf_tensor("%s_w%d" % (name, i), [128, KC, ncol], BF16)) for i in range(nbuf)]
        self.stg_b = P.bufs(nbuf)
        self.wb_b = P.bufs(nbuf)
        self.n = 0

    def load(self, W2d, r0, c0):
        P = self.k.P
        i = self.n % self.nbuf
        self.n += 1
        stg, wb = self.stg[i], self.wb[i]
        KC, ncol = self.KC, self.ncol
        P.dma("sp", lambda e: e.dma_start(
            out=stg[:], in_=W2d[r0:r0 + KC * 128, c0:c0 + ncol].rearrange("(c p) n -> p c n", p=128)),
            writes=[self.stg_b[i]])
        P.op("pool", lambda e: e.tensor_copy(out=wb[:], in_=stg[:]), reads=[self.stg_b[i]], writes=[self.wb_b[i]])
        return wb, self.wb_b[i]


def residual_store(k, hr, hrb, dc, tt):
    k.P.dma("sp", lambda e: e.dma_start(
        out=k.hT[dc * 128:(dc + 1) * 128, tt * TT:(tt + 1) * TT], in_=hr[:]),
        reads=[hrb], pwrites=[k.hT_b[tt]])


def residual_load(k, hr, hrb, dc, tt):
    k.P.dma("sp", lambda e: e.dma_start(
        out=hr[:], in_=k.hT[dc * 128:(dc + 1) * 128, tt * TT:(tt + 1) * TT]),
        reads=[k.hT_b[tt]], writes=[hrb])


POOL_H = 15


def phase_mix_pool(k):
    nc, P = k.nc, k.P
    wg = k.w["c_w_group"][0]
    scol = k.R.off["c_scale"]
    H = POOL_H
    W_ = TT + H
    with ExitStack() as st:
        wres = st.enter_context(nc.sbuf_tensor("pl_w", [128, 4, 4, 512], BF16))
        wres_b = P.buf()
        stg = [st.enter_context(nc.sbuf_tensor("pl_s%d" % i, [128, 4, 512], F32)) for i in range(2)]
        stg_b = P.bufs(2)
        invc = st.enter_context(nc.sbuf_tensor("pl_ic", [128, 64], F32))
        invc_b = P.buf()
        P.dma("sp", lambda e: e.dma_start(out=invc[:], in_=k.consts["pool_invc"]), writes=[invc_b])
        for g in range(4):
            sg, sgb = stg[g % 2], stg_b[g % 2]
            P.dma("sp", lambda e, sg=sg, g=g: e.dma_start(
                out=sg[:], in_=wg[g].rearrange("(c p) n -> p c n", p=128)), writes=[sgb])
            P.op("pool", lambda e, sg=sg, g=g: e.tensor_copy(out=wres[:, g, :, :], in_=sg[:]),
                 reads=[sgb], pwrites=[wres_b])
        xn = [st.enter_context(nc.sbuf_tensor("pl_x%d" % i, [128, DC, W_], BF16)) for i in range(2)]
        xn_b = P.bufs(2)
        pp = [[st.enter_context(nc.sbuf_tensor("pl_p%d%d" % (e_, i), [128, W_], F32)) for i in range(2)] for e_ in range(2)]
        pp_b = [P.bufs(2) for _ in range(2)]
        t16 = st.enter_context(nc.sbuf_tensor("pl_t16", [128, 16], F32))
        t16_b = P.buf()
        diff = st.enter_context(nc.sbuf_tensor("pl_d", [128, DC, TT], BF16))
        diff_b = P.buf()
        hres = [st.enter_context(nc.sbuf_tensor("pl_h%d" % i, [128, TT], F32)) for i in range(2)]
        hres_b = P.bufs(2)
        ps = [st.enter_context(nc.psum_tensor("pl_ps%d" % i, [128, TT], F32)) for i in range(2)]
        ps_b = P.pbufs(2)
        nn = 0
        for tt in range(NT):
            x_, xb = xn[tt % 2], xn_b[tt % 2]
            if tt == 0:
                P.op("pool", lambda e, x_=x_: e.memset(x_[:, :, 0:H], 0.0), pwrites=[xb])
                P.dma("sp", lambda e, x_=x_: e.dma_start(out=x_[:, :, H:W_], in_=fm(k.xnT, 0, DC, 0, TT)),
                      reads=[k.xnT_b[0]], pwrites=[xb])
            else:
                P.dma("sp", lambda e, x_=x_, tt=tt: e.dma_start(out=x_[:], in_=fm(k.xnT, 0, DC, tt * TT - H, W_)),
                      reads=[k.xnT_b[tt - 1], k.xnT_b[tt]], writes=[xb])
            for c in range(DC):
                g = c // 4
                w = 2 << g
                ei = c % 2
                eng = "dve" if ei == 0 else "pool"
                cur, curb = x_[:, c, :], xb
                for stp in range(g + 1):
                    sh = 1 << stp
                    nxt, nxtb = pp[ei][stp % 2], pp_b[ei][stp % 2]
                    P.op(eng, lambda e, nxt=nxt, cur=cur, sh=sh: e.tensor_tensor(
                        out=nxt[:, sh:W_], in0=cur[:, sh:W_], in1=cur[:, 0:W_ - sh], op=ALU.add),
                        reads=[curb], writes=[nxtb])
                    cur, curb = nxt[:], nxtb
                P.op("dve", lambda e, cur=cur, c=c, w=w, x_=x_: e.scalar_tensor_tensor(
                    out=diff[:, c, :], in0=cur[:, H:W_], scalar=1.0 / w, in1=x_[:, c, H:W_],
                    op0=ALU.mult, op1=ALU.subtract), reads=[curb, xb], pwrites=[diff_b])
                if tt == 0:
                    P.op("dve", lambda e, cur=cur, g=g: e.tensor_tensor(
                        out=t16[:], in0=cur[:, H:H + 16], in1=invc[:, g * 16:(g + 1) * 16], op=ALU.mult),
                        reads=[curb, invc_b], writes=[t16_b])
                    P.op("dve", lambda e, c=c, x_=x_: e.tensor_tensor(
                        out=diff[:, c, 0:16], in0=t16[:], in1=x_[:, c, H:H + 16], op=ALU.subtract),
                        reads=[t16_b, xb], pwrites=[diff_b])
            for n in range(DC):
                g, ni = n // 4, n % 4
                p_, pb_ = ps[nn % 2], ps_b[nn % 2]
                hr, hrb = hres[nn % 2], hres_b[nn % 2]
                residual_load(k, hr, hrb, n, tt)
                for kc in range(4):
                    P.op("pe", lambda e, p_=p_, g=g, kc=kc, ni=ni: e.matmul(
                        p_[:], lhsT=wres[:, g, kc, ni * 128:(ni + 1) * 128], rhs=diff[:, g * 4 + kc, :],
                        start=(kc == 0), stop=(kc == 3)), reads=[wres_b, diff_b], pwrites=[pb_])
                P.op("dve", lambda e, p_=p_, hr=hr, n=n: e.scalar_tensor_tensor(
                    out=hr[:], in0=p_[:], scalar=k.vecs[:, scol + n:scol + n + 1], in1=hr[:],
                    op0=ALU.mult, op1=ALU.add), reads=[pb_, hrb, k.vecs_b], writes=[hrb])
                residual_store(k, hr, hrb, n, tt)
                nn += 1
    P.barrier()


CONF_W = 31
CONF_H = CONF_W - 1


def phase_mix_conf(k):
    nc, P = k.nc, k.P
    R = k.R
    w1 = k.w["d_w_pw1"][0]
    w2 = k.w["d_w_pw2"][0]
    b1 = R.off["d_b_pw1"]
    wdw = [R.off["d_w_dw%d" % t] for t in range(CONF_W)]
    bdw = R.off["d_b_dw"]
    lng, lnb = R.off["d_ln_g"], R.off["d_ln_b"]
    b2 = R.off["d_b_pw2"]
    H = CONF_H
    W_ = TT + H
    with ExitStack() as st:
        xn = st.enter_context(nc.sbuf_tensor("cf_xn", [128, DC, TT], BF16))
        xn_b = P.buf()
        ws1 = WStream(k, st, "cf_w1", DC, 128, nbuf=3)
        ws2 = WStream(k, st, "cf_w2", DC, 128, nbuf=2)
        ub = st.enter_context(nc.sbuf_tensor("cf_ub", [128, DC, W_], F32))
        ub_b = [P.buf() for _ in range(DC)]
        gate = [st.enter_context(nc.sbuf_tensor("cf_g%d" % i, [128, TT], F32)) for i in range(2)]
        gate_b = P.bufs(2)
        v = st.enter_context(nc.sbuf_tensor("cf_v", [128, DC, TT], F32))
        v_b = [P.buf() for _ in range(DC)]
        sq = st.enter_context(nc.sbuf_tensor("cf_sq", [128, DC, TT], BF16))
        sq_b = P.buf()
        ones_f = st.enter_context(nc.sbuf_tensor("cf_1f", [128, 128], F32))
        ones_fb = P.buf()
        P.op("pool", lambda e: e.memset(ones_f[:], 1.0), writes=[ones_fb])
        mean = st.enter_context(nc.sbuf_tensor("cf_mean", [128, TT], F32))
        mean_b = P.buf()
        rstd = st.enter_context(nc.sbuf_tensor("cf_rstd", [128, TT], F32))
        rstd_b = P.buf()
        tmp = [st.enter_context(nc.sbuf_tensor("cf_t%d" % i, [128, TT], F32)) for i in range(2)]
        tmp_b = P.bufs(2)
        lo = st.enter_context(nc.sbuf_tensor("cf_lo", [128, DC, TT], BF16))
        lo_b = P.buf()
        hres = [st.enter_context(nc.sbuf_tensor("cf_h%d" % i, [128, TT], F32)) for i in range(2)]
        hres_b = P.bufs(2)
        pa = [st.enter_context(nc.psum_tensor("cf_pa%d" % i, [128, TT], F32)) for i in range(2)]
        pa_b = P.pbufs(2)
        pg = [st.enter_context(nc.psum_tensor("cf_pg%d" % i, [128, TT], F32)) for i in range(2)]
        pg_b = P.pbufs(2)
        pm = st.enter_context(nc.psum_tensor("cf_pm", [128, TT], F32))
        pm_b = P.pbuf()
        pq = st.enter_context(nc.psum_tensor("cf_pq", [128, TT], F32))
        pq_b = P.pbuf()
        po = [st.enter_context(nc.psum_tensor("cf_po%d" % i, [128, TT], F32)) for i in range(2)]
        po_b = P.pbufs(2)
        nj = 0
        nd = 0
        for tt in range(NT):
            P.dma("sp", lambda e, tt=tt: e.dma_start(out=xn[:], in_=fm(k.xnT, 0, DC, tt * TT, TT)),
                  reads=[k.xnT_b[tt]], writes=[xn_b])
            for j in range(DC):
                ubj = ub_b[j]
                if tt == 0:
                    P.op("pool", lambda e, j=j: e.memset(ub[:, j, 0:H], 0.0), pwrites=[ubj])
                else:
                    P.op("pool", lambda e, j=j: e.tensor_copy(out=ub[:, j, 0:H], in_=ub[:, j, TT:W_]),
                         reads=[ubj], writes=[ubj])
                wa, wab = ws1.load(w1, 0, j * 128)
                wgt, wgb = ws1.load(w1, 0, D + j * 128)
                pa_, pab = pa[nj % 2], pa_b[nj % 2]
                pg_, pgb = pg[nj % 2], pg_b[nj % 2]
                gt, gtb = gate[nj % 2], gate_b[nj % 2]
                for c in range(DC):
                    P.op("pe", lambda e, pa_=pa_, wa=wa, c=c: e.matmul(
                        pa_[:], lhsT=wa[:, c, :], rhs=xn[:, c, :], start=(c == 0), stop=(c == DC - 1)),
                        reads=[wab, xn_b], pwrites=[pab])
                for c in range(DC):
                    P.op("pe", lambda e, pg_=pg_, wgt=wgt, c=c: e.matmul(
                        pg_[:], lhsT=wgt[:, c, :], rhs=xn[:, c, :], start=(c == 0), stop=(c == DC - 1)),
                        reads=[wgb, xn_b], pwrites=[pgb])
                P.op("act", lambda e, gt=gt, pg_=pg_, j=j: e.activation(
                    out=gt[:], in_=pg_[:], func=AF.Sigmoid, bias=k.vecs[:, b1 + DC + j:b1 + DC + j + 1]),
                    reads=[pgb, k.vecs_b], writes=[gtb])
                P.op("dve", lambda e, gt=gt, pa_=pa_, j=j: e.scalar_tensor_tensor(
                    out=ub[:, j, H:W_], in0=pa_[:], scalar=k.vecs[:, b1 + j:b1 + j + 1], in1=gt[:],
                    op0=ALU.add, op1=ALU.mult), reads=[pab, gtb, k.vecs_b], pwrites=[ubj])
                P.op("act", lambda e, j=j: e.activation(
                    out=v[:, j, :], in_=ub[:, j, H:W_], func=AF.Identity,
                    scale=k.vecs[:, wdw[CONF_W - 1] + j:wdw[CONF_W - 1] + j + 1],
                    bias=k.vecs[:, bdw + j:bdw + j + 1]), reads=[ubj, k.vecs_b], writes=[v_b[j]])
                for tap in range(CONF_W - 1):
                    P.op("dve", lambda e, j=j, tap=tap: e.scalar_tensor_tensor(
                        out=v[:, j, :], in0=ub[:, j, tap:tap + TT],
                        scalar=k.vecs[:, wdw[tap] + j:wdw[tap] + j + 1], in1=v[:, j, :],
                        op0=ALU.mult, op1=ALU.add), reads=[ubj, v_b[j], k.vecs_b], writes=[v_b[j]])
                P.op("act", lambda e, j=j: e.activation(out=sq[:, j, :], in_=v[:, j, :], func=AF.Square),
                     reads=[v_b[j]], pwrites=[sq_b])
                nj += 1
            for c in range(DC):
                P.op("pe", lambda e, c=c: e.matmul(pm[:], lhsT=ones_f[:], rhs=v[:, c, :],
                                                   start=(c == 0), stop=(c == DC - 1)),
                     reads=[ones_fb, v_b[c]], pwrites=[pm_b])
            for c in range(DC):
                P.op("pe", lambda e, c=c: e.matmul(pq[:], lhsT=k.ones_bf[:], rhs=sq[:, c, :],
                                                   start=(c == 0), stop=(c == DC - 1)),
                     reads=[k.ones_b, sq_b], pwrites=[pq_b])
            P.op("act", lambda e: e.activation(out=mean[:], in_=pm[:], func=AF.Copy, scale=1.0 / D),
                 reads=[pm_b], writes=[mean_b])
            P.op("dve", lambda e: e.tensor_tensor(out=rstd[:], in0=mean[:], in1=mean[:], op=ALU.mult),
                 reads=[mean_b], writes=[rstd_b])
            P.op("dve", lambda e: e.scalar_tensor_tensor(
                out=rstd[:], in0=pq[:], scalar=1.0 / D, in1=rstd[:], op0=ALU.mult, op1=ALU.subtract),
                reads=[pq_b, rstd_b], writes=[rstd_b])
            P.op("act", lambda e: e.activation(out=rstd[:], in_=rstd[:], func=AF.Sqrt, bias=k.eps_t[:, 0:1]),
                 reads=[rstd_b, k.eps_b], writes=[rstd_b])
            P.op("dve", lambda e: e.reciprocal(out=rstd[:], in_=rstd[:]), reads=[rstd_b], writes=[rstd_b])
            for c in range(DC):
                t_, tb = tmp[c % 2], tmp_b[c % 2]
                P.op("pool", lambda e, t_=t_, c=c: e.tensor_tensor(out=t_[:], in0=v[:, c, :], in1=mean[:], op=ALU.subtract),
                     reads=[v_b[c], mean_b], writes=[tb])
                P.op("dve", lambda e, t_=t_, c=c: e.scalar_tensor_tensor(
                    out=t_[:], in0=t_[:], scalar=k.vecs[:, lng + c:lng + c + 1], in1=rstd[:],
                    op0=ALU.mult, op1=ALU.mult), reads=[tb, rstd_b, k.vecs_b], writes=[tb])
                P.op("act", lambda e, t_=t_, c=c: e.activation(
                    out=lo[:, c, :], in_=t_[:], func=AF.Silu, bias=k.vecs[:, lnb + c:lnb + c + 1]),
                    reads=[tb, k.vecs_b], pwrites=[lo_b])
            for dc in range(DC):
                w_, wb_ = ws2.load(w2, 0, dc * 128)
                p_, pb_ = po[nd % 2], po_b[nd % 2]
                hr, hrb = hres[nd % 2], hres_b[nd % 2]
                residual_load(k, hr, hrb, dc, tt)
                for c in range(DC):
                    P.op("pe", lambda e, p_=p_, w_=w_, c=c: e.matmul(
                        p_[:], lhsT=w_[:, c, :], rhs=lo[:, c, :], start=(c == 0), stop=(c == DC - 1)),
                        reads=[wb_, lo_b], pwrites=[pb_])
                P.op("dve", lambda e, p_=p_, hr=hr, dc=dc: e.scalar_tensor_tensor(
                    out=hr[:], in0=p_[:], scalar=k.vecs[:, b2 + dc:b2 + dc + 1], in1=hr[:],
                    op0=ALU.add, op1=ALU.add), reads=[pb_, hrb, k.vecs_b], writes=[hrb])
                residual_store(k, hr, hrb, dc, tt)
                nd += 1
    P.barrier()


HG_C = 64


def phase_mix_hgrn(k, L):
    nc, P = k.nc, k.P
    R = k.R
    win = k.w["b_w_in"][0]
    wo = k.w["b_w_o"][0]
    gn = R.off["b_g_norm"]
    NCH = TT // HG_C
    with ExitStack() as st:
        def sb(name, shape, dt=F32):
            return st.enter_context(nc.sbuf_tensor("hg_" + name, shape, dt))

        xn = sb("xn", [128, DC, TT], BF16); xn_b = P.buf()
        ws = WStream(k, st, "hg_wi", DC, 128, nbuf=4)
        wso = WStream(k, st, "hg_wo", DC, 128, nbuf=2)
        lb = sb("lb", [128, DC]); oml = sb("oml", [128, DC]); lbt = sb("lbt", [128, 4, DC]); lb_b = P.buf()
        ones64 = sb("ones64", [128, HG_C]); ones64_b = P.buf()
        mask = sb("mask", [128, TT]); mask_b = P.buf()
        identb = sb("identb", [128, 128], BF16); identb_b = P.buf()
        state = sb("state", [128, 16, 128]); state_b = [P.buf() for _ in range(16)]
        snap = sb("snap", [128, NCH + 1, 128], BF16); snap_b = [P.buf() for _ in range(NCH + 1)]
        sg = sb("sg", [128, TT]); sg_b = P.buf()
        lf = sb("lf", [128, TT]); lf_b = P.buf()
        kk = sb("kk", [128, TT]); kk_b = P.buf()
        a = sb("a", [128, TT]); a_b = P.buf()
        ea = sb("ea", [128, TT]); ea_b = P.buf()
        ena = sb("ena", [128, TT]); ena_b = P.buf()
        qs = sb("qs", [128, TT]); qs_b = P.buf()
        gs = sb("gs", [128, TT]); gs_b = P.buf()
        tmp = sb("tmp", [128, TT]); tmp_b = P.buf()
        rs = sb("rs", [128, TT]); rs_b = P.buf()
        qt = sb("qt", [128, TT], BF16); qt_b = P.buf()
        kt = sb("kt", [128, TT], BF16); kt_b = P.buf()
        kh = sb("kh", [128, TT], BF16); kh_b = P.buf()
        vb = sb("vb", [128, TT], BF16); vb_b = P.buf()
        osq = sb("osq", [128, TT], BF16); osq_b = P.buf()
        NB = TT // 128
        scb = sb("scb", [128, TT], BF16); scb_b = P.buf()
        vtok = sb("vtok", [128, NB, 128], BF16); vtok_b = P.buf()
        khA = sb("khA", [128, NB, 128], BF16); khA_b = P.buf()
        khB = sb("khB", [128, NB, 128], BF16); khB_b = P.buf()
        hmask = sb("hmask", [128, 2]); hmask_b = P.buf()
        ob = sb("ob", [128, DC, TT], BF16); ob_b = P.buf()
        hres = [sb("hr%d" % i, [128, TT]) for i in range(2)]; hres_b = P.bufs(2)
        pq = st.enter_context(nc.psum_tensor("hg_pq", [128, TT], F32)); pq_b = P.pbuf()
        pf = st.enter_context(nc.psum_tensor("hg_pf", [128, TT], F32)); pf_b = P.pbuf()
        pi = st.enter_context(nc.psum_tensor("hg_pi", [128, TT], F32)); pi_b = P.pbuf()
        pg = st.enter_context(nc.psum_tensor("hg_pg", [128, TT], F32)); pg_b = P.pbuf()
        po = st.enter_context(nc.psum_tensor("hg_po", [128, TT], F32)); po_b = P.pbuf()
        psc = st.enter_context(nc.psum_tensor("hg_psc", [128, TT], F32)); psc_b = P.pbuf()
        pkv = st.enter_context(nc.psum_tensor("hg_pkv", [128, 4, 128], F32)); pkv_b = [P.pbuf()] * 4
        ptr = st.enter_context(nc.psum_tensor("hg_ptr", [128, TT // 128, 128], BF16)); ptr_b = P.pbuf()

        P.dma("sp", lambda e: e.dma_start(out=mask[:], in_=k.consts["hg_mask"]), writes=[mask_b])
        P.dma("sp", lambda e: e.dma_start(out=hmask[:], in_=k.consts["half_mask"]), writes=[hmask_b])
        P.op("pool", lambda e: e.memset(ones64[:], 1.0), writes=[ones64_b])
        P.op("pool", lambda e: e.tensor_copy(out=identb[:], in_=k.ident[:]), reads=[k.ident_b], writes=[identb_b])
        P.op("pool", lambda e: e.memset(state[:], 0.0), writes=state_b)
        for l in range(DEPTH):
            c0 = R.off["b_lb%d" % l]
            P.op("act", lambda e, l=l, c0=c0: e.activation(out=lbt[:, l, :], in_=k.vecs[:, c0:c0 + DC], func=AF.Exp),
                 reads=[k.vecs_b], pwrites=[lb_b])
        P.op("dve", lambda e: e.tensor_tensor(out=oml[:], in0=lbt[:, 0, :], in1=lbt[:, 1, :], op=ALU.add),
             reads=[lb_b], pwrites=[lb_b])
        P.op("dve", lambda e: e.tensor_tensor(out=oml[:], in0=oml[:], in1=lbt[:, 2, :], op=ALU.add),
             reads=[lb_b], writes=[lb_b])
        P.op("dve", lambda e: e.tensor_tensor(out=oml[:], in0=oml[:], in1=lbt[:, 3, :], op=ALU.add),
             reads=[lb_b], writes=[lb_b])
        P.op("dve", lambda e: e.reciprocal(out=oml[:], in_=oml[:]), reads=[lb_b], writes=[lb_b])
        P.op("dve", lambda e: e.tensor_copy(out=lb[:], in_=lbt[:, 1, :]), reads=[lb_b], writes=[lb_b])
        for l in range(2, L + 1):
            P.op("dve", lambda e, l=l: e.tensor_tensor(out=lb[:], in0=lb[:], in1=lbt[:, l, :], op=ALU.add),
                 reads=[lb_b], writes=[lb_b])
        P.op("dve", lambda e: e.tensor_tensor(out=lb[:], in0=lb[:], in1=oml[:], op=ALU.mult),
             reads=[lb_b], writes=[lb_b])
        P.op("dve", lambda e: e.tensor_scalar(out=oml[:], in0=lb[:], scalar1=-1.0, scalar2=1.0,
                                              op0=ALU.mult, op1=ALU.add), reads=[lb_b], writes=[lb_b])
        nd = 0
        NH = 16
        for tt in range(NT):
            P.dma("sp", lambda e, tt=tt: e.dma_start(out=xn[:], in_=fm(k.xnT, 0, DC, tt * TT, TT)),
                  reads=[k.xnT_b[tt]], writes=[xn_b])
            for h in range(NH):
                sl = []
                for sec in range(4):
                    sl.append(ws.load(win, 0, sec * D + h * 128))
                for sec, (pt, ptb) in enumerate(((pq, pq_b), (pf, pf_b), (pi, pi_b), (pg, pg_b))):
                    w_, wb_ = sl[sec]
                    for c in range(DC):
                        P.op("pe", lambda e, pt=pt, w_=w_, c=c: e.matmul(
                            pt[:], lhsT=w_[:, c, :], rhs=xn[:, c, :], start=(c == 0), stop=(c == DC - 1)),
                            reads=[wb_, xn_b], pwrites=[ptb])
                P.op("act", lambda e: e.activation(out=sg[:], in_=pf[:], func=AF.Sigmoid), reads=[pf_b], writes=[sg_b])
                P.op("dve", lambda e, h=h: e.tensor_scalar(
                    out=sg[:], in0=sg[:], scalar1=oml[:, h:h + 1], scalar2=lb[:, h:h + 1],
                    op0=ALU.mult, op1=ALU.add), reads=[sg_b, lb_b], writes=[sg_b])
                P.op("act", lambda e: e.activation(out=lf[:], in_=sg[:], func=AF.Ln), reads=[sg_b], writes=[lf_b])
                P.op("pool", lambda e: e.tensor_scalar(out=kk[:], in0=sg[:], scalar1=-1.0, scalar2=1.0,
                                                       op0=ALU.mult, op1=ALU.add), reads=[sg_b], writes=[kk_b])
                for n in range(NCH):
                    cs = slice(n * HG_C, (n + 1) * HG_C)
                    P.op("dve", lambda e, cs=cs: e.tensor_tensor_scan(
                        out=a[:, cs], data0=ones64[:], data1=lf[:, cs], initial=0.0, op0=ALU.mult, op1=ALU.add),
                        reads=[lf_b, ones64_b], pwrites=[a_b])
                P.op("act", lambda e: e.activation(out=ea[:], in_=a[:], func=AF.Exp), reads=[a_b], writes=[ea_b])
                P.op("act", lambda e: e.activation(out=ena[:], in_=a[:], func=AF.Exp, scale=-1.0), reads=[a_b], writes=[ena_b])
                P.op("act", lambda e: e.activation(out=qs[:], in_=pq[:], func=AF.Silu), reads=[pq_b], writes=[qs_b])
                P.op("act", lambda e: e.activation(out=vb[:], in_=pi[:], func=AF.Copy), reads=[pi_b], writes=[vb_b])
                P.op("act", lambda e: e.activation(out=gs[:], in_=pg[:], func=AF.Silu), reads=[pg_b], writes=[gs_b])
                P.op("pool", lambda e: e.tensor_tensor(out=qt[:], in0=qs[:], in1=ea[:], op=ALU.mult),
                     reads=[qs_b, ea_b], writes=[qt_b])
                P.op("pool", lambda e: e.tensor_tensor(out=kt[:], in0=kk[:], in1=ena[:], op=ALU.mult),
                     reads=[kk_b, ena_b], writes=[kt_b])
                for n in range(NCH):
                    cs = slice(n * HG_C, (n + 1) * HG_C)
                    last = n * HG_C + HG_C - 1
                    P.op("dve", lambda e, cs=cs, last=last: e.tensor_scalar(
                        out=kh[:, cs], in0=kt[:, cs], scalar1=ea[:, last:last + 1], scalar2=None, op0=ALU.mult),
                        reads=[kt_b, ea_b], pwrites=[kh_b])
                for b in range(NB):
                    bs = slice(b * 128, (b + 1) * 128)
                    P.op("pe", lambda e, b=b, bs=bs: e.transpose(out=ptr[:, b, :], in_=vb[:, bs], identity=identb[:]),
                         reads=[vb_b, identb_b], pwrites=[ptr_b])
                P.op("act", lambda e: e.activation(out=vtok[:], in_=ptr[:], func=AF.Copy), reads=[ptr_b], writes=[vtok_b])
                for b in range(NB):
                    bs = slice(b * 128, (b + 1) * 128)
                    P.op("pe", lambda e, b=b, bs=bs: e.transpose(out=ptr[:, b, :], in_=kh[:, bs], identity=identb[:]),
                         reads=[kh_b, identb_b], pwrites=[ptr_b])
                P.op("act", lambda e: e.activation(out=khA[:], in_=ptr[:], func=AF.Copy, scale=hmask[:, 0:1]),
                     reads=[ptr_b, hmask_b], writes=[khA_b])
                P.op("dve", lambda e: e.tensor_scalar(out=khB[:], in0=ptr[:], scalar1=hmask[:, 1:2], scalar2=None, op0=ALU.mult),
                     reads=[ptr_b, hmask_b], writes=[khB_b])
                for b in range(NB):
                    bs = slice(b * 128, (b + 1) * 128)
                    P.op("pe", lambda e, bs=bs: e.matmul(psc[:, bs], lhsT=kt[:, bs], rhs=qt[:, bs], start=True, stop=True),
                         reads=[kt_b, qt_b], pwrites=[psc_b])
                P.op("dve", lambda e: e.tensor_tensor(out=scb[:], in0=psc[:], in1=mask[:], op=ALU.mult),
                     reads=[psc_b, mask_b], writes=[scb_b])
                P.op("act", lambda e, h=h: e.activation(out=snap[:, 0, :], in_=state[:, h, :], func=AF.Copy),
                     reads=[state_b[h]], writes=[snap_b[0]])
                for n in range(NCH):
                    last = n * HG_C + HG_C - 1
                    kx, kxb = (khA, khA_b) if n % 2 == 0 else (khB, khB_b)
                    P.op("pe", lambda e, n=n, kx=kx: e.matmul(pkv[:, n % 4, :], lhsT=kx[:, n // 2, :], rhs=vtok[:, n // 2, :],
                                                             start=True, stop=True),
                         reads=[kxb, vtok_b], writes=[pkv_b[n % 4]])
                    P.op("dve", lambda e, n=n, h=h, last=last: e.scalar_tensor_tensor(
                        out=state[:, h, :], in0=state[:, h, :], scalar=ea[:, last:last + 1], in1=pkv[:, n % 4, :],
                        op0=ALU.mult, op1=ALU.add), reads=[state_b[h], ea_b, pkv_b[n % 4]], writes=[state_b[h]])
                    P.op("act", lambda e, n=n, h=h: e.activation(out=snap[:, n + 1, :], in_=state[:, h, :], func=AF.Copy),
                         reads=[state_b[h]], writes=[snap_b[n + 1]])
                for b in range(NB):
                    bs = slice(b * 128, (b + 1) * 128)
                    P.op("pe", lambda e, b=b, bs=bs: e.matmul(po[:, bs], lhsT=vtok[:, b, :], rhs=scb[:, bs],
                                                              start=True, stop=False),
                         reads=[vtok_b, scb_b], pwrites=[po_b])
                    for n in (2 * b, 2 * b + 1):
                        cs = slice(n * HG_C, (n + 1) * HG_C)
                        P.op("pe", lambda e, n=n, cs=cs, b=b: e.matmul(po[:, cs], lhsT=snap[:, n, :], rhs=qt[:, cs],
                                                                      start=False, stop=(n == 2 * b + 1)),
                             reads=[snap_b[n], qt_b], pwrites=[po_b])
                P.op("act", lambda e: e.activation(out=osq[:], in_=po[:], func=AF.Square), reads=[po_b], writes=[osq_b])
                P.op("pe", lambda e: e.matmul(psc[:], lhsT=k.ones_bf[:], rhs=osq[:], start=True, stop=True),
                     reads=[osq_b, k.ones_b, scb_b], writes=[psc_b])
                P.op("act", lambda e: e.activation(out=rs[:], in_=psc[:], func=AF.Sqrt, bias=k.eps_t[:, 0:1], scale=1.0 / 128),
                     reads=[psc_b, k.eps_b], writes=[rs_b])
                P.op("dve", lambda e: e.reciprocal(out=rs[:], in_=rs[:]), reads=[rs_b], writes=[rs_b])
                P.op("dve", lambda e: e.tensor_tensor(out=tmp[:], in0=po[:], in1=rs[:], op=ALU.mult),
                     reads=[po_b, rs_b], writes=[tmp_b])
                P.op("dve", lambda e, h=h: e.scalar_tensor_tensor(
                    out=ob[:, h, :], in0=tmp[:], scalar=k.vecs[:, gn + h:gn + h + 1], in1=gs[:],
                    op0=ALU.mult, op1=ALU.mult), reads=[tmp_b, gs_b, k.vecs_b], pwrites=[ob_b])
            for dc in range(DC):
                w_, wb_ = wso.load(wo, 0, dc * 128)
                p_, pb_ = (pq, pq_b) if nd % 2 == 0 else (pf, pf_b)
                hr, hrb = hres[nd % 2], hres_b[nd % 2]
                residual_load(k, hr, hrb, dc, tt)
                for c in range(DC):
                    P.op("pe", lambda e, p_=p_, w_=w_, c=c: e.matmul(
                        p_[:], lhsT=w_[:, c, :], rhs=ob[:, c, :], start=(c == 0), stop=(c == DC - 1)),
                        reads=[wb_, ob_b], pwrites=[pb_])
                P.op("dve", lambda e, p_=p_, hr=hr: e.tensor_tensor(out=hr[:], in0=p_[:], in1=hr[:], op=ALU.add),
                     reads=[pb_, hrb], writes=[hrb])
                residual_store(k, hr, hrb, dc, tt)
                nd += 1
    P.barrier()


ATT_SCALE = 128 ** -0.5
NEG = -1.0e30
MNEG = -30000.0
NSB = S // 128


def t5_bucket_np(d):
    d = np.maximum(np.asarray(d, np.int64), 0)
    nf = np.maximum(d, 1).astype(np.float32)
    large = 16 + (np.log(nf / np.float32(16)) / np.float32(math.log(128 / 16)) * np.float32(16)).astype(np.int32)
    large = np.minimum(large, 31)
    return np.where(d < 16, d, large).astype(np.int64)


def dsa_layout_inputs(inp):
    out = {}
    out["a_gq_b"] = np.ascontiguousarray(np.broadcast_to(inp["a_g_q"][0][None, :], (128, 512)), dtype=np.float32)
    out["a_gkv_b"] = np.ascontiguousarray(np.broadcast_to(inp["a_g_kv"][0][None, :], (128, 256)), dtype=np.float32)
    rb = np.asarray(inp["rel_bias"], np.float32)
    out["a_cvec"] = np.ascontiguousarray(np.broadcast_to(rb[31][None, :], (128, 16)), dtype=np.float32)
    sl = np.arange(128)[:, None, None]
    r = np.arange(5)[None, :, None]
    ql = np.arange(512)[None, None, :]
    bidx = t5_bucket_np(ql - sl + 128 - 128 * r)
    out["a_bt"] = np.ascontiguousarray(np.moveaxis(rb[bidx], -1, 0), dtype=np.float32)
    return out


DSA_LAYOUT_SHAPES = {"a_gq_b": [128, 512], "a_gkv_b": [128, 256], "a_cvec": [128, 16], "a_bt": [16, 128, 5, 512]}


def phase_dsa_a(k):
    nc, P = k.nc, k.P
    win = k.w["a_w_in"][0]
    with ExitStack() as st:
        def sb(name, shape, dt=F32):
            return st.enter_context(nc.sbuf_tensor("da_" + name, shape, dt))

        xn = sb("xn", [128, DC, TT], BF16); xn_b = P.buf()
        wst = [sb("wst%d" % i, [128, 4, 848]) for i in range(2)]; wst_b = P.bufs(2)
        wbf = sb("wbf", [128, DC, 848], BF16); wbf_b = P.buf()
        gq = sb("gq", [128, 512]); gkv = sb("gkv", [128, 256]); g_b = P.buf()
        identb = sb("identb", [128, 128], BF16); identb_b = P.buf()
        junk = sb("junk", [128, 512], BF16); junk_b = P.buf()
        ss = sb("ss", [128, 2]); ss_b = P.buf()
        cqn = sb("cqn", [128, 512], BF16); cqn_b = P.buf()
        ckvn = [sb("ckvn%d" % i, [128, 256], BF16) for i in range(2)]; ckvn_b = P.bufs(2)
        kix = sb("kix", [128, 128], BF16); kix_b = P.buf()
        widx = sb("widx", [128, NSB, 16]); widx_b = P.buf()
        cqT = [sb("cqT%d" % i, [128, 4, TT], BF16) for i in range(2)]; cqT_b = P.bufs(2)
        ckvT = [sb("ckvT%d" % i, [128, 2, TT], BF16) for i in range(2)]; ckvT_b = P.bufs(2)
        kixT = [sb("kixT%d" % i, [128, TT], BF16) for i in range(2)]; kixT_b = P.bufs(2)
        pA = [st.enter_context(nc.psum_tensor("da_pA%d" % i, [128, 512], F32)) for i in range(2)]; pA_b = P.pbufs(2)
        pB = [st.enter_context(nc.psum_tensor("da_pB%d" % i, [128, 512], F32)) for i in range(2)]; pB_b = P.pbufs(2)
        ptr = [st.enter_context(nc.psum_tensor("da_ptr%d" % i, [128, 8, 128], BF16)) for i in range(2)]; ptr_b = P.pbufs(2)

        P.dma("sp", lambda e: e.dma_start(out=gq[:], in_=k.lay["a_gq_b"]), pwrites=[g_b])
        P.dma("sp", lambda e: e.dma_start(out=gkv[:], in_=k.lay["a_gkv_b"]), pwrites=[g_b])
        P.op("pool", lambda e: e.tensor_copy(out=identb[:], in_=k.ident[:]), reads=[k.ident_b], writes=[identb_b])
        for i in range(4):
            w_, wb_ = wst[i % 2], wst_b[i % 2]
            P.dma("sp", lambda e, w_=w_, i=i: e.dma_start(
                out=w_[:], in_=win[i * 512:(i + 1) * 512, :].rearrange("(c p) n -> p c n", p=128)), writes=[wb_])
            P.op("pool", lambda e, w_=w_, i=i: e.tensor_copy(out=wbf[:, i * 4:(i + 1) * 4, :], in_=w_[:]),
                 reads=[wb_], pwrites=[wbf_b])
        n = 0
        for tt in range(NT):
            P.dma("sp", lambda e, tt=tt: e.dma_start(out=xn[:], in_=fm(k.xnT, 0, DC, tt * TT, TT)),
                  reads=[k.xnT_b[tt]], writes=[xn_b])
            cq_t, cq_tb = cqT[tt % 2], cqT_b[tt % 2]
            ckv_t, ckv_tb = ckvT[tt % 2], ckvT_b[tt % 2]
            kix_t, kix_tb = kixT[tt % 2], kixT_b[tt % 2]
            for sub in range(4):
                sblk = tt * 4 + sub
                ts_ = slice(sub * 128, (sub + 1) * 128)
                a_, ab_ = pA[n % 2], pA_b[n % 2]
                b_, bb_ = pB[n % 2], pB_b[n % 2]
                t_, tb_ = ptr[n % 2], ptr_b[n % 2]
                ck, ckb = ckvn[n % 2], ckvn_b[n % 2]
                for c in range(DC):
                    P.op("pe", lambda e, a_=a_, c=c, ts_=ts_: e.matmul(
                        a_[:], lhsT=xn[:, c, ts_], rhs=wbf[:, c, 0:512], start=(c == 0), stop=(c == DC - 1)),
                        reads=[xn_b, wbf_b], pwrites=[ab_])
                for c in range(DC):
                    P.op("pe", lambda e, b_=b_, c=c, ts_=ts_: e.matmul(
                        b_[:, 0:336], lhsT=xn[:, c, ts_], rhs=wbf[:, c, 512:848], start=(c == 0), stop=(c == DC - 1)),
                        reads=[xn_b, wbf_b], pwrites=[bb_])
                P.op("act", lambda e, a_=a_: e.activation(out=junk[:], in_=a_[:], func=AF.Square, accum_out=ss[:, 0:1]),
                     reads=[ab_], writes=[junk_b], pwrites=[ss_b])
                P.op("act", lambda e, b_=b_: e.activation(out=junk[:, 0:256], in_=b_[:, 0:256], func=AF.Square,
                                                          accum_out=ss[:, 1:2]),
                     reads=[bb_], writes=[junk_b], pwrites=[ss_b])
                P.op("act", lambda e: e.activation(out=ss[:, 0:1], in_=ss[:, 0:1], func=AF.Sqrt,
                                                   bias=k.eps_t[:, 0:1], scale=1.0 / 512), reads=[ss_b, k.eps_b], pwrites=[ss_b])
                P.op("act", lambda e: e.activation(out=ss[:, 1:2], in_=ss[:, 1:2], func=AF.Sqrt,
                                                   bias=k.eps_t[:, 0:1], scale=1.0 / 256), reads=[ss_b, k.eps_b], pwrites=[ss_b])
                P.op("dve", lambda e: e.reciprocal(out=ss[:], in_=ss[:]), reads=[ss_b], writes=[ss_b])
                P.op("dve", lambda e, a_=a_: e.scalar_tensor_tensor(
                    out=cqn[:], in0=a_[:], scalar=ss[:, 0:1], in1=gq[:], op0=ALU.mult, op1=ALU.mult),
                    reads=[ab_, ss_b, g_b], writes=[cqn_b])
                P.op("dve", lambda e, b_=b_, ck=ck: e.scalar_tensor_tensor(
                    out=ck[:], in0=b_[:, 0:256], scalar=ss[:, 1:2], in1=gkv[:], op0=ALU.mult, op1=ALU.mult),
                    reads=[bb_, ss_b, g_b], writes=[ckb])
                P.op("act", lambda e, b_=b_: e.activation(out=kix[:, 0:64], in_=b_[:, 256:320], func=AF.Copy),
                     reads=[bb_], pwrites=[kix_b])
                P.op("act", lambda e, b_=b_: e.activation(out=kix[:, 64:128], in_=b_[:, 256:320], func=AF.Copy),
                     reads=[bb_], pwrites=[kix_b])
                P.op("act", lambda e, b_=b_, sblk=sblk: e.activation(out=widx[:, sblk, :], in_=b_[:, 320:336], func=AF.Copy),
                     reads=[bb_], pwrites=[widx_b])
                P.dma("sp", lambda e, ck=ck, sblk=sblk: e.dma_start(
                    out=k.ckv_tok[sblk * 128:(sblk + 1) * 128, :], in_=ck[:]), reads=[ckb], pwrites=[k.dsa_b])
                for j in range(4):
                    P.op("pe", lambda e, t_=t_, j=j: e.transpose(out=t_[:, j, :], in_=cqn[:, j * 128:(j + 1) * 128],
                                                                identity=identb[:]),
                         reads=[cqn_b, identb_b], pwrites=[tb_])
                for j in range(2):
                    P.op("pe", lambda e, t_=t_, j=j, ck=ck: e.transpose(out=t_[:, 4 + j, :], in_=ck[:, j * 128:(j + 1) * 128],
                                                                       identity=identb[:]),
                         reads=[ckb, identb_b], pwrites=[tb_])
                P.op("pe", lambda e, t_=t_: e.transpose(out=t_[:, 6, :], in_=kix[:], identity=identb[:]),
                     reads=[kix_b, identb_b], pwrites=[tb_])
                P.op("act", lambda e, t_=t_, cq_t=cq_t, ts_=ts_: e.activation(out=cq_t[:, :, ts_], in_=t_[:, 0:4, :], func=AF.Copy),
                     reads=[tb_], pwrites=[cq_tb])
                P.op("dve", lambda e, t_=t_, ckv_t=ckv_t, ts_=ts_: e.tensor_copy(out=ckv_t[:, :, ts_], in_=t_[:, 4:6, :]),
                     reads=[tb_], pwrites=[ckv_tb])
                P.op("dve", lambda e, t_=t_, kix_t=kix_t, ts_=ts_: e.tensor_copy(out=kix_t[:, ts_], in_=t_[:, 6, :]),
                     reads=[tb_], pwrites=[kix_tb])
                n += 1
            c0 = tt * TT
            P.dma("sp", lambda e, cq_t=cq_t, c0=c0: e.dma_start(out=fm(k.cqT, 0, 4, c0, TT), in_=cq_t[:]),
                  reads=[cq_tb], pwrites=[k.dsa_b])
            P.dma("sp", lambda e, ckv_t=ckv_t, c0=c0: e.dma_start(out=fm(k.ckvT, 0, 2, c0, TT), in_=ckv_t[:]),
                  reads=[ckv_tb], pwrites=[k.dsa_b])
            P.dma("sp", lambda e, kix_t=kix_t, c0=c0: e.dma_start(out=k.kidxT[:, c0:c0 + TT], in_=kix_t[:]),
                  reads=[kix_tb], pwrites=[k.dsa_b])
        P.dma("sp", lambda e: e.dma_start(out=k.widx, in_=widx[:]), reads=[widx_b], pwrites=[k.dsa_b])
    P.barrier()


def phase_dsa_b(k):
    nc, P = k.nc, k.P
    wq = k.w["a_w_qidx"][0]
    import os
    NQB = int(os.environ.get("DSA_NQB", str(NSB)))
    with ExitStack() as st:
        def sb(name, shape, dt=F32):
            return st.enter_context(nc.sbuf_tensor("db_" + name, shape, dt))

        kixT = sb("kixT", [128, S], BF16); kixT_b = P.buf()
        hmask = sb("hmask", [128, 2]); hmask_b = P.buf()
        widx = sb("widx", [128, NSB, 16]); widx_b = P.buf()
        wst = sb("wst", [128, 4, 1024]); wst_b = P.buf()
        wqb = sb("wqb", [128, 4, 1024], BF16); wqb_b = P.buf()
        identb = sb("identb", [128, 128], BF16); identb_b = P.buf()
        cm = sb("cm", [128, 128]); cm30 = sb("cm30", [128, 128], BF16); cm_b = P.buf()
        neg30 = sb("neg30", [128, 3, 128], BF16); neg30_b = P.buf()
        cq = [sb("cq%d" % i, [128, 4, 128], BF16) for i in range(2)]; cq_b = P.bufs(2)
        qixA = [sb("qixA%d" % i, [128, 8, 128], BF16) for i in range(2)]; qixA_b = P.bufs(2)
        qixB = [sb("qixB%d" % i, [128, 8, 128], BF16) for i in range(2)]; qixB_b = P.bufs(2)
        rl = [sb("rl%d" % i, [128, 512]) for i in range(4)]; rl_b = P.bufs(4)
        acc = [sb("acc%d" % i, [128, S]) for i in range(2)]; acc_b = P.bufs(2)
        m8 = sb("m8", [128, 8]); m8_b = P.buf()
        mq = [sb("mq%d" % i, [128, S], BF16) for i in range(2)]; mq_b = P.bufs(2)
        mT = [sb("mT%d" % i, [128, NSB, 128], BF16) for i in range(2)]; mT_b = P.bufs(2)
        pqi = [st.enter_context(nc.psum_tensor("db_pqi%d" % i, [128, 4, 128], F32)) for i in range(2)]; pqi_b = P.pbufs(2)
        ps = [st.enter_context(nc.psum_tensor("db_ps%d" % i, [128, 512], F32)) for i in range(4)]; ps_b = P.pbufs(4)
        ptr = [st.enter_context(nc.psum_tensor("db_ptr%d" % i, [128, 8, 128], BF16)) for i in range(2)]; ptr_b = P.pbufs(2)

        P.dma("sp", lambda e: e.dma_start(out=kixT[:], in_=k.kidxT), reads=[k.dsa_b], writes=[kixT_b])
        P.dma("sp", lambda e: e.dma_start(out=widx[:], in_=k.widx), reads=[k.dsa_b], writes=[widx_b])
        P.dma("sp", lambda e: e.dma_start(out=hmask[:], in_=k.consts["half_mask"]), writes=[hmask_b])
        P.dma("sp", lambda e: e.dma_start(out=wst[:], in_=wq.rearrange("(c p) n -> p c n", p=128)), writes=[wst_b])
        P.op("pool", lambda e: e.tensor_copy(out=wqb[:], in_=wst[:]), reads=[wst_b], writes=[wqb_b])
        P.op("pool", lambda e: e.tensor_copy(out=identb[:], in_=k.ident[:]), reads=[k.ident_b], writes=[identb_b])
        P.dma("sp", lambda e: e.dma_start(out=cm[:], in_=k.consts["dsa_cm"]), pwrites=[cm_b])
        P.op("pool", lambda e: e.memset(neg30[:], MNEG), writes=[neg30_b])
        P.op("dve", lambda e: e.tensor_scalar(out=cm30[:], in0=cm[:], scalar1=-1.0, scalar2=MNEG,
                                              op0=ALU.is_lt, op1=ALU.mult), reads=[cm_b], pwrites=[cm_b])
        npe = 0
        ntr = 0
        for qb in range(NQB):
            Lq = (qb + 1) * 128
            c_, cb_ = cq[qb % 2], cq_b[qb % 2]
            qxA, qxAb = qixA[qb % 2], qixA_b[qb % 2]
            qxB, qxBb = qixB[qb % 2], qixB_b[qb % 2]
            ac, acb = acc[qb % 2], acc_b[qb % 2]
            m_, mb_ = mq[qb % 2], mq_b[qb % 2]
            mt, mtb = mT[qb % 2], mT_b[qb % 2]
            P.dma("sp", lambda e, c_=c_, qb=qb: e.dma_start(out=c_[:], in_=fm(k.cqT, 0, 4, qb * 128, 128)),
                  reads=[k.dsa_b], writes=[cb_])
            if qb >= 2:
                for hg in range(2):
                    pq_, pqb = pqi[hg % 2], pqi_b[hg % 2]
                    for hh in range(4):
                        hp = hg * 4 + hh
                        for c in range(4):
                            P.op("pe", lambda e, pq_=pq_, hh=hh, hp=hp, c=c, c_=c_: e.matmul(
                                pq_[:, hh, :], lhsT=wqb[:, c, hp * 128:(hp + 1) * 128], rhs=c_[:, c, :],
                                start=(c == 0), stop=(c == 3)), reads=[wqb_b, cb_], pwrites=[pqb])
                    P.op("act", lambda e, pq_=pq_, qxA=qxA, hg=hg: e.activation(
                        out=qxA[:, hg * 4:(hg + 1) * 4, :], in_=pq_[:], func=AF.Copy, scale=hmask[:, 0:1]),
                        reads=[pqb, hmask_b], pwrites=[qxAb])
                    P.op("dve", lambda e, pq_=pq_, qxB=qxB, hg=hg: e.tensor_scalar(
                        out=qxB[:, hg * 4:(hg + 1) * 4, :], in0=pq_[:], scalar1=hmask[:, 1:2], scalar2=None, op0=ALU.mult),
                        reads=[pqb, hmask_b], pwrites=[qxBb])
                nkt = (Lq + 511) // 512
                for kt in range(nkt):
                    wd = min(512, Lq - kt * 512)
                    ks = slice(kt * 512, kt * 512 + wd)
                    for h in range(16):
                        p_, pb_ = ps[npe % 4], ps_b[npe % 4]
                        r_, rb_ = rl[npe % 4], rl_b[npe % 4]
                        npe += 1
                        qx, qxb = (qxA, qxAb) if h % 2 == 0 else (qxB, qxBb)
                        P.op("pe", lambda e, p_=p_, qx=qx, h=h, ks=ks, wd=wd: e.matmul(
                            p_[:, 0:wd], lhsT=qx[:, h // 2, :], rhs=kixT[:, ks], start=True, stop=True),
                            reads=[qxb, kixT_b], writes=[pb_])
                        P.op("act", lambda e, p_=p_, r_=r_, wd=wd: e.activation(out=r_[:, 0:wd], in_=p_[:, 0:wd], func=AF.Relu),
                             reads=[pb_], writes=[rb_])
                        if h == 0:
                            P.op("dve", lambda e, r_=r_, ac=ac, ks=ks, wd=wd, qb=qb: e.tensor_scalar(
                                out=ac[:, ks], in0=r_[:, 0:wd], scalar1=widx[:, qb, 0:1], scalar2=None, op0=ALU.mult),
                                reads=[rb_, widx_b], pwrites=[acb])
                        else:
                            P.op("dve", lambda e, r_=r_, ac=ac, ks=ks, wd=wd, qb=qb, h=h: e.scalar_tensor_tensor(
                                out=ac[:, ks], in0=r_[:, 0:wd], scalar=widx[:, qb, h:h + 1], in1=ac[:, ks],
                                op0=ALU.mult, op1=ALU.add), reads=[rb_, widx_b, acb], pwrites=[acb])
                dg = slice(Lq - 128, Lq)
                P.op("dve", lambda e, ac=ac, dg=dg: e.tensor_tensor(out=ac[:, dg], in0=ac[:, dg], in1=cm[:], op=ALU.add),
                     reads=[acb, cm_b], writes=[acb])
                for rnd in range(32):
                    P.op("dve", lambda e, ac=ac, Lq=Lq: e.max(out=m8[:], in_=ac[:, 0:Lq]), reads=[acb], writes=[m8_b])
                    P.op("dve", lambda e, ac=ac, Lq=Lq: e.match_replace(
                        out=ac[:, 0:Lq], in_to_replace=m8[:], in_values=ac[:, 0:Lq], imm_value=NEG),
                        reads=[acb, m8_b], writes=[acb])
                P.op("dve", lambda e, ac=ac, m_=m_, Lq=Lq: e.tensor_scalar(
                    out=m_[:, 0:Lq], in0=ac[:, 0:Lq], scalar1=-5.0e29, scalar2=MNEG, op0=ALU.is_gt, op1=ALU.mult),
                    reads=[acb], writes=[mb_])
                P.op("pool", lambda e, m_=m_, dg=dg: e.tensor_tensor(out=m_[:, dg], in0=m_[:, dg], in1=cm30[:], op=ALU.add),
                     reads=[mb_, cm_b], writes=[mb_])
            else:
                P.op("pool", lambda e, m_=m_, Lq=Lq: e.memset(m_[:, 0:Lq], 0.0), writes=[mb_])
                dg = slice(Lq - 128, Lq)
                P.op("pool", lambda e, m_=m_, dg=dg: e.tensor_copy(out=m_[:, dg], in_=cm30[:]), reads=[mb_, cm_b], writes=[mb_])
            for b0 in range(0, qb + 1, 8):
                nb = min(8, qb + 1 - b0)
                t_, tb_ = ptr[ntr % 2], ptr_b[ntr % 2]
                ntr += 1
                for j in range(nb):
                    P.op("pe", lambda e, t_=t_, j=j, m_=m_, b0=b0: e.transpose(
                        out=t_[:, j, :], in_=m_[:, (b0 + j) * 128:(b0 + j + 1) * 128], identity=identb[:]),
                        reads=[mb_, identb_b], pwrites=[tb_])
                P.op("act", lambda e, t_=t_, mt=mt, b0=b0, nb=nb: e.activation(
                    out=mt[:, b0:b0 + nb, :], in_=t_[:, 0:nb, :], func=AF.Copy), reads=[tb_], pwrites=[mtb])
            P.dma("sp", lambda e, mt=mt, qb=qb: e.dma_start(
                out=k.maskT[:, 0:qb + 1, qb * 128:(qb + 1) * 128], in_=mt[:, 0:qb + 1, :]),
                reads=[mtb], pwrites=[k.mask_b])
            nfill = 3 - (qb % 4)
            if nfill > 0:
                P.dma("sp", lambda e, qb=qb, nfill=nfill: e.dma_start(
                    out=k.maskT[:, qb + 1:qb + 1 + nfill, qb * 128:(qb + 1) * 128], in_=neg30[:, 0:nfill, :]),
                    reads=[neg30_b], pwrites=[k.mask_b])
    P.barrier()


def phase_dsa_c(k):
    nc, P = k.nc, k.P
    wuq = k.w["a_w_uq"][0]
    wuk = k.w["a_w_uk"][0]
    wuv = k.w["a_w_uv"][0]
    wo = k.w["a_w_o"][0]
    import os
    NG = int(os.environ.get("DSA_NG", str(NT)))
    with ExitStack() as st:
        def sb(name, shape, dt=F32):
            return st.enter_context(nc.sbuf_tensor("dc_" + name, shape, dt))

        ckvT = sb("ckvT", [128, 2, S], BF16); ckvT_b = P.buf()
        ckvk = sb("ckvk", [128, NSB, 256], BF16); ckvk_b = P.buf()
        wst = [sb("wst%d" % i, [128, 2048]) for i in range(2)]; wst_b = P.bufs(2)
        wuqb = sb("wuqb", [128, 4, 2048], BF16); wuqb_b = P.buf()
        wukb = sb("wukb", [128, 16, 256], BF16); wukb_b = P.buf()
        wuvb = sb("wuvb", [128, 16, 2, 128], BF16); wuvb_b = P.buf()
        cvec = sb("cvec", [128, 16]); cvec_b = P.buf()
        cq = sb("cq", [128, 4, TT], BF16); cq_b = P.buf()
        mk = sb("mk", [128, NSB, TT], BF16); mk_b = P.buf()
        bt = [sb("bt%d" % i, [128, 5, TT]) for i in range(2)]; bt_b = P.bufs(2)
        qT = sb("qT", [128, TT], BF16); qT_b = P.buf()
        ql = [sb("ql%d" % i, [128, 2, TT], BF16) for i in range(2)]; ql_b = P.bufs(2)
        tmp = [sb("tmp%d" % i, [128, TT]) for i in range(2)]; tmp_b = P.bufs(2)
        pT = [sb("pT%d" % i, [128, TT], BF16) for i in range(2)]; pT_b = P.bufs(2)
        rden = sb("rden", [128, TT]); rden_b = P.buf()
        oln = sb("oln", [128, 2, TT], BF16); oln_b = P.buf()
        oT = sb("oT", [128, 16, TT], BF16); oT_b = P.buf()
        wso = WStream(k, st, "dc_wo", DC, 128, nbuf=2)
        hres = [sb("hr%d" % i, [128, TT]) for i in range(2)]; hres_b = P.bufs(2)
        pm = [st.enter_context(nc.psum_tensor("dc_pm%d" % i, [128, TT], F32)) for i in range(2)]; pm_b = P.pbufs(2)
        pl = [st.enter_context(nc.psum_tensor("dc_pl%d" % i, [128, TT], F32)) for i in range(2)]; pl_b = P.pbufs(2)
        po = [st.enter_context(nc.psum_tensor("dc_po%d" % i, [128, TT], F32)) for i in range(2)]; po_b = P.pbufs(2)
        pden = st.enter_context(nc.psum_tensor("dc_pden", [128, TT], F32)); pden_b = P.pbuf()

        P.dma("sp", lambda e: e.dma_start(out=ckvT[:], in_=fm(k.ckvT, 0, 2, 0, S)), reads=[k.dsa_b], writes=[ckvT_b])
        P.dma("sp", lambda e: e.dma_start(out=ckvk[:], in_=k.ckv_tok.rearrange("(b p) c -> p b c", p=128)),
              reads=[k.dsa_b], writes=[ckvk_b])
        P.dma("sp", lambda e: e.dma_start(out=cvec[:], in_=k.lay["a_cvec"]), writes=[cvec_b])
        nw = 0
        for c in range(4):
            w_, wb_ = wst[nw % 2], wst_b[nw % 2]; nw += 1
            P.dma("sp", lambda e, w_=w_, c=c: e.dma_start(out=w_[:], in_=wuq[c * 128:(c + 1) * 128, :]), writes=[wb_])
            P.op("pool", lambda e, w_=w_, c=c: e.tensor_copy(out=wuqb[:, c, :], in_=w_[:]), reads=[wb_], pwrites=[wuqb_b])
        for hg in range(2):
            w_, wb_ = wst[nw % 2], wst_b[nw % 2]; nw += 1
            P.dma("sp", lambda e, w_=w_, hg=hg: e.dma_start(
                out=w_[:].rearrange("p (h c) -> p h c", h=8), in_=wuk[hg * 8:(hg + 1) * 8].rearrange("h d c -> d h c")),
                writes=[wb_])
            P.op("pool", lambda e, w_=w_, hg=hg: e.tensor_copy(
                out=wukb[:, hg * 8:(hg + 1) * 8, :], in_=w_[:].rearrange("p (h c) -> p h c", h=8)),
                reads=[wb_], pwrites=[wukb_b])
        for hg in range(2):
            w_, wb_ = wst[nw % 2], wst_b[nw % 2]; nw += 1
            P.dma("sp", lambda e, w_=w_, hg=hg: e.dma_start(
                out=w_[:].rearrange("p (h a d) -> p h a d", h=8, a=2),
                in_=wuv[hg * 8:(hg + 1) * 8].rearrange("h (a p) d -> p h a d", p=128)), writes=[wb_])
            P.op("pool", lambda e, w_=w_, hg=hg: e.tensor_copy(
                out=wuvb[:, hg * 8:(hg + 1) * 8, :, :], in_=w_[:].rearrange("p (h a d) -> p h a d", h=8, a=2)),
                reads=[wb_], pwrites=[wuvb_b])
        nl = 0
        nd = 0
        nh = 0
        for g in range(NG):
            q0 = g * TT
            nsb = 4 * g + 4
            P.dma("sp", lambda e, q0=q0: e.dma_start(out=cq[:], in_=fm(k.cqT, 0, 4, q0, TT)), reads=[k.dsa_b], writes=[cq_b])
            P.dma("sp", lambda e, q0=q0, nsb=nsb: e.dma_start(out=mk[:, 0:nsb, :], in_=k.maskT[:, 0:nsb, q0:q0 + TT]),
                  reads=[k.mask_b], writes=[mk_b])
            for h in range(16):
                b_, bb_ = bt[nh % 2], bt_b[nh % 2]
                q_, qb_ = ql[nh % 2], ql_b[nh % 2]
                nh += 1
                P.dma("sp", lambda e, b_=b_, h=h: e.dma_start(out=b_[:], in_=k.lay["a_bt"][h]), writes=[bb_])
                pq_, pqb = pm[0], pm_b[0]
                for c in range(4):
                    P.op("pe", lambda e, pq_=pq_, c=c, h=h: e.matmul(
                        pq_[:], lhsT=wuqb[:, c, h * 128:(h + 1) * 128], rhs=cq[:, c, :], start=(c == 0), stop=(c == 3)),
                        reads=[wuqb_b, cq_b], pwrites=[pqb])
                P.op("act", lambda e, pq_=pq_: e.activation(out=qT[:], in_=pq_[:], func=AF.Copy), reads=[pqb], writes=[qT_b])
                for cc in range(2):
                    pc_, pcb = pm[1], pm_b[1]
                    P.op("pe", lambda e, pc_=pc_, cc=cc, h=h: e.matmul(
                        pc_[:], lhsT=wukb[:, h, cc * 128:(cc + 1) * 128], rhs=qT[:], start=True, stop=True),
                        reads=[wukb_b, qT_b], writes=[pcb])
                    P.op("act", lambda e, pc_=pc_, q_=q_, cc=cc: e.activation(out=q_[:, cc, :], in_=pc_[:], func=AF.Copy),
                         reads=[pcb], pwrites=[qb_])
                for sbk in range(nsb):
                    ss_ = slice(sbk * 128, (sbk + 1) * 128)
                    l_, lb_ = pl[nl % 2], pl_b[nl % 2]
                    t_, tb_ = tmp[nl % 2], tmp_b[nl % 2]
                    p_, pb_ = pT[nl % 2], pT_b[nl % 2]
                    nl += 1
                    for cc in range(2):
                        P.op("pe", lambda e, l_=l_, cc=cc, ss_=ss_, q_=q_: e.matmul(
                            l_[:], lhsT=ckvT[:, cc, ss_], rhs=q_[:, cc, :], start=(cc == 0), stop=(cc == 1)),
                            reads=[ckvT_b, qb_], pwrites=[lb_])
                    P.op("dve", lambda e, l_=l_, t_=t_, sbk=sbk: e.scalar_tensor_tensor(
                        out=t_[:], in0=l_[:], scalar=ATT_SCALE, in1=mk[:, sbk, :], op0=ALU.mult, op1=ALU.add),
                        reads=[lb_, mk_b], writes=[tb_])
                    r = sbk - (4 * g - 1)
                    if r >= 0:
                        P.op("pool", lambda e, t_=t_, b_=b_, r=r: e.tensor_tensor(out=t_[:], in0=t_[:], in1=b_[:, r, :], op=ALU.add),
                             reads=[tb_, bb_], writes=[tb_])
                        P.op("act", lambda e, t_=t_, p_=p_: e.activation(out=p_[:], in_=t_[:], func=AF.Exp),
                             reads=[tb_], writes=[pb_])
                    else:
                        P.op("act", lambda e, t_=t_, p_=p_, h=h: e.activation(
                            out=p_[:], in_=t_[:], func=AF.Exp, bias=cvec[:, h:h + 1]), reads=[tb_, cvec_b], writes=[pb_])
                    for cc in range(2):
                        P.op("pe", lambda e, cc=cc, sbk=sbk, p_=p_, nsb=nsb: e.matmul(
                            po[cc][:], lhsT=ckvk[:, sbk, cc * 128:(cc + 1) * 128], rhs=p_[:],
                            start=(sbk == 0), stop=(sbk == nsb - 1)), reads=[ckvk_b, pb_], pwrites=[po_b[cc]])
                    P.op("pe", lambda e, sbk=sbk, p_=p_, nsb=nsb: e.matmul(
                        pden[:], lhsT=k.ones_bf[:], rhs=p_[:], start=(sbk == 0), stop=(sbk == nsb - 1)),
                        reads=[k.ones_b, pb_], pwrites=[pden_b])
                P.op("dve", lambda e: e.reciprocal(out=rden[:], in_=pden[:]), reads=[pden_b], writes=[rden_b])
                for cc in range(2):
                    P.op("dve", lambda e, cc=cc: e.tensor_tensor(out=oln[:, cc, :], in0=po[cc][:], in1=rden[:], op=ALU.mult),
                         reads=[po_b[cc], rden_b], pwrites=[oln_b])
                pq_, pqb = pm[0], pm_b[0]
                for cc in range(2):
                    P.op("pe", lambda e, pq_=pq_, cc=cc, h=h: e.matmul(
                        pq_[:], lhsT=wuvb[:, h, cc, :], rhs=oln[:, cc, :], start=(cc == 0), stop=(cc == 1)),
                        reads=[wuvb_b, oln_b], pwrites=[pqb])
                P.op("act", lambda e, pq_=pq_, h=h: e.activation(out=oT[:, h, :], in_=pq_[:], func=AF.Copy),
                     reads=[pqb], pwrites=[oT_b])
            for dc in range(DC):
                w_, wb_ = wso.load(wo, 0, dc * 128)
                p_, pb_ = pm[nd % 2], pm_b[nd % 2]
                hr, hrb = hres[nd % 2], hres_b[nd % 2]
                residual_load(k, hr, hrb, dc, g)
                for c in range(DC):
                    P.op("pe", lambda e, p_=p_, w_=w_, c=c: e.matmul(
                        p_[:], lhsT=w_[:, c, :], rhs=oT[:, c, :], start=(c == 0), stop=(c == DC - 1)),
                        reads=[wb_, oT_b], pwrites=[pb_])
                P.op("dve", lambda e, p_=p_, hr=hr: e.tensor_tensor(out=hr[:], in0=p_[:], in1=hr[:], op=ALU.add),
                     reads=[pb_, hrb], writes=[hrb])
                residual_store(k, hr, hrb, dc, g)
                nd += 1
    P.barrier()


WEIGHT_SHAPES = {
    "a_w_in": [1, 2048, 848], "a_w_uq": [1, 512, 2048], "a_w_qidx": [1, 512, 1024],
    "a_w_uk": [1, 16, 128, 256], "a_w_uv": [1, 16, 256, 128], "a_w_o": [1, 2048, 2048],
    "b_w_in": [1, 2048, 8192], "b_w_o": [1, 2048, 2048],
    "c_w_group": [1, 4, 512, 512],
    "d_w_pw1": [1, 2048, 4096], "d_w_pw2": [1, 2048, 2048],
    "ffn_w_up": [4, 2048, 11264], "ffn_w_down": [4, 5632, 2048],
}

PLAN_FULL = ["in"] + sum([["norm_mix%d" % i, "mix%d" % i, "norm_ffn%d" % i, "ffn%d" % i] for i in range(DEPTH)], []) + ["out"]


def plan_weights(plan):
    ws = set()
    for p in plan:
        if p.startswith("ffn"):
            ws.update(["ffn_w_up", "ffn_w_down"])
        if p == "mix0":
            ws.update(["a_w_in", "a_w_uq", "a_w_qidx", "a_w_uk", "a_w_uv", "a_w_o"])
        if p == "mix1":
            ws.update(["b_w_in", "b_w_o"])
        if p == "mix2":
            ws.update(["c_w_group"])
        if p == "mix3":
            ws.update(["d_w_pw1", "d_w_pw2"])
    return sorted(ws)


def build_nc(plan=PLAN_FULL, raw_out=False):
    nc = bass.Bass("TRN2", target_bir_lowering=False)
    k = K()
    k.nc = nc
    k.raw_out = raw_out
    k.R = vec_registry()
    k.x = nc.dram_tensor("x", [S, D], F32, kind="ExternalInput").ap()
    k.out = nc.dram_tensor("out", [S, D], F32, kind="ExternalOutput").ap()
    vecs_d = nc.dram_tensor("vecs", [128, k.R.n], F32, kind="ExternalInput").ap()
    ident_d = nc.dram_tensor("ident", [128, 128], F32, kind="ExternalInput").ap()
    k.consts = {}
    for name, arr in make_consts().items():
        k.consts[name] = nc.dram_tensor("c_" + name, list(arr.shape), F32, kind="ExternalInput").ap()
    k.w = {}
    for name in plan_weights(plan):
        k.w[name] = nc.dram_tensor(name, WEIGHT_SHAPES[name], F32, kind="ExternalInput").ap()
    k.lay = {}
    if "mix0" in plan:
        for name, shp in DSA_LAYOUT_SHAPES.items():
            k.lay[name] = nc.dram_tensor(name, shp, F32, kind="ExternalInput").ap()
        k.cqT = nc.dram_tensor("s_cqT", [512, S], BF16).ap()
        k.ckvT = nc.dram_tensor("s_ckvT", [256, S], BF16).ap()
        k.ckv_tok = nc.dram_tensor("s_ckvtok", [S, 256], BF16).ap()
        k.kidxT = nc.dram_tensor("s_kidxT", [128, S], BF16).ap()
        k.widx = nc.dram_tensor("s_widx", [128, NSB, 16], F32).ap()
        k.maskT = nc.dram_tensor("s_maskT", [128, NSB, S], BF16).ap()
    k.gT = nc.dram_tensor("s_gT", [DFF, S], BF16).ap()
    k.hT = nc.dram_tensor("hT", [D, S], F32).ap()
    k.xnT = nc.dram_tensor("xnT", [D, S], BF16).ap()
    with ExitStack() as st:
        P = Prog(nc, st)
        k.P = P
        k.hT_b = P.bufs(NT)
        k.xnT_b = P.bufs(NT)
        k.out_b = P.buf()
        k.dsa_b = P.buf()
        k.gT_b = P.buf()
        k.mask_b = P.buf()
        k.vecs = st.enter_context(nc.sbuf_tensor("vecs_t", [128, k.R.n], F32))
        k.vecs_b = P.buf()
        k.ident = st.enter_context(nc.sbuf_tensor("ident_t", [128, 128], F32))
        k.ident_b = P.buf()
        k.ones_bf = st.enter_context(nc.sbuf_tensor("ones_bf", [128, 128], BF16))
        k.ones_b = P.buf()
        k.eps_t = st.enter_context(nc.sbuf_tensor("eps_t", [128, 1], F32))
        k.eps_b = P.buf()
        P.dma("sp", lambda e: e.dma_start(out=k.vecs[:], in_=vecs_d), writes=[k.vecs_b])
        P.dma("sp", lambda e: e.dma_start(out=k.ident[:], in_=ident_d), writes=[k.ident_b])
        P.op("pool", lambda e: e.memset(k.ones_bf[:], 1.0), writes=[k.ones_b])
        P.op("pool", lambda e: e.memset(k.eps_t[:], EPS), writes=[k.eps_b])
        k.nc = NCProxy(nc)
        for p in plan:
            k.nc.tag += 1
            if p == "in":
                phase_in(k)
            elif p == "out":
                phase_out(k)
            elif p.startswith("norm_"):
                phase_norm(k, p)
            elif p.startswith("ffn"):
                phase_ffn(k, int(p[3:]))
            elif p == "mix0":
                phase_dsa_a(k)
                phase_dsa_b(k)
                phase_dsa_c(k)
            elif p == "mix1":
                phase_mix_hgrn(k, 1)
            elif p == "mix2":
                phase_mix_pool(k)
            elif p == "mix3":
                phase_mix_conf(k)
            else:
                raise NotImplementedError(p)
        P.barrier()
        P.emit()
    return nc


def make_consts():
    c = {}
    invc = np.zeros((128, 64), np.float32)
    for g, w in enumerate((2, 4, 8, 16)):
        for t in range(16):
            invc[:, g * 16 + t] = 1.0 / min(t + 1, w)
    c["pool_invc"] = invc
    blk = np.zeros((128, 128), np.float32)
    blk[0:64, 0:64] = np.triu(np.ones((64, 64), np.float32))
    blk[64:128, 64:128] = np.triu(np.ones((64, 64), np.float32))
    c["hg_mask"] = np.ascontiguousarray(np.tile(blk, (1, TT // 128)))
    hmk = np.zeros((128, 2), np.float32)
    hmk[0:64, 0] = 1.0
    hmk[64:128, 1] = 1.0
    c["half_mask"] = hmk
    cm = np.zeros((128, 128), np.float32)
    cm[np.triu_indices(128, 1)] = -1.0e30
    c["dsa_cm"] = cm
    return c


def make_in_maps(inp, plan, n_cores=8, xs=None):
    vecs = pack_vecs(inp)
    consts = make_consts()
    ident = np.eye(128, dtype=np.float32)
    wnames = plan_weights(plan)
    lay = dsa_layout_inputs(inp) if "mix0" in plan else {}
    maps = []
    for c in range(n_cores):
        m = {"x": np.ascontiguousarray(inp["x"][c] if xs is None else xs[c]), "vecs": vecs, "ident": ident}
        for w in wnames:
            m[w] = np.ascontiguousarray(inp[w], dtype=np.float32)
        for cn, arr in consts.items():
            m["c_" + cn] = arr
        m.update(lay)
        maps.append(m)
    return maps


def kernel(**inputs):
    inp = {k_: np.asarray(v) for k_, v in inputs.items()}
    nc = build_nc(PLAN_FULL)
    maps = make_in_maps(inp, PLAN_FULL, 8)
    res = run_bass_kernel_spmd(nc, maps, core_ids=list(range(8)))
    return np.stack([np.asarray(r["out"]) for r in res.results], axis=0).astype(np.float32)
```

```python
from contextlib import ExitStack
import math
import numpy as np
import concourse.bass as bass
import concourse.mybir as mybir
from concourse.bass_utils import run_bass_kernel_spmd

F32 = mybir.dt.float32
BF16 = mybir.dt.bfloat16
AF = mybir.ActivationFunctionType
ALU = mybir.AluOpType
AX = mybir.AxisListType

D = 2048
S = 4096
DC = D // 128
TT = 512
NT = S // TT
DFF = 5632
FC = DFF // 128
EPS = 1e-6
DEPTH = 4
NDMA_SEMS = 8


class Buf:
    __slots__ = ("name", "w", "wold", "r", "open", "excl")

    def __init__(self, name):
        self.name = name
        self.excl = False
        self.w = {}
        self.wold = {}
        self.r = {}
        self.open = False


def _mx(d, k, v):
    if d.get(k, 0) < v:
        d[k] = v


class Prog:
    STREAMS = ("pe", "act", "dve", "pool", "sp")

    def __init__(self, nc, stack):
        self.nc = nc
        self.ops = {s: [] for s in self.STREAMS}
        self.sems = {}
        self.known = {s: {} for s in self.STREAMS}
        self.cnt = {}
        for s in ("pe", "act", "dve", "pool"):
            self.sems[s] = stack.enter_context(nc.semaphore("c_" + s))
            self.cnt[s] = 0
        self.dma_sems = {}
        self.dma_n = {}
        for q in ("sp", "act", "pool"):
            self.dma_sems[q] = []
            for i in range(NDMA_SEMS):
                k = "d_%s%d" % (q, i)
                self.sems[k] = stack.enter_context(nc.semaphore(k))
                self.cnt[k] = 0
                self.dma_sems[q].append(k)
            self.dma_n[q] = 0
        self.nbuf = 0

    def buf(self, name=None):
        self.nbuf += 1
        return Buf(name or "b%d" % self.nbuf)

    def bufs(self, n, name=None):
        return [self.buf() for _ in range(n)]

    def pbuf(self):
        b = self.buf()
        b.excl = True
        return b

    def pbufs(self, n):
        return [self.pbuf() for _ in range(n)]

    def _deps(self, stream, reads, writes, pwrites, own_key):
        need = {}

        def add(k, v, same_ok):
            if k == own_key and same_ok:
                return
            _mx(need, k, v)

        for b in reads:
            for k, v in b.w.items():
                add(k, v, False)
            for k, v in b.wold.items():
                add(k, v, False)
        for b in writes:
            for dd in (b.w, b.wold, b.r):
                for k, v in dd.items():
                    add(k, v, True)
        for b in pwrites:
            if not b.open:
                for k, v in b.w.items():
                    _mx(b.wold, k, v)
                b.w = {}
                b.open = True
            for dd in (b.wold, b.r):
                for k, v in dd.items():
                    add(k, v, True)
        kn = self.known[stream]
        waits = []
        for k, v in need.items():
            if kn.get(k, 0) < v:
                kn[k] = v
                waits.append((k, v))
        return waits

    def _commit(self, tok, reads, writes, pwrites):
        k, v = tok
        for b in reads:
            _mx(b.r, k, v)
            b.open = False
        for b in writes:
            b.w = {k: v}
            b.wold = {}
            b.r = {}
            b.open = False
        for b in pwrites:
            _mx(b.w, k, v)

    def op(self, stream, fn, reads=(), writes=(), pwrites=()):
        if stream != "pe" and any(b.excl for b in reads):
            writes = list(writes) + [b for b in reads if b.excl]
            reads = [b for b in reads if not b.excl]
        waits = self._deps(stream, reads, writes, pwrites, stream)
        self.cnt[stream] += 1
        tok = (stream, self.cnt[stream])
        self.ops[stream].append((waits, fn, (stream, 1)))
        self._commit(tok, reads, writes, pwrites)
        return tok

    def dma(self, q, fn, reads=(), writes=(), pwrites=()):
        i = self.dma_n[q]
        self.dma_n[q] += 1
        key = self.dma_sems[q][i % NDMA_SEMS]
        waits = self._deps(q, reads, writes, pwrites, None)
        prev = self.cnt[key]
        kn = self.known[q]
        if prev > 0 and kn.get(key, 0) < prev:
            kn[key] = prev
            waits.append((key, prev))
        self.cnt[key] = prev + 16
        tok = (key, prev + 16)
        self.ops[q].append((waits, fn, (key, 16)))
        self._commit(tok, reads, writes, pwrites)
        return tok

    def barrier(self):
        cur = dict(self.cnt)
        for s in self.STREAMS:
            waits = []
            for k, v in cur.items():
                if v > 0 and self.known[s].get(k, 0) < v:
                    self.known[s][k] = v
                    waits.append((k, v))
            if waits:
                self.ops[s].append((waits, None, None))

    def emit(self):
        nc = self.nc
        sems = self.sems

        def run(stream):
            def body(eng):
                for waits, fn, inc in self.ops[stream]:
                    for k, v in waits:
                        eng.wait_ge(sems[k], v)
                    if fn is not None:
                        fn(eng).then_inc(sems[inc[0]], inc[1])
            return body

        with nc.Block() as block:
            block.tensor(run("pe"))
            block.scalar(run("act"))
            block.vector(run("dve"))
            block.gpsimd(run("pool"))
            block.sync(run("sp"))


class VecReg:
    def __init__(self):
        self.off = {}
        self.n = 0

    def add(self, name, ncols):
        self.off[name] = self.n
        self.n += ncols


def vec_registry():
    R = VecReg()
    for i in range(DEPTH):
        R.add("norm_mix%d" % i, DC)
        R.add("norm_ffn%d" % i, DC)
        R.add("ffn_b_conv%d" % i, 2 * FC)
        for k in range(3):
            R.add("ffn_w_conv%d_%d" % (i, k), 2 * FC)
    R.add("final_norm", DC)
    R.add("c_scale", DC)
    R.add("d_b_pw1", 2 * DC)
    for k in range(31):
        R.add("d_w_dw%d" % k, DC)
    R.add("d_b_dw", DC)
    R.add("d_ln_g", DC)
    R.add("d_ln_b", DC)
    R.add("d_b_pw2", DC)
    R.add("b_g_norm", DC)
    for i in range(DEPTH):
        R.add("b_lb%d" % i, DC)
    return R


def _cols(v):
    v = np.ascontiguousarray(v, dtype=np.float32).reshape(-1, 128)
    return v.T


def pack_vecs(inp):
    R = vec_registry()
    out = np.zeros((128, R.n), np.float32)

    def put(name, v):
        c = _cols(v)
        out[:, R.off[name]:R.off[name] + c.shape[1]] = c

    for i in range(DEPTH):
        put("norm_mix%d" % i, inp["norm_mix"][i])
        put("norm_ffn%d" % i, inp["norm_ffn"][i])
        put("ffn_b_conv%d" % i, inp["ffn_b_conv"][i])
        for k in range(3):
            put("ffn_w_conv%d_%d" % (i, k), inp["ffn_w_conv"][i, k])
        put("b_lb%d" % i, inp["b_lower_bounds"][i])
    put("final_norm", inp["final_norm"])
    put("c_scale", inp["c_scale"][0])
    put("d_b_pw1", inp["d_b_pw1"][0])
    for k in range(31):
        put("d_w_dw%d" % k, inp["d_w_dw"][0, k])
    put("d_b_dw", inp["d_b_dw"][0])
    put("d_ln_g", inp["d_ln_g"][0])
    put("d_ln_b", inp["d_ln_b"][0])
    put("d_b_pw2", inp["d_b_pw2"][0])
    put("b_g_norm", inp["b_g_norm"][0])
    return out


class K:
    pass


class NCProxy:
    def __init__(self, nc):
        self._nc = nc
        self.tag = 0

    def sbuf_tensor(self, name, *a, **kw):
        return self._nc.sbuf_tensor("%s_%d" % (name, self.tag), *a, **kw)

    def psum_tensor(self, name, *a, **kw):
        return self._nc.psum_tensor("%s_%d" % (name, self.tag), *a, **kw)

    def __getattr__(self, n):
        return getattr(self._nc, n)


def fm(ap2d, c0, nck, t0, nt):
    return ap2d[c0 * 128:(c0 + nck) * 128, t0:t0 + nt].rearrange("(c p) t -> p c t", p=128)


def phase_in(k):
    nc, P = k.nc, k.P
    with ExitStack() as st:
        xin = [st.enter_context(nc.sbuf_tensor("pi_x%d" % i, [128, D], F32)) for i in range(2)]
        xin_b = P.bufs(2)
        stg = [st.enter_context(nc.sbuf_tensor("pi_s%d" % i, [128, DC, TT], F32)) for i in range(2)]
        stg_b = P.bufs(2)
        ps = [st.enter_context(nc.psum_tensor("pi_p%d" % i, [128, 512], F32)) for i in range(4)]
        ps_b = P.pbufs(4)
        n = 0
        for tt in range(NT):
            sg, sgb = stg[tt % 2], stg_b[tt % 2]
            for sub in range(4):
                si = tt * 4 + sub
                xt, xb = xin[si % 2], xin_b[si % 2]
                P.dma("sp", lambda e, xt=xt, si=si: e.dma_start(out=xt[:], in_=k.x[si * 128:(si + 1) * 128, :]),
                      writes=[xb])
                for cg in range(4):
                    pt, pb = ps[n % 4], ps_b[n % 4]
                    for ci in range(4):
                        c = cg * 4 + ci
                        P.op("pe", lambda e, pt=pt, xt=xt, c=c, ci=ci: e.transpose(
                            out=pt[:, ci * 128:(ci + 1) * 128], in_=xt[:, c * 128:(c + 1) * 128],
                            identity=k.ident[:]), reads=[xb, k.ident_b], pwrites=[pb])
                    eng = "act" if n % 2 == 0 else "dve"
                    if eng == "act":
                        P.op("act", lambda e, pt=pt, sg=sg, cg=cg, sub=sub: e.activation(
                            out=sg[:, cg * 4:(cg + 1) * 4, sub * 128:(sub + 1) * 128],
                            in_=pt[:].rearrange("p (c s) -> p c s", c=4), func=AF.Copy),
                            reads=[pb], pwrites=[sgb])
                    else:
                        P.op("dve", lambda e, pt=pt, sg=sg, cg=cg, sub=sub: e.tensor_copy(
                            out=sg[:, cg * 4:(cg + 1) * 4, sub * 128:(sub + 1) * 128],
                            in_=pt[:].rearrange("p (c s) -> p c s", c=4)),
                            reads=[pb], pwrites=[sgb])
                    n += 1
            P.dma("sp", lambda e, sg=sg, tt=tt: e.dma_start(out=fm(k.hT, 0, DC, tt * TT, TT), in_=sg[:]),
                  reads=[sgb], writes=[k.hT_b[tt]])
    P.barrier()


def norm_tile(k, st_tiles, src_h, src_hb, gcol, out_tile, out_b, tag):
    nc, P = k.nc, k.P
    sq, sq_b, pss, pss_b, rb, rb_b = st_tiles
    for c in range(DC):
        P.op("act", lambda e, c=c: e.activation(out=sq[:, c, :], in_=src_h[:, c, :], func=AF.Square),
             reads=[src_hb], pwrites=[sq_b])
    for c in range(DC):
        P.op("pe", lambda e, c=c: e.matmul(pss[:], lhsT=k.ones_bf[:], rhs=sq[:, c, :],
                                           start=(c == 0), stop=(c == DC - 1)),
             reads=[sq_b, k.ones_b], pwrites=[pss_b])
    P.op("act", lambda e: e.activation(out=rb[:], in_=pss[:], func=AF.Sqrt, bias=k.eps_t[:, 0:1], scale=1.0 / D),
         reads=[pss_b, k.eps_b], writes=[rb_b])
    P.op("dve", lambda e: e.reciprocal(out=rb[:], in_=rb[:]), reads=[rb_b], writes=[rb_b])
    for c in range(DC):
        P.op("dve", lambda e, c=c: e.scalar_tensor_tensor(
            out=out_tile[:, c, :], in0=src_h[:, c, :], scalar=k.vecs[:, gcol + c:gcol + c + 1],
            in1=rb[:], op0=ALU.mult, op1=ALU.mult),
            reads=[src_hb, rb_b, k.vecs_b], pwrites=[out_b])


def phase_norm(k, gname):
    nc, P = k.nc, k.P
    gcol = k.R.off[gname]
    with ExitStack() as st:
        hin = [st.enter_context(nc.sbuf_tensor("pn_h%d" % i, [128, DC, TT], F32)) for i in range(2)]
        hin_b = P.bufs(2)
        sq = st.enter_context(nc.sbuf_tensor("pn_sq", [128, DC, TT], BF16))
        rb = st.enter_context(nc.sbuf_tensor("pn_rb", [128, TT], F32))
        xo = [st.enter_context(nc.sbuf_tensor("pn_o%d" % i, [128, DC, TT], BF16)) for i in range(2)]
        xo_b = P.bufs(2)
        pss = st.enter_context(nc.psum_tensor("pn_ps", [128, TT], F32))
        tiles = (sq, P.buf(), pss, P.pbuf(), rb, P.buf())
        for tt in range(NT):
            h, hb = hin[tt % 2], hin_b[tt % 2]
            o, ob = xo[tt % 2], xo_b[tt % 2]
            P.dma("sp", lambda e, h=h, tt=tt: e.dma_start(out=h[:], in_=fm(k.hT, 0, DC, tt * TT, TT)),
                  reads=[k.hT_b[tt]], writes=[hb])
            norm_tile(k, tiles, h, hb, gcol, o, ob, "pn")
            P.dma("sp", lambda e, o=o, tt=tt: e.dma_start(out=fm(k.xnT, 0, DC, tt * TT, TT), in_=o[:]),
                  reads=[ob], writes=[k.xnT_b[tt]])
    P.barrier()


def phase_out(k):
    nc, P = k.nc, k.P
    gcol = k.R.off["final_norm"]
    with ExitStack() as st:
        hin = [st.enter_context(nc.sbuf_tensor("po_h%d" % i, [128, DC, TT], F32)) for i in range(2)]
        hin_b = P.bufs(2)
        sq = st.enter_context(nc.sbuf_tensor("po_sq", [128, DC, TT], BF16))
        rb = st.enter_context(nc.sbuf_tensor("po_rb", [128, TT], F32))
        xo = st.enter_context(nc.sbuf_tensor("po_o", [128, DC, TT], F32))
        xo_b = P.buf()
        og = [st.enter_context(nc.sbuf_tensor("po_g%d" % i, [128, D], F32)) for i in range(2)]
        og_b = P.bufs(2)
        pss = st.enter_context(nc.psum_tensor("po_ps", [128, TT], F32))
        ps = [st.enter_context(nc.psum_tensor("po_p%d" % i, [128, 512], F32)) for i in range(4)]
        ps_b = P.pbufs(4)
        tiles = (sq, P.buf(), pss, P.pbuf(), rb, P.buf())
        n = 0
        toks = []
        for tt in range(NT):
            h, hb = hin[tt % 2], hin_b[tt % 2]
            P.dma("sp", lambda e, h=h, tt=tt: e.dma_start(out=h[:], in_=fm(k.hT, 0, DC, tt * TT, TT)),
                  reads=[k.hT_b[tt]], writes=[hb])
            if k.raw_out:
                src, srcb = h, hb
            else:
                norm_tile(k, tiles, h, hb, gcol, xo, xo_b, "po")
                src, srcb = xo, xo_b
            for sub in range(4):
                si = tt * 4 + sub
                o, ob = og[si % 2], og_b[si % 2]
                for cg in range(4):
                    pt, pb = ps[n % 4], ps_b[n % 4]
                    for ci in range(4):
                        c = cg * 4 + ci
                        P.op("pe", lambda e, pt=pt, src=src, c=c, ci=ci, sub=sub: e.transpose(
                            out=pt[:, ci * 128:(ci + 1) * 128], in_=src[:, c, sub * 128:(sub + 1) * 128],
                            identity=k.ident[:]), reads=[srcb, k.ident_b], pwrites=[pb])
                    if n % 2 == 0:
                        P.op("act", lambda e, pt=pt, o=o, cg=cg: e.activation(
                            out=o[:, cg * 512:(cg + 1) * 512], in_=pt[:], func=AF.Copy),
                            reads=[pb], pwrites=[ob])
                    else:
                        P.op("dve", lambda e, pt=pt, o=o, cg=cg: e.tensor_copy(
                            out=o[:, cg * 512:(cg + 1) * 512], in_=pt[:]),
                            reads=[pb], pwrites=[ob])
                    n += 1
                toks.append(P.dma("sp", lambda e, o=o, si=si: e.dma_start(
                    out=k.out[si * 128:(si + 1) * 128, :], in_=o[:]), reads=[ob], writes=[k.out_b]))
    P.barrier()


def phase_ffn(k, L):
    phase_ffn_up(k, L)
    phase_ffn_down(k, L)


def phase_ffn_up(k, L):
    nc, P = k.nc, k.P
    R = k.R
    wup = k.w["ffn_w_up"][L]
    bcol = R.off["ffn_b_conv%d" % L]
    wcol = [R.off["ffn_w_conv%d_%d" % (L, t)] for t in range(3)]
    with ExitStack() as st:
        xn = st.enter_context(nc.sbuf_tensor("fu_xn", [128, DC, S], BF16))
        xn_b = P.bufs(NT)
        sup = st.enter_context(nc.sbuf_tensor("fu_su", [128, 2, DC, 128], F32))
        sup_b = P.buf()
        wub = [st.enter_context(nc.sbuf_tensor("fu_wu%d" % i, [128, 2, DC, 128], BF16)) for i in range(2)]
        wub_b = P.bufs(2)
        ub = [st.enter_context(nc.sbuf_tensor("fu_ub%d" % i, [128, 2, TT + 2], F32)) for i in range(2)]
        ub_b = P.bufs(2)
        acc = [st.enter_context(nc.sbuf_tensor("fu_ac%d" % i, [128, 2, TT], F32)) for i in range(2)]
        acc_b = P.bufs(2)
        sil = [st.enter_context(nc.sbuf_tensor("fu_si%d" % i, [128, TT], F32)) for i in range(2)]
        sil_b = P.bufs(2)
        grow = [st.enter_context(nc.sbuf_tensor("fu_g%d" % i, [128, S], BF16)) for i in range(2)]
        grow_b = P.bufs(2)
        pu = [st.enter_context(nc.psum_tensor("fu_pu%d" % i, [128, 2, TT], F32)) for i in range(2)]
        pu_b = P.pbufs(2)
        for tt in range(NT):
            P.dma("sp", lambda e, tt=tt: e.dma_start(out=xn[:, :, tt * TT:(tt + 1) * TT], in_=fm(k.xnT, 0, DC, tt * TT, TT)),
                  reads=[k.xnT_b[tt]], writes=[xn_b[tt]])
        nu = 0
        for j in range(FC):
            w_, wb_ = wub[j % 2], wub_b[j % 2]
            gr, grb = grow[j % 2], grow_b[j % 2]
            for half in range(2):
                n0 = half * DFF + j * 128
                P.dma("sp", lambda e, half=half, n0=n0: e.dma_start(
                    out=sup[:, half, :, :], in_=wup[:, n0:n0 + 128].rearrange("(c p) n -> p c n", p=128)),
                    pwrites=[sup_b])
            P.op("pool", lambda e, w_=w_: e.tensor_copy(out=w_[:], in_=sup[:]), reads=[sup_b], writes=[wb_])
            for tt in range(NT):
                ts_ = slice(tt * TT, (tt + 1) * TT)
                u_, ubb = ub[nu % 2], ub_b[nu % 2]
                up_, upb = ub[(nu + 1) % 2], ub_b[(nu + 1) % 2]
                a_, ab_ = acc[nu % 2], acc_b[nu % 2]
                si_, sib = sil[nu % 2], sil_b[nu % 2]
                p_, pb_ = pu[nu % 2], pu_b[nu % 2]
                for half in range(2):
                    for c in range(DC):
                        P.op("pe", lambda e, p_=p_, w_=w_, half=half, c=c, ts_=ts_: e.matmul(
                            p_[:, half, :], lhsT=w_[:, half, c, :], rhs=xn[:, c, ts_],
                            start=(c == 0), stop=(c == DC - 1)),
                            reads=[wb_, xn_b[tt]], pwrites=[pb_])
                if tt == 0:
                    P.op("pool", lambda e, u_=u_: e.memset(u_[:, :, 0:2], 0.0), pwrites=[ubb])
                else:
                    P.op("pool", lambda e, u_=u_, up_=up_: e.tensor_copy(out=u_[:, :, 0:2], in_=up_[:, :, TT:TT + 2]),
                         reads=[upb], pwrites=[ubb])
                P.op("act", lambda e, u_=u_, p_=p_: e.activation(
                    out=u_[:, :, 2:TT + 2], in_=p_[:], func=AF.Copy), reads=[pb_], pwrites=[ubb])
                for half in range(2):
                    col = half * FC + j
                    P.op("dve", lambda e, a_=a_, u_=u_, half=half, col=col: e.tensor_scalar(
                        out=a_[:, half, :], in0=u_[:, half, 2:TT + 2],
                        scalar1=k.vecs[:, wcol[2] + col:wcol[2] + col + 1],
                        scalar2=k.vecs[:, bcol + col:bcol + col + 1], op0=ALU.mult, op1=ALU.add),
                        reads=[ubb, k.vecs_b], pwrites=[ab_])
                for tap in (1, 0):
                    for half in range(2):
                        col = half * FC + j
                        P.op("dve", lambda e, a_=a_, u_=u_, half=half, col=col, tap=tap: e.scalar_tensor_tensor(
                            out=a_[:, half, :], in0=u_[:, half, tap:tap + TT],
                            scalar=k.vecs[:, wcol[tap] + col:wcol[tap] + col + 1],
                            in1=a_[:, half, :], op0=ALU.mult, op1=ALU.add),
                            reads=[ubb, k.vecs_b, ab_], pwrites=[ab_])
                P.op("act", lambda e, si_=si_, a_=a_: e.activation(out=si_[:], in_=a_[:, 0, :], func=AF.Silu),
                     reads=[ab_], writes=[sib])
                P.op("pool", lambda e, si_=si_, a_=a_, gr=gr, ts_=ts_: e.tensor_tensor(
                    out=gr[:, ts_], in0=si_[:], in1=a_[:, 1, :], op=ALU.mult),
                    reads=[sib, ab_], pwrites=[grb])
                nu += 1
            P.dma("sp", lambda e, gr=gr, j=j: e.dma_start(out=k.gT[j * 128:(j + 1) * 128, :], in_=gr[:]),
                  reads=[grb], pwrites=[k.gT_b])
    P.barrier()


FD_T = 1024


def phase_ffn_down(k, L):
    nc, P = k.nc, k.P
    wdn = k.w["ffn_w_down"][L]
    NTD = S // FD_T
    with ExitStack() as st:
        g = st.enter_context(nc.sbuf_tensor("fd_g", [128, FC, FD_T], BF16))
        g_b = P.bufs(4)
        sdn = [st.enter_context(nc.sbuf_tensor("fd_sd%d" % i, [128, 22, 256], F32)) for i in range(2)]
        sdn_b = P.bufs(2)
        wdb = [st.enter_context(nc.sbuf_tensor("fd_wd%d" % i, [128, FC, 256], BF16)) for i in range(2)]
        wdb_b = P.bufs(2)
        hres = [st.enter_context(nc.sbuf_tensor("fd_hr%d" % i, [128, TT], F32)) for i in range(4)]
        hres_b = P.bufs(4)
        pd = [st.enter_context(nc.psum_tensor("fd_pd%d" % i, [128, TT], F32)) for i in range(4)]
        pd_b = P.pbufs(4)
        nd = 0
        nw = 0
        for t2 in range(NTD):
            t0 = t2 * FD_T
            for q4 in range(4):
                P.dma("sp", lambda e, q4=q4, t0=t0: e.dma_start(
                    out=g[:, q4 * 11:(q4 + 1) * 11, :], in_=fm(k.gT, q4 * 11, 11, t0, FD_T)),
                    reads=[k.gT_b], writes=[g_b[q4]])
            for dp in range(DC // 2):
                w_, wb_ = wdb[nw % 2], wdb_b[nw % 2]
                nw += 1
                for hf in range(2):
                    s_, sb_ = sdn[hf], sdn_b[hf]
                    P.dma("sp", lambda e, s_=s_, hf=hf, dp=dp: e.dma_start(
                        out=s_[:], in_=wdn[hf * 22 * 128:(hf + 1) * 22 * 128, dp * 256:(dp + 1) * 256].rearrange(
                            "(c p) n -> p c n", p=128)), writes=[sb_])
                    P.op("pool", lambda e, s_=s_, w_=w_, hf=hf: e.tensor_copy(
                        out=w_[:, hf * 22:(hf + 1) * 22, :], in_=s_[:]), reads=[sb_], pwrites=[wb_])
                for di in range(2):
                    dc = dp * 2 + di
                    for th in range(FD_T // TT):
                        tt = (t0 // TT) + th
                        p_, pb_ = pd[nd % 4], pd_b[nd % 4]
                        hr, hrb = hres[nd % 4], hres_b[nd % 4]
                        nd += 1
                        residual_load(k, hr, hrb, dc, tt)
                        for c in range(FC):
                            P.op("pe", lambda e, p_=p_, w_=w_, c=c, di=di, th=th: e.matmul(
                                p_[:], lhsT=w_[:, c, di * 128:(di + 1) * 128], rhs=g[:, c, th * TT:(th + 1) * TT],
                                start=(c == 0), stop=(c == FC - 1)),
                                reads=[wb_, g_b[c // 11]], pwrites=[pb_])
                        P.op("dve", lambda e, hr=hr, p_=p_: e.tensor_tensor(out=hr[:], in0=p_[:], in1=hr[:], op=ALU.add),
                             reads=[pb_, hrb], writes=[hrb])
                        residual_store(k, hr, hrb, dc, tt)
    P.barrier()


class WStream:
    def __init__(self, k, st, name, KC, ncol=128, nbuf=2, nstg=None):
        nc, P = k.nc, k.P
        nstg = nstg or nbuf
        self.k, self.KC, self.ncol, self.nbuf, self.nstg = k, KC, ncol, nbuf, nstg
        self.stg = [st.enter_context(nc.sbuf_tensor("%s_s%d" % (name, i), [128, KC, ncol], F32)) for i in range(nstg)]
        self.wb = [st.enter_context(nc.sbuf_tensor("%s_w%d" % (name, i), [128, KC, ncol], BF16)) for i in range(nbuf)]
        self.stg_b = P.bufs(nstg)
        self.wb_b = P.bufs(nbuf)
        self.n = 0

    def load(self, W2d, r0, c0):
        P = self.k.P
        i = self.n % self.nbuf
        si = self.n % self.nstg
        self.n += 1
        stg, wb = self.stg[si], self.wb[i]
        KC, ncol = self.KC, self.ncol
        P.dma("sp", lambda e: e.dma_start(
            out=stg[:], in_=W2d[r0:r0 + KC * 128, c0:c0 + ncol].rearrange("(c p) n -> p c n", p=128)),
            writes=[self.stg_b[si]])
        P.op("pool", lambda e: e.tensor_copy(out=wb[:], in_=stg[:]), reads=[self.stg_b[si]], writes=[self.wb_b[i]])
        return wb, self.wb_b[i]


def residual_store(k, hr, hrb, dc, tt):
    k.P.dma("sp", lambda e: e.dma_start(
        out=k.hT[dc * 128:(dc + 1) * 128, tt * TT:(tt + 1) * TT], in_=hr[:]),
        reads=[hrb], pwrites=[k.hT_b[tt]])


def residual_load(k, hr, hrb, dc, tt):
    k.P.dma("sp", lambda e: e.dma_start(
        out=hr[:], in_=k.hT[dc * 128:(dc + 1) * 128, tt * TT:(tt + 1) * TT]),
        reads=[k.hT_b[tt]], writes=[hrb])


POOL_H = 15


def phase_mix_pool(k):
    nc, P = k.nc, k.P
    wg = k.w["c_w_group"][0]
    scol = k.R.off["c_scale"]
    H = POOL_H
    W_ = TT + H
    with ExitStack() as st:
        wres = st.enter_context(nc.sbuf_tensor("pl_w", [128, 4, 4, 512], BF16))
        wres_b = P.buf()
        stg = [st.enter_context(nc.sbuf_tensor("pl_s%d" % i, [128, 4, 512], F32)) for i in range(2)]
        stg_b = P.bufs(2)
        invc = st.enter_context(nc.sbuf_tensor("pl_ic", [128, 64], F32))
        invc_b = P.buf()
        P.dma("sp", lambda e: e.dma_start(out=invc[:], in_=k.consts["pool_invc"]), writes=[invc_b])
        for g in range(4):
            sg, sgb = stg[g % 2], stg_b[g % 2]
            P.dma("sp", lambda e, sg=sg, g=g: e.dma_start(
                out=sg[:], in_=wg[g].rearrange("(c p) n -> p c n", p=128)), writes=[sgb])
            P.op("pool", lambda e, sg=sg, g=g: e.tensor_copy(out=wres[:, g, :, :], in_=sg[:]),
                 reads=[sgb], pwrites=[wres_b])
        xn = [st.enter_context(nc.sbuf_tensor("pl_x%d" % i, [128, DC, W_], BF16)) for i in range(2)]
        xn_b = P.bufs(2)
        pp = [[st.enter_context(nc.sbuf_tensor("pl_p%d%d" % (e_, i), [128, W_], F32)) for i in range(2)] for e_ in range(2)]
        pp_b = [P.bufs(2) for _ in range(2)]
        t16 = st.enter_context(nc.sbuf_tensor("pl_t16", [128, 16], F32))
        t16_b = P.buf()
        diff = st.enter_context(nc.sbuf_tensor("pl_d", [128, DC, TT], BF16))
        diff_b = P.buf()
        hres = [st.enter_context(nc.sbuf_tensor("pl_h%d" % i, [128, TT], F32)) for i in range(2)]
        hres_b = P.bufs(2)
        ps = [st.enter_context(nc.psum_tensor("pl_ps%d" % i, [128, TT], F32)) for i in range(2)]
        ps_b = P.pbufs(2)
        nn = 0
        for tt in range(NT):
            x_, xb = xn[tt % 2], xn_b[tt % 2]
            if tt == 0:
                P.op("pool", lambda e, x_=x_: e.memset(x_[:, :, 0:H], 0.0), pwrites=[xb])
                P.dma("sp", lambda e, x_=x_: e.dma_start(out=x_[:, :, H:W_], in_=fm(k.xnT, 0, DC, 0, TT)),
                      reads=[k.xnT_b[0]], pwrites=[xb])
            else:
                P.dma("sp", lambda e, x_=x_, tt=tt: e.dma_start(out=x_[:], in_=fm(k.xnT, 0, DC, tt * TT - H, W_)),
                      reads=[k.xnT_b[tt - 1], k.xnT_b[tt]], writes=[xb])
            for c in range(DC):
                g = c // 4
                w = 2 << g
                ei = c % 2
                eng = "dve" if ei == 0 else "pool"
                cur, curb = x_[:, c, :], xb
                for stp in range(g + 1):
                    sh = 1 << stp
                    nxt, nxtb = pp[ei][stp % 2], pp_b[ei][stp % 2]
                    P.op(eng, lambda e, nxt=nxt, cur=cur, sh=sh: e.tensor_tensor(
                        out=nxt[:, sh:W_], in0=cur[:, sh:W_], in1=cur[:, 0:W_ - sh], op=ALU.add),
                        reads=[curb], writes=[nxtb])
                    cur, curb = nxt[:], nxtb
                P.op("dve", lambda e, cur=cur, c=c, w=w, x_=x_: e.scalar_tensor_tensor(
                    out=diff[:, c, :], in0=cur[:, H:W_], scalar=1.0 / w, in1=x_[:, c, H:W_],
                    op0=ALU.mult, op1=ALU.subtract), reads=[curb, xb], pwrites=[diff_b])
                if tt == 0:
                    P.op("dve", lambda e, cur=cur, g=g: e.tensor_tensor(
                        out=t16[:], in0=cur[:, H:H + 16], in1=invc[:, g * 16:(g + 1) * 16], op=ALU.mult),
                        reads=[curb, invc_b], writes=[t16_b])
                    P.op("dve", lambda e, c=c, x_=x_: e.tensor_tensor(
                        out=diff[:, c, 0:16], in0=t16[:], in1=x_[:, c, H:H + 16], op=ALU.subtract),
                        reads=[t16_b, xb], pwrites=[diff_b])
            for n in range(DC):
                g, ni = n // 4, n % 4
                p_, pb_ = ps[nn % 2], ps_b[nn % 2]
                hr, hrb = hres[nn % 2], hres_b[nn % 2]
                residual_load(k, hr, hrb, n, tt)
                for kc in range(4):
                    P.op("pe", lambda e, p_=p_, g=g, kc=kc, ni=ni: e.matmul(
                        p_[:], lhsT=wres[:, g, kc, ni * 128:(ni + 1) * 128], rhs=diff[:, g * 4 + kc, :],
                        start=(kc == 0), stop=(kc == 3)), reads=[wres_b, diff_b], pwrites=[pb_])
                P.op("dve", lambda e, p_=p_, hr=hr, n=n: e.scalar_tensor_tensor(
                    out=hr[:], in0=p_[:], scalar=k.vecs[:, scol + n:scol + n + 1], in1=hr[:],
                    op0=ALU.mult, op1=ALU.add), reads=[pb_, hrb, k.vecs_b], writes=[hrb])
                residual_store(k, hr, hrb, n, tt)
                nn += 1
    P.barrier()


CONF_W = 31
CONF_H = CONF_W - 1


def phase_mix_conf(k):
    nc, P = k.nc, k.P
    R = k.R
    w1 = k.w["d_w_pw1"][0]
    w2 = k.w["d_w_pw2"][0]
    b1 = R.off["d_b_pw1"]
    wdw = [R.off["d_w_dw%d" % t] for t in range(CONF_W)]
    bdw = R.off["d_b_dw"]
    lng, lnb = R.off["d_ln_g"], R.off["d_ln_b"]
    b2 = R.off["d_b_pw2"]
    H = CONF_H
    W_ = TT + H
    with ExitStack() as st:
        xn = st.enter_context(nc.sbuf_tensor("cf_xn", [128, DC, TT], BF16))
        xn_b = P.buf()
        ws1 = WStream(k, st, "cf_w1", DC, 256, nbuf=2)
        ws2 = WStream(k, st, "cf_w2", DC, 128, nbuf=2, nstg=1)
        ub = st.enter_context(nc.sbuf_tensor("cf_ub", [128, DC, W_], F32))
        ub_b = [P.buf() for _ in range(DC)]
        gate = [st.enter_context(nc.sbuf_tensor("cf_g%d" % i, [128, TT], F32)) for i in range(2)]
        gate_b = P.bufs(2)
        v = st.enter_context(nc.sbuf_tensor("cf_v", [128, DC, TT], F32))
        v_b = [P.buf() for _ in range(DC)]
        sq = st.enter_context(nc.sbuf_tensor("cf_sq", [128, DC, TT], BF16))
        sq_b = P.buf()
        ones_f = st.enter_context(nc.sbuf_tensor("cf_1f", [128, 128], F32))
        ones_fb = P.buf()
        P.op("pool", lambda e: e.memset(ones_f[:], 1.0), writes=[ones_fb])
        mean = st.enter_context(nc.sbuf_tensor("cf_mean", [128, TT], F32))
        mean_b = P.buf()
        rstd = st.enter_context(nc.sbuf_tensor("cf_rstd", [128, TT], F32))
        rstd_b = P.buf()
        tmp = [st.enter_context(nc.sbuf_tensor("cf_t%d" % i, [128, TT], F32)) for i in range(1)] * 2
        tmp_b = [P.buf()] * 2
        lo = st.enter_context(nc.sbuf_tensor("cf_lo", [128, DC, TT], BF16))
        lo_b = P.buf()
        hres = [st.enter_context(nc.sbuf_tensor("cf_h%d" % i, [128, TT], F32)) for i in range(2)]
        hres_b = P.bufs(2)
        pa = [st.enter_context(nc.psum_tensor("cf_pa%d" % i, [128, TT], F32)) for i in range(2)]
        pa_b = P.pbufs(2)
        pg = [st.enter_context(nc.psum_tensor("cf_pg%d" % i, [128, TT], F32)) for i in range(2)]
        pg_b = P.pbufs(2)
        pm = st.enter_context(nc.psum_tensor("cf_pm", [128, TT], F32))
        pm_b = P.pbuf()
        pq = st.enter_context(nc.psum_tensor("cf_pq", [128, TT], F32))
        pq_b = P.pbuf()
        po = [st.enter_context(nc.psum_tensor("cf_po%d" % i, [128, TT], F32)) for i in range(2)]
        po_b = P.pbufs(2)
        nj = 0
        nd = 0
        for tt in range(NT):
            P.dma("sp", lambda e, tt=tt: e.dma_start(out=xn[:], in_=fm(k.xnT, 0, DC, tt * TT, TT)),
                  reads=[k.xnT_b[tt]], writes=[xn_b])
            for jp in range(DC // 2):
                wa, wab = ws1.load(w1, 0, jp * 256)
                wgt, wgb = ws1.load(w1, 0, D + jp * 256)
                for sub in range(2):
                    j = jp * 2 + sub
                    ubj = ub_b[j]
                    if tt == 0:
                        P.op("pool", lambda e, j=j: e.memset(ub[:, j, 0:H], 0.0), pwrites=[ubj])
                    else:
                        P.op("pool", lambda e, j=j: e.tensor_copy(out=ub[:, j, 0:H], in_=ub[:, j, TT:W_]),
                             reads=[ubj], writes=[ubj])
                    pa_, pab = pa[sub], pa_b[sub]
                    pg_, pgb = pg[sub], pg_b[sub]
                    gt, gtb = gate[sub], gate_b[sub]
                    for c in range(DC):
                        P.op("pe", lambda e, pa_=pa_, wa=wa, c=c, sub=sub: e.matmul(
                            pa_[:], lhsT=wa[:, c, sub * 128:(sub + 1) * 128], rhs=xn[:, c, :],
                            start=(c == 0), stop=(c == DC - 1)), reads=[wab, xn_b], pwrites=[pab])
                    for c in range(DC):
                        P.op("pe", lambda e, pg_=pg_, wgt=wgt, c=c, sub=sub: e.matmul(
                            pg_[:], lhsT=wgt[:, c, sub * 128:(sub + 1) * 128], rhs=xn[:, c, :],
                            start=(c == 0), stop=(c == DC - 1)), reads=[wgb, xn_b], pwrites=[pgb])
                    P.op("act", lambda e, gt=gt, pg_=pg_, j=j: e.activation(
                        out=gt[:], in_=pg_[:], func=AF.Sigmoid, bias=k.vecs[:, b1 + DC + j:b1 + DC + j + 1]),
                        reads=[pgb, k.vecs_b], writes=[gtb])
                    P.op("dve", lambda e, gt=gt, pa_=pa_, j=j: e.scalar_tensor_tensor(
                        out=ub[:, j, H:W_], in0=pa_[:], scalar=k.vecs[:, b1 + j:b1 + j + 1], in1=gt[:],
                        op0=ALU.add, op1=ALU.mult), reads=[pab, gtb, k.vecs_b], pwrites=[ubj])
                    P.op("act", lambda e, j=j: e.activation(
                        out=v[:, j, :], in_=ub[:, j, H:W_], func=AF.Identity,
                        scale=k.vecs[:, wdw[CONF_W - 1] + j:wdw[CONF_W - 1] + j + 1],
                        bias=k.vecs[:, bdw + j:bdw + j + 1]), reads=[ubj, k.vecs_b], writes=[v_b[j]])
                for tap in range(CONF_W - 1):
                    for sub in range(2):
                        j = jp * 2 + sub
                        P.op("dve", lambda e, j=j, tap=tap: e.scalar_tensor_tensor(
                            out=v[:, j, :], in0=ub[:, j, tap:tap + TT],
                            scalar=k.vecs[:, wdw[tap] + j:wdw[tap] + j + 1], in1=v[:, j, :],
                            op0=ALU.mult, op1=ALU.add), reads=[ub_b[j], v_b[j], k.vecs_b], writes=[v_b[j]])
                for sub in range(2):
                    j = jp * 2 + sub
                    P.op("act", lambda e, j=j: e.activation(out=sq[:, j, :], in_=v[:, j, :], func=AF.Square),
                         reads=[v_b[j]], pwrites=[sq_b])
            for c in range(DC):
                P.op("pe", lambda e, c=c: e.matmul(pm[:], lhsT=ones_f[:], rhs=v[:, c, :],
                                                   start=(c == 0), stop=(c == DC - 1)),
                     reads=[ones_fb, v_b[c]], pwrites=[pm_b])
            for c in range(DC):
                P.op("pe", lambda e, c=c: e.matmul(pq[:], lhsT=k.ones_bf[:], rhs=sq[:, c, :],
                                                   start=(c == 0), stop=(c == DC - 1)),
                     reads=[k.ones_b, sq_b], pwrites=[pq_b])
            P.op("act", lambda e: e.activation(out=mean[:], in_=pm[:], func=AF.Copy, scale=1.0 / D),
                 reads=[pm_b], writes=[mean_b])
            P.op("dve", lambda e: e.tensor_tensor(out=rstd[:], in0=mean[:], in1=mean[:], op=ALU.mult),
                 reads=[mean_b], writes=[rstd_b])
            P.op("dve", lambda e: e.scalar_tensor_tensor(
                out=rstd[:], in0=pq[:], scalar=1.0 / D, in1=rstd[:], op0=ALU.mult, op1=ALU.subtract),
                reads=[pq_b, rstd_b], writes=[rstd_b])
            P.op("act", lambda e: e.activation(out=rstd[:], in_=rstd[:], func=AF.Sqrt, bias=k.eps_t[:, 0:1]),
                 reads=[rstd_b, k.eps_b], writes=[rstd_b])
            P.op("dve", lambda e: e.reciprocal(out=rstd[:], in_=rstd[:]), reads=[rstd_b], writes=[rstd_b])
            for c in range(DC):
                t_, tb = tmp[c % 2], tmp_b[c % 2]
                P.op("pool", lambda e, t_=t_, c=c: e.tensor_tensor(out=t_[:], in0=v[:, c, :], in1=mean[:], op=ALU.subtract),
                     reads=[v_b[c], mean_b], writes=[tb])
                P.op("dve", lambda e, t_=t_, c=c: e.scalar_tensor_tensor(
                    out=t_[:], in0=t_[:], scalar=k.vecs[:, lng + c:lng + c + 1], in1=rstd[:],
                    op0=ALU.mult, op1=ALU.mult), reads=[tb, rstd_b, k.vecs_b], writes=[tb])
                P.op("act", lambda e, t_=t_, c=c: e.activation(
                    out=lo[:, c, :], in_=t_[:], func=AF.Silu, bias=k.vecs[:, lnb + c:lnb + c + 1]),
                    reads=[tb, k.vecs_b], pwrites=[lo_b])
            for dc in range(DC):
                w_, wb_ = ws2.load(w2, 0, dc * 128)
                p_, pb_ = po[nd % 2], po_b[nd % 2]
                hr, hrb = hres[nd % 2], hres_b[nd % 2]
                residual_load(k, hr, hrb, dc, tt)
                for c in range(DC):
                    P.op("pe", lambda e, p_=p_, w_=w_, c=c: e.matmul(
                        p_[:], lhsT=w_[:, c, :], rhs=lo[:, c, :], start=(c == 0), stop=(c == DC - 1)),
                        reads=[wb_, lo_b], pwrites=[pb_])
                P.op("dve", lambda e, p_=p_, hr=hr, dc=dc: e.scalar_tensor_tensor(
                    out=hr[:], in0=p_[:], scalar=k.vecs[:, b2 + dc:b2 + dc + 1], in1=hr[:],
                    op0=ALU.add, op1=ALU.add), reads=[pb_, hrb, k.vecs_b], writes=[hrb])
                residual_store(k, hr, hrb, dc, tt)
                nd += 1
    P.barrier()


HG_C = 64


def phase_mix_hgrn(k, L):
    nc, P = k.nc, k.P
    R = k.R
    win = k.w["b_w_in"][0]
    wo = k.w["b_w_o"][0]
    gn = R.off["b_g_norm"]
    NCH = TT // HG_C
    with ExitStack() as st:
        def sb(name, shape, dt=F32):
            return st.enter_context(nc.sbuf_tensor("hg_" + name, shape, dt))

        xn = sb("xn", [128, DC, TT], BF16); xn_b = P.buf()
        ws = WStream(k, st, "hg_wi", DC, 256, nbuf=4, nstg=2)
        wso = WStream(k, st, "hg_wo", DC, 128, nbuf=2)
        lb = sb("lb", [128, DC]); oml = sb("oml", [128, DC]); lbt = sb("lbt", [128, 4, DC]); lb_b = P.buf()
        ones64 = sb("ones64", [128, HG_C]); ones64_b = P.buf()
        mask = sb("mask", [128, TT]); mask_b = P.buf()
        identb = sb("identb", [128, 128], BF16); identb_b = P.buf()
        state = sb("state", [128, 16, 128]); state_b = [P.buf() for _ in range(16)]
        snap = sb("snap", [128, NCH + 1, 128], BF16); snap_b = [P.buf() for _ in range(NCH + 1)]
        sg = sb("sg", [128, TT]); sg_b = P.buf()
        lf = sb("lf", [128, TT]); lf_b = P.buf()
        kk = sb("kk", [128, TT]); kk_b = P.buf()
        a = sb("a", [128, TT]); a_b = P.buf()
        ea = sb("ea", [128, TT]); ea_b = P.buf()
        ena = sb("ena", [128, TT]); ena_b = P.buf()
        qs = sb("qs", [128, TT]); qs_b = P.buf()
        gs = sb("gs", [128, TT]); gs_b = P.buf()
        tmp = sb("tmp", [128, TT]); tmp_b = P.buf()
        rs = sb("rs", [128, TT]); rs_b = P.buf()
        qt = sb("qt", [128, TT], BF16); qt_b = P.buf()
        kt = sb("kt", [128, TT], BF16); kt_b = P.buf()
        kh = sb("kh", [128, TT], BF16); kh_b = P.buf()
        vb = sb("vb", [128, TT], BF16); vb_b = P.buf()
        osq = sb("osq", [128, TT], BF16); osq_b = P.buf()
        NB = TT // 128
        scb = sb("scb", [128, TT], BF16); scb_b = P.buf()
        vtok = sb("vtok", [128, NB, 128], BF16); vtok_b = P.buf()
        khA = sb("khA", [128, NB, 128], BF16); khA_b = P.buf()
        khB = sb("khB", [128, NB, 128], BF16); khB_b = P.buf()
        hmask = sb("hmask", [128, 2]); hmask_b = P.buf()
        ob = sb("ob", [128, DC, TT], BF16); ob_b = P.buf()
        hres = [sb("hr%d" % i, [128, TT]) for i in range(2)]; hres_b = P.bufs(2)
        pq = st.enter_context(nc.psum_tensor("hg_pq", [128, TT], F32)); pq_b = P.pbuf()
        pf = st.enter_context(nc.psum_tensor("hg_pf", [128, TT], F32)); pf_b = P.pbuf()
        pi = st.enter_context(nc.psum_tensor("hg_pi", [128, TT], F32)); pi_b = P.pbuf()
        pg = st.enter_context(nc.psum_tensor("hg_pg", [128, TT], F32)); pg_b = P.pbuf()
        po = st.enter_context(nc.psum_tensor("hg_po", [128, TT], F32)); po_b = P.pbuf()
        psc = st.enter_context(nc.psum_tensor("hg_psc", [128, TT], F32)); psc_b = P.pbuf()
        pkv = st.enter_context(nc.psum_tensor("hg_pkv", [128, 4, 128], F32)); pkv_b = [P.pbuf()] * 4
        ptr = st.enter_context(nc.psum_tensor("hg_ptr", [128, TT // 128, 128], BF16)); ptr_b = P.pbuf()

        P.dma("sp", lambda e: e.dma_start(out=mask[:], in_=k.consts["hg_mask"]), writes=[mask_b])
        P.dma("sp", lambda e: e.dma_start(out=hmask[:], in_=k.consts["half_mask"]), writes=[hmask_b])
        P.op("pool", lambda e: e.memset(ones64[:], 1.0), writes=[ones64_b])
        P.op("pool", lambda e: e.tensor_copy(out=identb[:], in_=k.ident[:]), reads=[k.ident_b], writes=[identb_b])
        P.op("pool", lambda e: e.memset(state[:], 0.0), writes=state_b)
        for l in range(DEPTH):
            c0 = R.off["b_lb%d" % l]
            P.op("act", lambda e, l=l, c0=c0: e.activation(out=lbt[:, l, :], in_=k.vecs[:, c0:c0 + DC], func=AF.Exp),
                 reads=[k.vecs_b], pwrites=[lb_b])
        P.op("dve", lambda e: e.tensor_tensor(out=oml[:], in0=lbt[:, 0, :], in1=lbt[:, 1, :], op=ALU.add),
             reads=[lb_b], pwrites=[lb_b])
        P.op("dve", lambda e: e.tensor_tensor(out=oml[:], in0=oml[:], in1=lbt[:, 2, :], op=ALU.add),
             reads=[lb_b], writes=[lb_b])
        P.op("dve", lambda e: e.tensor_tensor(out=oml[:], in0=oml[:], in1=lbt[:, 3, :], op=ALU.add),
             reads=[lb_b], writes=[lb_b])
        P.op("dve", lambda e: e.reciprocal(out=oml[:], in_=oml[:]), reads=[lb_b], writes=[lb_b])
        P.op("dve", lambda e: e.tensor_copy(out=lb[:], in_=lbt[:, 1, :]), reads=[lb_b], writes=[lb_b])
        for l in range(2, L + 1):
            P.op("dve", lambda e, l=l: e.tensor_tensor(out=lb[:], in0=lb[:], in1=lbt[:, l, :], op=ALU.add),
                 reads=[lb_b], writes=[lb_b])
        P.op("dve", lambda e: e.tensor_tensor(out=lb[:], in0=lb[:], in1=oml[:], op=ALU.mult),
             reads=[lb_b], writes=[lb_b])
        P.op("dve", lambda e: e.tensor_scalar(out=oml[:], in0=lb[:], scalar1=-1.0, scalar2=1.0,
                                              op0=ALU.mult, op1=ALU.add), reads=[lb_b], writes=[lb_b])
        nd = 0
        NH = 16
        for tt in range(NT):
            P.dma("sp", lambda e, tt=tt: e.dma_start(out=xn[:], in_=fm(k.xnT, 0, DC, tt * TT, TT)),
                  reads=[k.xnT_b[tt]], writes=[xn_b])
            for h in range(NH):
                hs = h % 2
                if hs == 0:
                    sl = []
                    for sec in range(4):
                        sl.append(ws.load(win, 0, sec * D + h * 128))
                for sec, (pt, ptb) in enumerate(((pq, pq_b), (pf, pf_b), (pi, pi_b), (pg, pg_b))):
                    w_, wb_ = sl[sec]
                    for c in range(DC):
                        P.op("pe", lambda e, pt=pt, w_=w_, c=c, hs=hs: e.matmul(
                            pt[:], lhsT=w_[:, c, hs * 128:(hs + 1) * 128], rhs=xn[:, c, :], start=(c == 0), stop=(c == DC - 1)),
                            reads=[wb_, xn_b], pwrites=[ptb])
                P.op("act", lambda e: e.activation(out=sg[:], in_=pf[:], func=AF.Sigmoid), reads=[pf_b], writes=[sg_b])
                P.op("dve", lambda e, h=h: e.tensor_scalar(
                    out=sg[:], in0=sg[:], scalar1=oml[:, h:h + 1], scalar2=lb[:, h:h + 1],
                    op0=ALU.mult, op1=ALU.add), reads=[sg_b, lb_b], writes=[sg_b])
                P.op("act", lambda e: e.activation(out=lf[:], in_=sg[:], func=AF.Ln), reads=[sg_b], writes=[lf_b])
                P.op("pool", lambda e: e.tensor_scalar(out=kk[:], in0=sg[:], scalar1=-1.0, scalar2=1.0,
                                                       op0=ALU.mult, op1=ALU.add), reads=[sg_b], writes=[kk_b])
                for n in range(NCH):
                    cs = slice(n * HG_C, (n + 1) * HG_C)
                    P.op("dve", lambda e, cs=cs: e.tensor_tensor_scan(
                        out=a[:, cs], data0=ones64[:], data1=lf[:, cs], initial=0.0, op0=ALU.mult, op1=ALU.add),
                        reads=[lf_b, ones64_b], pwrites=[a_b])
                P.op("act", lambda e: e.activation(out=ea[:], in_=a[:], func=AF.Exp), reads=[a_b], writes=[ea_b])
                P.op("act", lambda e: e.activation(out=ena[:], in_=a[:], func=AF.Exp, scale=-1.0), reads=[a_b], writes=[ena_b])
                P.op("act", lambda e: e.activation(out=qs[:], in_=pq[:], func=AF.Silu), reads=[pq_b], writes=[qs_b])
                P.op("act", lambda e: e.activation(out=vb[:], in_=pi[:], func=AF.Copy), reads=[pi_b], writes=[vb_b])
                P.op("act", lambda e: e.activation(out=gs[:], in_=pg[:], func=AF.Silu), reads=[pg_b], writes=[gs_b])
                P.op("pool", lambda e: e.tensor_tensor(out=qt[:], in0=qs[:], in1=ea[:], op=ALU.mult),
                     reads=[qs_b, ea_b], writes=[qt_b])
                P.op("pool", lambda e: e.tensor_tensor(out=kt[:], in0=kk[:], in1=ena[:], op=ALU.mult),
                     reads=[kk_b, ena_b], writes=[kt_b])
                for n in range(NCH):
                    cs = slice(n * HG_C, (n + 1) * HG_C)
                    last = n * HG_C + HG_C - 1
                    P.op("dve", lambda e, cs=cs, last=last: e.tensor_scalar(
                        out=kh[:, cs], in0=kt[:, cs], scalar1=ea[:, last:last + 1], scalar2=None, op0=ALU.mult),
                        reads=[kt_b, ea_b], pwrites=[kh_b])
                for b in range(NB):
                    bs = slice(b * 128, (b + 1) * 128)
                    P.op("pe", lambda e, b=b, bs=bs: e.transpose(out=ptr[:, b, :], in_=vb[:, bs], identity=identb[:]),
                         reads=[vb_b, identb_b], pwrites=[ptr_b])
                P.op("act", lambda e: e.activation(out=vtok[:], in_=ptr[:], func=AF.Copy), reads=[ptr_b], writes=[vtok_b])
                for b in range(NB):
                    bs = slice(b * 128, (b + 1) * 128)
                    P.op("pe", lambda e, b=b, bs=bs: e.transpose(out=ptr[:, b, :], in_=kh[:, bs], identity=identb[:]),
                         reads=[kh_b, identb_b], pwrites=[ptr_b])
                P.op("act", lambda e: e.activation(out=khA[:], in_=ptr[:], func=AF.Copy, scale=hmask[:, 0:1]),
                     reads=[ptr_b, hmask_b], writes=[khA_b])
                P.op("dve", lambda e: e.tensor_scalar(out=khB[:], in0=ptr[:], scalar1=hmask[:, 1:2], scalar2=None, op0=ALU.mult),
                     reads=[ptr_b, hmask_b], writes=[khB_b])
                for b in range(NB):
                    bs = slice(b * 128, (b + 1) * 128)
                    P.op("pe", lambda e, bs=bs: e.matmul(psc[:, bs], lhsT=kt[:, bs], rhs=qt[:, bs], start=True, stop=True),
                         reads=[kt_b, qt_b], pwrites=[psc_b])
                P.op("dve", lambda e: e.tensor_tensor(out=scb[:], in0=psc[:], in1=mask[:], op=ALU.mult),
                     reads=[psc_b, mask_b], writes=[scb_b])
                P.op("act", lambda e, h=h: e.activation(out=snap[:, 0, :], in_=state[:, h, :], func=AF.Copy),
                     reads=[state_b[h]], writes=[snap_b[0]])
                for n in range(NCH):
                    last = n * HG_C + HG_C - 1
                    kx, kxb = (khA, khA_b) if n % 2 == 0 else (khB, khB_b)
                    P.op("pe", lambda e, n=n, kx=kx: e.matmul(pkv[:, n % 4, :], lhsT=kx[:, n // 2, :], rhs=vtok[:, n // 2, :],
                                                             start=True, stop=True),
                         reads=[kxb, vtok_b], writes=[pkv_b[n % 4]])
                    P.op("dve", lambda e, n=n, h=h, last=last: e.scalar_tensor_tensor(
                        out=state[:, h, :], in0=state[:, h, :], scalar=ea[:, last:last + 1], in1=pkv[:, n % 4, :],
                        op0=ALU.mult, op1=ALU.add), reads=[state_b[h], ea_b, pkv_b[n % 4]], writes=[state_b[h]])
                    P.op("act", lambda e, n=n, h=h: e.activation(out=snap[:, n + 1, :], in_=state[:, h, :], func=AF.Copy),
                         reads=[state_b[h]], writes=[snap_b[n + 1]])
                for b in range(NB):
                    bs = slice(b * 128, (b + 1) * 128)
                    P.op("pe", lambda e, b=b, bs=bs: e.matmul(po[:, bs], lhsT=vtok[:, b, :], rhs=scb[:, bs],
                                                              start=True, stop=False),
                         reads=[vtok_b, scb_b], pwrites=[po_b])
                    for n in (2 * b, 2 * b + 1):
                        cs = slice(n * HG_C, (n + 1) * HG_C)
                        P.op("pe", lambda e, n=n, cs=cs, b=b: e.matmul(po[:, cs], lhsT=snap[:, n, :], rhs=qt[:, cs],
                                                                      start=False, stop=(n == 2 * b + 1)),
                             reads=[snap_b[n], qt_b], pwrites=[po_b])
                P.op("act", lambda e: e.activation(out=osq[:], in_=po[:], func=AF.Square), reads=[po_b], writes=[osq_b])
                P.op("pe", lambda e: e.matmul(psc[:], lhsT=k.ones_bf[:], rhs=osq[:], start=True, stop=True),
                     reads=[osq_b, k.ones_b, scb_b], writes=[psc_b])
                P.op("act", lambda e: e.activation(out=rs[:], in_=psc[:], func=AF.Sqrt, bias=k.eps_t[:, 0:1], scale=1.0 / 128),
                     reads=[psc_b, k.eps_b], writes=[rs_b])
                P.op("dve", lambda e: e.reciprocal(out=rs[:], in_=rs[:]), reads=[rs_b], writes=[rs_b])
                P.op("dve", lambda e: e.tensor_tensor(out=tmp[:], in0=po[:], in1=rs[:], op=ALU.mult),
                     reads=[po_b, rs_b], writes=[tmp_b])
                P.op("dve", lambda e, h=h: e.scalar_tensor_tensor(
                    out=ob[:, h, :], in0=tmp[:], scalar=k.vecs[:, gn + h:gn + h + 1], in1=gs[:],
                    op0=ALU.mult, op1=ALU.mult), reads=[tmp_b, gs_b, k.vecs_b], pwrites=[ob_b])
            for dc in range(DC):
                w_, wb_ = wso.load(wo, 0, dc * 128)
                p_, pb_ = (pq, pq_b) if nd % 2 == 0 else (pf, pf_b)
                hr, hrb = hres[nd % 2], hres_b[nd % 2]
                residual_load(k, hr, hrb, dc, tt)
                for c in range(DC):
                    P.op("pe", lambda e, p_=p_, w_=w_, c=c: e.matmul(
                        p_[:], lhsT=w_[:, c, :], rhs=ob[:, c, :], start=(c == 0), stop=(c == DC - 1)),
                        reads=[wb_, ob_b], pwrites=[pb_])
                P.op("dve", lambda e, p_=p_, hr=hr: e.tensor_tensor(out=hr[:], in0=p_[:], in1=hr[:], op=ALU.add),
                     reads=[pb_, hrb], writes=[hrb])
                residual_store(k, hr, hrb, dc, tt)
                nd += 1
    P.barrier()


ATT_SCALE = 128 ** -0.5
NEG = -1.0e30
MNEG = -30000.0
NSB = S // 128


def t5_bucket_np(d):
    d = np.maximum(np.asarray(d, np.int64), 0)
    nf = np.maximum(d, 1).astype(np.float32)
    large = 16 + (np.log(nf / np.float32(16)) / np.float32(math.log(128 / 16)) * np.float32(16)).astype(np.int32)
    large = np.minimum(large, 31)
    return np.where(d < 16, d, large).astype(np.int64)


def dsa_layout_inputs(inp):
    out = {}
    out["a_gq_b"] = np.ascontiguousarray(np.broadcast_to(inp["a_g_q"][0][None, :], (128, 512)), dtype=np.float32)
    out["a_gkv_b"] = np.ascontiguousarray(np.broadcast_to(inp["a_g_kv"][0][None, :], (128, 256)), dtype=np.float32)
    rb = np.asarray(inp["rel_bias"], np.float32)
    out["a_cvec"] = np.ascontiguousarray(np.broadcast_to(rb[31][None, :], (128, 16)), dtype=np.float32)
    sl = np.arange(128)[:, None, None]
    r = np.arange(5)[None, :, None]
    ql = np.arange(512)[None, None, :]
    bidx = t5_bucket_np(ql - sl + 128 - 128 * r)
    out["a_bt"] = np.ascontiguousarray(np.moveaxis(rb[bidx], -1, 0), dtype=np.float32)
    return out


DSA_LAYOUT_SHAPES = {"a_gq_b": [128, 512], "a_gkv_b": [128, 256], "a_cvec": [128, 16], "a_bt": [16, 128, 5, 512]}


def phase_dsa_a(k):
    nc, P = k.nc, k.P
    win = k.w["a_w_in"][0]
    with ExitStack() as st:
        def sb(name, shape, dt=F32):
            return st.enter_context(nc.sbuf_tensor("da_" + name, shape, dt))

        xn = sb("xn", [128, DC, TT], BF16); xn_b = P.buf()
        wst = [sb("wst%d" % i, [128, 4, 848]) for i in range(2)]; wst_b = P.bufs(2)
        wbf = sb("wbf", [128, DC, 848], BF16); wbf_b = P.buf()
        gq = sb("gq", [128, 512]); gkv = sb("gkv", [128, 256]); g_b = P.buf()
        identb = sb("identb", [128, 128], BF16); identb_b = P.buf()
        junk = sb("junk", [128, 512], BF16); junk_b = P.buf()
        ss = sb("ss", [128, 2]); ss_b = P.buf()
        cqn = sb("cqn", [128, 512], BF16); cqn_b = P.buf()
        ckvn = [sb("ckvn%d" % i, [128, 256], BF16) for i in range(2)]; ckvn_b = P.bufs(2)
        kix = sb("kix", [128, 128], BF16); kix_b = P.buf()
        widx = sb("widx", [128, NSB, 16]); widx_b = P.buf()
        cqT = [sb("cqT%d" % i, [128, 4, TT], BF16) for i in range(2)]; cqT_b = P.bufs(2)
        ckvT = [sb("ckvT%d" % i, [128, 2, TT], BF16) for i in range(2)]; ckvT_b = P.bufs(2)
        kixT = [sb("kixT%d" % i, [128, TT], BF16) for i in range(2)]; kixT_b = P.bufs(2)
        pA = [st.enter_context(nc.psum_tensor("da_pA%d" % i, [128, 512], F32)) for i in range(2)]; pA_b = P.pbufs(2)
        pB = [st.enter_context(nc.psum_tensor("da_pB%d" % i, [128, 512], F32)) for i in range(2)]; pB_b = P.pbufs(2)
        ptr = [st.enter_context(nc.psum_tensor("da_ptr%d" % i, [128, 8, 128], BF16)) for i in range(2)]; ptr_b = P.pbufs(2)

        P.dma("sp", lambda e: e.dma_start(out=gq[:], in_=k.lay["a_gq_b"]), pwrites=[g_b])
        P.dma("sp", lambda e: e.dma_start(out=gkv[:], in_=k.lay["a_gkv_b"]), pwrites=[g_b])
        P.op("pool", lambda e: e.tensor_copy(out=identb[:], in_=k.ident[:]), reads=[k.ident_b], writes=[identb_b])
        for i in range(4):
            w_, wb_ = wst[i % 2], wst_b[i % 2]
            P.dma("sp", lambda e, w_=w_, i=i: e.dma_start(
                out=w_[:], in_=win[i * 512:(i + 1) * 512, :].rearrange("(c p) n -> p c n", p=128)), writes=[wb_])
            P.op("pool", lambda e, w_=w_, i=i: e.tensor_copy(out=wbf[:, i * 4:(i + 1) * 4, :], in_=w_[:]),
                 reads=[wb_], pwrites=[wbf_b])
        n = 0
        for tt in range(NT):
            P.dma("sp", lambda e, tt=tt: e.dma_start(out=xn[:], in_=fm(k.xnT, 0, DC, tt * TT, TT)),
                  reads=[k.xnT_b[tt]], writes=[xn_b])
            cq_t, cq_tb = cqT[tt % 2], cqT_b[tt % 2]
            ckv_t, ckv_tb = ckvT[tt % 2], ckvT_b[tt % 2]
            kix_t, kix_tb = kixT[tt % 2], kixT_b[tt % 2]
            for sub in range(4):
                sblk = tt * 4 + sub
                ts_ = slice(sub * 128, (sub + 1) * 128)
                a_, ab_ = pA[n % 2], pA_b[n % 2]
                b_, bb_ = pB[n % 2], pB_b[n % 2]
                t_, tb_ = ptr[n % 2], ptr_b[n % 2]
                ck, ckb = ckvn[n % 2], ckvn_b[n % 2]
                for c in range(DC):
                    P.op("pe", lambda e, a_=a_, c=c, ts_=ts_: e.matmul(
                        a_[:], lhsT=xn[:, c, ts_], rhs=wbf[:, c, 0:512], start=(c == 0), stop=(c == DC - 1)),
                        reads=[xn_b, wbf_b], pwrites=[ab_])
                for c in range(DC):
                    P.op("pe", lambda e, b_=b_, c=c, ts_=ts_: e.matmul(
                        b_[:, 0:336], lhsT=xn[:, c, ts_], rhs=wbf[:, c, 512:848], start=(c == 0), stop=(c == DC - 1)),
                        reads=[xn_b, wbf_b], pwrites=[bb_])
                P.op("act", lambda e, a_=a_: e.activation(out=junk[:], in_=a_[:], func=AF.Square, accum_out=ss[:, 0:1]),
                     reads=[ab_], writes=[junk_b], pwrites=[ss_b])
                P.op("act", lambda e, b_=b_: e.activation(out=junk[:, 0:256], in_=b_[:, 0:256], func=AF.Square,
                                                          accum_out=ss[:, 1:2]),
                     reads=[bb_], writes=[junk_b], pwrites=[ss_b])
                P.op("act", lambda e: e.activation(out=ss[:, 0:1], in_=ss[:, 0:1], func=AF.Sqrt,
                                                   bias=k.eps_t[:, 0:1], scale=1.0 / 512), reads=[ss_b, k.eps_b], pwrites=[ss_b])
                P.op("act", lambda e: e.activation(out=ss[:, 1:2], in_=ss[:, 1:2], func=AF.Sqrt,
                                                   bias=k.eps_t[:, 0:1], scale=1.0 / 256), reads=[ss_b, k.eps_b], pwrites=[ss_b])
                P.op("dve", lambda e: e.reciprocal(out=ss[:], in_=ss[:]), reads=[ss_b], writes=[ss_b])
                P.op("dve", lambda e, a_=a_: e.scalar_tensor_tensor(
                    out=cqn[:], in0=a_[:], scalar=ss[:, 0:1], in1=gq[:], op0=ALU.mult, op1=ALU.mult),
                    reads=[ab_, ss_b, g_b], writes=[cqn_b])
                P.op("dve", lambda e, b_=b_, ck=ck: e.scalar_tensor_tensor(
                    out=ck[:], in0=b_[:, 0:256], scalar=ss[:, 1:2], in1=gkv[:], op0=ALU.mult, op1=ALU.mult),
                    reads=[bb_, ss_b, g_b], writes=[ckb])
                P.op("act", lambda e, b_=b_: e.activation(out=kix[:, 0:64], in_=b_[:, 256:320], func=AF.Copy),
                     reads=[bb_], pwrites=[kix_b])
                P.op("act", lambda e, b_=b_: e.activation(out=kix[:, 64:128], in_=b_[:, 256:320], func=AF.Copy),
                     reads=[bb_], pwrites=[kix_b])
                P.op("act", lambda e, b_=b_, sblk=sblk: e.activation(out=widx[:, sblk, :], in_=b_[:, 320:336], func=AF.Copy),
                     reads=[bb_], pwrites=[widx_b])
                P.dma("sp", lambda e, ck=ck, sblk=sblk: e.dma_start(
                    out=k.ckv_tok[sblk * 128:(sblk + 1) * 128, :], in_=ck[:]), reads=[ckb], pwrites=[k.dsa_b])
                for j in range(4):
                    P.op("pe", lambda e, t_=t_, j=j: e.transpose(out=t_[:, j, :], in_=cqn[:, j * 128:(j + 1) * 128],
                                                                identity=identb[:]),
                         reads=[cqn_b, identb_b], pwrites=[tb_])
                for j in range(2):
                    P.op("pe", lambda e, t_=t_, j=j, ck=ck: e.transpose(out=t_[:, 4 + j, :], in_=ck[:, j * 128:(j + 1) * 128],
                                                                       identity=identb[:]),
                         reads=[ckb, identb_b], pwrites=[tb_])
                P.op("pe", lambda e, t_=t_: e.transpose(out=t_[:, 6, :], in_=kix[:], identity=identb[:]),
                     reads=[kix_b, identb_b], pwrites=[tb_])
                P.op("act", lambda e, t_=t_, cq_t=cq_t, ts_=ts_: e.activation(out=cq_t[:, :, ts_], in_=t_[:, 0:4, :], func=AF.Copy),
                     reads=[tb_], pwrites=[cq_tb])
                P.op("dve", lambda e, t_=t_, ckv_t=ckv_t, ts_=ts_: e.tensor_copy(out=ckv_t[:, :, ts_], in_=t_[:, 4:6, :]),
                     reads=[tb_], pwrites=[ckv_tb])
                P.op("dve", lambda e, t_=t_, kix_t=kix_t, ts_=ts_: e.tensor_copy(out=kix_t[:, ts_], in_=t_[:, 6, :]),
                     reads=[tb_], pwrites=[kix_tb])
                n += 1
            c0 = tt * TT
            P.dma("sp", lambda e, cq_t=cq_t, c0=c0: e.dma_start(out=fm(k.cqT, 0, 4, c0, TT), in_=cq_t[:]),
                  reads=[cq_tb], pwrites=[k.dsa_b])
            P.dma("sp", lambda e, ckv_t=ckv_t, c0=c0: e.dma_start(out=fm(k.ckvT, 0, 2, c0, TT), in_=ckv_t[:]),
                  reads=[ckv_tb], pwrites=[k.dsa_b])
            P.dma("sp", lambda e, kix_t=kix_t, c0=c0: e.dma_start(out=k.kidxT[:, c0:c0 + TT], in_=kix_t[:]),
                  reads=[kix_tb], pwrites=[k.dsa_b])
        P.dma("sp", lambda e: e.dma_start(out=k.widx, in_=widx[:]), reads=[widx_b], pwrites=[k.dsa_b])
    P.barrier()


def phase_dsa_b(k):
    nc, P = k.nc, k.P
    wq = k.w["a_w_qidx"][0]
    import os
    NQB = int(os.environ.get("DSA_NQB", str(NSB)))
    with ExitStack() as st:
        def sb(name, shape, dt=F32):
            return st.enter_context(nc.sbuf_tensor("db_" + name, shape, dt))

        kixT = sb("kixT", [128, S], BF16); kixT_b = P.buf()
        hmask = sb("hmask", [128, 2]); hmask_b = P.buf()
        widx = sb("widx", [128, NSB, 16]); widx_b = P.buf()
        wst = sb("wst", [128, 4, 1024]); wst_b = P.buf()
        wqb = sb("wqb", [128, 4, 1024], BF16); wqb_b = P.buf()
        identb = sb("identb", [128, 128], BF16); identb_b = P.buf()
        cm = sb("cm", [128, 128]); cm30 = sb("cm30", [128, 128], BF16); cm_b = P.buf()
        neg30 = sb("neg30", [128, 3, 128], BF16); neg30_b = P.buf()
        cq = [sb("cq%d" % i, [128, 4, 128], BF16) for i in range(2)]; cq_b = P.bufs(2)
        qixA = [sb("qixA%d" % i, [128, 8, 128], BF16) for i in range(2)]; qixA_b = P.bufs(2)
        qixB = [sb("qixB%d" % i, [128, 8, 128], BF16) for i in range(2)]; qixB_b = P.bufs(2)
        rl = [sb("rl%d" % i, [128, 512]) for i in range(4)]; rl_b = P.bufs(4)
        acc = [sb("acc%d" % i, [128, S]) for i in range(2)]; acc_b = P.bufs(2)
        m8 = sb("m8", [128, 8]); m8_b = P.buf()
        mq = [sb("mq%d" % i, [128, S], BF16) for i in range(2)]; mq_b = P.bufs(2)
        mT = [sb("mT%d" % i, [128, NSB, 128], BF16) for i in range(2)]; mT_b = P.bufs(2)
        pqi = [st.enter_context(nc.psum_tensor("db_pqi%d" % i, [128, 4, 128], F32)) for i in range(2)]; pqi_b = P.pbufs(2)
        ps = [st.enter_context(nc.psum_tensor("db_ps%d" % i, [128, 512], F32)) for i in range(4)]; ps_b = P.pbufs(4)
        ptr = [st.enter_context(nc.psum_tensor("db_ptr%d" % i, [128, 8, 128], BF16)) for i in range(2)]; ptr_b = P.pbufs(2)

        P.dma("sp", lambda e: e.dma_start(out=kixT[:], in_=k.kidxT), reads=[k.dsa_b], writes=[kixT_b])
        P.dma("sp", lambda e: e.dma_start(out=widx[:], in_=k.widx), reads=[k.dsa_b], writes=[widx_b])
        P.dma("sp", lambda e: e.dma_start(out=hmask[:], in_=k.consts["half_mask"]), writes=[hmask_b])
        P.dma("sp", lambda e: e.dma_start(out=wst[:], in_=wq.rearrange("(c p) n -> p c n", p=128)), writes=[wst_b])
        P.op("pool", lambda e: e.tensor_copy(out=wqb[:], in_=wst[:]), reads=[wst_b], writes=[wqb_b])
        P.op("pool", lambda e: e.tensor_copy(out=identb[:], in_=k.ident[:]), reads=[k.ident_b], writes=[identb_b])
        P.dma("sp", lambda e: e.dma_start(out=cm[:], in_=k.consts["dsa_cm"]), pwrites=[cm_b])
        P.op("pool", lambda e: e.memset(neg30[:], MNEG), writes=[neg30_b])
        P.op("dve", lambda e: e.tensor_scalar(out=cm30[:], in0=cm[:], scalar1=-1.0, scalar2=MNEG,
                                              op0=ALU.is_lt, op1=ALU.mult), reads=[cm_b], pwrites=[cm_b])
        npe = 0
        ntr = 0
        for qb in range(NQB):
            Lq = (qb + 1) * 128
            c_, cb_ = cq[qb % 2], cq_b[qb % 2]
            qxA, qxAb = qixA[qb % 2], qixA_b[qb % 2]
            qxB, qxBb = qixB[qb % 2], qixB_b[qb % 2]
            ac, acb = acc[qb % 2], acc_b[qb % 2]
            m_, mb_ = mq[qb % 2], mq_b[qb % 2]
            mt, mtb = mT[qb % 2], mT_b[qb % 2]
            P.dma("sp", lambda e, c_=c_, qb=qb: e.dma_start(out=c_[:], in_=fm(k.cqT, 0, 4, qb * 128, 128)),
                  reads=[k.dsa_b], writes=[cb_])
            if qb >= 2:
                for hg in range(2):
                    pq_, pqb = pqi[hg % 2], pqi_b[hg % 2]
                    for hh in range(4):
                        hp = hg * 4 + hh
                        for c in range(4):
                            P.op("pe", lambda e, pq_=pq_, hh=hh, hp=hp, c=c, c_=c_: e.matmul(
                                pq_[:, hh, :], lhsT=wqb[:, c, hp * 128:(hp + 1) * 128], rhs=c_[:, c, :],
                                start=(c == 0), stop=(c == 3)), reads=[wqb_b, cb_], pwrites=[pqb])
                    P.op("act", lambda e, pq_=pq_, qxA=qxA, hg=hg: e.activation(
                        out=qxA[:, hg * 4:(hg + 1) * 4, :], in_=pq_[:], func=AF.Copy, scale=hmask[:, 0:1]),
                        reads=[pqb, hmask_b], pwrites=[qxAb])
                    P.op("dve", lambda e, pq_=pq_, qxB=qxB, hg=hg: e.tensor_scalar(
                        out=qxB[:, hg * 4:(hg + 1) * 4, :], in0=pq_[:], scalar1=hmask[:, 1:2], scalar2=None, op0=ALU.mult),
                        reads=[pqb, hmask_b], pwrites=[qxBb])
                nkt = (Lq + 511) // 512
                for kt in range(nkt):
                    wd = min(512, Lq - kt * 512)
                    ks = slice(kt * 512, kt * 512 + wd)
                    for h in range(16):
                        p_, pb_ = ps[npe % 4], ps_b[npe % 4]
                        r_, rb_ = rl[npe % 4], rl_b[npe % 4]
                        npe += 1
                        qx, qxb = (qxA, qxAb) if h % 2 == 0 else (qxB, qxBb)
                        P.op("pe", lambda e, p_=p_, qx=qx, h=h, ks=ks, wd=wd: e.matmul(
                            p_[:, 0:wd], lhsT=qx[:, h // 2, :], rhs=kixT[:, ks], start=True, stop=True),
                            reads=[qxb, kixT_b], writes=[pb_])
                        P.op("act", lambda e, p_=p_, r_=r_, wd=wd: e.activation(out=r_[:, 0:wd], in_=p_[:, 0:wd], func=AF.Relu),
                             reads=[pb_], writes=[rb_])
                        if h == 0:
                            P.op("dve", lambda e, r_=r_, ac=ac, ks=ks, wd=wd, qb=qb: e.tensor_scalar(
                                out=ac[:, ks], in0=r_[:, 0:wd], scalar1=widx[:, qb, 0:1], scalar2=None, op0=ALU.mult),
                                reads=[rb_, widx_b], pwrites=[acb])
                        else:
                            P.op("dve", lambda e, r_=r_, ac=ac, ks=ks, wd=wd, qb=qb, h=h: e.scalar_tensor_tensor(
                                out=ac[:, ks], in0=r_[:, 0:wd], scalar=widx[:, qb, h:h + 1], in1=ac[:, ks],
                                op0=ALU.mult, op1=ALU.add), reads=[rb_, widx_b, acb], pwrites=[acb])
                dg = slice(Lq - 128, Lq)
                P.op("dve", lambda e, ac=ac, dg=dg: e.tensor_tensor(out=ac[:, dg], in0=ac[:, dg], in1=cm[:], op=ALU.add),
                     reads=[acb, cm_b], writes=[acb])
                for rnd in range(32):
                    P.op("dve", lambda e, ac=ac, Lq=Lq: e.max(out=m8[:], in_=ac[:, 0:Lq]), reads=[acb], writes=[m8_b])
                    P.op("dve", lambda e, ac=ac, Lq=Lq: e.match_replace(
                        out=ac[:, 0:Lq], in_to_replace=m8[:], in_values=ac[:, 0:Lq], imm_value=NEG),
                        reads=[acb, m8_b], writes=[acb])
                P.op("dve", lambda e, ac=ac, m_=m_, Lq=Lq: e.tensor_scalar(
                    out=m_[:, 0:Lq], in0=ac[:, 0:Lq], scalar1=-5.0e29, scalar2=MNEG, op0=ALU.is_gt, op1=ALU.mult),
                    reads=[acb], writes=[mb_])
                P.op("pool", lambda e, m_=m_, dg=dg: e.tensor_tensor(out=m_[:, dg], in0=m_[:, dg], in1=cm30[:], op=ALU.add),
                     reads=[mb_, cm_b], writes=[mb_])
            else:
                P.op("pool", lambda e, m_=m_, Lq=Lq: e.memset(m_[:, 0:Lq], 0.0), writes=[mb_])
                dg = slice(Lq - 128, Lq)
                P.op("pool", lambda e, m_=m_, dg=dg: e.tensor_copy(out=m_[:, dg], in_=cm30[:]), reads=[mb_, cm_b], writes=[mb_])
            for b0 in range(0, qb + 1, 8):
                nb = min(8, qb + 1 - b0)
                t_, tb_ = ptr[ntr % 2], ptr_b[ntr % 2]
                ntr += 1
                for j in range(nb):
                    P.op("pe", lambda e, t_=t_, j=j, m_=m_, b0=b0: e.transpose(
                        out=t_[:, j, :], in_=m_[:, (b0 + j) * 128:(b0 + j + 1) * 128], identity=identb[:]),
                        reads=[mb_, identb_b], pwrites=[tb_])
                P.op("act", lambda e, t_=t_, mt=mt, b0=b0, nb=nb: e.activation(
                    out=mt[:, b0:b0 + nb, :], in_=t_[:, 0:nb, :], func=AF.Copy), reads=[tb_], pwrites=[mtb])
            P.dma("sp", lambda e, mt=mt, qb=qb: e.dma_start(
                out=k.maskT[:, 0:qb + 1, qb * 128:(qb + 1) * 128], in_=mt[:, 0:qb + 1, :]),
                reads=[mtb], pwrites=[k.mask_b])
            nfill = 3 - (qb % 4)
            if nfill > 0:
                P.dma("sp", lambda e, qb=qb, nfill=nfill: e.dma_start(
                    out=k.maskT[:, qb + 1:qb + 1 + nfill, qb * 128:(qb + 1) * 128], in_=neg30[:, 0:nfill, :]),
                    reads=[neg30_b], pwrites=[k.mask_b])
    P.barrier()


def phase_dsa_c(k):
    nc, P = k.nc, k.P
    wuq = k.w["a_w_uq"][0]
    wuk = k.w["a_w_uk"][0]
    wuv = k.w["a_w_uv"][0]
    wo = k.w["a_w_o"][0]
    import os
    NG = int(os.environ.get("DSA_NG", str(NT)))
    with ExitStack() as st:
        def sb(name, shape, dt=F32):
            return st.enter_context(nc.sbuf_tensor("dc_" + name, shape, dt))

        ckvT = sb("ckvT", [128, 2, S], BF16); ckvT_b = P.buf()
        ckvk = sb("ckvk", [128, NSB, 256], BF16); ckvk_b = P.buf()
        wst = [sb("wst%d" % i, [128, 2048]) for i in range(2)]; wst_b = P.bufs(2)
        wuqb = sb("wuqb", [128, 4, 2048], BF16); wuqb_b = P.buf()
        wukb = sb("wukb", [128, 16, 256], BF16); wukb_b = P.buf()
        wuvb = sb("wuvb", [128, 16, 2, 128], BF16); wuvb_b = P.buf()
        cvec = sb("cvec", [128, 16]); cvec_b = P.buf()
        cq = sb("cq", [128, 4, TT], BF16); cq_b = P.buf()
        mk = sb("mk", [128, NSB, TT], BF16); mk_b = P.buf()
        bt = [sb("bt0", [128, 5, TT])] * 2; bt_b = [P.buf()] * 2
        qT = sb("qT", [128, TT], BF16); qT_b = P.buf()
        ql = [sb("ql%d" % i, [128, 2, TT], BF16) for i in range(2)]; ql_b = P.bufs(2)
        pT = [sb("pT%d" % i, [128, TT], BF16) for i in range(2)]; pT_b = P.bufs(2)
        rden = sb("rden", [128, TT]); rden_b = P.buf()
        oln = sb("oln", [128, 2, TT], BF16); oln_b = P.buf()
        oT = sb("oT", [128, 16, TT], BF16); oT_b = P.buf()
        wso = WStream(k, st, "dc_wo", DC, 128, nbuf=2)
        hres = [sb("hr%d" % i, [128, TT]) for i in range(2)]; hres_b = P.bufs(2)
        pm = [st.enter_context(nc.psum_tensor("dc_pm%d" % i, [128, TT], F32)) for i in range(2)]; pm_b = P.pbufs(2)
        pl = [st.enter_context(nc.psum_tensor("dc_pl%d" % i, [128, TT], F32)) for i in range(2)]; pl_b = P.pbufs(2)
        po = [st.enter_context(nc.psum_tensor("dc_po%d" % i, [128, TT], F32)) for i in range(2)]; po_b = P.pbufs(2)
        pden = st.enter_context(nc.psum_tensor("dc_pden", [128, TT], F32)); pden_b = P.pbuf()

        P.dma("sp", lambda e: e.dma_start(out=ckvT[:], in_=fm(k.ckvT, 0, 2, 0, S)), reads=[k.dsa_b], writes=[ckvT_b])
        P.dma("sp", lambda e: e.dma_start(out=ckvk[:], in_=k.ckv_tok.rearrange("(b p) c -> p b c", p=128)),
              reads=[k.dsa_b], writes=[ckvk_b])
        P.dma("sp", lambda e: e.dma_start(out=cvec[:], in_=k.lay["a_cvec"]), writes=[cvec_b])
        nw = 0
        for c in range(4):
            w_, wb_ = wst[nw % 2], wst_b[nw % 2]; nw += 1
            P.dma("sp", lambda e, w_=w_, c=c: e.dma_start(out=w_[:], in_=wuq[c * 128:(c + 1) * 128, :]), writes=[wb_])
            P.op("pool", lambda e, w_=w_, c=c: e.tensor_copy(out=wuqb[:, c, :], in_=w_[:]), reads=[wb_], pwrites=[wuqb_b])
        for hg in range(2):
            w_, wb_ = wst[nw % 2], wst_b[nw % 2]; nw += 1
            P.dma("sp", lambda e, w_=w_, hg=hg: e.dma_start(
                out=w_[:].rearrange("p (h c) -> p h c", h=8), in_=wuk[hg * 8:(hg + 1) * 8].rearrange("h d c -> d h c")),
                writes=[wb_])
            P.op("pool", lambda e, w_=w_, hg=hg: e.tensor_copy(
                out=wukb[:, hg * 8:(hg + 1) * 8, :], in_=w_[:].rearrange("p (h c) -> p h c", h=8)),
                reads=[wb_], pwrites=[wukb_b])
        for hg in range(2):
            w_, wb_ = wst[nw % 2], wst_b[nw % 2]; nw += 1
            P.dma("sp", lambda e, w_=w_, hg=hg: e.dma_start(
                out=w_[:].rearrange("p (h a d) -> p h a d", h=8, a=2),
                in_=wuv[hg * 8:(hg + 1) * 8].rearrange("h (a p) d -> p h a d", p=128)), writes=[wb_])
            P.op("pool", lambda e, w_=w_, hg=hg: e.tensor_copy(
                out=wuvb[:, hg * 8:(hg + 1) * 8, :, :], in_=w_[:].rearrange("p (h a d) -> p h a d", h=8, a=2)),
                reads=[wb_], pwrites=[wuvb_b])
        nd = 0
        identb = sb("identb", [128, 128], BF16); identb_b = P.buf()
        btb = [sb("btb%d" % i, [128, 5, TT], BF16) for i in range(2)]; btb_b = P.bufs(2)
        P.op("pool", lambda e: e.tensor_copy(out=identb[:], in_=k.ident[:]), reads=[k.ident_b], writes=[identb_b])
        for g in range(NG):
            q0 = g * TT
            nsb = 4 * g + 4
            P.dma("sp", lambda e, q0=q0: e.dma_start(out=cq[:], in_=fm(k.cqT, 0, 4, q0, TT)), reads=[k.dsa_b], writes=[cq_b])
            P.dma("sp", lambda e, q0=q0, nsb=nsb: e.dma_start(out=mk[:, 0:nsb, :], in_=k.maskT[:, 0:nsb, q0:q0 + TT]),
                  reads=[k.mask_b], writes=[mk_b])
            units = [(h, sbk) for h in range(16) for sbk in range(nsb)]

            def stage_a(u, g=g, nsb=nsb):
                h, sbk = units[u]
                q_, qb_ = ql[h % 2], ql_b[h % 2]
                b_, bb_ = bt[h % 2], bt_b[h % 2]
                bb16, bb16_b = btb[h % 2], btb_b[h % 2]
                if sbk == 0:
                    P.dma("sp", lambda e: e.dma_start(out=b_[:], in_=k.lay["a_bt"][h]), writes=[bb_])
                    P.op("pool", lambda e: e.tensor_copy(out=bb16[:], in_=b_[:]), reads=[bb_], writes=[bb16_b])
                    for c in range(4):
                        P.op("pe", lambda e, c=c: e.matmul(
                            pm[0][:], lhsT=wuqb[:, c, h * 128:(h + 1) * 128], rhs=cq[:, c, :], start=(c == 0), stop=(c == 3)),
                            reads=[wuqb_b, cq_b], pwrites=[pm_b[0]])
                    P.op("act", lambda e: e.activation(out=qT[:], in_=pm[0][:], func=AF.Copy), reads=[pm_b[0]], writes=[qT_b])
                    for cc in range(2):
                        P.op("pe", lambda e, cc=cc: e.matmul(
                            pm[1][:], lhsT=wukb[:, h, cc * 128:(cc + 1) * 128], rhs=qT[:], start=True, stop=True),
                            reads=[wukb_b, qT_b], writes=[pm_b[1]])
                        P.op("act", lambda e, cc=cc: e.activation(out=q_[:, cc, :], in_=pm[1][:], func=AF.Copy, scale=ATT_SCALE),
                             reads=[pm_b[1]], pwrites=[qb_])
                ss_ = slice(sbk * 128, (sbk + 1) * 128)
                l_, lb_ = pl[u % 2], pl_b[u % 2]
                r = sbk - (4 * g - 1)
                for cc in range(2):
                    P.op("pe", lambda e, cc=cc: e.matmul(
                        l_[:], lhsT=ckvT[:, cc, ss_], rhs=q_[:, cc, :], start=(cc == 0), stop=False),
                        reads=[ckvT_b, qb_], pwrites=[lb_])
                P.op("pe", lambda e: e.matmul(l_[:], lhsT=identb[:], rhs=mk[:, sbk, :], start=False, stop=(r < 0)),
                     reads=[identb_b, mk_b], pwrites=[lb_])
                if r >= 0:
                    P.op("pe", lambda e: e.matmul(l_[:], lhsT=identb[:], rhs=bb16[:, r, :], start=False, stop=True),
                         reads=[identb_b, bb16_b], pwrites=[lb_])

            def stage_b(u, g=g):
                h, sbk = units[u]
                l_, lb_ = pl[u % 2], pl_b[u % 2]
                p_, pb_ = pT[u % 2], pT_b[u % 2]
                r = sbk - (4 * g - 1)
                if r >= 0:
                    P.op("act", lambda e: e.activation(out=p_[:], in_=l_[:], func=AF.Exp), reads=[lb_], writes=[pb_])
                else:
                    P.op("act", lambda e: e.activation(out=p_[:], in_=l_[:], func=AF.Exp, bias=cvec[:, h:h + 1]),
                         reads=[lb_, cvec_b], writes=[pb_])

            def stage_c(u, nsb=nsb):
                h, sbk = units[u]
                p_, pb_ = pT[u % 2], pT_b[u % 2]
                for cc in range(2):
                    P.op("pe", lambda e, cc=cc: e.matmul(
                        po[cc][:], lhsT=ckvk[:, sbk, cc * 128:(cc + 1) * 128], rhs=p_[:],
                        start=(sbk == 0), stop=(sbk == nsb - 1)), reads=[ckvk_b, pb_], pwrites=[po_b[cc]])
                P.op("pe", lambda e: e.matmul(
                    pden[:], lhsT=k.ones_bf[:], rhs=p_[:], start=(sbk == 0), stop=(sbk == nsb - 1)),
                    reads=[k.ones_b, pb_], pwrites=[pden_b])
                if sbk == nsb - 1:
                    P.op("dve", lambda e: e.reciprocal(out=rden[:], in_=pden[:]), reads=[pden_b], writes=[rden_b])
                    for cc in range(2):
                        P.op("dve", lambda e, cc=cc: e.tensor_tensor(out=oln[:, cc, :], in0=po[cc][:], in1=rden[:], op=ALU.mult),
                             reads=[po_b[cc], rden_b], pwrites=[oln_b])
                    for cc in range(2):
                        P.op("pe", lambda e, cc=cc: e.matmul(
                            pm[0][:], lhsT=wuvb[:, h, cc, :], rhs=oln[:, cc, :], start=(cc == 0), stop=(cc == 1)),
                            reads=[wuvb_b, oln_b], pwrites=[pm_b[0]])
                    P.op("act", lambda e: e.activation(out=oT[:, h, :], in_=pm[0][:], func=AF.Copy),
                         reads=[pm_b[0]], pwrites=[oT_b])

            nun = len(units)
            stage_a(0)
            stage_b(0)
            for u in range(nun):
                if u + 1 < nun:
                    stage_a(u + 1)
                    stage_b(u + 1)
                stage_c(u)
            for dc in range(DC):
                w_, wb_ = wso.load(wo, 0, dc * 128)
                p_, pb_ = pm[nd % 2], pm_b[nd % 2]
                hr, hrb = hres[nd % 2], hres_b[nd % 2]
                residual_load(k, hr, hrb, dc, g)
                for c in range(DC):
                    P.op("pe", lambda e, p_=p_, w_=w_, c=c: e.matmul(
                        p_[:], lhsT=w_[:, c, :], rhs=oT[:, c, :], start=(c == 0), stop=(c == DC - 1)),
                        reads=[wb_, oT_b], pwrites=[pb_])
                P.op("dve", lambda e, p_=p_, hr=hr: e.tensor_tensor(out=hr[:], in0=p_[:], in1=hr[:], op=ALU.add),
                     reads=[pb_, hrb], writes=[hrb])
                residual_store(k, hr, hrb, dc, g)
                nd += 1
    P.barrier()


WEIGHT_SHAPES = {
    "a_w_in": [1, 2048, 848], "a_w_uq": [1, 512, 2048], "a_w_qidx": [1, 512, 1024],
    "a_w_uk": [1, 16, 128, 256], "a_w_uv": [1, 16, 256, 128], "a_w_o": [1, 2048, 2048],
    "b_w_in": [1, 2048, 8192], "b_w_o": [1, 2048, 2048],
    "c_w_group": [1, 4, 512, 512],
    "d_w_pw1": [1, 2048, 4096], "d_w_pw2": [1, 2048, 2048],
    "ffn_w_up": [4, 2048, 11264], "ffn_w_down": [4, 5632, 2048],
}

PLAN_FULL = ["in"] + sum([["norm_mix%d" % i, "mix%d" % i, "norm_ffn%d" % i, "ffn%d" % i] for i in range(DEPTH)], []) + ["out"]


def plan_weights(plan):
    ws = set()
    for p in plan:
        if p.startswith("ffn"):
            ws.update(["ffn_w_up", "ffn_w_down"])
        if p == "mix0":
            ws.update(["a_w_in", "a_w_uq", "a_w_qidx", "a_w_uk", "a_w_uv", "a_w_o"])
        if p == "mix1":
            ws.update(["b_w_in", "b_w_o"])
        if p == "mix2":
            ws.update(["c_w_group"])
        if p == "mix3":
            ws.update(["d_w_pw1", "d_w_pw2"])
    return sorted(ws)


def build_nc(plan=PLAN_FULL, raw_out=False):
    nc = bass.Bass("TRN2", target_bir_lowering=False)
    k = K()
    k.nc = nc
    k.raw_out = raw_out
    k.R = vec_registry()
    k.x = nc.dram_tensor("x", [S, D], F32, kind="ExternalInput").ap()
    k.out = nc.dram_tensor("out", [S, D], F32, kind="ExternalOutput").ap()
    vecs_d = nc.dram_tensor("vecs", [128, k.R.n], F32, kind="ExternalInput").ap()
    ident_d = nc.dram_tensor("ident", [128, 128], F32, kind="ExternalInput").ap()
    k.consts = {}
    for name, arr in make_consts().items():
        k.consts[name] = nc.dram_tensor("c_" + name, list(arr.shape), F32, kind="ExternalInput").ap()
    k.w = {}
    for name in plan_weights(plan):
        k.w[name] = nc.dram_tensor(name, WEIGHT_SHAPES[name], F32, kind="ExternalInput").ap()
    k.lay = {}
    if "mix0" in plan:
        for name, shp in DSA_LAYOUT_SHAPES.items():
            k.lay[name] = nc.dram_tensor(name, shp, F32, kind="ExternalInput").ap()
        k.cqT = nc.dram_tensor("s_cqT", [512, S], BF16).ap()
        k.ckvT = nc.dram_tensor("s_ckvT", [256, S], BF16).ap()
        k.ckv_tok = nc.dram_tensor("s_ckvtok", [S, 256], BF16).ap()
        k.kidxT = nc.dram_tensor("s_kidxT", [128, S], BF16).ap()
        k.widx = nc.dram_tensor("s_widx", [128, NSB, 16], F32).ap()
        k.maskT = nc.dram_tensor("s_maskT", [128, NSB, S], BF16).ap()
    k.gT = nc.dram_tensor("s_gT", [DFF, S], BF16).ap()
    k.hT = nc.dram_tensor("hT", [D, S], F32).ap()
    k.xnT = nc.dram_tensor("xnT", [D, S], BF16).ap()
    with ExitStack() as st:
        P = Prog(nc, st)
        k.P = P
        k.hT_b = P.bufs(NT)
        k.xnT_b = P.bufs(NT)
        k.out_b = P.buf()
        k.dsa_b = P.buf()
        k.gT_b = P.buf()
        k.mask_b = P.buf()
        k.vecs = st.enter_context(nc.sbuf_tensor("vecs_t", [128, k.R.n], F32))
        k.vecs_b = P.buf()
        k.ident = st.enter_context(nc.sbuf_tensor("ident_t", [128, 128], F32))
        k.ident_b = P.buf()
        k.ones_bf = st.enter_context(nc.sbuf_tensor("ones_bf", [128, 128], BF16))
        k.ones_b = P.buf()
        k.eps_t = st.enter_context(nc.sbuf_tensor("eps_t", [128, 1], F32))
        k.eps_b = P.buf()
        P.dma("sp", lambda e: e.dma_start(out=k.vecs[:], in_=vecs_d), writes=[k.vecs_b])
        P.dma("sp", lambda e: e.dma_start(out=k.ident[:], in_=ident_d), writes=[k.ident_b])
        P.op("pool", lambda e: e.memset(k.ones_bf[:], 1.0), writes=[k.ones_b])
        P.op("pool", lambda e: e.memset(k.eps_t[:], EPS), writes=[k.eps_b])
        k.nc = NCProxy(nc)
        for p in plan:
            k.nc.tag += 1
            if p == "in":
                phase_in(k)
            elif p == "out":
                phase_out(k)
            elif p.startswith("norm_"):
                phase_norm(k, p)
            elif p.startswith("ffn"):
                phase_ffn(k, int(p[3:]))
            elif p == "mix0":
                phase_dsa_a(k)
                phase_dsa_b(k)
                phase_dsa_c(k)
            elif p == "mix1":
                phase_mix_hgrn(k, 1)
            elif p == "mix2":
                phase_mix_pool(k)
            elif p == "mix3":
                phase_mix_conf(k)
            else:
                raise NotImplementedError(p)
        P.barrier()
        P.emit()
    return nc


def make_consts():
    c = {}
    invc = np.zeros((128, 64), np.float32)
    for g, w in enumerate((2, 4, 8, 16)):
        for t in range(16):
            invc[:, g * 16 + t] = 1.0 / min(t + 1, w)
    c["pool_invc"] = invc
    blk = np.zeros((128, 128), np.float32)
    blk[0:64, 0:64] = np.triu(np.ones((64, 64), np.float32))
    blk[64:128, 64:128] = np.triu(np.ones((64, 64), np.float32))
    c["hg_mask"] = np.ascontiguousarray(np.tile(blk, (1, TT // 128)))
    hmk = np.zeros((128, 2), np.float32)
    hmk[0:64, 0] = 1.0
    hmk[64:128, 1] = 1.0
    c["half_mask"] = hmk
    cm = np.zeros((128, 128), np.float32)
    cm[np.triu_indices(128, 1)] = -1.0e30
    c["dsa_cm"] = cm
    return c


def make_in_maps(inp, plan, n_cores=8, xs=None):
    vecs = pack_vecs(inp)
    consts = make_consts()
    ident = np.eye(128, dtype=np.float32)
    wnames = plan_weights(plan)
    lay = dsa_layout_inputs(inp) if "mix0" in plan else {}
    maps = []
    for c in range(n_cores):
        m = {"x": np.ascontiguousarray(inp["x"][c] if xs is None else xs[c]), "vecs": vecs, "ident": ident}
        for w in wnames:
            m[w] = np.ascontiguousarray(inp[w], dtype=np.float32)
        for cn, arr in consts.items():
            m["c_" + cn] = arr
        m.update(lay)
        maps.append(m)
    return maps


def kernel(**inputs):
    inp = {k_: np.asarray(v) for k_, v in inputs.items()}
    nc = build_nc(PLAN_FULL)
    maps = make_in_maps(inp, PLAN_FULL, 8)
    res = run_bass_kernel_spmd(nc, maps, core_ids=list(range(8)))
    return np.stack([np.asarray(r["out"]) for r in res.results], axis=0).astype(np.float32)
```

```python
from contextlib import ExitStack
import math
import numpy as np
import concourse.bass as bass
import concourse.mybir as mybir
from concourse.bass_utils import run_bass_kernel_spmd

F32 = mybir.dt.float32
BF16 = mybir.dt.bfloat16
AF = mybir.ActivationFunctionType
ALU = mybir.AluOpType
AX = mybir.AxisListType

D = 2048
S = 4096
DC = D // 128
TT = 512
NT = S // TT
DFF = 5632
FC = DFF // 128
EPS = 1e-6
DEPTH = 4
NDMA_SEMS = 8


class Buf:
    __slots__ = ("name", "w", "wold", "r", "open", "excl")

    def __init__(self, name):
        self.name = name
        self.excl = False
        self.w = {}
        self.wold = {}
        self.r = {}
        self.open = False


def _mx(d, k, v):
    if d.get(k, 0) < v:
        d[k] = v


class Prog:
    STREAMS = ("pe", "act", "dve", "pool", "sp")

    def __init__(self, nc, stack):
        self.nc = nc
        self.ops = {s: [] for s in self.STREAMS}
        self.sems = {}
        self.known = {s: {} for s in self.STREAMS}
        self.cnt = {}
        for s in ("pe", "act", "dve", "pool"):
            self.sems[s] = stack.enter_context(nc.semaphore("c_" + s))
            self.cnt[s] = 0
        self.dma_sems = {}
        self.dma_n = {}
        for q in ("sp", "act", "pool"):
            self.dma_sems[q] = []
            for i in range(NDMA_SEMS):
                k = "d_%s%d" % (q, i)
                self.sems[k] = stack.enter_context(nc.semaphore(k))
                self.cnt[k] = 0
                self.dma_sems[q].append(k)
            self.dma_n[q] = 0
        self.nbuf = 0

    def buf(self, name=None):
        self.nbuf += 1
        return Buf(name or "b%d" % self.nbuf)

    def bufs(self, n, name=None):
        return [self.buf() for _ in range(n)]

    def pbuf(self):
        b = self.buf()
        b.excl = True
        return b

    def pbufs(self, n):
        return [self.pbuf() for _ in range(n)]

    def _deps(self, stream, reads, writes, pwrites, own_key):
        need = {}

        def add(k, v, same_ok):
            if k == own_key and same_ok:
                return
            _mx(need, k, v)

        for b in reads:
            for k, v in b.w.items():
                add(k, v, False)
            for k, v in b.wold.items():
                add(k, v, False)
        for b in writes:
            for dd in (b.w, b.wold, b.r):
                for k, v in dd.items():
                    add(k, v, True)
        for b in pwrites:
            if not b.open:
                for k, v in b.w.items():
                    _mx(b.wold, k, v)
                b.w = {}
                b.open = True
            for dd in (b.wold, b.r):
                for k, v in dd.items():
                    add(k, v, True)
        kn = self.known[stream]
        waits = []
        for k, v in need.items():
            if kn.get(k, 0) < v:
                kn[k] = v
                waits.append((k, v))
        return waits

    def _commit(self, tok, reads, writes, pwrites):
        k, v = tok
        for b in reads:
            _mx(b.r, k, v)
            b.open = False
        for b in writes:
            b.w = {k: v}
            b.wold = {}
            b.r = {}
            b.open = False
        for b in pwrites:
            _mx(b.w, k, v)

    def op(self, stream, fn, reads=(), writes=(), pwrites=()):
        if stream != "pe" and any(b.excl for b in reads):
            writes = list(writes) + [b for b in reads if b.excl]
            reads = [b for b in reads if not b.excl]
        waits = self._deps(stream, reads, writes, pwrites, stream)
        self.cnt[stream] += 1
        tok = (stream, self.cnt[stream])
        self.ops[stream].append((waits, fn, (stream, 1)))
        self._commit(tok, reads, writes, pwrites)
        return tok

    def dma(self, q, fn, reads=(), writes=(), pwrites=()):
        i = self.dma_n[q]
        self.dma_n[q] += 1
        key = self.dma_sems[q][i % NDMA_SEMS]
        waits = self._deps(q, reads, writes, pwrites, None)
        prev = self.cnt[key]
        kn = self.known[q]
        if prev > 0 and kn.get(key, 0) < prev:
            kn[key] = prev
            waits.append((key, prev))
        self.cnt[key] = prev + 16
        tok = (key, prev + 16)
        self.ops[q].append((waits, fn, (key, 16)))
        self._commit(tok, reads, writes, pwrites)
        return tok

    def barrier(self):
        cur = dict(self.cnt)
        for s in self.STREAMS:
            waits = []
            for k, v in cur.items():
                if v > 0 and self.known[s].get(k, 0) < v:
                    self.known[s][k] = v
                    waits.append((k, v))
            if waits:
                self.ops[s].append((waits, None, None))

    def emit(self):
        nc = self.nc
        sems = self.sems

        def run(stream):
            def body(eng):
                for waits, fn, inc in self.ops[stream]:
                    for k, v in waits:
                        eng.wait_ge(sems[k], v)
                    if fn is not None:
                        fn(eng).then_inc(sems[inc[0]], inc[1])
            return body

        with nc.Block() as block:
            block.tensor(run("pe"))
            block.scalar(run("act"))
            block.vector(run("dve"))
            block.gpsimd(run("pool"))
            block.sync(run("sp"))


class VecReg:
    def __init__(self):
        self.off = {}
        self.n = 0

    def add(self, name, ncols):
        self.off[name] = self.n
        self.n += ncols


def vec_registry():
    R = VecReg()
    for i in range(DEPTH):
        R.add("norm_mix%d" % i, DC)
        R.add("norm_ffn%d" % i, DC)
        R.add("ffn_b_conv%d" % i, 2 * FC)
        for k in range(3):
            R.add("ffn_w_conv%d_%d" % (i, k), 2 * FC)
    R.add("final_norm", DC)
    R.add("c_scale", DC)
    R.add("d_b_pw1", 2 * DC)
    for k in range(31):
        R.add("d_w_dw%d" % k, DC)
    R.add("d_b_dw", DC)
    R.add("d_ln_g", DC)
    R.add("d_ln_b", DC)
    R.add("d_b_pw2", DC)
    R.add("b_g_norm", DC)
    for i in range(DEPTH):
        R.add("b_lb%d" % i, DC)
    return R


def _cols(v):
    v = np.ascontiguousarray(v, dtype=np.float32).reshape(-1, 128)
    return v.T


def pack_vecs(inp):
    R = vec_registry()
    out = np.zeros((128, R.n), np.float32)

    def put(name, v):
        c = _cols(v)
        out[:, R.off[name]:R.off[name] + c.shape[1]] = c

    for i in range(DEPTH):
        put("norm_mix%d" % i, inp["norm_mix"][i])
        put("norm_ffn%d" % i, inp["norm_ffn"][i])
        put("ffn_b_conv%d" % i, inp["ffn_b_conv"][i])
        for k in range(3):
            put("ffn_w_conv%d_%d" % (i, k), inp["ffn_w_conv"][i, k])
        put("b_lb%d" % i, inp["b_lower_bounds"][i])
    put("final_norm", inp["final_norm"])
    put("c_scale", inp["c_scale"][0])
    put("d_b_pw1", inp["d_b_pw1"][0])
    for k in range(31):
        put("d_w_dw%d" % k, inp["d_w_dw"][0, k])
    put("d_b_dw", inp["d_b_dw"][0])
    put("d_ln_g", inp["d_ln_g"][0])
    put("d_ln_b", inp["d_ln_b"][0])
    put("d_b_pw2", inp["d_b_pw2"][0])
    put("b_g_norm", inp["b_g_norm"][0])
    return out


class K:
    pass


class NCProxy:
    def __init__(self, nc):
        self._nc = nc
        self.tag = 0

    def sbuf_tensor(self, name, *a, **kw):
        return self._nc.sbuf_tensor("%s_%d" % (name, self.tag), *a, **kw)

    def psum_tensor(self, name, *a, **kw):
        return self._nc.psum_tensor("%s_%d" % (name, self.tag), *a, **kw)

    def __getattr__(self, n):
        return getattr(self._nc, n)


def fm(ap2d, c0, nck, t0, nt):
    return ap2d[c0 * 128:(c0 + nck) * 128, t0:t0 + nt].rearrange("(c p) t -> p c t", p=128)


def phase_in(k):
    nc, P = k.nc, k.P
    with ExitStack() as st:
        xin = [st.enter_context(nc.sbuf_tensor("pi_x%d" % i, [128, D], F32)) for i in range(2)]
        xin_b = P.bufs(2)
        stg = [st.enter_context(nc.sbuf_tensor("pi_s%d" % i, [128, DC, TT], F32)) for i in range(2)]
        stg_b = P.bufs(2)
        ps = [st.enter_context(nc.psum_tensor("pi_p%d" % i, [128, 512], F32)) for i in range(4)]
        ps_b = P.pbufs(4)
        n = 0
        for tt in range(NT):
            sg, sgb = stg[tt % 2], stg_b[tt % 2]
            for sub in range(4):
                si = tt * 4 + sub
                xt, xb = xin[si % 2], xin_b[si % 2]
                P.dma("sp", lambda e, xt=xt, si=si: e.dma_start(out=xt[:], in_=k.x[si * 128:(si + 1) * 128, :]),
                      writes=[xb])
                for cg in range(4):
                    pt, pb = ps[n % 4], ps_b[n % 4]
                    for ci in range(4):
                        c = cg * 4 + ci
                        P.op("pe", lambda e, pt=pt, xt=xt, c=c, ci=ci: e.transpose(
                            out=pt[:, ci * 128:(ci + 1) * 128], in_=xt[:, c * 128:(c + 1) * 128],
                            identity=k.ident[:]), reads=[xb, k.ident_b], pwrites=[pb])
                    eng = "act" if n % 2 == 0 else "dve"
                    if eng == "act":
                        P.op("act", lambda e, pt=pt, sg=sg, cg=cg, sub=sub: e.activation(
                            out=sg[:, cg * 4:(cg + 1) * 4, sub * 128:(sub + 1) * 128],
                            in_=pt[:].rearrange("p (c s) -> p c s", c=4), func=AF.Copy),
                            reads=[pb], pwrites=[sgb])
                    else:
                        P.op("dve", lambda e, pt=pt, sg=sg, cg=cg, sub=sub: e.tensor_copy(
                            out=sg[:, cg * 4:(cg + 1) * 4, sub * 128:(sub + 1) * 128],
                            in_=pt[:].rearrange("p (c s) -> p c s", c=4)),
                            reads=[pb], pwrites=[sgb])
                    n += 1
            P.dma("sp", lambda e, sg=sg, tt=tt: e.dma_start(out=fm(k.hT, 0, DC, tt * TT, TT), in_=sg[:]),
                  reads=[sgb], writes=[k.hT_b[tt]])
    P.barrier()


def norm_tile(k, st_tiles, src_h, src_hb, gcol, out_tile, out_b, tag):
    nc, P = k.nc, k.P
    sq, sq_b, pss, pss_b, rb, rb_b = st_tiles
    for c in range(DC):
        P.op("act", lambda e, c=c: e.activation(out=sq[:, c, :], in_=src_h[:, c, :], func=AF.Square),
             reads=[src_hb], pwrites=[sq_b])
    for c in range(DC):
        P.op("pe", lambda e, c=c: e.matmul(pss[:], lhsT=k.ones_bf[:], rhs=sq[:, c, :],
                                           start=(c == 0), stop=(c == DC - 1)),
             reads=[sq_b, k.ones_b], pwrites=[pss_b])
    P.op("act", lambda e: e.activation(out=rb[:], in_=pss[:], func=AF.Sqrt, bias=k.eps_t[:, 0:1], scale=1.0 / D),
         reads=[pss_b, k.eps_b], writes=[rb_b])
    P.op("dve", lambda e: e.reciprocal(out=rb[:], in_=rb[:]), reads=[rb_b], writes=[rb_b])
    for c in range(DC):
        P.op("dve", lambda e, c=c: e.scalar_tensor_tensor(
            out=out_tile[:, c, :], in0=src_h[:, c, :], scalar=k.vecs[:, gcol + c:gcol + c + 1],
            in1=rb[:], op0=ALU.mult, op1=ALU.mult),
            reads=[src_hb, rb_b, k.vecs_b], pwrites=[out_b])


def phase_norm(k, gname):
    nc, P = k.nc, k.P
    gcol = k.R.off[gname]
    with ExitStack() as st:
        hin = [st.enter_context(nc.sbuf_tensor("pn_h%d" % i, [128, DC, TT], F32)) for i in range(2)]
        hin_b = P.bufs(2)
        sq = st.enter_context(nc.sbuf_tensor("pn_sq", [128, DC, TT], BF16))
        rb = st.enter_context(nc.sbuf_tensor("pn_rb", [128, TT], F32))
        xo = [st.enter_context(nc.sbuf_tensor("pn_o%d" % i, [128, DC, TT], BF16)) for i in range(2)]
        xo_b = P.bufs(2)
        pss = st.enter_context(nc.psum_tensor("pn_ps", [128, TT], F32))
        tiles = (sq, P.buf(), pss, P.pbuf(), rb, P.buf())
        for tt in range(NT):
            h, hb = hin[tt % 2], hin_b[tt % 2]
            o, ob = xo[tt % 2], xo_b[tt % 2]
            P.dma("sp", lambda e, h=h, tt=tt: e.dma_start(out=h[:], in_=fm(k.hT, 0, DC, tt * TT, TT)),
                  reads=[k.hT_b[tt]], writes=[hb])
            norm_tile(k, tiles, h, hb, gcol, o, ob, "pn")
            P.dma("sp", lambda e, o=o, tt=tt: e.dma_start(out=fm(k.xnT, 0, DC, tt * TT, TT), in_=o[:]),
                  reads=[ob], writes=[k.xnT_b[tt]])
    P.barrier()


def phase_out(k):
    nc, P = k.nc, k.P
    gcol = k.R.off["final_norm"]
    with ExitStack() as st:
        hin = [st.enter_context(nc.sbuf_tensor("po_h%d" % i, [128, DC, TT], F32)) for i in range(2)]
        hin_b = P.bufs(2)
        sq = st.enter_context(nc.sbuf_tensor("po_sq", [128, DC, TT], BF16))
        rb = st.enter_context(nc.sbuf_tensor("po_rb", [128, TT], F32))
        xo = st.enter_context(nc.sbuf_tensor("po_o", [128, DC, TT], F32))
        xo_b = P.buf()
        og = [st.enter_context(nc.sbuf_tensor("po_g%d" % i, [128, D], F32)) for i in range(2)]
        og_b = P.bufs(2)
        pss = st.enter_context(nc.psum_tensor("po_ps", [128, TT], F32))
        ps = [st.enter_context(nc.psum_tensor("po_p%d" % i, [128, 512], F32)) for i in range(4)]
        ps_b = P.pbufs(4)
        tiles = (sq, P.buf(), pss, P.pbuf(), rb, P.buf())
        n = 0
        toks = []
        for tt in range(NT):
            h, hb = hin[tt % 2], hin_b[tt % 2]
            P.dma("sp", lambda e, h=h, tt=tt: e.dma_start(out=h[:], in_=fm(k.hT, 0, DC, tt * TT, TT)),
                  reads=[k.hT_b[tt]], writes=[hb])
            if k.raw_out:
                src, srcb = h, hb
            else:
                norm_tile(k, tiles, h, hb, gcol, xo, xo_b, "po")
                src, srcb = xo, xo_b
            for sub in range(4):
                si = tt * 4 + sub
                o, ob = og[si % 2], og_b[si % 2]
                for cg in range(4):
                    pt, pb = ps[n % 4], ps_b[n % 4]
                    for ci in range(4):
                        c = cg * 4 + ci
                        P.op("pe", lambda e, pt=pt, src=src, c=c, ci=ci, sub=sub: e.transpose(
                            out=pt[:, ci * 128:(ci + 1) * 128], in_=src[:, c, sub * 128:(sub + 1) * 128],
                            identity=k.ident[:]), reads=[srcb, k.ident_b], pwrites=[pb])
                    if n % 2 == 0:
                        P.op("act", lambda e, pt=pt, o=o, cg=cg: e.activation(
                            out=o[:, cg * 512:(cg + 1) * 512], in_=pt[:], func=AF.Copy),
                            reads=[pb], pwrites=[ob])
                    else:
                        P.op("dve", lambda e, pt=pt, o=o, cg=cg: e.tensor_copy(
                            out=o[:, cg * 512:(cg + 1) * 512], in_=pt[:]),
                            reads=[pb], pwrites=[ob])
                    n += 1
                toks.append(P.dma("sp", lambda e, o=o, si=si: e.dma_start(
                    out=k.out[si * 128:(si + 1) * 128, :], in_=o[:]), reads=[ob], writes=[k.out_b]))
    P.barrier()


def phase_ffn(k, L):
    phase_ffn_up(k, L)
    phase_ffn_down(k, L)


def phase_ffn_up(k, L):
    nc, P = k.nc, k.P
    R = k.R
    wup = k.w["ffn_w_up"][L]
    bcol = R.off["ffn_b_conv%d" % L]
    wcol = [R.off["ffn_w_conv%d_%d" % (L, t)] for t in range(3)]
    with ExitStack() as st:
        xn = st.enter_context(nc.sbuf_tensor("fu_xn", [128, DC, S], BF16))
        xn_b = P.bufs(NT)
        sup = st.enter_context(nc.sbuf_tensor("fu_su", [128, 2, DC, 128], F32))
        sup_b = P.buf()
        wub = [st.enter_context(nc.sbuf_tensor("fu_wu%d" % i, [128, 2, DC, 128], BF16)) for i in range(2)]
        wub_b = P.bufs(2)
        ub = [st.enter_context(nc.sbuf_tensor("fu_ub%d" % i, [128, 2, TT + 2], F32)) for i in range(2)]
        ub_b = P.bufs(2)
        acc = [st.enter_context(nc.sbuf_tensor("fu_ac%d" % i, [128, 2, TT], F32)) for i in range(2)]
        acc_b = P.bufs(2)
        sil = [st.enter_context(nc.sbuf_tensor("fu_si%d" % i, [128, TT], F32)) for i in range(2)]
        sil_b = P.bufs(2)
        grow = [st.enter_context(nc.sbuf_tensor("fu_g%d" % i, [128, S], BF16)) for i in range(2)]
        grow_b = P.bufs(2)
        pu = [st.enter_context(nc.psum_tensor("fu_pu%d" % i, [128, 2, TT], F32)) for i in range(2)]
        pu_b = P.pbufs(2)
        for tt in range(NT):
            P.dma("sp", lambda e, tt=tt: e.dma_start(out=xn[:, :, tt * TT:(tt + 1) * TT], in_=fm(k.xnT, 0, DC, tt * TT, TT)),
                  reads=[k.xnT_b[tt]], writes=[xn_b[tt]])
        nu = 0
        for j in range(FC):
            w_, wb_ = wub[j % 2], wub_b[j % 2]
            gr, grb = grow[j % 2], grow_b[j % 2]
            for half in range(2):
                n0 = half * DFF + j * 128
                P.dma("sp", lambda e, half=half, n0=n0: e.dma_start(
                    out=sup[:, half, :, :], in_=wup[:, n0:n0 + 128].rearrange("(c p) n -> p c n", p=128)),
                    pwrites=[sup_b])
            P.op("pool", lambda e, w_=w_: e.tensor_copy(out=w_[:], in_=sup[:]), reads=[sup_b], writes=[wb_])
            for tt in range(NT):
                ts_ = slice(tt * TT, (tt + 1) * TT)
                u_, ubb = ub[nu % 2], ub_b[nu % 2]
                up_, upb = ub[(nu + 1) % 2], ub_b[(nu + 1) % 2]
                a_, ab_ = acc[nu % 2], acc_b[nu % 2]
                si_, sib = sil[nu % 2], sil_b[nu % 2]
                p_, pb_ = pu[nu % 2], pu_b[nu % 2]
                for half in range(2):
                    for c in range(DC):
                        P.op("pe", lambda e, p_=p_, w_=w_, half=half, c=c, ts_=ts_: e.matmul(
                            p_[:, half, :], lhsT=w_[:, half, c, :], rhs=xn[:, c, ts_],
                            start=(c == 0), stop=(c == DC - 1)),
                            reads=[wb_, xn_b[tt]], pwrites=[pb_])
                if tt == 0:
                    P.op("pool", lambda e, u_=u_: e.memset(u_[:, :, 0:2], 0.0), pwrites=[ubb])
                else:
                    P.op("pool", lambda e, u_=u_, up_=up_: e.tensor_copy(out=u_[:, :, 0:2], in_=up_[:, :, TT:TT + 2]),
                         reads=[upb], pwrites=[ubb])
                P.op("act", lambda e, u_=u_, p_=p_: e.activation(
                    out=u_[:, :, 2:TT + 2], in_=p_[:], func=AF.Copy), reads=[pb_], pwrites=[ubb])
                for half in range(2):
                    col = half * FC + j
                    P.op("dve", lambda e, a_=a_, u_=u_, half=half, col=col: e.tensor_scalar(
                        out=a_[:, half, :], in0=u_[:, half, 2:TT + 2],
                        scalar1=k.vecs[:, wcol[2] + col:wcol[2] + col + 1],
                        scalar2=k.vecs[:, bcol + col:bcol + col + 1], op0=ALU.mult, op1=ALU.add),
                        reads=[ubb, k.vecs_b], pwrites=[ab_])
                for tap in (1, 0):
                    for half in range(2):
                        col = half * FC + j
                        P.op("dve", lambda e, a_=a_, u_=u_, half=half, col=col, tap=tap: e.scalar_tensor_tensor(
                            out=a_[:, half, :], in0=u_[:, half, tap:tap + TT],
                            scalar=k.vecs[:, wcol[tap] + col:wcol[tap] + col + 1],
                            in1=a_[:, half, :], op0=ALU.mult, op1=ALU.add),
                            reads=[ubb, k.vecs_b, ab_], pwrites=[ab_])
                P.op("act", lambda e, si_=si_, a_=a_: e.activation(out=si_[:], in_=a_[:, 0, :], func=AF.Silu),
                     reads=[ab_], writes=[sib])
                P.op("pool", lambda e, si_=si_, a_=a_, gr=gr, ts_=ts_: e.tensor_tensor(
                    out=gr[:, ts_], in0=si_[:], in1=a_[:, 1, :], op=ALU.mult),
                    reads=[sib, ab_], pwrites=[grb])
                nu += 1
            P.dma("sp", lambda e, gr=gr, j=j: e.dma_start(out=k.gT[j * 128:(j + 1) * 128, :], in_=gr[:]),
                  reads=[grb], pwrites=[k.gT_b])
    P.barrier()


FD_T = 1024


def phase_ffn_down(k, L):
    nc, P = k.nc, k.P
    wdn = k.w["ffn_w_down"][L]
    NTD = S // FD_T
    with ExitStack() as st:
        g = st.enter_context(nc.sbuf_tensor("fd_g", [128, FC, FD_T], BF16))
        g_b = P.bufs(4)
        sdn = [st.enter_context(nc.sbuf_tensor("fd_sd%d" % i, [128, 22, 256], F32)) for i in range(2)]
        sdn_b = P.bufs(2)
        wdb = [st.enter_context(nc.sbuf_tensor("fd_wd%d" % i, [128, FC, 256], BF16)) for i in range(2)]
        wdb_b = P.bufs(2)
        hres = [st.enter_context(nc.sbuf_tensor("fd_hr%d" % i, [128, TT], F32)) for i in range(4)]
        hres_b = P.bufs(4)
        pd = [st.enter_context(nc.psum_tensor("fd_pd%d" % i, [128, TT], F32)) for i in range(4)]
        pd_b = P.pbufs(4)
        nd = 0
        nw = 0
        for t2 in range(NTD):
            t0 = t2 * FD_T
            for q4 in range(4):
                P.dma("sp", lambda e, q4=q4, t0=t0: e.dma_start(
                    out=g[:, q4 * 11:(q4 + 1) * 11, :], in_=fm(k.gT, q4 * 11, 11, t0, FD_T)),
                    reads=[k.gT_b], writes=[g_b[q4]])
            for dp in range(DC // 2):
                w_, wb_ = wdb[nw % 2], wdb_b[nw % 2]
                nw += 1
                for hf in range(2):
                    s_, sb_ = sdn[hf], sdn_b[hf]
                    P.dma("sp", lambda e, s_=s_, hf=hf, dp=dp: e.dma_start(
                        out=s_[:], in_=wdn[hf * 22 * 128:(hf + 1) * 22 * 128, dp * 256:(dp + 1) * 256].rearrange(
                            "(c p) n -> p c n", p=128)), writes=[sb_])
                    P.op("pool", lambda e, s_=s_, w_=w_, hf=hf: e.tensor_copy(
                        out=w_[:, hf * 22:(hf + 1) * 22, :], in_=s_[:]), reads=[sb_], pwrites=[wb_])
                for di in range(2):
                    dc = dp * 2 + di
                    for th in range(FD_T // TT):
                        tt = (t0 // TT) + th
                        p_, pb_ = pd[nd % 4], pd_b[nd % 4]
                        hr, hrb = hres[nd % 4], hres_b[nd % 4]
                        nd += 1
                        residual_load(k, hr, hrb, dc, tt)
                        for c in range(FC):
                            P.op("pe", lambda e, p_=p_, w_=w_, c=c, di=di, th=th: e.matmul(
                                p_[:], lhsT=w_[:, c, di * 128:(di + 1) * 128], rhs=g[:, c, th * TT:(th + 1) * TT],
                                start=(c == 0), stop=(c == FC - 1)),
                                reads=[wb_, g_b[c // 11]], pwrites=[pb_])
                        P.op("dve", lambda e, hr=hr, p_=p_: e.tensor_tensor(out=hr[:], in0=p_[:], in1=hr[:], op=ALU.add),
                             reads=[pb_, hrb], writes=[hrb])
                        residual_store(k, hr, hrb, dc, tt)
    P.barrier()


class WStream:
    def __init__(self, k, st, name, KC, ncol=128, nbuf=2, nstg=None):
        nc, P = k.nc, k.P
        nstg = nstg or nbuf
        self.k, self.KC, self.ncol, self.nbuf, self.nstg = k, KC, ncol, nbuf, nstg
        self.stg = [st.enter_context(nc.sbuf_tensor("%s_s%d" % (name, i), [128, KC, ncol], F32)) for i in range(nstg)]
        self.wb = [st.enter_context(nc.sbuf_tensor("%s_w%d" % (name, i), [128, KC, ncol], BF16)) for i in range(nbuf)]
        self.stg_b = P.bufs(nstg)
        self.wb_b = P.bufs(nbuf)
        self.n = 0

    def load(self, W2d, r0, c0):
        P = self.k.P
        i = self.n % self.nbuf
        si = self.n % self.nstg
        self.n += 1
        stg, wb = self.stg[si], self.wb[i]
        KC, ncol = self.KC, self.ncol
        P.dma("sp", lambda e: e.dma_start(
            out=stg[:], in_=W2d[r0:r0 + KC * 128, c0:c0 + ncol].rearrange("(c p) n -> p c n", p=128)),
            writes=[self.stg_b[si]])
        P.op("pool", lambda e: e.tensor_copy(out=wb[:], in_=stg[:]), reads=[self.stg_b[si]], writes=[self.wb_b[i]])
        return wb, self.wb_b[i]


def residual_store(k, hr, hrb, dc, tt):
    k.P.dma("sp", lambda e: e.dma_start(
        out=k.hT[dc * 128:(dc + 1) * 128, tt * TT:(tt + 1) * TT], in_=hr[:]),
        reads=[hrb], pwrites=[k.hT_b[tt]])


def residual_load(k, hr, hrb, dc, tt):
    k.P.dma("sp", lambda e: e.dma_start(
        out=hr[:], in_=k.hT[dc * 128:(dc + 1) * 128, tt * TT:(tt + 1) * TT]),
        reads=[k.hT_b[tt]], writes=[hrb])


POOL_H = 15


def phase_mix_pool(k):
    nc, P = k.nc, k.P
    wg = k.w["c_w_group"][0]
    scol = k.R.off["c_scale"]
    H = POOL_H
    W_ = TT + H
    with ExitStack() as st:
        wres = st.enter_context(nc.sbuf_tensor("pl_w", [128, 4, 4, 512], BF16))
        wres_b = P.buf()
        stg = [st.enter_context(nc.sbuf_tensor("pl_s%d" % i, [128, 4, 512], F32)) for i in range(2)]
        stg_b = P.bufs(2)
        invc = st.enter_context(nc.sbuf_tensor("pl_ic", [128, 64], F32))
        invc_b = P.buf()
        P.dma("sp", lambda e: e.dma_start(out=invc[:], in_=k.consts["pool_invc"]), writes=[invc_b])
        for g in range(4):
            sg, sgb = stg[g % 2], stg_b[g % 2]
            P.dma("sp", lambda e, sg=sg, g=g: e.dma_start(
                out=sg[:], in_=wg[g].rearrange("(c p) n -> p c n", p=128)), writes=[sgb])
            P.op("pool", lambda e, sg=sg, g=g: e.tensor_copy(out=wres[:, g, :, :], in_=sg[:]),
                 reads=[sgb], pwrites=[wres_b])
        xn = [st.enter_context(nc.sbuf_tensor("pl_x%d" % i, [128, DC, W_], BF16)) for i in range(2)]
        xn_b = P.bufs(2)
        pp = [[st.enter_context(nc.sbuf_tensor("pl_p%d%d" % (e_, i), [128, W_], F32)) for i in range(2)] for e_ in range(2)]
        pp_b = [P.bufs(2) for _ in range(2)]
        t16 = st.enter_context(nc.sbuf_tensor("pl_t16", [128, 16], F32))
        t16_b = P.buf()
        diff = st.enter_context(nc.sbuf_tensor("pl_d", [128, DC, TT], BF16))
        diff_b = P.buf()
        hres = [st.enter_context(nc.sbuf_tensor("pl_h%d" % i, [128, TT], F32)) for i in range(2)]
        hres_b = P.bufs(2)
        ps = [st.enter_context(nc.psum_tensor("pl_ps%d" % i, [128, TT], F32)) for i in range(2)]
        ps_b = P.pbufs(2)
        nn = 0
        for tt in range(NT):
            x_, xb = xn[tt % 2], xn_b[tt % 2]
            if tt == 0:
                P.op("pool", lambda e, x_=x_: e.memset(x_[:, :, 0:H], 0.0), pwrites=[xb])
                P.dma("sp", lambda e, x_=x_: e.dma_start(out=x_[:, :, H:W_], in_=fm(k.xnT, 0, DC, 0, TT)),
                      reads=[k.xnT_b[0]], pwrites=[xb])
            else:
                P.dma("sp", lambda e, x_=x_, tt=tt: e.dma_start(out=x_[:], in_=fm(k.xnT, 0, DC, tt * TT - H, W_)),
                      reads=[k.xnT_b[tt - 1], k.xnT_b[tt]], writes=[xb])
            for c in range(DC):
                g = c // 4
                w = 2 << g
                ei = c % 2
                eng = "dve" if ei == 0 else "pool"
                cur, curb = x_[:, c, :], xb
                for stp in range(g + 1):
                    sh = 1 << stp
                    nxt, nxtb = pp[ei][stp % 2], pp_b[ei][stp % 2]
                    P.op(eng, lambda e, nxt=nxt, cur=cur, sh=sh: e.tensor_tensor(
                        out=nxt[:, sh:W_], in0=cur[:, sh:W_], in1=cur[:, 0:W_ - sh], op=ALU.add),
                        reads=[curb], writes=[nxtb])
                    cur, curb = nxt[:], nxtb
                P.op("dve", lambda e, cur=cur, c=c, w=w, x_=x_: e.scalar_tensor_tensor(
                    out=diff[:, c, :], in0=cur[:, H:W_], scalar=1.0 / w, in1=x_[:, c, H:W_],
                    op0=ALU.mult, op1=ALU.subtract), reads=[curb, xb], pwrites=[diff_b])
                if tt == 0:
                    P.op("dve", lambda e, cur=cur, g=g: e.tensor_tensor(
                        out=t16[:], in0=cur[:, H:H + 16], in1=invc[:, g * 16:(g + 1) * 16], op=ALU.mult),
                        reads=[curb, invc_b], writes=[t16_b])
                    P.op("dve", lambda e, c=c, x_=x_: e.tensor_tensor(
                        out=diff[:, c, 0:16], in0=t16[:], in1=x_[:, c, H:H + 16], op=ALU.subtract),
                        reads=[t16_b, xb], pwrites=[diff_b])
            for n in range(DC):
                g, ni = n // 4, n % 4
                p_, pb_ = ps[nn % 2], ps_b[nn % 2]
                hr, hrb = hres[nn % 2], hres_b[nn % 2]
                residual_load(k, hr, hrb, n, tt)
                for kc in range(4):
                    P.op("pe", lambda e, p_=p_, g=g, kc=kc, ni=ni: e.matmul(
                        p_[:], lhsT=wres[:, g, kc, ni * 128:(ni + 1) * 128], rhs=diff[:, g * 4 + kc, :],
                        start=(kc == 0), stop=(kc == 3)), reads=[wres_b, diff_b], pwrites=[pb_])
                P.op("dve", lambda e, p_=p_, hr=hr, n=n: e.scalar_tensor_tensor(
                    out=hr[:], in0=p_[:], scalar=k.vecs[:, scol + n:scol + n + 1], in1=hr[:],
                    op0=ALU.mult, op1=ALU.add), reads=[pb_, hrb, k.vecs_b], writes=[hrb])
                residual_store(k, hr, hrb, n, tt)
                nn += 1
    P.barrier()


CONF_W = 31
CONF_H = CONF_W - 1


def phase_mix_conf(k):
    nc, P = k.nc, k.P
    R = k.R
    w1 = k.w["d_w_pw1"][0]
    w2 = k.w["d_w_pw2"][0]
    b1 = R.off["d_b_pw1"]
    wdw = [R.off["d_w_dw%d" % t] for t in range(CONF_W)]
    bdw = R.off["d_b_dw"]
    lng, lnb = R.off["d_ln_g"], R.off["d_ln_b"]
    b2 = R.off["d_b_pw2"]
    H = CONF_H
    W_ = TT + H
    with ExitStack() as st:
        xn = st.enter_context(nc.sbuf_tensor("cf_xn", [128, DC, TT], BF16))
        xn_b = P.buf()
        ws1 = WStream(k, st, "cf_w1", DC, 256, nbuf=2)
        ws2 = WStream(k, st, "cf_w2", DC, 128, nbuf=2, nstg=1)
        ub = st.enter_context(nc.sbuf_tensor("cf_ub", [128, DC, W_], BF16))
        dg = [st.enter_context(nc.sbuf_tensor("cf_dg%d" % i, [128, CONF_W, 128], BF16)) for i in range(2)]
        dg_b = P.bufs(2)
        identb = st.enter_context(nc.sbuf_tensor("cf_idb", [128, 128], BF16))
        identb_b = P.buf()
        P.op("pool", lambda e: e.tensor_copy(out=identb[:], in_=k.ident[:]), reads=[k.ident_b], writes=[identb_b])
        ub_b = [P.buf() for _ in range(DC)]
        gate = [st.enter_context(nc.sbuf_tensor("cf_g%d" % i, [128, TT], F32)) for i in range(2)]
        gate_b = P.bufs(2)
        v = st.enter_context(nc.sbuf_tensor("cf_v", [128, DC, TT], F32))
        v_b = [P.buf() for _ in range(DC)]
        sq = st.enter_context(nc.sbuf_tensor("cf_sq", [128, DC, TT], BF16))
        sq_b = P.buf()
        ones_f = st.enter_context(nc.sbuf_tensor("cf_1f", [128, 128], F32))
        ones_fb = P.buf()
        P.op("pool", lambda e: e.memset(ones_f[:], 1.0), writes=[ones_fb])
        mean = st.enter_context(nc.sbuf_tensor("cf_mean", [128, TT], F32))
        mean_b = P.buf()
        rstd = st.enter_context(nc.sbuf_tensor("cf_rstd", [128, TT], F32))
        rstd_b = P.buf()
        tmp = [st.enter_context(nc.sbuf_tensor("cf_t%d" % i, [128, TT], F32)) for i in range(1)] * 2
        tmp_b = [P.buf()] * 2
        lo = st.enter_context(nc.sbuf_tensor("cf_lo", [128, DC, TT], BF16))
        lo_b = P.buf()
        hres = [st.enter_context(nc.sbuf_tensor("cf_h%d" % i, [128, TT], F32)) for i in range(2)]
        hres_b = P.bufs(2)
        pa = [st.enter_context(nc.psum_tensor("cf_pa%d" % i, [128, TT], F32)) for i in range(2)]
        pa_b = P.pbufs(2)
        pg = [st.enter_context(nc.psum_tensor("cf_pg%d" % i, [128, TT], F32)) for i in range(2)]
        pg_b = P.pbufs(2)
        pm = st.enter_context(nc.psum_tensor("cf_pm", [128, TT], F32))
        pm_b = P.pbuf()
        pq = st.enter_context(nc.psum_tensor("cf_pq", [128, TT], F32))
        pq_b = P.pbuf()
        po = [st.enter_context(nc.psum_tensor("cf_po%d" % i, [128, TT], F32)) for i in range(2)]
        po_b = P.pbufs(2)
        nj = 0
        nd = 0
        for tt in range(NT):
            P.dma("sp", lambda e, tt=tt: e.dma_start(out=xn[:], in_=fm(k.xnT, 0, DC, tt * TT, TT)),
                  reads=[k.xnT_b[tt]], writes=[xn_b])
            def pe_part(jp, tt=tt):
                wa, wab = ws1.load(w1, 0, jp * 256)
                wgt, wgb = ws1.load(w1, 0, D + jp * 256)
                for sub in range(2):
                    j = jp * 2 + sub
                    ubj = ub_b[j]
                    if tt == 0:
                        P.op("pool", lambda e, j=j: e.memset(ub[:, j, 0:H], 0.0), pwrites=[ubj])
                    else:
                        P.op("pool", lambda e, j=j: e.tensor_copy(out=ub[:, j, 0:H], in_=ub[:, j, TT:W_]),
                             reads=[ubj], writes=[ubj])
                    pa_, pab = pa[sub], pa_b[sub]
                    pg_, pgb = pg[sub], pg_b[sub]
                    for c in range(DC):
                        P.op("pe", lambda e, pa_=pa_, c=c, sub=sub: e.matmul(
                            pa_[:], lhsT=wa[:, c, sub * 128:(sub + 1) * 128], rhs=xn[:, c, :],
                            start=(c == 0), stop=(c == DC - 1)), reads=[wab, xn_b], pwrites=[pab])
                    for c in range(DC):
                        P.op("pe", lambda e, pg_=pg_, c=c, sub=sub: e.matmul(
                            pg_[:], lhsT=wgt[:, c, sub * 128:(sub + 1) * 128], rhs=xn[:, c, :],
                            start=(c == 0), stop=(c == DC - 1)), reads=[wgb, xn_b], pwrites=[pgb])

            def glu_part(jp):
                for sub in range(2):
                    j = jp * 2 + sub
                    ubj = ub_b[j]
                    pa_, pab = pa[sub], pa_b[sub]
                    pg_, pgb = pg[sub], pg_b[sub]
                    gt, gtb = gate[sub], gate_b[sub]
                    P.op("act", lambda e, gt=gt, pg_=pg_, j=j: e.activation(
                        out=gt[:], in_=pg_[:], func=AF.Sigmoid, bias=k.vecs[:, b1 + DC + j:b1 + DC + j + 1]),
                        reads=[pgb, k.vecs_b], writes=[gtb])
                    P.op("dve", lambda e, gt=gt, pa_=pa_, j=j: e.scalar_tensor_tensor(
                        out=ub[:, j, H:W_], in0=pa_[:], scalar=k.vecs[:, b1 + j:b1 + j + 1], in1=gt[:],
                        op0=ALU.add, op1=ALU.mult), reads=[pab, gtb, k.vecs_b], pwrites=[ubj])

            def conv_part(jp):
                for sub in range(2):
                    j = jp * 2 + sub
                    dg_, dgb = dg[j % 2], dg_b[j % 2]
                    pc_, pcb = po[j % 2], po_b[j % 2]
                    c0 = wdw[0] + j
                    P.op("pool", lambda e, dg_=dg_, c0=c0: e.tensor_tensor(
                        out=dg_[:],
                        in0=identb[:].rearrange("p (o n) -> p o n", o=1).broadcast_to([128, CONF_W, 128]),
                        in1=k.vecs[:, c0:c0 + CONF_W * DC:DC].rearrange("p (t o) -> p t o", o=1).broadcast_to([128, CONF_W, 128]),
                        op=ALU.mult), reads=[identb_b, k.vecs_b], writes=[dgb])
                    for tap in range(CONF_W):
                        P.op("pe", lambda e, pc_=pc_, dg_=dg_, tap=tap, j=j: e.matmul(
                            pc_[:], lhsT=dg_[:, tap, :], rhs=ub[:, j, tap:tap + TT],
                            start=(tap == 0), stop=(tap == CONF_W - 1)), reads=[dgb, ub_b[j]], pwrites=[pcb])
                    P.op("act", lambda e, pc_=pc_, j=j: e.activation(
                        out=v[:, j, :], in_=pc_[:], func=AF.Identity, bias=k.vecs[:, bdw + j:bdw + j + 1]),
                        reads=[pcb, k.vecs_b], writes=[v_b[j]])
                    P.op("act", lambda e, j=j: e.activation(out=sq[:, j, :], in_=v[:, j, :], func=AF.Square),
                         reads=[v_b[j]], pwrites=[sq_b])

            NJP = DC // 2
            pe_part(0)
            glu_part(0)
            for jp in range(NJP):
                if jp + 1 < NJP:
                    pe_part(jp + 1)
                conv_part(jp)
                if jp + 1 < NJP:
                    glu_part(jp + 1)
            for c in range(DC):
                P.op("pe", lambda e, c=c: e.matmul(pm[:], lhsT=ones_f[:], rhs=v[:, c, :],
                                                   start=(c == 0), stop=(c == DC - 1)),
                     reads=[ones_fb, v_b[c]], pwrites=[pm_b])
            for c in range(DC):
                P.op("pe", lambda e, c=c: e.matmul(pq[:], lhsT=k.ones_bf[:], rhs=sq[:, c, :],
                                                   start=(c == 0), stop=(c == DC - 1)),
                     reads=[k.ones_b, sq_b], pwrites=[pq_b])
            P.op("act", lambda e: e.activation(out=mean[:], in_=pm[:], func=AF.Copy, scale=1.0 / D),
                 reads=[pm_b], writes=[mean_b])
            P.op("dve", lambda e: e.tensor_tensor(out=rstd[:], in0=mean[:], in1=mean[:], op=ALU.mult),
                 reads=[mean_b], writes=[rstd_b])
            P.op("dve", lambda e: e.scalar_tensor_tensor(
                out=rstd[:], in0=pq[:], scalar=1.0 / D, in1=rstd[:], op0=ALU.mult, op1=ALU.subtract),
                reads=[pq_b, rstd_b], writes=[rstd_b])
            P.op("act", lambda e: e.activation(out=rstd[:], in_=rstd[:], func=AF.Sqrt, bias=k.eps_t[:, 0:1]),
                 reads=[rstd_b, k.eps_b], writes=[rstd_b])
            P.op("dve", lambda e: e.reciprocal(out=rstd[:], in_=rstd[:]), reads=[rstd_b], writes=[rstd_b])
            for c in range(DC):
                t_, tb = tmp[c % 2], tmp_b[c % 2]
                P.op("pool", lambda e, t_=t_, c=c: e.tensor_tensor(out=t_[:], in0=v[:, c, :], in1=mean[:], op=ALU.subtract),
                     reads=[v_b[c], mean_b], writes=[tb])
                P.op("dve", lambda e, t_=t_, c=c: e.scalar_tensor_tensor(
                    out=t_[:], in0=t_[:], scalar=k.vecs[:, lng + c:lng + c + 1], in1=rstd[:],
                    op0=ALU.mult, op1=ALU.mult), reads=[tb, rstd_b, k.vecs_b], writes=[tb])
                P.op("act", lambda e, t_=t_, c=c: e.activation(
                    out=lo[:, c, :], in_=t_[:], func=AF.Silu, bias=k.vecs[:, lnb + c:lnb + c + 1]),
                    reads=[tb, k.vecs_b], pwrites=[lo_b])
            for dc in range(DC):
                w_, wb_ = ws2.load(w2, 0, dc * 128)
                p_, pb_ = po[nd % 2], po_b[nd % 2]
                hr, hrb = hres[nd % 2], hres_b[nd % 2]
                residual_load(k, hr, hrb, dc, tt)
                for c in range(DC):
                    P.op("pe", lambda e, p_=p_, w_=w_, c=c: e.matmul(
                        p_[:], lhsT=w_[:, c, :], rhs=lo[:, c, :], start=(c == 0), stop=(c == DC - 1)),
                        reads=[wb_, lo_b], pwrites=[pb_])
                P.op("dve", lambda e, p_=p_, hr=hr, dc=dc: e.scalar_tensor_tensor(
                    out=hr[:], in0=p_[:], scalar=k.vecs[:, b2 + dc:b2 + dc + 1], in1=hr[:],
                    op0=ALU.add, op1=ALU.add), reads=[pb_, hrb, k.vecs_b], writes=[hrb])
                residual_store(k, hr, hrb, dc, tt)
                nd += 1
    P.barrier()


HG_C = 64


def phase_mix_hgrn(k, L):
    nc, P = k.nc, k.P
    R = k.R
    win = k.w["b_w_in"][0]
    wo = k.w["b_w_o"][0]
    gn = R.off["b_g_norm"]
    NCH = TT // HG_C
    with ExitStack() as st:
        def sb(name, shape, dt=F32):
            return st.enter_context(nc.sbuf_tensor("hg_" + name, shape, dt))

        xn = sb("xn", [128, DC, TT], BF16); xn_b = P.buf()
        ws = WStream(k, st, "hg_wi", DC, 256, nbuf=4, nstg=2)
        wso = WStream(k, st, "hg_wo", DC, 128, nbuf=2)
        lb = sb("lb", [128, DC]); oml = sb("oml", [128, DC]); lbt = sb("lbt", [128, 4, DC]); lb_b = P.buf()
        ones64 = sb("ones64", [128, HG_C]); ones64_b = P.buf()
        mask = sb("mask", [128, TT]); mask_b = P.buf()
        identb = sb("identb", [128, 128], BF16); identb_b = P.buf()
        state = sb("state", [128, 16, 128]); state_b = [P.buf() for _ in range(16)]
        snap = sb("snap", [128, NCH + 1, 128], BF16); snap_b = [P.buf() for _ in range(NCH + 1)]
        sg = sb("sg", [128, TT]); sg_b = P.buf()
        lf = sb("lf", [128, TT]); lf_b = P.buf()
        kk = sb("kk", [128, TT]); kk_b = P.buf()
        a = sb("a", [128, TT]); a_b = P.buf()
        ea = sb("ea", [128, TT]); ea_b = P.buf()
        ena = sb("ena", [128, TT]); ena_b = P.buf()
        qs = sb("qs", [128, TT]); qs_b = P.buf()
        gs = sb("gs", [128, TT]); gs_b = P.buf()
        tmp = sb("tmp", [128, TT]); tmp_b = P.buf()
        rs = sb("rs", [128, TT]); rs_b = P.buf()
        qt = sb("qt", [128, TT], BF16); qt_b = P.buf()
        kt = sb("kt", [128, TT], BF16); kt_b = P.buf()
        kh = sb("kh", [128, TT], BF16); kh_b = P.buf()
        vb = sb("vb", [128, TT], BF16); vb_b = P.buf()
        osq = sb("osq", [128, TT], BF16); osq_b = P.buf()
        NB = TT // 128
        scb = sb("scb", [128, TT], BF16); scb_b = P.buf()
        vtok = sb("vtok", [128, NB, 128], BF16); vtok_b = P.buf()
        khA = sb("khA", [128, NB, 128], BF16); khA_b = P.buf()
        khB = sb("khB", [128, NB, 128], BF16); khB_b = P.buf()
        hmask = sb("hmask", [128, 2]); hmask_b = P.buf()
        ob = sb("ob", [128, DC, TT], BF16); ob_b = P.buf()
        hres = [sb("hr%d" % i, [128, TT]) for i in range(2)]; hres_b = P.bufs(2)
        pq = st.enter_context(nc.psum_tensor("hg_pq", [128, TT], F32)); pq_b = P.pbuf()
        pf = st.enter_context(nc.psum_tensor("hg_pf", [128, TT], F32)); pf_b = P.pbuf()
        pi = st.enter_context(nc.psum_tensor("hg_pi", [128, TT], F32)); pi_b = P.pbuf()
        pg = st.enter_context(nc.psum_tensor("hg_pg", [128, TT], F32)); pg_b = P.pbuf()
        po = st.enter_context(nc.psum_tensor("hg_po", [128, TT], F32)); po_b = P.pbuf()
        psc = st.enter_context(nc.psum_tensor("hg_psc", [128, TT], F32)); psc_b = P.pbuf()
        pkv = st.enter_context(nc.psum_tensor("hg_pkv", [128, 4, 128], F32)); pkv_b = [P.pbuf()] * 4
        ptr = st.enter_context(nc.psum_tensor("hg_ptr", [128, TT // 128, 128], BF16)); ptr_b = P.pbuf()

        P.dma("sp", lambda e: e.dma_start(out=mask[:], in_=k.consts["hg_mask"]), writes=[mask_b])
        P.dma("sp", lambda e: e.dma_start(out=hmask[:], in_=k.consts["half_mask"]), writes=[hmask_b])
        P.op("pool", lambda e: e.memset(ones64[:], 1.0), writes=[ones64_b])
        P.op("pool", lambda e: e.tensor_copy(out=identb[:], in_=k.ident[:]), reads=[k.ident_b], writes=[identb_b])
        P.op("pool", lambda e: e.memset(state[:], 0.0), writes=state_b)
        for l in range(DEPTH):
            c0 = R.off["b_lb%d" % l]
            P.op("act", lambda e, l=l, c0=c0: e.activation(out=lbt[:, l, :], in_=k.vecs[:, c0:c0 + DC], func=AF.Exp),
                 reads=[k.vecs_b], pwrites=[lb_b])
        P.op("dve", lambda e: e.tensor_tensor(out=oml[:], in0=lbt[:, 0, :], in1=lbt[:, 1, :], op=ALU.add),
             reads=[lb_b], pwrites=[lb_b])
        P.op("dve", lambda e: e.tensor_tensor(out=oml[:], in0=oml[:], in1=lbt[:, 2, :], op=ALU.add),
             reads=[lb_b], writes=[lb_b])
        P.op("dve", lambda e: e.tensor_tensor(out=oml[:], in0=oml[:], in1=lbt[:, 3, :], op=ALU.add),
             reads=[lb_b], writes=[lb_b])
        P.op("dve", lambda e: e.reciprocal(out=oml[:], in_=oml[:]), reads=[lb_b], writes=[lb_b])
        P.op("dve", lambda e: e.tensor_copy(out=lb[:], in_=lbt[:, 1, :]), reads=[lb_b], writes=[lb_b])
        for l in range(2, L + 1):
            P.op("dve", lambda e, l=l: e.tensor_tensor(out=lb[:], in0=lb[:], in1=lbt[:, l, :], op=ALU.add),
                 reads=[lb_b], writes=[lb_b])
        P.op("dve", lambda e: e.tensor_tensor(out=lb[:], in0=lb[:], in1=oml[:], op=ALU.mult),
             reads=[lb_b], writes=[lb_b])
        P.op("dve", lambda e: e.tensor_scalar(out=oml[:], in0=lb[:], scalar1=-1.0, scalar2=1.0,
                                              op0=ALU.mult, op1=ALU.add), reads=[lb_b], writes=[lb_b])
        nd = 0
        NH = 16
        sl_hold = []
        for tt in range(NT):
            P.dma("sp", lambda e, tt=tt: e.dma_start(out=xn[:], in_=fm(k.xnT, 0, DC, tt * TT, TT)),
                  reads=[k.xnT_b[tt]], writes=[xn_b])
            def emit_proj(h, sl):
                hs = h % 2
                if hs == 0:
                    del sl[:]
                    for sec in range(4):
                        sl.append(ws.load(win, 0, sec * D + h * 128))
                for sec, (pt, ptb) in enumerate(((pq, pq_b), (pf, pf_b), (pi, pi_b), (pg, pg_b))):
                    w_, wb_ = sl[sec]
                    for c in range(DC):
                        P.op("pe", lambda e, pt=pt, w_=w_, c=c, hs=hs: e.matmul(
                            pt[:], lhsT=w_[:, c, hs * 128:(hs + 1) * 128], rhs=xn[:, c, :], start=(c == 0), stop=(c == DC - 1)),
                            reads=[wb_, xn_b], pwrites=[ptb])

            emit_proj(0, sl_hold)
            for h in range(NH):
                P.op("act", lambda e: e.activation(out=sg[:], in_=pf[:], func=AF.Sigmoid), reads=[pf_b], writes=[sg_b])
                P.op("act", lambda e: e.activation(out=qs[:], in_=pq[:], func=AF.Silu), reads=[pq_b], writes=[qs_b])
                P.op("act", lambda e: e.activation(out=vb[:], in_=pi[:], func=AF.Copy), reads=[pi_b], writes=[vb_b])
                P.op("act", lambda e: e.activation(out=gs[:], in_=pg[:], func=AF.Silu), reads=[pg_b], writes=[gs_b])
                if h + 1 < NH:
                    emit_proj(h + 1, sl_hold)
                P.op("dve", lambda e, h=h: e.tensor_scalar(
                    out=sg[:], in0=sg[:], scalar1=oml[:, h:h + 1], scalar2=lb[:, h:h + 1],
                    op0=ALU.mult, op1=ALU.add), reads=[sg_b, lb_b], writes=[sg_b])
                P.op("act", lambda e: e.activation(out=lf[:], in_=sg[:], func=AF.Ln), reads=[sg_b], writes=[lf_b])
                P.op("pool", lambda e: e.tensor_scalar(out=kk[:], in0=sg[:], scalar1=-1.0, scalar2=1.0,
                                                       op0=ALU.mult, op1=ALU.add), reads=[sg_b], writes=[kk_b])
                for n in range(NCH):
                    cs = slice(n * HG_C, (n + 1) * HG_C)
                    P.op("dve", lambda e, cs=cs: e.tensor_tensor_scan(
                        out=a[:, cs], data0=ones64[:], data1=lf[:, cs], initial=0.0, op0=ALU.mult, op1=ALU.add),
                        reads=[lf_b, ones64_b], pwrites=[a_b])
                P.op("act", lambda e: e.activation(out=ea[:], in_=a[:], func=AF.Exp), reads=[a_b], writes=[ea_b])
                P.op("act", lambda e: e.activation(out=ena[:], in_=a[:], func=AF.Exp, scale=-1.0), reads=[a_b], writes=[ena_b])
                P.op("pool", lambda e: e.tensor_tensor(out=qt[:], in0=qs[:], in1=ea[:], op=ALU.mult),
                     reads=[qs_b, ea_b], writes=[qt_b])
                P.op("pool", lambda e: e.tensor_tensor(out=kt[:], in0=kk[:], in1=ena[:], op=ALU.mult),
                     reads=[kk_b, ena_b], writes=[kt_b])
                for n in range(NCH):
                    cs = slice(n * HG_C, (n + 1) * HG_C)
                    last = n * HG_C + HG_C - 1
                    P.op("dve", lambda e, cs=cs, last=last: e.tensor_scalar(
                        out=kh[:, cs], in0=kt[:, cs], scalar1=ea[:, last:last + 1], scalar2=None, op0=ALU.mult),
                        reads=[kt_b, ea_b], pwrites=[kh_b])
                for b in range(NB):
                    bs = slice(b * 128, (b + 1) * 128)
                    P.op("pe", lambda e, b=b, bs=bs: e.transpose(out=ptr[:, b, :], in_=vb[:, bs], identity=identb[:]),
                         reads=[vb_b, identb_b], pwrites=[ptr_b])
                P.op("act", lambda e: e.activation(out=vtok[:], in_=ptr[:], func=AF.Copy), reads=[ptr_b], writes=[vtok_b])
                for b in range(NB):
                    bs = slice(b * 128, (b + 1) * 128)
                    P.op("pe", lambda e, b=b, bs=bs: e.transpose(out=ptr[:, b, :], in_=kh[:, bs], identity=identb[:]),
                         reads=[kh_b, identb_b], pwrites=[ptr_b])
                P.op("act", lambda e: e.activation(out=khA[:], in_=ptr[:], func=AF.Copy, scale=hmask[:, 0:1]),
                     reads=[ptr_b, hmask_b], writes=[khA_b])
                P.op("dve", lambda e: e.tensor_scalar(out=khB[:], in0=ptr[:], scalar1=hmask[:, 1:2], scalar2=None, op0=ALU.mult),
                     reads=[ptr_b, hmask_b], writes=[khB_b])
                for b in range(NB):
                    bs = slice(b * 128, (b + 1) * 128)
                    P.op("pe", lambda e, bs=bs: e.matmul(psc[:, bs], lhsT=kt[:, bs], rhs=qt[:, bs], start=True, stop=True),
                         reads=[kt_b, qt_b], pwrites=[psc_b])
                P.op("dve", lambda e: e.tensor_tensor(out=scb[:], in0=psc[:], in1=mask[:], op=ALU.mult),
                     reads=[psc_b, mask_b], writes=[scb_b])
                P.op("act", lambda e, h=h: e.activation(out=snap[:, 0, :], in_=state[:, h, :], func=AF.Copy),
                     reads=[state_b[h]], writes=[snap_b[0]])
                for n in range(NCH):
                    last = n * HG_C + HG_C - 1
                    kx, kxb = (khA, khA_b) if n % 2 == 0 else (khB, khB_b)
                    P.op("pe", lambda e, n=n, kx=kx: e.matmul(pkv[:, n % 4, :], lhsT=kx[:, n // 2, :], rhs=vtok[:, n // 2, :],
                                                             start=True, stop=True),
                         reads=[kxb, vtok_b], writes=[pkv_b[n % 4]])
                    P.op("dve", lambda e, n=n, h=h, last=last: e.scalar_tensor_tensor(
                        out=state[:, h, :], in0=state[:, h, :], scalar=ea[:, last:last + 1], in1=pkv[:, n % 4, :],
                        op0=ALU.mult, op1=ALU.add), reads=[state_b[h], ea_b, pkv_b[n % 4]], writes=[state_b[h]])
                    P.op("act", lambda e, n=n, h=h: e.activation(out=snap[:, n + 1, :], in_=state[:, h, :], func=AF.Copy),
                         reads=[state_b[h]], writes=[snap_b[n + 1]])
                for b in range(NB):
                    bs = slice(b * 128, (b + 1) * 128)
                    P.op("pe", lambda e, b=b, bs=bs: e.matmul(po[:, bs], lhsT=vtok[:, b, :], rhs=scb[:, bs],
                                                              start=True, stop=False),
                         reads=[vtok_b, scb_b], pwrites=[po_b])
                    for n in (2 * b, 2 * b + 1):
                        cs = slice(n * HG_C, (n + 1) * HG_C)
                        P.op("pe", lambda e, n=n, cs=cs, b=b: e.matmul(po[:, cs], lhsT=snap[:, n, :], rhs=qt[:, cs],
                                                                      start=False, stop=(n == 2 * b + 1)),
                             reads=[snap_b[n], qt_b], pwrites=[po_b])
                P.op("act", lambda e: e.activation(out=osq[:], in_=po[:], func=AF.Square), reads=[po_b], writes=[osq_b])
                P.op("pe", lambda e: e.matmul(psc[:], lhsT=k.ones_bf[:], rhs=osq[:], start=True, stop=True),
                     reads=[osq_b, k.ones_b, scb_b], writes=[psc_b])
                P.op("act", lambda e: e.activation(out=rs[:], in_=psc[:], func=AF.Sqrt, bias=k.eps_t[:, 0:1], scale=1.0 / 128),
                     reads=[psc_b, k.eps_b], writes=[rs_b])
                P.op("dve", lambda e: e.reciprocal(out=rs[:], in_=rs[:]), reads=[rs_b], writes=[rs_b])
                P.op("dve", lambda e: e.tensor_tensor(out=tmp[:], in0=po[:], in1=rs[:], op=ALU.mult),
                     reads=[po_b, rs_b], writes=[tmp_b])
                P.op("dve", lambda e, h=h: e.scalar_tensor_tensor(
                    out=ob[:, h, :], in0=tmp[:], scalar=k.vecs[:, gn + h:gn + h + 1], in1=gs[:],
                    op0=ALU.mult, op1=ALU.mult), reads=[tmp_b, gs_b, k.vecs_b], pwrites=[ob_b])
            for dc in range(DC):
                w_, wb_ = wso.load(wo, 0, dc * 128)
                p_, pb_ = (pq, pq_b) if nd % 2 == 0 else (pf, pf_b)
                hr, hrb = hres[nd % 2], hres_b[nd % 2]
                residual_load(k, hr, hrb, dc, tt)
                for c in range(DC):
                    P.op("pe", lambda e, p_=p_, w_=w_, c=c: e.matmul(
                        p_[:], lhsT=w_[:, c, :], rhs=ob[:, c, :], start=(c == 0), stop=(c == DC - 1)),
                        reads=[wb_, ob_b], pwrites=[pb_])
                P.op("dve", lambda e, p_=p_, hr=hr: e.tensor_tensor(out=hr[:], in0=p_[:], in1=hr[:], op=ALU.add),
                     reads=[pb_, hrb], writes=[hrb])
                residual_store(k, hr, hrb, dc, tt)
                nd += 1
    P.barrier()


ATT_SCALE = 128 ** -0.5
NEG = -1.0e30
MNEG = -30000.0
NSB = S // 128


def t5_bucket_np(d):
    d = np.maximum(np.asarray(d, np.int64), 0)
    nf = np.maximum(d, 1).astype(np.float32)
    large = 16 + (np.log(nf / np.float32(16)) / np.float32(math.log(128 / 16)) * np.float32(16)).astype(np.int32)
    large = np.minimum(large, 31)
    return np.where(d < 16, d, large).astype(np.int64)


def dsa_layout_inputs(inp):
    out = {}
    out["a_gq_b"] = np.ascontiguousarray(np.broadcast_to(inp["a_g_q"][0][None, :], (128, 512)), dtype=np.float32)
    out["a_gkv_b"] = np.ascontiguousarray(np.broadcast_to(inp["a_g_kv"][0][None, :], (128, 256)), dtype=np.float32)
    rb = np.asarray(inp["rel_bias"], np.float32)
    out["a_cvec"] = np.ascontiguousarray(np.broadcast_to(rb[31][None, :], (128, 16)), dtype=np.float32)
    sl = np.arange(128)[:, None, None]
    r = np.arange(5)[None, :, None]
    ql = np.arange(512)[None, None, :]
    bidx = t5_bucket_np(ql - sl + 128 - 128 * r)
    out["a_bt"] = np.ascontiguousarray(np.moveaxis(rb[bidx], -1, 0), dtype=np.float32)
    return out


DSA_LAYOUT_SHAPES = {"a_gq_b": [128, 512], "a_gkv_b": [128, 256], "a_cvec": [128, 16], "a_bt": [16, 128, 5, 512]}


def phase_dsa_a(k):
    nc, P = k.nc, k.P
    win = k.w["a_w_in"][0]
    with ExitStack() as st:
        def sb(name, shape, dt=F32):
            return st.enter_context(nc.sbuf_tensor("da_" + name, shape, dt))

        xn = sb("xn", [128, DC, TT], BF16); xn_b = P.buf()
        wst = [sb("wst%d" % i, [128, 4, 848]) for i in range(2)]; wst_b = P.bufs(2)
        wbf = sb("wbf", [128, DC, 848], BF16); wbf_b = P.buf()
        gq = sb("gq", [128, 512]); gkv = sb("gkv", [128, 256]); g_b = P.buf()
        identb = sb("identb", [128, 128], BF16); identb_b = P.buf()
        junk = sb("junk", [128, 512], BF16); junk_b = P.buf()
        ss = sb("ss", [128, 2]); ss_b = P.buf()
        cqn = sb("cqn", [128, 512], BF16); cqn_b = P.buf()
        ckvn = [sb("ckvn%d" % i, [128, 256], BF16) for i in range(2)]; ckvn_b = P.bufs(2)
        kix = sb("kix", [128, 128], BF16); kix_b = P.buf()
        widx = sb("widx", [128, NSB, 16]); widx_b = P.buf()
        cqT = [sb("cqT%d" % i, [128, 4, TT], BF16) for i in range(2)]; cqT_b = P.bufs(2)
        ckvT = [sb("ckvT%d" % i, [128, 2, TT], BF16) for i in range(2)]; ckvT_b = P.bufs(2)
        kixT = [sb("kixT%d" % i, [128, TT], BF16) for i in range(2)]; kixT_b = P.bufs(2)
        pA = [st.enter_context(nc.psum_tensor("da_pA%d" % i, [128, 512], F32)) for i in range(2)]; pA_b = P.pbufs(2)
        pB = [st.enter_context(nc.psum_tensor("da_pB%d" % i, [128, 512], F32)) for i in range(2)]; pB_b = P.pbufs(2)
        ptr = [st.enter_context(nc.psum_tensor("da_ptr%d" % i, [128, 8, 128], BF16)) for i in range(2)]; ptr_b = P.pbufs(2)

        P.dma("sp", lambda e: e.dma_start(out=gq[:], in_=k.lay["a_gq_b"]), pwrites=[g_b])
        P.dma("sp", lambda e: e.dma_start(out=gkv[:], in_=k.lay["a_gkv_b"]), pwrites=[g_b])
        P.op("pool", lambda e: e.tensor_copy(out=identb[:], in_=k.ident[:]), reads=[k.ident_b], writes=[identb_b])
        for i in range(4):
            w_, wb_ = wst[i % 2], wst_b[i % 2]
            P.dma("sp", lambda e, w_=w_, i=i: e.dma_start(
                out=w_[:], in_=win[i * 512:(i + 1) * 512, :].rearrange("(c p) n -> p c n", p=128)), writes=[wb_])
            P.op("pool", lambda e, w_=w_, i=i: e.tensor_copy(out=wbf[:, i * 4:(i + 1) * 4, :], in_=w_[:]),
                 reads=[wb_], pwrites=[wbf_b])
        n = 0
        for tt in range(NT):
            P.dma("sp", lambda e, tt=tt: e.dma_start(out=xn[:], in_=fm(k.xnT, 0, DC, tt * TT, TT)),
                  reads=[k.xnT_b[tt]], writes=[xn_b])
            cq_t, cq_tb = cqT[tt % 2], cqT_b[tt % 2]
            ckv_t, ckv_tb = ckvT[tt % 2], ckvT_b[tt % 2]
            kix_t, kix_tb = kixT[tt % 2], kixT_b[tt % 2]
            for sub in range(4):
                sblk = tt * 4 + sub
                ts_ = slice(sub * 128, (sub + 1) * 128)
                a_, ab_ = pA[n % 2], pA_b[n % 2]
                b_, bb_ = pB[n % 2], pB_b[n % 2]
                t_, tb_ = ptr[n % 2], ptr_b[n % 2]
                ck, ckb = ckvn[n % 2], ckvn_b[n % 2]
                for c in range(DC):
                    P.op("pe", lambda e, a_=a_, c=c, ts_=ts_: e.matmul(
                        a_[:], lhsT=xn[:, c, ts_], rhs=wbf[:, c, 0:512], start=(c == 0), stop=(c == DC - 1)),
                        reads=[xn_b, wbf_b], pwrites=[ab_])
                for c in range(DC):
                    P.op("pe", lambda e, b_=b_, c=c, ts_=ts_: e.matmul(
                        b_[:, 0:336], lhsT=xn[:, c, ts_], rhs=wbf[:, c, 512:848], start=(c == 0), stop=(c == DC - 1)),
                        reads=[xn_b, wbf_b], pwrites=[bb_])
                P.op("act", lambda e, a_=a_: e.activation(out=junk[:], in_=a_[:], func=AF.Square, accum_out=ss[:, 0:1]),
                     reads=[ab_], writes=[junk_b], pwrites=[ss_b])
                P.op("act", lambda e, b_=b_: e.activation(out=junk[:, 0:256], in_=b_[:, 0:256], func=AF.Square,
                                                          accum_out=ss[:, 1:2]),
                     reads=[bb_], writes=[junk_b], pwrites=[ss_b])
                P.op("act", lambda e: e.activation(out=ss[:, 0:1], in_=ss[:, 0:1], func=AF.Sqrt,
                                                   bias=k.eps_t[:, 0:1], scale=1.0 / 512), reads=[ss_b, k.eps_b], pwrites=[ss_b])
                P.op("act", lambda e: e.activation(out=ss[:, 1:2], in_=ss[:, 1:2], func=AF.Sqrt,
                                                   bias=k.eps_t[:, 0:1], scale=1.0 / 256), reads=[ss_b, k.eps_b], pwrites=[ss_b])
                P.op("dve", lambda e: e.reciprocal(out=ss[:], in_=ss[:]), reads=[ss_b], writes=[ss_b])
                P.op("dve", lambda e, a_=a_: e.scalar_tensor_tensor(
                    out=cqn[:], in0=a_[:], scalar=ss[:, 0:1], in1=gq[:], op0=ALU.mult, op1=ALU.mult),
                    reads=[ab_, ss_b, g_b], writes=[cqn_b])
                P.op("dve", lambda e, b_=b_, ck=ck: e.scalar_tensor_tensor(
                    out=ck[:], in0=b_[:, 0:256], scalar=ss[:, 1:2], in1=gkv[:], op0=ALU.mult, op1=ALU.mult),
                    reads=[bb_, ss_b, g_b], writes=[ckb])
                P.op("act", lambda e, b_=b_: e.activation(out=kix[:, 0:64], in_=b_[:, 256:320], func=AF.Copy),
                     reads=[bb_], pwrites=[kix_b])
                P.op("act", lambda e, b_=b_: e.activation(out=kix[:, 64:128], in_=b_[:, 256:320], func=AF.Copy),
                     reads=[bb_], pwrites=[kix_b])
                P.op("act", lambda e, b_=b_, sblk=sblk: e.activation(out=widx[:, sblk, :], in_=b_[:, 320:336], func=AF.Copy),
                     reads=[bb_], pwrites=[widx_b])
                P.dma("sp", lambda e, ck=ck, sblk=sblk: e.dma_start(
                    out=k.ckv_tok[sblk * 128:(sblk + 1) * 128, :], in_=ck[:]), reads=[ckb], pwrites=[k.dsa_b])
                for j in range(4):
                    P.op("pe", lambda e, t_=t_, j=j: e.transpose(out=t_[:, j, :], in_=cqn[:, j * 128:(j + 1) * 128],
                                                                identity=identb[:]),
                         reads=[cqn_b, identb_b], pwrites=[tb_])
                for j in range(2):
                    P.op("pe", lambda e, t_=t_, j=j, ck=ck: e.transpose(out=t_[:, 4 + j, :], in_=ck[:, j * 128:(j + 1) * 128],
                                                                       identity=identb[:]),
                         reads=[ckb, identb_b], pwrites=[tb_])
                P.op("pe", lambda e, t_=t_: e.transpose(out=t_[:, 6, :], in_=kix[:], identity=identb[:]),
                     reads=[kix_b, identb_b], pwrites=[tb_])
                P.op("act", lambda e, t_=t_, cq_t=cq_t, ts_=ts_: e.activation(out=cq_t[:, :, ts_], in_=t_[:, 0:4, :], func=AF.Copy),
                     reads=[tb_], pwrites=[cq_tb])
                P.op("dve", lambda e, t_=t_, ckv_t=ckv_t, ts_=ts_: e.tensor_copy(out=ckv_t[:, :, ts_], in_=t_[:, 4:6, :]),
                     reads=[tb_], pwrites=[ckv_tb])
                P.op("dve", lambda e, t_=t_, kix_t=kix_t, ts_=ts_: e.tensor_copy(out=kix_t[:, ts_], in_=t_[:, 6, :]),
                     reads=[tb_], pwrites=[kix_tb])
                n += 1
            c0 = tt * TT
            P.dma("sp", lambda e, cq_t=cq_t, c0=c0: e.dma_start(out=fm(k.cqT, 0, 4, c0, TT), in_=cq_t[:]),
                  reads=[cq_tb], pwrites=[k.dsa_b])
            P.dma("sp", lambda e, ckv_t=ckv_t, c0=c0: e.dma_start(out=fm(k.ckvT, 0, 2, c0, TT), in_=ckv_t[:]),
                  reads=[ckv_tb], pwrites=[k.dsa_b])
            P.dma("sp", lambda e, kix_t=kix_t, c0=c0: e.dma_start(out=k.kidxT[:, c0:c0 + TT], in_=kix_t[:]),
                  reads=[kix_tb], pwrites=[k.dsa_b])
        P.dma("sp", lambda e: e.dma_start(out=k.widx, in_=widx[:]), reads=[widx_b], pwrites=[k.dsa_b])
    P.barrier()


def phase_dsa_b(k):
    nc, P = k.nc, k.P
    wq = k.w["a_w_qidx"][0]
    import os
    NQB = int(os.environ.get("DSA_NQB", str(NSB)))
    with ExitStack() as st:
        def sb(name, shape, dt=F32):
            return st.enter_context(nc.sbuf_tensor("db_" + name, shape, dt))

        kixT = sb("kixT", [128, S], BF16); kixT_b = P.buf()
        hmask = sb("hmask", [128, 2]); hmask_b = P.buf()
        widx = sb("widx", [128, NSB, 16]); widx_b = P.buf()
        wst = sb("wst", [128, 4, 1024]); wst_b = P.buf()
        wqb = sb("wqb", [128, 4, 1024], BF16); wqb_b = P.buf()
        identb = sb("identb", [128, 128], BF16); identb_b = P.buf()
        cm = sb("cm", [128, 128]); cm30 = sb("cm30", [128, 128], BF16); cm_b = P.buf()
        neg30 = sb("neg30", [128, 3, 128], BF16); neg30_b = P.buf()
        cq = [sb("cq%d" % i, [128, 4, 128], BF16) for i in range(2)]; cq_b = P.bufs(2)
        qixA = [sb("qixA%d" % i, [128, 8, 128], BF16) for i in range(2)]; qixA_b = P.bufs(2)
        qixB = [sb("qixB%d" % i, [128, 8, 128], BF16) for i in range(2)]; qixB_b = P.bufs(2)
        rl = [sb("rl%d" % i, [128, 512]) for i in range(4)]; rl_b = P.bufs(4)
        acc = [sb("acc%d" % i, [128, S]) for i in range(2)]; acc_b = P.bufs(2)
        m8 = sb("m8", [128, 8]); m8_b = P.buf()
        mq = [sb("mq%d" % i, [128, S], BF16) for i in range(2)]; mq_b = P.bufs(2)
        mT = [sb("mT%d" % i, [128, NSB, 128], BF16) for i in range(2)]; mT_b = P.bufs(2)
        pqi = [st.enter_context(nc.psum_tensor("db_pqi%d" % i, [128, 4, 128], F32)) for i in range(2)]; pqi_b = P.pbufs(2)
        ps = [st.enter_context(nc.psum_tensor("db_ps%d" % i, [128, 512], F32)) for i in range(4)]; ps_b = P.pbufs(4)
        ptr = [st.enter_context(nc.psum_tensor("db_ptr%d" % i, [128, 8, 128], BF16)) for i in range(2)]; ptr_b = P.pbufs(2)

        P.dma("sp", lambda e: e.dma_start(out=kixT[:], in_=k.kidxT), reads=[k.dsa_b], writes=[kixT_b])
        P.dma("sp", lambda e: e.dma_start(out=widx[:], in_=k.widx), reads=[k.dsa_b], writes=[widx_b])
        P.dma("sp", lambda e: e.dma_start(out=hmask[:], in_=k.consts["half_mask"]), writes=[hmask_b])
        P.dma("sp", lambda e: e.dma_start(out=wst[:], in_=wq.rearrange("(c p) n -> p c n", p=128)), writes=[wst_b])
        P.op("pool", lambda e: e.tensor_copy(out=wqb[:], in_=wst[:]), reads=[wst_b], writes=[wqb_b])
        P.op("pool", lambda e: e.tensor_copy(out=identb[:], in_=k.ident[:]), reads=[k.ident_b], writes=[identb_b])
        P.dma("sp", lambda e: e.dma_start(out=cm[:], in_=k.consts["dsa_cm"]), pwrites=[cm_b])
        P.op("pool", lambda e: e.memset(neg30[:], MNEG), writes=[neg30_b])
        P.op("dve", lambda e: e.tensor_scalar(out=cm30[:], in0=cm[:], scalar1=-1.0, scalar2=MNEG,
                                              op0=ALU.is_lt, op1=ALU.mult), reads=[cm_b], pwrites=[cm_b])
        npe = 0
        ntr = 0
        for qb in range(NQB):
            Lq = (qb + 1) * 128
            c_, cb_ = cq[qb % 2], cq_b[qb % 2]
            qxA, qxAb = qixA[qb % 2], qixA_b[qb % 2]
            qxB, qxBb = qixB[qb % 2], qixB_b[qb % 2]
            ac, acb = acc[qb % 2], acc_b[qb % 2]
            m_, mb_ = mq[qb % 2], mq_b[qb % 2]
            mt, mtb = mT[qb % 2], mT_b[qb % 2]
            P.dma("sp", lambda e, c_=c_, qb=qb: e.dma_start(out=c_[:], in_=fm(k.cqT, 0, 4, qb * 128, 128)),
                  reads=[k.dsa_b], writes=[cb_])
            if qb >= 2:
                for hg in range(2):
                    pq_, pqb = pqi[hg % 2], pqi_b[hg % 2]
                    for hh in range(4):
                        hp = hg * 4 + hh
                        for c in range(4):
                            P.op("pe", lambda e, pq_=pq_, hh=hh, hp=hp, c=c, c_=c_: e.matmul(
                                pq_[:, hh, :], lhsT=wqb[:, c, hp * 128:(hp + 1) * 128], rhs=c_[:, c, :],
                                start=(c == 0), stop=(c == 3)), reads=[wqb_b, cb_], pwrites=[pqb])
                    P.op("act", lambda e, pq_=pq_, qxA=qxA, hg=hg: e.activation(
                        out=qxA[:, hg * 4:(hg + 1) * 4, :], in_=pq_[:], func=AF.Copy, scale=hmask[:, 0:1]),
                        reads=[pqb, hmask_b], pwrites=[qxAb])
                    P.op("dve", lambda e, pq_=pq_, qxB=qxB, hg=hg: e.tensor_scalar(
                        out=qxB[:, hg * 4:(hg + 1) * 4, :], in0=pq_[:], scalar1=hmask[:, 1:2], scalar2=None, op0=ALU.mult),
                        reads=[pqb, hmask_b], pwrites=[qxBb])
                nkt = (Lq + 511) // 512
                for kt in range(nkt):
                    wd = min(512, Lq - kt * 512)
                    ks = slice(kt * 512, kt * 512 + wd)
                    for h in range(16):
                        p_, pb_ = ps[npe % 4], ps_b[npe % 4]
                        r_, rb_ = rl[npe % 4], rl_b[npe % 4]
                        npe += 1
                        qx, qxb = (qxA, qxAb) if h % 2 == 0 else (qxB, qxBb)
                        P.op("pe", lambda e, p_=p_, qx=qx, h=h, ks=ks, wd=wd: e.matmul(
                            p_[:, 0:wd], lhsT=qx[:, h // 2, :], rhs=kixT[:, ks], start=True, stop=True),
                            reads=[qxb, kixT_b], writes=[pb_])
                        P.op("act", lambda e, p_=p_, r_=r_, wd=wd: e.activation(out=r_[:, 0:wd], in_=p_[:, 0:wd], func=AF.Relu),
                             reads=[pb_], writes=[rb_])
                        if h == 0:
                            P.op("dve", lambda e, r_=r_, ac=ac, ks=ks, wd=wd, qb=qb: e.tensor_scalar(
                                out=ac[:, ks], in0=r_[:, 0:wd], scalar1=widx[:, qb, 0:1], scalar2=None, op0=ALU.mult),
                                reads=[rb_, widx_b], pwrites=[acb])
                        else:
                            P.op("dve", lambda e, r_=r_, ac=ac, ks=ks, wd=wd, qb=qb, h=h: e.scalar_tensor_tensor(
                                out=ac[:, ks], in0=r_[:, 0:wd], scalar=widx[:, qb, h:h + 1], in1=ac[:, ks],
                                op0=ALU.mult, op1=ALU.add), reads=[rb_, widx_b, acb], pwrites=[acb])
                dg = slice(Lq - 128, Lq)
                P.op("dve", lambda e, ac=ac, dg=dg: e.tensor_tensor(out=ac[:, dg], in0=ac[:, dg], in1=cm[:], op=ALU.add),
                     reads=[acb, cm_b], writes=[acb])
                for rnd in range(32):
                    P.op("dve", lambda e, ac=ac, Lq=Lq: e.max(out=m8[:], in_=ac[:, 0:Lq]), reads=[acb], writes=[m8_b])
                    P.op("dve", lambda e, ac=ac, Lq=Lq: e.match_replace(
                        out=ac[:, 0:Lq], in_to_replace=m8[:], in_values=ac[:, 0:Lq], imm_value=NEG),
                        reads=[acb, m8_b], writes=[acb])
                P.op("dve", lambda e, ac=ac, m_=m_, Lq=Lq: e.tensor_scalar(
                    out=m_[:, 0:Lq], in0=ac[:, 0:Lq], scalar1=-5.0e29, scalar2=MNEG, op0=ALU.is_gt, op1=ALU.mult),
                    reads=[acb], writes=[mb_])
                P.op("pool", lambda e, m_=m_, dg=dg: e.tensor_tensor(out=m_[:, dg], in0=m_[:, dg], in1=cm30[:], op=ALU.add),
                     reads=[mb_, cm_b], writes=[mb_])
            else:
                P.op("pool", lambda e, m_=m_, Lq=Lq: e.memset(m_[:, 0:Lq], 0.0), writes=[mb_])
                dg = slice(Lq - 128, Lq)
                P.op("pool", lambda e, m_=m_, dg=dg: e.tensor_copy(out=m_[:, dg], in_=cm30[:]), reads=[mb_, cm_b], writes=[mb_])
            for b0 in range(0, qb + 1, 8):
                nb = min(8, qb + 1 - b0)
                t_, tb_ = ptr[ntr % 2], ptr_b[ntr % 2]
                ntr += 1
                for j in range(nb):
                    P.op("pe", lambda e, t_=t_, j=j, m_=m_, b0=b0: e.transpose(
                        out=t_[:, j, :], in_=m_[:, (b0 + j) * 128:(b0 + j + 1) * 128], identity=identb[:]),
                        reads=[mb_, identb_b], pwrites=[tb_])
                P.op("act", lambda e, t_=t_, mt=mt, b0=b0, nb=nb: e.activation(
                    out=mt[:, b0:b0 + nb, :], in_=t_[:, 0:nb, :], func=AF.Copy), reads=[tb_], pwrites=[mtb])
            P.dma("sp", lambda e, mt=mt, qb=qb: e.dma_start(
                out=k.maskT[:, 0:qb + 1, qb * 128:(qb + 1) * 128], in_=mt[:, 0:qb + 1, :]),
                reads=[mtb], pwrites=[k.mask_b])
            nfill = 3 - (qb % 4)
            if nfill > 0:
                P.dma("sp", lambda e, qb=qb, nfill=nfill: e.dma_start(
                    out=k.maskT[:, qb + 1:qb + 1 + nfill, qb * 128:(qb + 1) * 128], in_=neg30[:, 0:nfill, :]),
                    reads=[neg30_b], pwrites=[k.mask_b])
    P.barrier()


def phase_dsa_c(k):
    nc, P = k.nc, k.P
    wuq = k.w["a_w_uq"][0]
    wuk = k.w["a_w_uk"][0]
    wuv = k.w["a_w_uv"][0]
    wo = k.w["a_w_o"][0]
    import os
    NG = int(os.environ.get("DSA_NG", str(NT)))
    with ExitStack() as st:
        def sb(name, shape, dt=F32):
            return st.enter_context(nc.sbuf_tensor("dc_" + name, shape, dt))

        ckvT = sb("ckvT", [128, 2, S], BF16); ckvT_b = P.buf()
        ckvk = sb("ckvk", [128, NSB, 256], BF16); ckvk_b = P.buf()
        wst = [sb("wst%d" % i, [128, 2048]) for i in range(2)]; wst_b = P.bufs(2)
        wuqb = sb("wuqb", [128, 4, 2048], BF16); wuqb_b = P.buf()
        wukb = sb("wukb", [128, 16, 256], BF16); wukb_b = P.buf()
        wuvb = sb("wuvb", [128, 16, 2, 128], BF16); wuvb_b = P.buf()
        cvec = sb("cvec", [128, 16]); cvec_b = P.buf()
        cq = sb("cq", [128, 4, TT], BF16); cq_b = P.buf()
        mk = sb("mk", [128, NSB, TT], BF16); mk_b = P.buf()
        bt = [sb("bt0", [128, 5, TT])] * 2; bt_b = [P.buf()] * 2
        qT = sb("qT", [128, TT], BF16); qT_b = P.buf()
        ql = [sb("ql%d" % i, [128, 2, TT], BF16) for i in range(2)]; ql_b = P.bufs(2)
        pT = [sb("pT%d" % i, [128, TT], BF16) for i in range(2)]; pT_b = P.bufs(2)
        rden = sb("rden", [128, TT]); rden_b = P.buf()
        oln = sb("oln", [128, 2, TT], BF16); oln_b = P.buf()
        oT = sb("oT", [128, 16, TT], BF16); oT_b = P.buf()
        wso = WStream(k, st, "dc_wo", DC, 128, nbuf=2)
        hres = [sb("hr%d" % i, [128, TT]) for i in range(2)]; hres_b = P.bufs(2)
        pm = [st.enter_context(nc.psum_tensor("dc_pm%d" % i, [128, TT], F32)) for i in range(2)]; pm_b = P.pbufs(2)
        pl = [st.enter_context(nc.psum_tensor("dc_pl%d" % i, [128, TT], F32)) for i in range(2)]; pl_b = P.pbufs(2)
        po = [st.enter_context(nc.psum_tensor("dc_po%d" % i, [128, TT], F32)) for i in range(2)]; po_b = P.pbufs(2)
        pden = st.enter_context(nc.psum_tensor("dc_pden", [128, TT], F32)); pden_b = P.pbuf()

        P.dma("sp", lambda e: e.dma_start(out=ckvT[:], in_=fm(k.ckvT, 0, 2, 0, S)), reads=[k.dsa_b], writes=[ckvT_b])
        P.dma("sp", lambda e: e.dma_start(out=ckvk[:], in_=k.ckv_tok.rearrange("(b p) c -> p b c", p=128)),
              reads=[k.dsa_b], writes=[ckvk_b])
        P.dma("sp", lambda e: e.dma_start(out=cvec[:], in_=k.lay["a_cvec"]), writes=[cvec_b])
        nw = 0
        for c in range(4):
            w_, wb_ = wst[nw % 2], wst_b[nw % 2]; nw += 1
            P.dma("sp", lambda e, w_=w_, c=c: e.dma_start(out=w_[:], in_=wuq[c * 128:(c + 1) * 128, :]), writes=[wb_])
            P.op("pool", lambda e, w_=w_, c=c: e.tensor_copy(out=wuqb[:, c, :], in_=w_[:]), reads=[wb_], pwrites=[wuqb_b])
        for hg in range(2):
            w_, wb_ = wst[nw % 2], wst_b[nw % 2]; nw += 1
            P.dma("sp", lambda e, w_=w_, hg=hg: e.dma_start(
                out=w_[:].rearrange("p (h c) -> p h c", h=8), in_=wuk[hg * 8:(hg + 1) * 8].rearrange("h d c -> d h c")),
                writes=[wb_])
            P.op("pool", lambda e, w_=w_, hg=hg: e.tensor_copy(
                out=wukb[:, hg * 8:(hg + 1) * 8, :], in_=w_[:].rearrange("p (h c) -> p h c", h=8)),
                reads=[wb_], pwrites=[wukb_b])
        for hg in range(2):
            w_, wb_ = wst[nw % 2], wst_b[nw % 2]; nw += 1
            P.dma("sp", lambda e, w_=w_, hg=hg: e.dma_start(
                out=w_[:].rearrange("p (h a d) -> p h a d", h=8, a=2),
                in_=wuv[hg * 8:(hg + 1) * 8].rearrange("h (a p) d -> p h a d", p=128)), writes=[wb_])
            P.op("pool", lambda e, w_=w_, hg=hg: e.tensor_copy(
                out=wuvb[:, hg * 8:(hg + 1) * 8, :, :], in_=w_[:].rearrange("p (h a d) -> p h a d", h=8, a=2)),
                reads=[wb_], pwrites=[wuvb_b])
        nd = 0
        identb = sb("identb", [128, 128], BF16); identb_b = P.buf()
        btb = [sb("btb%d" % i, [128, 5, TT], BF16) for i in range(2)]; btb_b = P.bufs(2)
        P.op("pool", lambda e: e.tensor_copy(out=identb[:], in_=k.ident[:]), reads=[k.ident_b], writes=[identb_b])
        for g in range(NG):
            q0 = g * TT
            nsb = 4 * g + 4
            P.dma("sp", lambda e, q0=q0: e.dma_start(out=cq[:], in_=fm(k.cqT, 0, 4, q0, TT)), reads=[k.dsa_b], writes=[cq_b])
            P.dma("sp", lambda e, q0=q0, nsb=nsb: e.dma_start(out=mk[:, 0:nsb, :], in_=k.maskT[:, 0:nsb, q0:q0 + TT]),
                  reads=[k.mask_b], writes=[mk_b])
            units = [(h, sbk) for h in range(16) for sbk in range(nsb)]

            def stage_a(u, g=g, nsb=nsb):
                h, sbk = units[u]
                q_, qb_ = ql[h % 2], ql_b[h % 2]
                b_, bb_ = bt[h % 2], bt_b[h % 2]
                bb16, bb16_b = btb[h % 2], btb_b[h % 2]
                if sbk == 0:
                    P.dma("sp", lambda e: e.dma_start(out=b_[:], in_=k.lay["a_bt"][h]), writes=[bb_])
                    P.op("pool", lambda e: e.tensor_copy(out=bb16[:], in_=b_[:]), reads=[bb_], writes=[bb16_b])
                    for c in range(4):
                        P.op("pe", lambda e, c=c: e.matmul(
                            pm[0][:], lhsT=wuqb[:, c, h * 128:(h + 1) * 128], rhs=cq[:, c, :], start=(c == 0), stop=(c == 3)),
                            reads=[wuqb_b, cq_b], pwrites=[pm_b[0]])
                    P.op("act", lambda e: e.activation(out=qT[:], in_=pm[0][:], func=AF.Copy), reads=[pm_b[0]], writes=[qT_b])
                    for cc in range(2):
                        P.op("pe", lambda e, cc=cc: e.matmul(
                            pm[1][:], lhsT=wukb[:, h, cc * 128:(cc + 1) * 128], rhs=qT[:], start=True, stop=True),
                            reads=[wukb_b, qT_b], writes=[pm_b[1]])
                        P.op("act", lambda e, cc=cc: e.activation(out=q_[:, cc, :], in_=pm[1][:], func=AF.Copy, scale=ATT_SCALE),
                             reads=[pm_b[1]], pwrites=[qb_])
                ss_ = slice(sbk * 128, (sbk + 1) * 128)
                l_, lb_ = pl[u % 2], pl_b[u % 2]
                r = sbk - (4 * g - 1)
                for cc in range(2):
                    P.op("pe", lambda e, cc=cc: e.matmul(
                        l_[:], lhsT=ckvT[:, cc, ss_], rhs=q_[:, cc, :], start=(cc == 0), stop=False),
                        reads=[ckvT_b, qb_], pwrites=[lb_])
                P.op("pe", lambda e: e.matmul(l_[:], lhsT=identb[:], rhs=mk[:, sbk, :], start=False, stop=(r < 0)),
                     reads=[identb_b, mk_b], pwrites=[lb_])
                if r >= 0:
                    P.op("pe", lambda e: e.matmul(l_[:], lhsT=identb[:], rhs=bb16[:, r, :], start=False, stop=True),
                         reads=[identb_b, bb16_b], pwrites=[lb_])

            def stage_b(u, g=g):
                h, sbk = units[u]
                l_, lb_ = pl[u % 2], pl_b[u % 2]
                p_, pb_ = pT[u % 2], pT_b[u % 2]
                r = sbk - (4 * g - 1)
                if r >= 0:
                    P.op("act", lambda e: e.activation(out=p_[:], in_=l_[:], func=AF.Exp), reads=[lb_], writes=[pb_])
                else:
                    P.op("act", lambda e: e.activation(out=p_[:], in_=l_[:], func=AF.Exp, bias=cvec[:, h:h + 1]),
                         reads=[lb_, cvec_b], writes=[pb_])

            def stage_c(u, nsb=nsb):
                h, sbk = units[u]
                p_, pb_ = pT[u % 2], pT_b[u % 2]
                for cc in range(2):
                    P.op("pe", lambda e, cc=cc: e.matmul(
                        po[cc][:], lhsT=ckvk[:, sbk, cc * 128:(cc + 1) * 128], rhs=p_[:],
                        start=(sbk == 0), stop=(sbk == nsb - 1)), reads=[ckvk_b, pb_], pwrites=[po_b[cc]])
                P.op("pe", lambda e: e.matmul(
                    pden[:], lhsT=k.ones_bf[:], rhs=p_[:], start=(sbk == 0), stop=(sbk == nsb - 1)),
                    reads=[k.ones_b, pb_], pwrites=[pden_b])
                if sbk == nsb - 1:
                    P.op("dve", lambda e: e.reciprocal(out=rden[:], in_=pden[:]), reads=[pden_b], writes=[rden_b])
                    for cc in range(2):
                        P.op("dve", lambda e, cc=cc: e.tensor_tensor(out=oln[:, cc, :], in0=po[cc][:], in1=rden[:], op=ALU.mult),
                             reads=[po_b[cc], rden_b], pwrites=[oln_b])
                    for cc in range(2):
                        P.op("pe", lambda e, cc=cc: e.matmul(
                            pm[0][:], lhsT=wuvb[:, h, cc, :], rhs=oln[:, cc, :], start=(cc == 0), stop=(cc == 1)),
                            reads=[wuvb_b, oln_b], pwrites=[pm_b[0]])
                    P.op("act", lambda e: e.activation(out=oT[:, h, :], in_=pm[0][:], func=AF.Copy),
                         reads=[pm_b[0]], pwrites=[oT_b])

            nun = len(units)
            stage_a(0)
            stage_b(0)
            for u in range(nun):
                if u + 1 < nun:
                    stage_a(u + 1)
                    stage_b(u + 1)
                stage_c(u)
            for dc in range(DC):
                w_, wb_ = wso.load(wo, 0, dc * 128)
                p_, pb_ = pm[nd % 2], pm_b[nd % 2]
                hr, hrb = hres[nd % 2], hres_b[nd % 2]
                residual_load(k, hr, hrb, dc, g)
                for c in range(DC):
                    P.op("pe", lambda e, p_=p_, w_=w_, c=c: e.matmul(
                        p_[:], lhsT=w_[:, c, :], rhs=oT[:, c, :], start=(c == 0), stop=(c == DC - 1)),
                        reads=[wb_, oT_b], pwrites=[pb_])
                P.op("dve", lambda e, p_=p_, hr=hr: e.tensor_tensor(out=hr[:], in0=p_[:], in1=hr[:], op=ALU.add),
                     reads=[pb_, hrb], writes=[hrb])
                residual_store(k, hr, hrb, dc, g)
                nd += 1
    P.barrier()


WEIGHT_SHAPES = {
    "a_w_in": [1, 2048, 848], "a_w_uq": [1, 512, 2048], "a_w_qidx": [1, 512, 1024],
    "a_w_uk": [1, 16, 128, 256], "a_w_uv": [1, 16, 256, 128], "a_w_o": [1, 2048, 2048],
    "b_w_in": [1, 2048, 8192], "b_w_o": [1, 2048, 2048],
    "c_w_group": [1, 4, 512, 512],
    "d_w_pw1": [1, 2048, 4096], "d_w_pw2": [1, 2048, 2048],
    "ffn_w_up": [4, 2048, 11264], "ffn_w_down": [4, 5632, 2048],
}

PLAN_FULL = ["in"] + sum([["norm_mix%d" % i, "mix%d" % i, "norm_ffn%d" % i, "ffn%d" % i] for i in range(DEPTH)], []) + ["out"]


def plan_weights(plan):
    ws = set()
    for p in plan:
        if p.startswith("ffn"):
            ws.update(["ffn_w_up", "ffn_w_down"])
        if p == "mix0":
            ws.update(["a_w_in", "a_w_uq", "a_w_qidx", "a_w_uk", "a_w_uv", "a_w_o"])
        if p == "mix1":
            ws.update(["b_w_in", "b_w_o"])
        if p == "mix2":
            ws.update(["c_w_group"])
        if p == "mix3":
            ws.update(["d_w_pw1", "d_w_pw2"])
    return sorted(ws)


def build_nc(plan=PLAN_FULL, raw_out=False):
    nc = bass.Bass("TRN2", target_bir_lowering=False)
    k = K()
    k.nc = nc
    k.raw_out = raw_out
    k.R = vec_registry()
    k.x = nc.dram_tensor("x", [S, D], F32, kind="ExternalInput").ap()
    k.out = nc.dram_tensor("out", [S, D], F32, kind="ExternalOutput").ap()
    vecs_d = nc.dram_tensor("vecs", [128, k.R.n], F32, kind="ExternalInput").ap()
    ident_d = nc.dram_tensor("ident", [128, 128], F32, kind="ExternalInput").ap()
    k.consts = {}
    for name, arr in make_consts().items():
        k.consts[name] = nc.dram_tensor("c_" + name, list(arr.shape), F32, kind="ExternalInput").ap()
    k.w = {}
    for name in plan_weights(plan):
        k.w[name] = nc.dram_tensor(name, WEIGHT_SHAPES[name], F32, kind="ExternalInput").ap()
    k.lay = {}
    if "mix0" in plan:
        for name, shp in DSA_LAYOUT_SHAPES.items():
            k.lay[name] = nc.dram_tensor(name, shp, F32, kind="ExternalInput").ap()
        k.cqT = nc.dram_tensor("s_cqT", [512, S], BF16).ap()
        k.ckvT = nc.dram_tensor("s_ckvT", [256, S], BF16).ap()
        k.ckv_tok = nc.dram_tensor("s_ckvtok", [S, 256], BF16).ap()
        k.kidxT = nc.dram_tensor("s_kidxT", [128, S], BF16).ap()
        k.widx = nc.dram_tensor("s_widx", [128, NSB, 16], F32).ap()
        k.maskT = nc.dram_tensor("s_maskT", [128, NSB, S], BF16).ap()
    k.gT = nc.dram_tensor("s_gT", [DFF, S], BF16).ap()
    k.hT = nc.dram_tensor("hT", [D, S], F32).ap()
    k.xnT = nc.dram_tensor("xnT", [D, S], BF16).ap()
    with ExitStack() as st:
        P = Prog(nc, st)
        k.P = P
        k.hT_b = P.bufs(NT)
        k.xnT_b = P.bufs(NT)
        k.out_b = P.buf()
        k.dsa_b = P.buf()
        k.gT_b = P.buf()
        k.mask_b = P.buf()
        k.vecs = st.enter_context(nc.sbuf_tensor("vecs_t", [128, k.R.n], F32))
        k.vecs_b = P.buf()
        k.ident = st.enter_context(nc.sbuf_tensor("ident_t", [128, 128], F32))
        k.ident_b = P.buf()
        k.ones_bf = st.enter_context(nc.sbuf_tensor("ones_bf", [128, 128], BF16))
        k.ones_b = P.buf()
        k.eps_t = st.enter_context(nc.sbuf_tensor("eps_t", [128, 1], F32))
        k.eps_b = P.buf()
        P.dma("sp", lambda e: e.dma_start(out=k.vecs[:], in_=vecs_d), writes=[k.vecs_b])
        P.dma("sp", lambda e: e.dma_start(out=k.ident[:], in_=ident_d), writes=[k.ident_b])
        P.op("pool", lambda e: e.memset(k.ones_bf[:], 1.0), writes=[k.ones_b])
        P.op("pool", lambda e: e.memset(k.eps_t[:], EPS), writes=[k.eps_b])
        k.nc = NCProxy(nc)
        for p in plan:
            k.nc.tag += 1
            if p == "in":
                phase_in(k)
            elif p == "out":
                phase_out(k)
            elif p.startswith("norm_"):
                phase_norm(k, p)
            elif p.startswith("ffn"):
                phase_ffn(k, int(p[3:]))
            elif p == "mix0":
                phase_dsa_a(k)
                phase_dsa_b(k)
                phase_dsa_c(k)
            elif p == "mix1":
                phase_mix_hgrn(k, 1)
            elif p == "mix2":
                phase_mix_pool(k)
            elif p == "mix3":
                phase_mix_conf(k)
            else:
                raise NotImplementedError(p)
        P.barrier()
        P.emit()
    return nc


def make_consts():
    c = {}
    invc = np.zeros((128, 64), np.float32)
    for g, w in enumerate((2, 4, 8, 16)):
        for t in range(16):
            invc[:, g * 16 + t] = 1.0 / min(t + 1, w)
    c["pool_invc"] = invc
    blk = np.zeros((128, 128), np.float32)
    blk[0:64, 0:64] = np.triu(np.ones((64, 64), np.float32))
    blk[64:128, 64:128] = np.triu(np.ones((64, 64), np.float32))
    c["hg_mask"] = np.ascontiguousarray(np.tile(blk, (1, TT // 128)))
    hmk = np.zeros((128, 2), np.float32)
    hmk[0:64, 0] = 1.0
    hmk[64:128, 1] = 1.0
    c["half_mask"] = hmk
    cm = np.zeros((128, 128), np.float32)
    cm[np.triu_indices(128, 1)] = -1.0e30
    c["dsa_cm"] = cm
    return c


def make_in_maps(inp, plan, n_cores=8, xs=None):
    vecs = pack_vecs(inp)
    consts = make_consts()
    ident = np.eye(128, dtype=np.float32)
    wnames = plan_weights(plan)
    lay = dsa_layout_inputs(inp) if "mix0" in plan else {}
    maps = []
    for c in range(n_cores):
        m = {"x": np.ascontiguousarray(inp["x"][c] if xs is None else xs[c]), "vecs": vecs, "ident": ident}
        for w in wnames:
            m[w] = np.ascontiguousarray(inp[w], dtype=np.float32)
        for cn, arr in consts.items():
            m["c_" + cn] = arr
        m.update(lay)
        maps.append(m)
    return maps


def kernel(**inputs):
    inp = {k_: np.asarray(v) for k_, v in inputs.items()}
    nc = build_nc(PLAN_FULL)
    maps = make_in_maps(inp, PLAN_FULL, 8)
    res = run_bass_kernel_spmd(nc, maps, core_ids=list(range(8)))
    return np.stack([np.asarray(r["out"]) for r in res.results], axis=0).astype(np.float32)
```

```python
from contextlib import ExitStack
import math
import numpy as np
import concourse.bass as bass
import concourse.mybir as mybir
from concourse.bass_utils import run_bass_kernel_spmd

F32 = mybir.dt.float32
BF16 = mybir.dt.bfloat16
AF = mybir.ActivationFunctionType
ALU = mybir.AluOpType
AX = mybir.AxisListType

D = 2048
S = 4096
DC = D // 128
TT = 512
NT = S // TT
DFF = 5632
FC = DFF // 128
EPS = 1e-6
DEPTH = 4
NDMA_SEMS = 8


class Buf:
    __slots__ = ("name", "w", "wold", "r", "open", "excl")

    def __init__(self, name):
        self.name = name
        self.excl = False
        self.w = {}
        self.wold = {}
        self.r = {}
        self.open = False


def _mx(d, k, v):
    if d.get(k, 0) < v:
        d[k] = v


class Prog:
    STREAMS = ("pe", "act", "dve", "pool", "sp")

    def __init__(self, nc, stack):
        self.nc = nc
        self.ops = {s: [] for s in self.STREAMS}
        self.sems = {}
        self.known = {s: {} for s in self.STREAMS}
        self.cnt = {}
        for s in ("pe", "act", "dve", "pool"):
            self.sems[s] = stack.enter_context(nc.semaphore("c_" + s))
            self.cnt[s] = 0
        self.dma_sems = {}
        self.dma_n = {}
        for q in ("sp", "act", "pool"):
            self.dma_sems[q] = []
            for i in range(NDMA_SEMS):
                k = "d_%s%d" % (q, i)
                self.sems[k] = stack.enter_context(nc.semaphore(k))
                self.cnt[k] = 0
                self.dma_sems[q].append(k)
            self.dma_n[q] = 0
        self.nbuf = 0

    def buf(self, name=None):
        self.nbuf += 1
        return Buf(name or "b%d" % self.nbuf)

    def bufs(self, n, name=None):
        return [self.buf() for _ in range(n)]

    def pbuf(self):
        b = self.buf()
        b.excl = True
        return b

    def pbufs(self, n):
        return [self.pbuf() for _ in range(n)]

    def _deps(self, stream, reads, writes, pwrites, own_key):
        need = {}

        def add(k, v, same_ok):
            if k == own_key and same_ok:
                return
            _mx(need, k, v)

        for b in reads:
            for k, v in b.w.items():
                add(k, v, False)
            for k, v in b.wold.items():
                add(k, v, False)
        for b in writes:
            for dd in (b.w, b.wold, b.r):
                for k, v in dd.items():
                    add(k, v, True)
        for b in pwrites:
            if not b.open:
                for k, v in b.w.items():
                    _mx(b.wold, k, v)
                b.w = {}
                b.open = True
            for dd in (b.wold, b.r):
                for k, v in dd.items():
                    add(k, v, True)
        kn = self.known[stream]
        waits = []
        for k, v in need.items():
            if kn.get(k, 0) < v:
                kn[k] = v
                waits.append((k, v))
        return waits

    def _commit(self, tok, reads, writes, pwrites):
        k, v = tok
        for b in reads:
            _mx(b.r, k, v)
            b.open = False
        for b in writes:
            b.w = {k: v}
            b.wold = {}
            b.r = {}
            b.open = False
        for b in pwrites:
            _mx(b.w, k, v)

    def op(self, stream, fn, reads=(), writes=(), pwrites=()):
        if stream != "pe" and any(b.excl for b in reads):
            writes = list(writes) + [b for b in reads if b.excl]
            reads = [b for b in reads if not b.excl]
        waits = self._deps(stream, reads, writes, pwrites, stream)
        self.cnt[stream] += 1
        tok = (stream, self.cnt[stream])
        self.ops[stream].append((waits, fn, (stream, 1)))
        self._commit(tok, reads, writes, pwrites)
        return tok

    def dma(self, q, fn, reads=(), writes=(), pwrites=()):
        i = self.dma_n[q]
        self.dma_n[q] += 1
        key = self.dma_sems[q][i % NDMA_SEMS]
        waits = self._deps(q, reads, writes, pwrites, None)
        prev = self.cnt[key]
        kn = self.known[q]
        if prev > 0 and kn.get(key, 0) < prev:
            kn[key] = prev
            waits.append((key, prev))
        self.cnt[key] = prev + 16
        tok = (key, prev + 16)
        self.ops[q].append((waits, fn, (key, 16)))
        self._commit(tok, reads, writes, pwrites)
        return tok

    def barrier(self):
        cur = dict(self.cnt)
        for s in self.STREAMS:
            waits = []
            for k, v in cur.items():
                if v > 0 and self.known[s].get(k, 0) < v:
                    self.known[s][k] = v
                    waits.append((k, v))
            if waits:
                self.ops[s].append((waits, None, None))

    def emit(self):
        nc = self.nc
        sems = self.sems

        def run(stream):
            def body(eng):
                for waits, fn, inc in self.ops[stream]:
                    for k, v in waits:
                        eng.wait_ge(sems[k], v)
                    if fn is not None:
                        fn(eng).then_inc(sems[inc[0]], inc[1])
            return body

        with nc.Block() as block:
            block.tensor(run("pe"))
            block.scalar(run("act"))
            block.vector(run("dve"))
            block.gpsimd(run("pool"))
            block.sync(run("sp"))


class VecReg:
    def __init__(self):
        self.off = {}
        self.n = 0

    def add(self, name, ncols):
        self.off[name] = self.n
        self.n += ncols


def vec_registry():
    R = VecReg()
    for i in range(DEPTH):
        R.add("norm_mix%d" % i, DC)
        R.add("norm_ffn%d" % i, DC)
        R.add("ffn_b_conv%d" % i, 2 * FC)
        for k in range(3):
            R.add("ffn_w_conv%d_%d" % (i, k), 2 * FC)
    R.add("final_norm", DC)
    R.add("c_scale", DC)
    R.add("d_b_pw1", 2 * DC)
    for k in range(31):
        R.add("d_w_dw%d" % k, DC)
    R.add("d_b_dw", DC)
    R.add("d_ln_g", DC)
    R.add("d_ln_b", DC)
    R.add("d_b_pw2", DC)
    R.add("b_g_norm", DC)
    for i in range(DEPTH):
        R.add("b_lb%d" % i, DC)
    return R


def _cols(v):
    v = np.ascontiguousarray(v, dtype=np.float32).reshape(-1, 128)
    return v.T


def pack_vecs(inp):
    R = vec_registry()
    out = np.zeros((128, R.n), np.float32)

    def put(name, v):
        c = _cols(v)
        out[:, R.off[name]:R.off[name] + c.shape[1]] = c

    for i in range(DEPTH):
        put("norm_mix%d" % i, inp["norm_mix"][i])
        put("norm_ffn%d" % i, inp["norm_ffn"][i])
        put("ffn_b_conv%d" % i, inp["ffn_b_conv"][i])
        for k in range(3):
            put("ffn_w_conv%d_%d" % (i, k), inp["ffn_w_conv"][i, k])
        put("b_lb%d" % i, inp["b_lower_bounds"][i])
    put("final_norm", inp["final_norm"])
    put("c_scale", inp["c_scale"][0])
    put("d_b_pw1", inp["d_b_pw1"][0])
    for k in range(31):
        put("d_w_dw%d" % k, inp["d_w_dw"][0, k])
    put("d_b_dw", inp["d_b_dw"][0])
    put("d_ln_g", inp["d_ln_g"][0])
    put("d_ln_b", inp["d_ln_b"][0])
    put("d_b_pw2", inp["d_b_pw2"][0])
    put("b_g_norm", inp["b_g_norm"][0])
    return out


class K:
    pass


class NCProxy:
    def __init__(self, nc):
        self._nc = nc
        self.tag = 0

    def sbuf_tensor(self, name, *a, **kw):
        return self._nc.sbuf_tensor("%s_%d" % (name, self.tag), *a, **kw)

    def psum_tensor(self, name, *a, **kw):
        return self._nc.psum_tensor("%s_%d" % (name, self.tag), *a, **kw)

    def __getattr__(self, n):
        return getattr(self._nc, n)


def fm(ap2d, c0, nck, t0, nt):
    return ap2d[c0 * 128:(c0 + nck) * 128, t0:t0 + nt].rearrange("(c p) t -> p c t", p=128)


def phase_in(k):
    nc, P = k.nc, k.P
    with ExitStack() as st:
        xin = [st.enter_context(nc.sbuf_tensor("pi_x%d" % i, [128, D], F32)) for i in range(2)]
        xin_b = P.bufs(2)
        stg = [st.enter_context(nc.sbuf_tensor("pi_s%d" % i, [128, DC, TT], F32)) for i in range(2)]
        stg_b = P.bufs(2)
        ps = [st.enter_context(nc.psum_tensor("pi_p%d" % i, [128, 512], F32)) for i in range(4)]
        ps_b = P.pbufs(4)
        n = 0
        for tt in range(NT):
            sg, sgb = stg[tt % 2], stg_b[tt % 2]
            for sub in range(4):
                si = tt * 4 + sub
                xt, xb = xin[si % 2], xin_b[si % 2]
                P.dma("sp", lambda e, xt=xt, si=si: e.dma_start(out=xt[:], in_=k.x[si * 128:(si + 1) * 128, :]),
                      writes=[xb])
                for cg in range(4):
                    pt, pb = ps[n % 4], ps_b[n % 4]
                    for ci in range(4):
                        c = cg * 4 + ci
                        P.op("pe", lambda e, pt=pt, xt=xt, c=c, ci=ci: e.transpose(
                            out=pt[:, ci * 128:(ci + 1) * 128], in_=xt[:, c * 128:(c + 1) * 128],
                            identity=k.ident[:]), reads=[xb, k.ident_b], pwrites=[pb])
                    eng = "act" if n % 2 == 0 else "dve"
                    if eng == "act":
                        P.op("act", lambda e, pt=pt, sg=sg, cg=cg, sub=sub: e.activation(
                            out=sg[:, cg * 4:(cg + 1) * 4, sub * 128:(sub + 1) * 128],
                            in_=pt[:].rearrange("p (c s) -> p c s", c=4), func=AF.Copy),
                            reads=[pb], pwrites=[sgb])
                    else:
                        P.op("dve", lambda e, pt=pt, sg=sg, cg=cg, sub=sub: e.tensor_copy(
                            out=sg[:, cg * 4:(cg + 1) * 4, sub * 128:(sub + 1) * 128],
                            in_=pt[:].rearrange("p (c s) -> p c s", c=4)),
                            reads=[pb], pwrites=[sgb])
                    n += 1
            P.dma("sp", lambda e, sg=sg, tt=tt: e.dma_start(out=fm(k.hT, 0, DC, tt * TT, TT), in_=sg[:]),
                  reads=[sgb], writes=[k.hT_b[tt]])
    P.barrier()


def norm_tile(k, st_tiles, src_h, src_hb, gcol, out_tile, out_b, tag):
    nc, P = k.nc, k.P
    sq, sq_b, pss, pss_b, rb, rb_b = st_tiles
    for c in range(DC):
        P.op("act", lambda e, c=c: e.activation(out=sq[:, c, :], in_=src_h[:, c, :], func=AF.Square),
             reads=[src_hb], pwrites=[sq_b])
    for c in range(DC):
        P.op("pe", lambda e, c=c: e.matmul(pss[:], lhsT=k.ones_bf[:], rhs=sq[:, c, :],
                                           start=(c == 0), stop=(c == DC - 1)),
             reads=[sq_b, k.ones_b], pwrites=[pss_b])
    P.op("act", lambda e: e.activation(out=rb[:], in_=pss[:], func=AF.Sqrt, bias=k.eps_t[:, 0:1], scale=1.0 / D),
         reads=[pss_b, k.eps_b], writes=[rb_b])
    P.op("dve", lambda e: e.reciprocal(out=rb[:], in_=rb[:]), reads=[rb_b], writes=[rb_b])
    for c in range(DC):
        P.op("dve", lambda e, c=c: e.scalar_tensor_tensor(
            out=out_tile[:, c, :], in0=src_h[:, c, :], scalar=k.vecs[:, gcol + c:gcol + c + 1],
            in1=rb[:], op0=ALU.mult, op1=ALU.mult),
            reads=[src_hb, rb_b, k.vecs_b], pwrites=[out_b])


def phase_norm(k, gname):
    nc, P = k.nc, k.P
    gcol = k.R.off[gname]
    with ExitStack() as st:
        hin = [st.enter_context(nc.sbuf_tensor("pn_h%d" % i, [128, DC, TT], F32)) for i in range(2)]
        hin_b = P.bufs(2)
        sq = st.enter_context(nc.sbuf_tensor("pn_sq", [128, DC, TT], BF16))
        rb = st.enter_context(nc.sbuf_tensor("pn_rb", [128, TT], F32))
        xo = [st.enter_context(nc.sbuf_tensor("pn_o%d" % i, [128, DC, TT], BF16)) for i in range(2)]
        xo_b = P.bufs(2)
        pss = st.enter_context(nc.psum_tensor("pn_ps", [128, TT], F32))
        tiles = (sq, P.buf(), pss, P.pbuf(), rb, P.buf())
        for tt in range(NT):
            h, hb = hin[tt % 2], hin_b[tt % 2]
            o, ob = xo[tt % 2], xo_b[tt % 2]
            P.dma("sp", lambda e, h=h, tt=tt: e.dma_start(out=h[:], in_=fm(k.hT, 0, DC, tt * TT, TT)),
                  reads=[k.hT_b[tt]], writes=[hb])
            norm_tile(k, tiles, h, hb, gcol, o, ob, "pn")
            P.dma("sp", lambda e, o=o, tt=tt: e.dma_start(out=fm(k.xnT, 0, DC, tt * TT, TT), in_=o[:]),
                  reads=[ob], writes=[k.xnT_b[tt]])
    P.barrier()


def phase_out(k):
    nc, P = k.nc, k.P
    gcol = k.R.off["final_norm"]
    with ExitStack() as st:
        hin = [st.enter_context(nc.sbuf_tensor("po_h%d" % i, [128, DC, TT], F32)) for i in range(2)]
        hin_b = P.bufs(2)
        sq = st.enter_context(nc.sbuf_tensor("po_sq", [128, DC, TT], BF16))
        rb = st.enter_context(nc.sbuf_tensor("po_rb", [128, TT], F32))
        xo = st.enter_context(nc.sbuf_tensor("po_o", [128, DC, TT], F32))
        xo_b = P.buf()
        og = [st.enter_context(nc.sbuf_tensor("po_g%d" % i, [128, D], F32)) for i in range(2)]
        og_b = P.bufs(2)
        pss = st.enter_context(nc.psum_tensor("po_ps", [128, TT], F32))
        ps = [st.enter_context(nc.psum_tensor("po_p%d" % i, [128, 512], F32)) for i in range(4)]
        ps_b = P.pbufs(4)
        tiles = (sq, P.buf(), pss, P.pbuf(), rb, P.buf())
        n = 0
        toks = []
        for tt in range(NT):
            h, hb = hin[tt % 2], hin_b[tt % 2]
            P.dma("sp", lambda e, h=h, tt=tt: e.dma_start(out=h[:], in_=fm(k.hT, 0, DC, tt * TT, TT)),
                  reads=[k.hT_b[tt]], writes=[hb])
            if k.raw_out:
                src, srcb = h, hb
            else:
                norm_tile(k, tiles, h, hb, gcol, xo, xo_b, "po")
                src, srcb = xo, xo_b
            for sub in range(4):
                si = tt * 4 + sub
                o, ob = og[si % 2], og_b[si % 2]
                for cg in range(4):
                    pt, pb = ps[n % 4], ps_b[n % 4]
                    for ci in range(4):
                        c = cg * 4 + ci
                        P.op("pe", lambda e, pt=pt, src=src, c=c, ci=ci, sub=sub: e.transpose(
                            out=pt[:, ci * 128:(ci + 1) * 128], in_=src[:, c, sub * 128:(sub + 1) * 128],
                            identity=k.ident[:]), reads=[srcb, k.ident_b], pwrites=[pb])
                    if n % 2 == 0:
                        P.op("act", lambda e, pt=pt, o=o, cg=cg: e.activation(
                            out=o[:, cg * 512:(cg + 1) * 512], in_=pt[:], func=AF.Copy),
                            reads=[pb], pwrites=[ob])
                    else:
                        P.op("dve", lambda e, pt=pt, o=o, cg=cg: e.tensor_copy(
                            out=o[:, cg * 512:(cg + 1) * 512], in_=pt[:]),
                            reads=[pb], pwrites=[ob])
                    n += 1
                toks.append(P.dma("sp", lambda e, o=o, si=si: e.dma_start(
                    out=k.out[si * 128:(si + 1) * 128, :], in_=o[:]), reads=[ob], writes=[k.out_b]))
    P.barrier()


def phase_ffn(k, L):
    phase_ffn_up(k, L)
    phase_ffn_down(k, L)


def phase_ffn_up(k, L):
    nc, P = k.nc, k.P
    R = k.R
    wup = k.w["ffn_w_up"][L]
    bcol = R.off["ffn_b_conv%d" % L]
    wcol = [R.off["ffn_w_conv%d_%d" % (L, t)] for t in range(3)]
    with ExitStack() as st:
        xn = st.enter_context(nc.sbuf_tensor("fu_xn", [128, DC, S], BF16))
        xn_b = P.bufs(NT)
        sup = st.enter_context(nc.sbuf_tensor("fu_su", [128, 2, DC, 128], F32))
        sup_b = P.buf()
        wub = [st.enter_context(nc.sbuf_tensor("fu_wu%d" % i, [128, 2, DC, 128], BF16)) for i in range(2)]
        wub_b = P.bufs(2)
        ub = [st.enter_context(nc.sbuf_tensor("fu_ub%d" % i, [128, 2, TT + 2], F32)) for i in range(2)]
        ub_b = P.bufs(2)
        acc = [st.enter_context(nc.sbuf_tensor("fu_ac%d" % i, [128, 2, TT], F32)) for i in range(2)]
        acc_b = P.bufs(2)
        sil = [st.enter_context(nc.sbuf_tensor("fu_si%d" % i, [128, TT], F32)) for i in range(2)]
        sil_b = P.bufs(2)
        grow = [st.enter_context(nc.sbuf_tensor("fu_g%d" % i, [128, S], BF16)) for i in range(2)]
        grow_b = P.bufs(2)
        pu = [st.enter_context(nc.psum_tensor("fu_pu%d" % i, [128, 2, TT], F32)) for i in range(2)]
        pu_b = P.pbufs(2)
        for tt in range(NT):
            P.dma("sp", lambda e, tt=tt: e.dma_start(out=xn[:, :, tt * TT:(tt + 1) * TT], in_=fm(k.xnT, 0, DC, tt * TT, TT)),
                  reads=[k.xnT_b[tt]], writes=[xn_b[tt]])
        nu = 0

        def emit_w(j):
            w_, wb_ = wub[j % 2], wub_b[j % 2]
            for half in range(2):
                n0 = half * DFF + j * 128
                P.dma("sp", lambda e, half=half, n0=n0: e.dma_start(
                    out=sup[:, half, :, :], in_=wup[:, n0:n0 + 128].rearrange("(c p) n -> p c n", p=128)),
                    pwrites=[sup_b])
            P.op("pool", lambda e, w_=w_: e.tensor_copy(out=w_[:], in_=sup[:]), reads=[sup_b], writes=[wb_])

        emit_w(0)
        for j in range(FC):
            w_, wb_ = wub[j % 2], wub_b[j % 2]
            gr, grb = grow[j % 2], grow_b[j % 2]
            if j + 1 < FC:
                emit_w(j + 1)
            for tt in range(NT):
                ts_ = slice(tt * TT, (tt + 1) * TT)
                u_, ubb = ub[nu % 2], ub_b[nu % 2]
                up_, upb = ub[(nu + 1) % 2], ub_b[(nu + 1) % 2]
                a_, ab_ = acc[nu % 2], acc_b[nu % 2]
                si_, sib = sil[nu % 2], sil_b[nu % 2]
                p_, pb_ = pu[nu % 2], pu_b[nu % 2]
                for half in range(2):
                    for c in range(DC):
                        P.op("pe", lambda e, p_=p_, w_=w_, half=half, c=c, ts_=ts_: e.matmul(
                            p_[:, half, :], lhsT=w_[:, half, c, :], rhs=xn[:, c, ts_],
                            start=(c == 0), stop=(c == DC - 1)),
                            reads=[wb_, xn_b[tt]], pwrites=[pb_])
                if tt == 0:
                    P.op("pool", lambda e, u_=u_: e.memset(u_[:, :, 0:2], 0.0), pwrites=[ubb])
                else:
                    P.op("pool", lambda e, u_=u_, up_=up_: e.tensor_copy(out=u_[:, :, 0:2], in_=up_[:, :, TT:TT + 2]),
                         reads=[upb], pwrites=[ubb])
                P.op("act", lambda e, u_=u_, p_=p_: e.activation(
                    out=u_[:, :, 2:TT + 2], in_=p_[:], func=AF.Copy), reads=[pb_], pwrites=[ubb])
                for half in range(2):
                    col = half * FC + j
                    P.op("dve", lambda e, a_=a_, u_=u_, half=half, col=col: e.tensor_scalar(
                        out=a_[:, half, :], in0=u_[:, half, 2:TT + 2],
                        scalar1=k.vecs[:, wcol[2] + col:wcol[2] + col + 1],
                        scalar2=k.vecs[:, bcol + col:bcol + col + 1], op0=ALU.mult, op1=ALU.add),
                        reads=[ubb, k.vecs_b], pwrites=[ab_])
                for tap in (1, 0):
                    for half in range(2):
                        col = half * FC + j
                        P.op("dve", lambda e, a_=a_, u_=u_, half=half, col=col, tap=tap: e.scalar_tensor_tensor(
                            out=a_[:, half, :], in0=u_[:, half, tap:tap + TT],
                            scalar=k.vecs[:, wcol[tap] + col:wcol[tap] + col + 1],
                            in1=a_[:, half, :], op0=ALU.mult, op1=ALU.add),
                            reads=[ubb, k.vecs_b, ab_], pwrites=[ab_])
                P.op("act", lambda e, si_=si_, a_=a_: e.activation(out=si_[:], in_=a_[:, 0, :], func=AF.Silu),
                     reads=[ab_], writes=[sib])
                P.op("pool", lambda e, si_=si_, a_=a_, gr=gr, ts_=ts_: e.tensor_tensor(
                    out=gr[:, ts_], in0=si_[:], in1=a_[:, 1, :], op=ALU.mult),
                    reads=[sib, ab_], pwrites=[grb])
                nu += 1
            P.dma("sp", lambda e, gr=gr, j=j: e.dma_start(out=k.gT[j * 128:(j + 1) * 128, :], in_=gr[:]),
                  reads=[grb], pwrites=[k.gT_b])
    P.barrier()


FD_T = 1024


def phase_ffn_down(k, L):
    nc, P = k.nc, k.P
    wdn = k.w["ffn_w_down"][L]
    NTD = S // FD_T
    with ExitStack() as st:
        g = st.enter_context(nc.sbuf_tensor("fd_g", [128, FC, FD_T], BF16))
        g_b = P.bufs(4)
        sdn = [st.enter_context(nc.sbuf_tensor("fd_sd%d" % i, [128, 22, 256], F32)) for i in range(2)]
        sdn_b = P.bufs(2)
        wdb = [st.enter_context(nc.sbuf_tensor("fd_wd%d" % i, [128, FC, 256], BF16)) for i in range(2)]
        wdb_b = P.bufs(2)
        hres = [st.enter_context(nc.sbuf_tensor("fd_hr%d" % i, [128, TT], F32)) for i in range(4)]
        hres_b = P.bufs(4)
        pd = [st.enter_context(nc.psum_tensor("fd_pd%d" % i, [128, TT], F32)) for i in range(4)]
        pd_b = P.pbufs(4)
        nd = 0
        nw = 0

        def emit_wd(i):
            dp = i % (DC // 2)
            w_, wb_ = wdb[i % 2], wdb_b[i % 2]
            for hf in range(2):
                s_, sb_ = sdn[hf], sdn_b[hf]
                P.dma("sp", lambda e, s_=s_, hf=hf, dp=dp: e.dma_start(
                    out=s_[:], in_=wdn[hf * 22 * 128:(hf + 1) * 22 * 128, dp * 256:(dp + 1) * 256].rearrange(
                        "(c p) n -> p c n", p=128)), writes=[sb_])
                P.op("pool", lambda e, s_=s_, w_=w_, hf=hf: e.tensor_copy(
                    out=w_[:, hf * 22:(hf + 1) * 22, :], in_=s_[:]), reads=[sb_], pwrites=[wb_])

        for t2 in range(NTD):
            t0 = t2 * FD_T
            for q4 in range(4):
                P.dma("sp", lambda e, q4=q4, t0=t0: e.dma_start(
                    out=g[:, q4 * 11:(q4 + 1) * 11, :], in_=fm(k.gT, q4 * 11, 11, t0, FD_T)),
                    reads=[k.gT_b], writes=[g_b[q4]])
            for dp in range(DC // 2):
                if nw == 0:
                    emit_wd(0)
                w_, wb_ = wdb[nw % 2], wdb_b[nw % 2]
                nw += 1
                if nw < NTD * (DC // 2):
                    emit_wd(nw)
                for di in range(2):
                    dc = dp * 2 + di
                    for th in range(FD_T // TT):
                        tt = (t0 // TT) + th
                        p_, pb_ = pd[nd % 4], pd_b[nd % 4]
                        hr, hrb = hres[nd % 4], hres_b[nd % 4]
                        nd += 1
                        residual_load(k, hr, hrb, dc, tt)
                        for c in range(FC):
                            P.op("pe", lambda e, p_=p_, w_=w_, c=c, di=di, th=th: e.matmul(
                                p_[:], lhsT=w_[:, c, di * 128:(di + 1) * 128], rhs=g[:, c, th * TT:(th + 1) * TT],
                                start=(c == 0), stop=(c == FC - 1)),
                                reads=[wb_, g_b[c // 11]], pwrites=[pb_])
                        P.op("dve", lambda e, hr=hr, p_=p_: e.tensor_tensor(out=hr[:], in0=p_[:], in1=hr[:], op=ALU.add),
                             reads=[pb_, hrb], writes=[hrb])
                        residual_store(k, hr, hrb, dc, tt)
    P.barrier()


class WStream:
    def __init__(self, k, st, name, KC, ncol=128, nbuf=2, nstg=None):
        nc, P = k.nc, k.P
        nstg = nstg or nbuf
        self.k, self.KC, self.ncol, self.nbuf, self.nstg = k, KC, ncol, nbuf, nstg
        self.stg = [st.enter_context(nc.sbuf_tensor("%s_s%d" % (name, i), [128, KC, ncol], F32)) for i in range(nstg)]
        self.wb = [st.enter_context(nc.sbuf_tensor("%s_w%d" % (name, i), [128, KC, ncol], BF16)) for i in range(nbuf)]
        self.stg_b = P.bufs(nstg)
        self.wb_b = P.bufs(nbuf)
        self.n = 0

    def load(self, W2d, r0, c0):
        P = self.k.P
        i = self.n % self.nbuf
        si = self.n % self.nstg
        self.n += 1
        stg, wb = self.stg[si], self.wb[i]
        KC, ncol = self.KC, self.ncol
        P.dma("sp", lambda e: e.dma_start(
            out=stg[:], in_=W2d[r0:r0 + KC * 128, c0:c0 + ncol].rearrange("(c p) n -> p c n", p=128)),
            writes=[self.stg_b[si]])
        P.op("pool", lambda e: e.tensor_copy(out=wb[:], in_=stg[:]), reads=[self.stg_b[si]], writes=[self.wb_b[i]])
        return wb, self.wb_b[i]


def residual_store(k, hr, hrb, dc, tt):
    k.P.dma("sp", lambda e: e.dma_start(
        out=k.hT[dc * 128:(dc + 1) * 128, tt * TT:(tt + 1) * TT], in_=hr[:]),
        reads=[hrb], pwrites=[k.hT_b[tt]])


def residual_load(k, hr, hrb, dc, tt):
    k.P.dma("sp", lambda e: e.dma_start(
        out=hr[:], in_=k.hT[dc * 128:(dc + 1) * 128, tt * TT:(tt + 1) * TT]),
        reads=[k.hT_b[tt]], writes=[hrb])


POOL_H = 15


def phase_mix_pool(k):
    nc, P = k.nc, k.P
    wg = k.w["c_w_group"][0]
    scol = k.R.off["c_scale"]
    H = POOL_H
    W_ = TT + H
    with ExitStack() as st:
        wres = st.enter_context(nc.sbuf_tensor("pl_w", [128, 4, 4, 512], BF16))
        wres_b = P.buf()
        stg = [st.enter_context(nc.sbuf_tensor("pl_s%d" % i, [128, 4, 512], F32)) for i in range(2)]
        stg_b = P.bufs(2)
        invc = st.enter_context(nc.sbuf_tensor("pl_ic", [128, 64], F32))
        invc_b = P.buf()
        P.dma("sp", lambda e: e.dma_start(out=invc[:], in_=k.consts["pool_invc"]), writes=[invc_b])
        for g in range(4):
            sg, sgb = stg[g % 2], stg_b[g % 2]
            P.dma("sp", lambda e, sg=sg, g=g: e.dma_start(
                out=sg[:], in_=wg[g].rearrange("(c p) n -> p c n", p=128)), writes=[sgb])
            P.op("pool", lambda e, sg=sg, g=g: e.tensor_copy(out=wres[:, g, :, :], in_=sg[:]),
                 reads=[sgb], pwrites=[wres_b])
        xn = [st.enter_context(nc.sbuf_tensor("pl_x%d" % i, [128, DC, W_], BF16)) for i in range(2)]
        xn_b = P.bufs(2)
        pp = [[st.enter_context(nc.sbuf_tensor("pl_p%d%d" % (e_, i), [128, W_], F32)) for i in range(2)] for e_ in range(2)]
        pp_b = [P.bufs(2) for _ in range(2)]
        t16 = st.enter_context(nc.sbuf_tensor("pl_t16", [128, 16], F32))
        t16_b = P.buf()
        diff = st.enter_context(nc.sbuf_tensor("pl_d", [128, DC, TT], BF16))
        diff_b = P.buf()
        hres = [st.enter_context(nc.sbuf_tensor("pl_h%d" % i, [128, TT], F32)) for i in range(2)]
        hres_b = P.bufs(2)
        ps = [st.enter_context(nc.psum_tensor("pl_ps%d" % i, [128, TT], F32)) for i in range(2)]
        ps_b = P.pbufs(2)
        nn = 0
        for tt in range(NT):
            x_, xb = xn[tt % 2], xn_b[tt % 2]
            if tt == 0:
                P.op("pool", lambda e, x_=x_: e.memset(x_[:, :, 0:H], 0.0), pwrites=[xb])
                P.dma("sp", lambda e, x_=x_: e.dma_start(out=x_[:, :, H:W_], in_=fm(k.xnT, 0, DC, 0, TT)),
                      reads=[k.xnT_b[0]], pwrites=[xb])
            else:
                P.dma("sp", lambda e, x_=x_, tt=tt: e.dma_start(out=x_[:], in_=fm(k.xnT, 0, DC, tt * TT - H, W_)),
                      reads=[k.xnT_b[tt - 1], k.xnT_b[tt]], writes=[xb])
            for c in range(DC):
                g = c // 4
                w = 2 << g
                ei = c % 2
                eng = "dve" if ei == 0 else "pool"
                cur, curb = x_[:, c, :], xb
                for stp in range(g + 1):
                    sh = 1 << stp
                    nxt, nxtb = pp[ei][stp % 2], pp_b[ei][stp % 2]
                    P.op(eng, lambda e, nxt=nxt, cur=cur, sh=sh: e.tensor_tensor(
                        out=nxt[:, sh:W_], in0=cur[:, sh:W_], in1=cur[:, 0:W_ - sh], op=ALU.add),
                        reads=[curb], writes=[nxtb])
                    cur, curb = nxt[:], nxtb
                P.op("dve", lambda e, cur=cur, c=c, w=w, x_=x_: e.scalar_tensor_tensor(
                    out=diff[:, c, :], in0=cur[:, H:W_], scalar=1.0 / w, in1=x_[:, c, H:W_],
                    op0=ALU.mult, op1=ALU.subtract), reads=[curb, xb], pwrites=[diff_b])
                if tt == 0:
                    P.op("dve", lambda e, cur=cur, g=g: e.tensor_tensor(
                        out=t16[:], in0=cur[:, H:H + 16], in1=invc[:, g * 16:(g + 1) * 16], op=ALU.mult),
                        reads=[curb, invc_b], writes=[t16_b])
                    P.op("dve", lambda e, c=c, x_=x_: e.tensor_tensor(
                        out=diff[:, c, 0:16], in0=t16[:], in1=x_[:, c, H:H + 16], op=ALU.subtract),
                        reads=[t16_b, xb], pwrites=[diff_b])
            for n in range(DC):
                g, ni = n // 4, n % 4
                p_, pb_ = ps[nn % 2], ps_b[nn % 2]
                hr, hrb = hres[nn % 2], hres_b[nn % 2]
                residual_load(k, hr, hrb, n, tt)
                for kc in range(4):
                    P.op("pe", lambda e, p_=p_, g=g, kc=kc, ni=ni: e.matmul(
                        p_[:], lhsT=wres[:, g, kc, ni * 128:(ni + 1) * 128], rhs=diff[:, g * 4 + kc, :],
                        start=(kc == 0), stop=(kc == 3)), reads=[wres_b, diff_b], pwrites=[pb_])
                P.op("dve", lambda e, p_=p_, hr=hr, n=n: e.scalar_tensor_tensor(
                    out=hr[:], in0=p_[:], scalar=k.vecs[:, scol + n:scol + n + 1], in1=hr[:],
                    op0=ALU.mult, op1=ALU.add), reads=[pb_, hrb, k.vecs_b], writes=[hrb])
                residual_store(k, hr, hrb, n, tt)
                nn += 1
    P.barrier()


CONF_W = 31
CONF_H = CONF_W - 1


def phase_mix_conf(k):
    nc, P = k.nc, k.P
    R = k.R
    w1 = k.w["d_w_pw1"][0]
    w2 = k.w["d_w_pw2"][0]
    b1 = R.off["d_b_pw1"]
    wdw = [R.off["d_w_dw%d" % t] for t in range(CONF_W)]
    bdw = R.off["d_b_dw"]
    lng, lnb = R.off["d_ln_g"], R.off["d_ln_b"]
    b2 = R.off["d_b_pw2"]
    H = CONF_H
    W_ = TT + H
    with ExitStack() as st:
        xn = st.enter_context(nc.sbuf_tensor("cf_xn", [128, DC, TT], BF16))
        xn_b = P.buf()
        ws1 = WStream(k, st, "cf_w1", DC, 256, nbuf=2)
        ws2 = WStream(k, st, "cf_w2", DC, 128, nbuf=2, nstg=1)
        ub = st.enter_context(nc.sbuf_tensor("cf_ub", [128, DC, W_], BF16))
        dg = [st.enter_context(nc.sbuf_tensor("cf_dg%d" % i, [128, CONF_W, 128], BF16)) for i in range(2)]
        dg_b = P.bufs(2)
        identb = st.enter_context(nc.sbuf_tensor("cf_idb", [128, 128], BF16))
        identb_b = P.buf()
        P.op("pool", lambda e: e.tensor_copy(out=identb[:], in_=k.ident[:]), reads=[k.ident_b], writes=[identb_b])
        ub_b = [P.buf() for _ in range(DC)]
        gate = [st.enter_context(nc.sbuf_tensor("cf_g%d" % i, [128, TT], F32)) for i in range(2)]
        gate_b = P.bufs(2)
        v = st.enter_context(nc.sbuf_tensor("cf_v", [128, DC, TT], F32))
        v_b = [P.buf() for _ in range(DC)]
        sq = st.enter_context(nc.sbuf_tensor("cf_sq", [128, DC, TT], BF16))
        sq_b = P.buf()
        ones_f = st.enter_context(nc.sbuf_tensor("cf_1f", [128, 128], F32))
        ones_fb = P.buf()
        P.op("pool", lambda e: e.memset(ones_f[:], 1.0), writes=[ones_fb])
        mean = st.enter_context(nc.sbuf_tensor("cf_mean", [128, TT], F32))
        mean_b = P.buf()
        rstd = st.enter_context(nc.sbuf_tensor("cf_rstd", [128, TT], F32))
        rstd_b = P.buf()
        tmp = [st.enter_context(nc.sbuf_tensor("cf_t%d" % i, [128, TT], F32)) for i in range(1)] * 2
        tmp_b = [P.buf()] * 2
        lo = st.enter_context(nc.sbuf_tensor("cf_lo", [128, DC, TT], BF16))
        lo_b = P.buf()
        hres = [st.enter_context(nc.sbuf_tensor("cf_h%d" % i, [128, TT], F32)) for i in range(2)]
        hres_b = P.bufs(2)
        pa = [st.enter_context(nc.psum_tensor("cf_pa%d" % i, [128, TT], F32)) for i in range(2)]
        pa_b = P.pbufs(2)
        pg = [st.enter_context(nc.psum_tensor("cf_pg%d" % i, [128, TT], F32)) for i in range(2)]
        pg_b = P.pbufs(2)
        pm = st.enter_context(nc.psum_tensor("cf_pm", [128, TT], F32))
        pm_b = P.pbuf()
        pq = st.enter_context(nc.psum_tensor("cf_pq", [128, TT], F32))
        pq_b = P.pbuf()
        po = [st.enter_context(nc.psum_tensor("cf_po%d" % i, [128, TT], F32)) for i in range(2)]
        po_b = P.pbufs(2)
        nj = 0
        nd = 0
        for tt in range(NT):
            P.dma("sp", lambda e, tt=tt: e.dma_start(out=xn[:], in_=fm(k.xnT, 0, DC, tt * TT, TT)),
                  reads=[k.xnT_b[tt]], writes=[xn_b])
            def pe_part(jp, tt=tt):
                wa, wab = ws1.load(w1, 0, jp * 256)
                wgt, wgb = ws1.load(w1, 0, D + jp * 256)
                for sub in range(2):
                    j = jp * 2 + sub
                    ubj = ub_b[j]
                    if tt == 0:
                        P.op("pool", lambda e, j=j: e.memset(ub[:, j, 0:H], 0.0), pwrites=[ubj])
                    else:
                        P.op("pool", lambda e, j=j: e.tensor_copy(out=ub[:, j, 0:H], in_=ub[:, j, TT:W_]),
                             reads=[ubj], writes=[ubj])
                    pa_, pab = pa[sub], pa_b[sub]
                    pg_, pgb = pg[sub], pg_b[sub]
                    for c in range(DC):
                        P.op("pe", lambda e, pa_=pa_, c=c, sub=sub: e.matmul(
                            pa_[:], lhsT=wa[:, c, sub * 128:(sub + 1) * 128], rhs=xn[:, c, :],
                            start=(c == 0), stop=(c == DC - 1)), reads=[wab, xn_b], pwrites=[pab])
                    for c in range(DC):
                        P.op("pe", lambda e, pg_=pg_, c=c, sub=sub: e.matmul(
                            pg_[:], lhsT=wgt[:, c, sub * 128:(sub + 1) * 128], rhs=xn[:, c, :],
                            start=(c == 0), stop=(c == DC - 1)), reads=[wgb, xn_b], pwrites=[pgb])

            def glu_part(jp):
                for sub in range(2):
                    j = jp * 2 + sub
                    ubj = ub_b[j]
                    pa_, pab = pa[sub], pa_b[sub]
                    pg_, pgb = pg[sub], pg_b[sub]
                    gt, gtb = gate[sub], gate_b[sub]
                    P.op("act", lambda e, gt=gt, pg_=pg_, j=j: e.activation(
                        out=gt[:], in_=pg_[:], func=AF.Sigmoid, bias=k.vecs[:, b1 + DC + j:b1 + DC + j + 1]),
                        reads=[pgb, k.vecs_b], writes=[gtb])
                    P.op("dve", lambda e, gt=gt, pa_=pa_, j=j: e.scalar_tensor_tensor(
                        out=ub[:, j, H:W_], in0=pa_[:], scalar=k.vecs[:, b1 + j:b1 + j + 1], in1=gt[:],
                        op0=ALU.add, op1=ALU.mult), reads=[pab, gtb, k.vecs_b], pwrites=[ubj])

            def conv_part(jp):
                for sub in range(2):
                    j = jp * 2 + sub
                    dg_, dgb = dg[j % 2], dg_b[j % 2]
                    pc_, pcb = po[j % 2], po_b[j % 2]
                    c0 = wdw[0] + j
                    P.op("pool", lambda e, dg_=dg_, c0=c0: e.tensor_tensor(
                        out=dg_[:],
                        in0=identb[:].rearrange("p (o n) -> p o n", o=1).broadcast_to([128, CONF_W, 128]),
                        in1=k.vecs[:, c0:c0 + CONF_W * DC:DC].rearrange("p (t o) -> p t o", o=1).broadcast_to([128, CONF_W, 128]),
                        op=ALU.mult), reads=[identb_b, k.vecs_b], writes=[dgb])
                    for tap in range(CONF_W):
                        P.op("pe", lambda e, pc_=pc_, dg_=dg_, tap=tap, j=j: e.matmul(
                            pc_[:], lhsT=dg_[:, tap, :], rhs=ub[:, j, tap:tap + TT],
                            start=(tap == 0), stop=(tap == CONF_W - 1)), reads=[dgb, ub_b[j]], pwrites=[pcb])
                    P.op("act", lambda e, pc_=pc_, j=j: e.activation(
                        out=v[:, j, :], in_=pc_[:], func=AF.Identity, bias=k.vecs[:, bdw + j:bdw + j + 1]),
                        reads=[pcb, k.vecs_b], writes=[v_b[j]])
                    P.op("act", lambda e, j=j: e.activation(out=sq[:, j, :], in_=v[:, j, :], func=AF.Square),
                         reads=[v_b[j]], pwrites=[sq_b])

            NJP = DC // 2
            pe_part(0)
            glu_part(0)
            for jp in range(NJP):
                if jp + 1 < NJP:
                    pe_part(jp + 1)
                conv_part(jp)
                if jp + 1 < NJP:
                    glu_part(jp + 1)
            for c in range(DC):
                P.op("pe", lambda e, c=c: e.matmul(pm[:], lhsT=ones_f[:], rhs=v[:, c, :],
                                                   start=(c == 0), stop=(c == DC - 1)),
                     reads=[ones_fb, v_b[c]], pwrites=[pm_b])
            for c in range(DC):
                P.op("pe", lambda e, c=c: e.matmul(pq[:], lhsT=k.ones_bf[:], rhs=sq[:, c, :],
                                                   start=(c == 0), stop=(c == DC - 1)),
                     reads=[k.ones_b, sq_b], pwrites=[pq_b])
            P.op("act", lambda e: e.activation(out=mean[:], in_=pm[:], func=AF.Copy, scale=1.0 / D),
                 reads=[pm_b], writes=[mean_b])
            P.op("dve", lambda e: e.tensor_tensor(out=rstd[:], in0=mean[:], in1=mean[:], op=ALU.mult),
                 reads=[mean_b], writes=[rstd_b])
            P.op("dve", lambda e: e.scalar_tensor_tensor(
                out=rstd[:], in0=pq[:], scalar=1.0 / D, in1=rstd[:], op0=ALU.mult, op1=ALU.subtract),
                reads=[pq_b, rstd_b], writes=[rstd_b])
            P.op("act", lambda e: e.activation(out=rstd[:], in_=rstd[:], func=AF.Sqrt, bias=k.eps_t[:, 0:1]),
                 reads=[rstd_b, k.eps_b], writes=[rstd_b])
            P.op("dve", lambda e: e.reciprocal(out=rstd[:], in_=rstd[:]), reads=[rstd_b], writes=[rstd_b])
            for c in range(DC):
                t_, tb = tmp[c % 2], tmp_b[c % 2]
                P.op("pool", lambda e, t_=t_, c=c: e.tensor_tensor(out=t_[:], in0=v[:, c, :], in1=mean[:], op=ALU.subtract),
                     reads=[v_b[c], mean_b], writes=[tb])
                P.op("dve", lambda e, t_=t_, c=c: e.scalar_tensor_tensor(
                    out=t_[:], in0=t_[:], scalar=k.vecs[:, lng + c:lng + c + 1], in1=rstd[:],
                    op0=ALU.mult, op1=ALU.mult), reads=[tb, rstd_b, k.vecs_b], writes=[tb])
                P.op("act", lambda e, t_=t_, c=c: e.activation(
                    out=lo[:, c, :], in_=t_[:], func=AF.Silu, bias=k.vecs[:, lnb + c:lnb + c + 1]),
                    reads=[tb, k.vecs_b], pwrites=[lo_b])
            for dc in range(DC):
                w_, wb_ = ws2.load(w2, 0, dc * 128)
                p_, pb_ = po[nd % 2], po_b[nd % 2]
                hr, hrb = hres[nd % 2], hres_b[nd % 2]
                residual_load(k, hr, hrb, dc, tt)
                for c in range(DC):
                    P.op("pe", lambda e, p_=p_, w_=w_, c=c: e.matmul(
                        p_[:], lhsT=w_[:, c, :], rhs=lo[:, c, :], start=(c == 0), stop=(c == DC - 1)),
                        reads=[wb_, lo_b], pwrites=[pb_])
                P.op("dve", lambda e, p_=p_, hr=hr, dc=dc: e.scalar_tensor_tensor(
                    out=hr[:], in0=p_[:], scalar=k.vecs[:, b2 + dc:b2 + dc + 1], in1=hr[:],
                    op0=ALU.add, op1=ALU.add), reads=[pb_, hrb, k.vecs_b], writes=[hrb])
                residual_store(k, hr, hrb, dc, tt)
                nd += 1
    P.barrier()


HG_C = 64


def phase_mix_hgrn(k, L):
    nc, P = k.nc, k.P
    R = k.R
    win = k.w["b_w_in"][0]
    wo = k.w["b_w_o"][0]
    gn = R.off["b_g_norm"]
    NCH = TT // HG_C
    with ExitStack() as st:
        def sb(name, shape, dt=F32):
            return st.enter_context(nc.sbuf_tensor("hg_" + name, shape, dt))

        xn = sb("xn", [128, DC, TT], BF16); xn_b = P.buf()
        ws = WStream(k, st, "hg_wi", DC, 256, nbuf=4, nstg=2)
        wso = WStream(k, st, "hg_wo", DC, 128, nbuf=2)
        lb = sb("lb", [128, DC]); oml = sb("oml", [128, DC]); lbt = sb("lbt", [128, 4, DC]); lb_b = P.buf()
        ones64 = sb("ones64", [128, HG_C]); ones64_b = P.buf()
        mask = sb("mask", [128, TT]); mask_b = P.buf()
        identb = sb("identb", [128, 128], BF16); identb_b = P.buf()
        state = sb("state", [128, 16, 128]); state_b = [P.buf() for _ in range(16)]
        snap = sb("snap", [128, NCH + 1, 128], BF16); snap_b = [P.buf() for _ in range(NCH + 1)]
        sg = sb("sg", [128, TT]); sg_b = P.buf()
        lf = sb("lf", [128, TT]); lf_b = P.buf()
        kk = sb("kk", [128, TT]); kk_b = P.buf()
        a = sb("a", [128, TT]); a_b = P.buf()
        ea = sb("ea", [128, TT]); ea_b = P.buf()
        ena = sb("ena", [128, TT]); ena_b = P.buf()
        qs = sb("qs", [128, TT]); qs_b = P.buf()
        gs = sb("gs", [128, TT]); gs_b = P.buf()
        tmp = sb("tmp", [128, TT]); tmp_b = P.buf()
        rs = sb("rs", [128, TT]); rs_b = P.buf()
        qt = sb("qt", [128, TT], BF16); qt_b = P.buf()
        kt = sb("kt", [128, TT], BF16); kt_b = P.buf()
        kh = sb("kh", [128, TT], BF16); kh_b = P.buf()
        vb = sb("vb", [128, TT], BF16); vb_b = P.buf()
        osq = sb("osq", [128, TT], BF16); osq_b = P.buf()
        NB = TT // 128
        scb = sb("scb", [128, TT], BF16); scb_b = P.buf()
        vtok = sb("vtok", [128, NB, 128], BF16); vtok_b = P.buf()
        khA = sb("khA", [128, NB, 128], BF16); khA_b = P.buf()
        khB = sb("khB", [128, NB, 128], BF16); khB_b = P.buf()
        hmask = sb("hmask", [128, 2]); hmask_b = P.buf()
        ob = sb("ob", [128, DC, TT], BF16); ob_b = P.buf()
        hres = [sb("hr%d" % i, [128, TT]) for i in range(2)]; hres_b = P.bufs(2)
        pq = st.enter_context(nc.psum_tensor("hg_pq", [128, TT], F32)); pq_b = P.pbuf()
        pf = st.enter_context(nc.psum_tensor("hg_pf", [128, TT], F32)); pf_b = P.pbuf()
        pi = st.enter_context(nc.psum_tensor("hg_pi", [128, TT], F32)); pi_b = P.pbuf()
        pg = st.enter_context(nc.psum_tensor("hg_pg", [128, TT], F32)); pg_b = P.pbuf()
        po = st.enter_context(nc.psum_tensor("hg_po", [128, TT], F32)); po_b = P.pbuf()
        psc = st.enter_context(nc.psum_tensor("hg_psc", [128, TT], F32)); psc_b = P.pbuf()
        pkv = st.enter_context(nc.psum_tensor("hg_pkv", [128, 4, 128], F32)); pkv_b = [P.pbuf()] * 4
        ptr = st.enter_context(nc.psum_tensor("hg_ptr", [128, TT // 128, 128], BF16)); ptr_b = P.pbuf()

        P.dma("sp", lambda e: e.dma_start(out=mask[:], in_=k.consts["hg_mask"]), writes=[mask_b])
        P.dma("sp", lambda e: e.dma_start(out=hmask[:], in_=k.consts["half_mask"]), writes=[hmask_b])
        P.op("pool", lambda e: e.memset(ones64[:], 1.0), writes=[ones64_b])
        P.op("pool", lambda e: e.tensor_copy(out=identb[:], in_=k.ident[:]), reads=[k.ident_b], writes=[identb_b])
        P.op("pool", lambda e: e.memset(state[:], 0.0), writes=state_b)
        for l in range(DEPTH):
            c0 = R.off["b_lb%d" % l]
            P.op("act", lambda e, l=l, c0=c0: e.activation(out=lbt[:, l, :], in_=k.vecs[:, c0:c0 + DC], func=AF.Exp),
                 reads=[k.vecs_b], pwrites=[lb_b])
        P.op("dve", lambda e: e.tensor_tensor(out=oml[:], in0=lbt[:, 0, :], in1=lbt[:, 1, :], op=ALU.add),
             reads=[lb_b], pwrites=[lb_b])
        P.op("dve", lambda e: e.tensor_tensor(out=oml[:], in0=oml[:], in1=lbt[:, 2, :], op=ALU.add),
             reads=[lb_b], writes=[lb_b])
        P.op("dve", lambda e: e.tensor_tensor(out=oml[:], in0=oml[:], in1=lbt[:, 3, :], op=ALU.add),
             reads=[lb_b], writes=[lb_b])
        P.op("dve", lambda e: e.reciprocal(out=oml[:], in_=oml[:]), reads=[lb_b], writes=[lb_b])
        P.op("dve", lambda e: e.tensor_copy(out=lb[:], in_=lbt[:, 1, :]), reads=[lb_b], writes=[lb_b])
        for l in range(2, L + 1):
            P.op("dve", lambda e, l=l: e.tensor_tensor(out=lb[:], in0=lb[:], in1=lbt[:, l, :], op=ALU.add),
                 reads=[lb_b], writes=[lb_b])
        P.op("dve", lambda e: e.tensor_tensor(out=lb[:], in0=lb[:], in1=oml[:], op=ALU.mult),
             reads=[lb_b], writes=[lb_b])
        P.op("dve", lambda e: e.tensor_scalar(out=oml[:], in0=lb[:], scalar1=-1.0, scalar2=1.0,
                                              op0=ALU.mult, op1=ALU.add), reads=[lb_b], writes=[lb_b])
        nd = 0
        NH = 16
        sl_hold = []
        for tt in range(NT):
            P.dma("sp", lambda e, tt=tt: e.dma_start(out=xn[:], in_=fm(k.xnT, 0, DC, tt * TT, TT)),
                  reads=[k.xnT_b[tt]], writes=[xn_b])
            def emit_proj(h, sl):
                hs = h % 2
                if hs == 0:
                    del sl[:]
                    for sec in range(4):
                        sl.append(ws.load(win, 0, sec * D + h * 128))
                for sec, (pt, ptb) in enumerate(((pq, pq_b), (pf, pf_b), (pi, pi_b), (pg, pg_b))):
                    w_, wb_ = sl[sec]
                    for c in range(DC):
                        P.op("pe", lambda e, pt=pt, w_=w_, c=c, hs=hs: e.matmul(
                            pt[:], lhsT=w_[:, c, hs * 128:(hs + 1) * 128], rhs=xn[:, c, :], start=(c == 0), stop=(c == DC - 1)),
                            reads=[wb_, xn_b], pwrites=[ptb])

            emit_proj(0, sl_hold)
            for h in range(NH):
                P.op("act", lambda e: e.activation(out=sg[:], in_=pf[:], func=AF.Sigmoid), reads=[pf_b], writes=[sg_b])
                P.op("act", lambda e: e.activation(out=qs[:], in_=pq[:], func=AF.Silu), reads=[pq_b], writes=[qs_b])
                P.op("act", lambda e: e.activation(out=vb[:], in_=pi[:], func=AF.Copy), reads=[pi_b], writes=[vb_b])
                P.op("act", lambda e: e.activation(out=gs[:], in_=pg[:], func=AF.Silu), reads=[pg_b], writes=[gs_b])
                if h + 1 < NH:
                    emit_proj(h + 1, sl_hold)
                P.op("dve", lambda e, h=h: e.tensor_scalar(
                    out=sg[:], in0=sg[:], scalar1=oml[:, h:h + 1], scalar2=lb[:, h:h + 1],
                    op0=ALU.mult, op1=ALU.add), reads=[sg_b, lb_b], writes=[sg_b])
                P.op("act", lambda e: e.activation(out=lf[:], in_=sg[:], func=AF.Ln), reads=[sg_b], writes=[lf_b])
                P.op("pool", lambda e: e.tensor_scalar(out=kk[:], in0=sg[:], scalar1=-1.0, scalar2=1.0,
                                                       op0=ALU.mult, op1=ALU.add), reads=[sg_b], writes=[kk_b])
                for n in range(NCH):
                    cs = slice(n * HG_C, (n + 1) * HG_C)
                    P.op("dve", lambda e, cs=cs: e.tensor_tensor_scan(
                        out=a[:, cs], data0=ones64[:], data1=lf[:, cs], initial=0.0, op0=ALU.mult, op1=ALU.add),
                        reads=[lf_b, ones64_b], pwrites=[a_b])
                P.op("act", lambda e: e.activation(out=ea[:], in_=a[:], func=AF.Exp), reads=[a_b], writes=[ea_b])
                P.op("act", lambda e: e.activation(out=ena[:], in_=a[:], func=AF.Exp, scale=-1.0), reads=[a_b], writes=[ena_b])
                P.op("pool", lambda e: e.tensor_tensor(out=qt[:], in0=qs[:], in1=ea[:], op=ALU.mult),
                     reads=[qs_b, ea_b], writes=[qt_b])
                P.op("pool", lambda e: e.tensor_tensor(out=kt[:], in0=kk[:], in1=ena[:], op=ALU.mult),
                     reads=[kk_b, ena_b], writes=[kt_b])
                for n in range(NCH):
                    cs = slice(n * HG_C, (n + 1) * HG_C)
                    last = n * HG_C + HG_C - 1
                    P.op("dve", lambda e, cs=cs, last=last: e.tensor_scalar(
                        out=kh[:, cs], in0=kt[:, cs], scalar1=ea[:, last:last + 1], scalar2=None, op0=ALU.mult),
                        reads=[kt_b, ea_b], pwrites=[kh_b])
                for b in range(NB):
                    bs = slice(b * 128, (b + 1) * 128)
                    P.op("pe", lambda e, b=b, bs=bs: e.transpose(out=ptr[:, b, :], in_=vb[:, bs], identity=identb[:]),
                         reads=[vb_b, identb_b], pwrites=[ptr_b])
                P.op("act", lambda e: e.activation(out=vtok[:], in_=ptr[:], func=AF.Copy), reads=[ptr_b], writes=[vtok_b])
                for b in range(NB):
                    bs = slice(b * 128, (b + 1) * 128)
                    P.op("pe", lambda e, b=b, bs=bs: e.transpose(out=ptr[:, b, :], in_=kh[:, bs], identity=identb[:]),
                         reads=[kh_b, identb_b], pwrites=[ptr_b])
                P.op("act", lambda e: e.activation(out=khA[:], in_=ptr[:], func=AF.Copy, scale=hmask[:, 0:1]),
                     reads=[ptr_b, hmask_b], writes=[khA_b])
                P.op("dve", lambda e: e.tensor_scalar(out=khB[:], in0=ptr[:], scalar1=hmask[:, 1:2], scalar2=None, op0=ALU.mult),
                     reads=[ptr_b, hmask_b], writes=[khB_b])
                for b in range(NB):
                    bs = slice(b * 128, (b + 1) * 128)
                    P.op("pe", lambda e, bs=bs: e.matmul(psc[:, bs], lhsT=kt[:, bs], rhs=qt[:, bs], start=True, stop=True),
                         reads=[kt_b, qt_b], pwrites=[psc_b])
                P.op("dve", lambda e: e.tensor_tensor(out=scb[:], in0=psc[:], in1=mask[:], op=ALU.mult),
                     reads=[psc_b, mask_b], writes=[scb_b])
                P.op("act", lambda e, h=h: e.activation(out=snap[:, 0, :], in_=state[:, h, :], func=AF.Copy),
                     reads=[state_b[h]], writes=[snap_b[0]])
                for n in range(NCH):
                    last = n * HG_C + HG_C - 1
                    kx, kxb = (khA, khA_b) if n % 2 == 0 else (khB, khB_b)
                    P.op("pe", lambda e, n=n, kx=kx: e.matmul(pkv[:, n % 4, :], lhsT=kx[:, n // 2, :], rhs=vtok[:, n // 2, :],
                                                             start=True, stop=True),
                         reads=[kxb, vtok_b], writes=[pkv_b[n % 4]])
                    P.op("dve", lambda e, n=n, h=h, last=last: e.scalar_tensor_tensor(
                        out=state[:, h, :], in0=state[:, h, :], scalar=ea[:, last:last + 1], in1=pkv[:, n % 4, :],
                        op0=ALU.mult, op1=ALU.add), reads=[state_b[h], ea_b, pkv_b[n % 4]], writes=[state_b[h]])
                    P.op("act", lambda e, n=n, h=h: e.activation(out=snap[:, n + 1, :], in_=state[:, h, :], func=AF.Copy),
                         reads=[state_b[h]], writes=[snap_b[n + 1]])
                for b in range(NB):
                    bs = slice(b * 128, (b + 1) * 128)
                    P.op("pe", lambda e, b=b, bs=bs: e.matmul(po[:, bs], lhsT=vtok[:, b, :], rhs=scb[:, bs],
                                                              start=True, stop=False),
                         reads=[vtok_b, scb_b], pwrites=[po_b])
                    for n in (2 * b, 2 * b + 1):
                        cs = slice(n * HG_C, (n + 1) * HG_C)
                        P.op("pe", lambda e, n=n, cs=cs, b=b: e.matmul(po[:, cs], lhsT=snap[:, n, :], rhs=qt[:, cs],
                                                                      start=False, stop=(n == 2 * b + 1)),
                             reads=[snap_b[n], qt_b], pwrites=[po_b])
                P.op("act", lambda e: e.activation(out=osq[:], in_=po[:], func=AF.Square), reads=[po_b], writes=[osq_b])
                P.op("pe", lambda e: e.matmul(psc[:], lhsT=k.ones_bf[:], rhs=osq[:], start=True, stop=True),
                     reads=[osq_b, k.ones_b, scb_b], writes=[psc_b])
                P.op("act", lambda e: e.activation(out=rs[:], in_=psc[:], func=AF.Sqrt, bias=k.eps_t[:, 0:1], scale=1.0 / 128),
                     reads=[psc_b, k.eps_b], writes=[rs_b])
                P.op("dve", lambda e: e.reciprocal(out=rs[:], in_=rs[:]), reads=[rs_b], writes=[rs_b])
                P.op("dve", lambda e: e.tensor_tensor(out=tmp[:], in0=po[:], in1=rs[:], op=ALU.mult),
                     reads=[po_b, rs_b], writes=[tmp_b])
                P.op("dve", lambda e, h=h: e.scalar_tensor_tensor(
                    out=ob[:, h, :], in0=tmp[:], scalar=k.vecs[:, gn + h:gn + h + 1], in1=gs[:],
                    op0=ALU.mult, op1=ALU.mult), reads=[tmp_b, gs_b, k.vecs_b], pwrites=[ob_b])
            for dc in range(DC):
                w_, wb_ = wso.load(wo, 0, dc * 128)
                p_, pb_ = (pq, pq_b) if nd % 2 == 0 else (pf, pf_b)
                hr, hrb = hres[nd % 2], hres_b[nd % 2]
                residual_load(k, hr, hrb, dc, tt)
                for c in range(DC):
                    P.op("pe", lambda e, p_=p_, w_=w_, c=c: e.matmul(
                        p_[:], lhsT=w_[:, c, :], rhs=ob[:, c, :], start=(c == 0), stop=(c == DC - 1)),
                        reads=[wb_, ob_b], pwrites=[pb_])
                P.op("dve", lambda e, p_=p_, hr=hr: e.tensor_tensor(out=hr[:], in0=p_[:], in1=hr[:], op=ALU.add),
                     reads=[pb_, hrb], writes=[hrb])
                residual_store(k, hr, hrb, dc, tt)
                nd += 1
    P.barrier()


ATT_SCALE = 128 ** -0.5
NEG = -1.0e30
MNEG = -30000.0
NSB = S // 128


def t5_bucket_np(d):
    d = np.maximum(np.asarray(d, np.int64), 0)
    nf = np.maximum(d, 1).astype(np.float32)
    large = 16 + (np.log(nf / np.float32(16)) / np.float32(math.log(128 / 16)) * np.float32(16)).astype(np.int32)
    large = np.minimum(large, 31)
    return np.where(d < 16, d, large).astype(np.int64)


def dsa_layout_inputs(inp):
    out = {}
    out["a_gq_b"] = np.ascontiguousarray(np.broadcast_to(inp["a_g_q"][0][None, :], (128, 512)), dtype=np.float32)
    out["a_gkv_b"] = np.ascontiguousarray(np.broadcast_to(inp["a_g_kv"][0][None, :], (128, 256)), dtype=np.float32)
    rb = np.asarray(inp["rel_bias"], np.float32)
    out["a_cvec"] = np.ascontiguousarray(np.broadcast_to(rb[31][None, :], (128, 16)), dtype=np.float32)
    sl = np.arange(128)[:, None, None]
    r = np.arange(5)[None, :, None]
    ql = np.arange(512)[None, None, :]
    bidx = t5_bucket_np(ql - sl + 128 - 128 * r)
    out["a_bt"] = np.ascontiguousarray(np.moveaxis(rb[bidx], -1, 0), dtype=np.float32)
    return out


DSA_LAYOUT_SHAPES = {"a_gq_b": [128, 512], "a_gkv_b": [128, 256], "a_cvec": [128, 16], "a_bt": [16, 128, 5, 512]}


def phase_dsa_a(k):
    nc, P = k.nc, k.P
    win = k.w["a_w_in"][0]
    with ExitStack() as st:
        def sb(name, shape, dt=F32):
            return st.enter_context(nc.sbuf_tensor("da_" + name, shape, dt))

        xn = sb("xn", [128, DC, TT], BF16); xn_b = P.buf()
        wst = [sb("wst%d" % i, [128, 4, 848]) for i in range(2)]; wst_b = P.bufs(2)
        wbf = sb("wbf", [128, DC, 848], BF16); wbf_b = P.buf()
        gq = sb("gq", [128, 512]); gkv = sb("gkv", [128, 256]); g_b = P.buf()
        identb = sb("identb", [128, 128], BF16); identb_b = P.buf()
        junk = sb("junk", [128, 512], BF16); junk_b = P.buf()
        ss = sb("ss", [128, 2]); ss_b = P.buf()
        cqn = sb("cqn", [128, 512], BF16); cqn_b = P.buf()
        ckvn = [sb("ckvn%d" % i, [128, 256], BF16) for i in range(2)]; ckvn_b = P.bufs(2)
        kix = sb("kix", [128, 128], BF16); kix_b = P.buf()
        widx = sb("widx", [128, NSB, 16]); widx_b = P.buf()
        cqT = [sb("cqT%d" % i, [128, 4, TT], BF16) for i in range(2)]; cqT_b = P.bufs(2)
        ckvT = [sb("ckvT%d" % i, [128, 2, TT], BF16) for i in range(2)]; ckvT_b = P.bufs(2)
        kixT = [sb("kixT%d" % i, [128, TT], BF16) for i in range(2)]; kixT_b = P.bufs(2)
        pA = [st.enter_context(nc.psum_tensor("da_pA%d" % i, [128, 512], F32)) for i in range(2)]; pA_b = P.pbufs(2)
        pB = [st.enter_context(nc.psum_tensor("da_pB%d" % i, [128, 512], F32)) for i in range(2)]; pB_b = P.pbufs(2)
        ptr = [st.enter_context(nc.psum_tensor("da_ptr%d" % i, [128, 8, 128], BF16)) for i in range(2)]; ptr_b = P.pbufs(2)

        P.dma("sp", lambda e: e.dma_start(out=gq[:], in_=k.lay["a_gq_b"]), pwrites=[g_b])
        P.dma("sp", lambda e: e.dma_start(out=gkv[:], in_=k.lay["a_gkv_b"]), pwrites=[g_b])
        P.op("pool", lambda e: e.tensor_copy(out=identb[:], in_=k.ident[:]), reads=[k.ident_b], writes=[identb_b])
        for i in range(4):
            w_, wb_ = wst[i % 2], wst_b[i % 2]
            P.dma("sp", lambda e, w_=w_, i=i: e.dma_start(
                out=w_[:], in_=win[i * 512:(i + 1) * 512, :].rearrange("(c p) n -> p c n", p=128)), writes=[wb_])
            P.op("pool", lambda e, w_=w_, i=i: e.tensor_copy(out=wbf[:, i * 4:(i + 1) * 4, :], in_=w_[:]),
                 reads=[wb_], pwrites=[wbf_b])
        n = 0
        for tt in range(NT):
            P.dma("sp", lambda e, tt=tt: e.dma_start(out=xn[:], in_=fm(k.xnT, 0, DC, tt * TT, TT)),
                  reads=[k.xnT_b[tt]], writes=[xn_b])
            cq_t, cq_tb = cqT[tt % 2], cqT_b[tt % 2]
            ckv_t, ckv_tb = ckvT[tt % 2], ckvT_b[tt % 2]
            kix_t, kix_tb = kixT[tt % 2], kixT_b[tt % 2]
            for sub in range(4):
                sblk = tt * 4 + sub
                ts_ = slice(sub * 128, (sub + 1) * 128)
                a_, ab_ = pA[n % 2], pA_b[n % 2]
                b_, bb_ = pB[n % 2], pB_b[n % 2]
                t_, tb_ = ptr[n % 2], ptr_b[n % 2]
                ck, ckb = ckvn[n % 2], ckvn_b[n % 2]
                for c in range(DC):
                    P.op("pe", lambda e, a_=a_, c=c, ts_=ts_: e.matmul(
                        a_[:], lhsT=xn[:, c, ts_], rhs=wbf[:, c, 0:512], start=(c == 0), stop=(c == DC - 1)),
                        reads=[xn_b, wbf_b], pwrites=[ab_])
                for c in range(DC):
                    P.op("pe", lambda e, b_=b_, c=c, ts_=ts_: e.matmul(
                        b_[:, 0:336], lhsT=xn[:, c, ts_], rhs=wbf[:, c, 512:848], start=(c == 0), stop=(c == DC - 1)),
                        reads=[xn_b, wbf_b], pwrites=[bb_])
                P.op("act", lambda e, a_=a_: e.activation(out=junk[:], in_=a_[:], func=AF.Square, accum_out=ss[:, 0:1]),
                     reads=[ab_], writes=[junk_b], pwrites=[ss_b])
                P.op("act", lambda e, b_=b_: e.activation(out=junk[:, 0:256], in_=b_[:, 0:256], func=AF.Square,
                                                          accum_out=ss[:, 1:2]),
                     reads=[bb_], writes=[junk_b], pwrites=[ss_b])
                P.op("act", lambda e: e.activation(out=ss[:, 0:1], in_=ss[:, 0:1], func=AF.Sqrt,
                                                   bias=k.eps_t[:, 0:1], scale=1.0 / 512), reads=[ss_b, k.eps_b], pwrites=[ss_b])
                P.op("act", lambda e: e.activation(out=ss[:, 1:2], in_=ss[:, 1:2], func=AF.Sqrt,
                                                   bias=k.eps_t[:, 0:1], scale=1.0 / 256), reads=[ss_b, k.eps_b], pwrites=[ss_b])
                P.op("dve", lambda e: e.reciprocal(out=ss[:], in_=ss[:]), reads=[ss_b], writes=[ss_b])
                P.op("dve", lambda e, a_=a_: e.scalar_tensor_tensor(
                    out=cqn[:], in0=a_[:], scalar=ss[:, 0:1], in1=gq[:], op0=ALU.mult, op1=ALU.mult),
                    reads=[ab_, ss_b, g_b], writes=[cqn_b])
                P.op("dve", lambda e, b_=b_, ck=ck: e.scalar_tensor_tensor(
                    out=ck[:], in0=b_[:, 0:256], scalar=ss[:, 1:2], in1=gkv[:], op0=ALU.mult, op1=ALU.mult),
                    reads=[bb_, ss_b, g_b], writes=[ckb])
                P.op("act", lambda e, b_=b_: e.activation(out=kix[:, 0:64], in_=b_[:, 256:320], func=AF.Copy),
                     reads=[bb_], pwrites=[kix_b])
                P.op("act", lambda e, b_=b_: e.activation(out=kix[:, 64:128], in_=b_[:, 256:320], func=AF.Copy),
                     reads=[bb_], pwrites=[kix_b])
                P.op("act", lambda e, b_=b_, sblk=sblk: e.activation(out=widx[:, sblk, :], in_=b_[:, 320:336], func=AF.Copy),
                     reads=[bb_], pwrites=[widx_b])
                P.dma("sp", lambda e, ck=ck, sblk=sblk: e.dma_start(
                    out=k.ckv_tok[sblk * 128:(sblk + 1) * 128, :], in_=ck[:]), reads=[ckb], pwrites=[k.dsa_b])
                for j in range(4):
                    P.op("pe", lambda e, t_=t_, j=j: e.transpose(out=t_[:, j, :], in_=cqn[:, j * 128:(j + 1) * 128],
                                                                identity=identb[:]),
                         reads=[cqn_b, identb_b], pwrites=[tb_])
                for j in range(2):
                    P.op("pe", lambda e, t_=t_, j=j, ck=ck: e.transpose(out=t_[:, 4 + j, :], in_=ck[:, j * 128:(j + 1) * 128],
                                                                       identity=identb[:]),
                         reads=[ckb, identb_b], pwrites=[tb_])
                P.op("pe", lambda e, t_=t_: e.transpose(out=t_[:, 6, :], in_=kix[:], identity=identb[:]),
                     reads=[kix_b, identb_b], pwrites=[tb_])
                P.op("act", lambda e, t_=t_, cq_t=cq_t, ts_=ts_: e.activation(out=cq_t[:, :, ts_], in_=t_[:, 0:4, :], func=AF.Copy),
                     reads=[tb_], pwrites=[cq_tb])
                P.op("dve", lambda e, t_=t_, ckv_t=ckv_t, ts_=ts_: e.tensor_copy(out=ckv_t[:, :, ts_], in_=t_[:, 4:6, :]),
                     reads=[tb_], pwrites=[ckv_tb])
                P.op("dve", lambda e, t_=t_, kix_t=kix_t, ts_=ts_: e.tensor_copy(out=kix_t[:, ts_], in_=t_[:, 6, :]),
                     reads=[tb_], pwrites=[kix_tb])
                n += 1
            c0 = tt * TT
            P.dma("sp", lambda e, cq_t=cq_t, c0=c0: e.dma_start(out=fm(k.cqT, 0, 4, c0, TT), in_=cq_t[:]),
                  reads=[cq_tb], pwrites=[k.dsa_b])
            P.dma("sp", lambda e, ckv_t=ckv_t, c0=c0: e.dma_start(out=fm(k.ckvT, 0, 2, c0, TT), in_=ckv_t[:]),
                  reads=[ckv_tb], pwrites=[k.dsa_b])
            P.dma("sp", lambda e, kix_t=kix_t, c0=c0: e.dma_start(out=k.kidxT[:, c0:c0 + TT], in_=kix_t[:]),
                  reads=[kix_tb], pwrites=[k.dsa_b])
        P.dma("sp", lambda e: e.dma_start(out=k.widx, in_=widx[:]), reads=[widx_b], pwrites=[k.dsa_b])
    P.barrier()


def phase_dsa_b(k):
    nc, P = k.nc, k.P
    wq = k.w["a_w_qidx"][0]
    import os
    NQB = int(os.environ.get("DSA_NQB", str(NSB)))
    with ExitStack() as st:
        def sb(name, shape, dt=F32):
            return st.enter_context(nc.sbuf_tensor("db_" + name, shape, dt))

        kixT = sb("kixT", [128, S], BF16); kixT_b = P.buf()
        hmask = sb("hmask", [128, 2]); hmask_b = P.buf()
        widx = sb("widx", [128, NSB, 16]); widx_b = P.buf()
        wst = sb("wst", [128, 4, 1024]); wst_b = P.buf()
        wqb = sb("wqb", [128, 4, 1024], BF16); wqb_b = P.buf()
        identb = sb("identb", [128, 128], BF16); identb_b = P.buf()
        cm = sb("cm", [128, 128]); cm30 = sb("cm30", [128, 128], BF16); cm_b = P.buf()
        neg30 = sb("neg30", [128, 3, 128], BF16); neg30_b = P.buf()
        cq = [sb("cq%d" % i, [128, 4, 128], BF16) for i in range(2)]; cq_b = P.bufs(2)
        qixA = [sb("qixA%d" % i, [128, 8, 128], BF16) for i in range(2)]; qixA_b = P.bufs(2)
        qixB = [sb("qixB%d" % i, [128, 8, 128], BF16) for i in range(2)]; qixB_b = P.bufs(2)
        rl = [sb("rl%d" % i, [128, 512]) for i in range(4)]; rl_b = P.bufs(4)
        acc = [sb("acc%d" % i, [128, S]) for i in range(2)]; acc_b = P.bufs(2)
        m8 = sb("m8", [128, 8]); m8_b = P.buf()
        mq = [sb("mq%d" % i, [128, S], BF16) for i in range(2)]; mq_b = P.bufs(2)
        mT = [sb("mT%d" % i, [128, NSB, 128], BF16) for i in range(2)]; mT_b = P.bufs(2)
        pqi = [st.enter_context(nc.psum_tensor("db_pqi%d" % i, [128, 4, 128], F32)) for i in range(2)]; pqi_b = P.pbufs(2)
        ps = [st.enter_context(nc.psum_tensor("db_ps%d" % i, [128, 512], F32)) for i in range(4)]; ps_b = P.pbufs(4)
        ptr = [st.enter_context(nc.psum_tensor("db_ptr%d" % i, [128, 8, 128], BF16)) for i in range(2)]; ptr_b = P.pbufs(2)

        P.dma("sp", lambda e: e.dma_start(out=kixT[:], in_=k.kidxT), reads=[k.dsa_b], writes=[kixT_b])
        P.dma("sp", lambda e: e.dma_start(out=widx[:], in_=k.widx), reads=[k.dsa_b], writes=[widx_b])
        P.dma("sp", lambda e: e.dma_start(out=hmask[:], in_=k.consts["half_mask"]), writes=[hmask_b])
        P.dma("sp", lambda e: e.dma_start(out=wst[:], in_=wq.rearrange("(c p) n -> p c n", p=128)), writes=[wst_b])
        P.op("pool", lambda e: e.tensor_copy(out=wqb[:], in_=wst[:]), reads=[wst_b], writes=[wqb_b])
        P.op("pool", lambda e: e.tensor_copy(out=identb[:], in_=k.ident[:]), reads=[k.ident_b], writes=[identb_b])
        P.dma("sp", lambda e: e.dma_start(out=cm[:], in_=k.consts["dsa_cm"]), pwrites=[cm_b])
        P.op("pool", lambda e: e.memset(neg30[:], MNEG), writes=[neg30_b])
        P.op("dve", lambda e: e.tensor_scalar(out=cm30[:], in0=cm[:], scalar1=-1.0, scalar2=MNEG,
                                              op0=ALU.is_lt, op1=ALU.mult), reads=[cm_b], pwrites=[cm_b])
        npe = 0
        ntr = 0
        for qb in range(NQB):
            Lq = (qb + 1) * 128
            c_, cb_ = cq[qb % 2], cq_b[qb % 2]
            qxA, qxAb = qixA[qb % 2], qixA_b[qb % 2]
            qxB, qxBb = qixB[qb % 2], qixB_b[qb % 2]
            ac, acb = acc[qb % 2], acc_b[qb % 2]
            m_, mb_ = mq[qb % 2], mq_b[qb % 2]
            mt, mtb = mT[qb % 2], mT_b[qb % 2]
            P.dma("sp", lambda e, c_=c_, qb=qb: e.dma_start(out=c_[:], in_=fm(k.cqT, 0, 4, qb * 128, 128)),
                  reads=[k.dsa_b], writes=[cb_])
            if qb >= 2:
                for hg in range(2):
                    pq_, pqb = pqi[hg % 2], pqi_b[hg % 2]
                    for hh in range(4):
                        hp = hg * 4 + hh
                        for c in range(4):
                            P.op("pe", lambda e, pq_=pq_, hh=hh, hp=hp, c=c, c_=c_: e.matmul(
                                pq_[:, hh, :], lhsT=wqb[:, c, hp * 128:(hp + 1) * 128], rhs=c_[:, c, :],
                                start=(c == 0), stop=(c == 3)), reads=[wqb_b, cb_], pwrites=[pqb])
                    P.op("act", lambda e, pq_=pq_, qxA=qxA, hg=hg: e.activation(
                        out=qxA[:, hg * 4:(hg + 1) * 4, :], in_=pq_[:], func=AF.Copy, scale=hmask[:, 0:1]),
                        reads=[pqb, hmask_b], pwrites=[qxAb])
                    P.op("dve", lambda e, pq_=pq_, qxB=qxB, hg=hg: e.tensor_scalar(
                        out=qxB[:, hg * 4:(hg + 1) * 4, :], in0=pq_[:], scalar1=hmask[:, 1:2], scalar2=None, op0=ALU.mult),
                        reads=[pqb, hmask_b], pwrites=[qxBb])
                nkt = (Lq + 511) // 512
                for kt in range(nkt):
                    wd = min(512, Lq - kt * 512)
                    ks = slice(kt * 512, kt * 512 + wd)
                    for h in range(16):
                        p_, pb_ = ps[npe % 4], ps_b[npe % 4]
                        r_, rb_ = rl[npe % 4], rl_b[npe % 4]
                        npe += 1
                        qx, qxb = (qxA, qxAb) if h % 2 == 0 else (qxB, qxBb)
                        P.op("pe", lambda e, p_=p_, qx=qx, h=h, ks=ks, wd=wd: e.matmul(
                            p_[:, 0:wd], lhsT=qx[:, h // 2, :], rhs=kixT[:, ks], start=True, stop=True),
                            reads=[qxb, kixT_b], writes=[pb_])
                        P.op("act", lambda e, p_=p_, r_=r_, wd=wd: e.activation(out=r_[:, 0:wd], in_=p_[:, 0:wd], func=AF.Relu),
                             reads=[pb_], writes=[rb_])
                        if h == 0:
                            P.op("dve", lambda e, r_=r_, ac=ac, ks=ks, wd=wd, qb=qb: e.tensor_scalar(
                                out=ac[:, ks], in0=r_[:, 0:wd], scalar1=widx[:, qb, 0:1], scalar2=None, op0=ALU.mult),
                                reads=[rb_, widx_b], pwrites=[acb])
                        else:
                            P.op("dve", lambda e, r_=r_, ac=ac, ks=ks, wd=wd, qb=qb, h=h: e.scalar_tensor_tensor(
                                out=ac[:, ks], in0=r_[:, 0:wd], scalar=widx[:, qb, h:h + 1], in1=ac[:, ks],
                                op0=ALU.mult, op1=ALU.add), reads=[rb_, widx_b, acb], pwrites=[acb])
                dg = slice(Lq - 128, Lq)
                P.op("dve", lambda e, ac=ac, dg=dg: e.tensor_tensor(out=ac[:, dg], in0=ac[:, dg], in1=cm[:], op=ALU.add),
                     reads=[acb, cm_b], writes=[acb])
                for rnd in range(32):
                    P.op("dve", lambda e, ac=ac, Lq=Lq: e.max(out=m8[:], in_=ac[:, 0:Lq]), reads=[acb], writes=[m8_b])
                    P.op("dve", lambda e, ac=ac, Lq=Lq: e.match_replace(
                        out=ac[:, 0:Lq], in_to_replace=m8[:], in_values=ac[:, 0:Lq], imm_value=NEG),
                        reads=[acb, m8_b], writes=[acb])
                P.op("dve", lambda e, ac=ac, m_=m_, Lq=Lq: e.tensor_scalar(
                    out=m_[:, 0:Lq], in0=ac[:, 0:Lq], scalar1=-5.0e29, scalar2=MNEG, op0=ALU.is_gt, op1=ALU.mult),
                    reads=[acb], writes=[mb_])
                P.op("pool", lambda e, m_=m_, dg=dg: e.tensor_tensor(out=m_[:, dg], in0=m_[:, dg], in1=cm30[:], op=ALU.add),
                     reads=[mb_, cm_b], writes=[mb_])
            else:
                P.op("pool", lambda e, m_=m_, Lq=Lq: e.memset(m_[:, 0:Lq], 0.0), writes=[mb_])
                dg = slice(Lq - 128, Lq)
                P.op("pool", lambda e, m_=m_, dg=dg: e.tensor_copy(out=m_[:, dg], in_=cm30[:]), reads=[mb_, cm_b], writes=[mb_])
            for b0 in range(0, qb + 1, 8):
                nb = min(8, qb + 1 - b0)
                t_, tb_ = ptr[ntr % 2], ptr_b[ntr % 2]
                ntr += 1
                for j in range(nb):
                    P.op("pe", lambda e, t_=t_, j=j, m_=m_, b0=b0: e.transpose(
                        out=t_[:, j, :], in_=m_[:, (b0 + j) * 128:(b0 + j + 1) * 128], identity=identb[:]),
                        reads=[mb_, identb_b], pwrites=[tb_])
                P.op("act", lambda e, t_=t_, mt=mt, b0=b0, nb=nb: e.activation(
                    out=mt[:, b0:b0 + nb, :], in_=t_[:, 0:nb, :], func=AF.Copy), reads=[tb_], pwrites=[mtb])
            P.dma("sp", lambda e, mt=mt, qb=qb: e.dma_start(
                out=k.maskT[:, 0:qb + 1, qb * 128:(qb + 1) * 128], in_=mt[:, 0:qb + 1, :]),
                reads=[mtb], pwrites=[k.mask_b])
            nfill = 3 - (qb % 4)
            if nfill > 0:
                P.dma("sp", lambda e, qb=qb, nfill=nfill: e.dma_start(
                    out=k.maskT[:, qb + 1:qb + 1 + nfill, qb * 128:(qb + 1) * 128], in_=neg30[:, 0:nfill, :]),
                    reads=[neg30_b], pwrites=[k.mask_b])
    P.barrier()


def phase_dsa_c(k):
    nc, P = k.nc, k.P
    wuq = k.w["a_w_uq"][0]
    wuk = k.w["a_w_uk"][0]
    wuv = k.w["a_w_uv"][0]
    wo = k.w["a_w_o"][0]
    import os
    NG = int(os.environ.get("DSA_NG", str(NT)))
    with ExitStack() as st:
        def sb(name, shape, dt=F32):
            return st.enter_context(nc.sbuf_tensor("dc_" + name, shape, dt))

        ckvT = sb("ckvT", [128, 2, S], BF16); ckvT_b = P.buf()
        ckvk = sb("ckvk", [128, NSB, 256], BF16); ckvk_b = P.buf()
        wst = [sb("wst%d" % i, [128, 2048]) for i in range(2)]; wst_b = P.bufs(2)
        wuqb = sb("wuqb", [128, 4, 2048], BF16); wuqb_b = P.buf()
        wukb = sb("wukb", [128, 16, 256], BF16); wukb_b = P.buf()
        wuvb = sb("wuvb", [128, 16, 2, 128], BF16); wuvb_b = P.buf()
        cvec = sb("cvec", [128, 16]); cvec_b = P.buf()
        cq = sb("cq", [128, 4, TT], BF16); cq_b = P.buf()
        mk = sb("mk", [128, NSB, TT], BF16); mk_b = P.buf()
        bt = [sb("bt0", [128, 5, TT])] * 2; bt_b = [P.buf()] * 2
        qT = sb("qT", [128, TT], BF16); qT_b = P.buf()
        ql = [sb("ql%d" % i, [128, 2, TT], BF16) for i in range(2)]; ql_b = P.bufs(2)
        pT = [sb("pT%d" % i, [128, TT], BF16) for i in range(2)]; pT_b = P.bufs(2)
        rden = sb("rden", [128, TT]); rden_b = P.buf()
        oln = sb("oln", [128, 2, TT], BF16); oln_b = P.buf()
        oT = sb("oT", [128, 16, TT], BF16); oT_b = P.buf()
        wso = WStream(k, st, "dc_wo", DC, 128, nbuf=2)
        hres = [sb("hr%d" % i, [128, TT]) for i in range(2)]; hres_b = P.bufs(2)
        pm = [st.enter_context(nc.psum_tensor("dc_pm%d" % i, [128, TT], F32)) for i in range(2)]; pm_b = P.pbufs(2)
        pl = [st.enter_context(nc.psum_tensor("dc_pl%d" % i, [128, TT], F32)) for i in range(2)]; pl_b = P.pbufs(2)
        po = [st.enter_context(nc.psum_tensor("dc_po%d" % i, [128, TT], F32)) for i in range(2)]; po_b = P.pbufs(2)
        pden = st.enter_context(nc.psum_tensor("dc_pden", [128, TT], F32)); pden_b = P.pbuf()

        P.dma("sp", lambda e: e.dma_start(out=ckvT[:], in_=fm(k.ckvT, 0, 2, 0, S)), reads=[k.dsa_b], writes=[ckvT_b])
        P.dma("sp", lambda e: e.dma_start(out=ckvk[:], in_=k.ckv_tok.rearrange("(b p) c -> p b c", p=128)),
              reads=[k.dsa_b], writes=[ckvk_b])
        P.dma("sp", lambda e: e.dma_start(out=cvec[:], in_=k.lay["a_cvec"]), writes=[cvec_b])
        nw = 0
        for c in range(4):
            w_, wb_ = wst[nw % 2], wst_b[nw % 2]; nw += 1
            P.dma("sp", lambda e, w_=w_, c=c: e.dma_start(out=w_[:], in_=wuq[c * 128:(c + 1) * 128, :]), writes=[wb_])
            P.op("pool", lambda e, w_=w_, c=c: e.tensor_copy(out=wuqb[:, c, :], in_=w_[:]), reads=[wb_], pwrites=[wuqb_b])
        for hg in range(2):
            w_, wb_ = wst[nw % 2], wst_b[nw % 2]; nw += 1
            P.dma("sp", lambda e, w_=w_, hg=hg: e.dma_start(
                out=w_[:].rearrange("p (h c) -> p h c", h=8), in_=wuk[hg * 8:(hg + 1) * 8].rearrange("h d c -> d h c")),
                writes=[wb_])
            P.op("pool", lambda e, w_=w_, hg=hg: e.tensor_copy(
                out=wukb[:, hg * 8:(hg + 1) * 8, :], in_=w_[:].rearrange("p (h c) -> p h c", h=8)),
                reads=[wb_], pwrites=[wukb_b])
        for hg in range(2):
            w_, wb_ = wst[nw % 2], wst_b[nw % 2]; nw += 1
            P.dma("sp", lambda e, w_=w_, hg=hg: e.dma_start(
                out=w_[:].rearrange("p (h a d) -> p h a d", h=8, a=2),
                in_=wuv[hg * 8:(hg + 1) * 8].rearrange("h (a p) d -> p h a d", p=128)), writes=[wb_])
            P.op("pool", lambda e, w_=w_, hg=hg: e.tensor_copy(
                out=wuvb[:, hg * 8:(hg + 1) * 8, :, :], in_=w_[:].rearrange("p (h a d) -> p h a d", h=8, a=2)),
                reads=[wb_], pwrites=[wuvb_b])
        nd = 0
        identb = sb("identb", [128, 128], BF16); identb_b = P.buf()
        btb = [sb("btb%d" % i, [128, 5, TT], BF16) for i in range(2)]; btb_b = P.bufs(2)
        P.op("pool", lambda e: e.tensor_copy(out=identb[:], in_=k.ident[:]), reads=[k.ident_b], writes=[identb_b])
        for g in range(NG):
            q0 = g * TT
            nsb = 4 * g + 4
            P.dma("sp", lambda e, q0=q0: e.dma_start(out=cq[:], in_=fm(k.cqT, 0, 4, q0, TT)), reads=[k.dsa_b], writes=[cq_b])
            P.dma("sp", lambda e, q0=q0, nsb=nsb: e.dma_start(out=mk[:, 0:nsb, :], in_=k.maskT[:, 0:nsb, q0:q0 + TT]),
                  reads=[k.mask_b], writes=[mk_b])
            units = [(h, sbk) for h in range(16) for sbk in range(nsb)]

            def stage_a(u, g=g, nsb=nsb):
                h, sbk = units[u]
                q_, qb_ = ql[h % 2], ql_b[h % 2]
                b_, bb_ = bt[h % 2], bt_b[h % 2]
                bb16, bb16_b = btb[h % 2], btb_b[h % 2]
                if sbk == 0:
                    P.dma("sp", lambda e: e.dma_start(out=b_[:], in_=k.lay["a_bt"][h]), writes=[bb_])
                    P.op("pool", lambda e: e.tensor_copy(out=bb16[:], in_=b_[:]), reads=[bb_], writes=[bb16_b])
                    for c in range(4):
                        P.op("pe", lambda e, c=c: e.matmul(
                            pm[0][:], lhsT=wuqb[:, c, h * 128:(h + 1) * 128], rhs=cq[:, c, :], start=(c == 0), stop=(c == 3)),
                            reads=[wuqb_b, cq_b], pwrites=[pm_b[0]])
                    P.op("act", lambda e: e.activation(out=qT[:], in_=pm[0][:], func=AF.Copy), reads=[pm_b[0]], writes=[qT_b])
                    for cc in range(2):
                        P.op("pe", lambda e, cc=cc: e.matmul(
                            pm[1][:], lhsT=wukb[:, h, cc * 128:(cc + 1) * 128], rhs=qT[:], start=True, stop=True),
                            reads=[wukb_b, qT_b], writes=[pm_b[1]])
                        P.op("act", lambda e, cc=cc: e.activation(out=q_[:, cc, :], in_=pm[1][:], func=AF.Copy, scale=ATT_SCALE),
                             reads=[pm_b[1]], pwrites=[qb_])
                ss_ = slice(sbk * 128, (sbk + 1) * 128)
                l_, lb_ = pl[u % 2], pl_b[u % 2]
                r = sbk - (4 * g - 1)
                for cc in range(2):
                    P.op("pe", lambda e, cc=cc: e.matmul(
                        l_[:], lhsT=ckvT[:, cc, ss_], rhs=q_[:, cc, :], start=(cc == 0), stop=False),
                        reads=[ckvT_b, qb_], pwrites=[lb_])
                P.op("pe", lambda e: e.matmul(l_[:], lhsT=identb[:], rhs=mk[:, sbk, :], start=False, stop=(r < 0)),
                     reads=[identb_b, mk_b], pwrites=[lb_])
                if r >= 0:
                    P.op("pe", lambda e: e.matmul(l_[:], lhsT=identb[:], rhs=bb16[:, r, :], start=False, stop=True),
                         reads=[identb_b, bb16_b], pwrites=[lb_])

            def stage_b(u, g=g):
                h, sbk = units[u]
                l_, lb_ = pl[u % 2], pl_b[u % 2]
                p_, pb_ = pT[u % 2], pT_b[u % 2]
                r = sbk - (4 * g - 1)
                if r >= 0:
                    P.op("act", lambda e: e.activation(out=p_[:], in_=l_[:], func=AF.Exp), reads=[lb_], writes=[pb_])
                else:
                    P.op("act", lambda e: e.activation(out=p_[:], in_=l_[:], func=AF.Exp, bias=cvec[:, h:h + 1]),
                         reads=[lb_, cvec_b], writes=[pb_])

            def stage_c(u, nsb=nsb):
                h, sbk = units[u]
                p_, pb_ = pT[u % 2], pT_b[u % 2]
                for cc in range(2):
                    P.op("pe", lambda e, cc=cc: e.matmul(
                        po[cc][:], lhsT=ckvk[:, sbk, cc * 128:(cc + 1) * 128], rhs=p_[:],
                        start=(sbk == 0), stop=(sbk == nsb - 1)), reads=[ckvk_b, pb_], pwrites=[po_b[cc]])
                P.op("pe", lambda e: e.matmul(
                    pden[:], lhsT=k.ones_bf[:], rhs=p_[:], start=(sbk == 0), stop=(sbk == nsb - 1)),
                    reads=[k.ones_b, pb_], pwrites=[pden_b])
                if sbk == nsb - 1:
                    P.op("dve", lambda e: e.reciprocal(out=rden[:], in_=pden[:]), reads=[pden_b], writes=[rden_b])
                    for cc in range(2):
                        P.op("dve", lambda e, cc=cc: e.tensor_tensor(out=oln[:, cc, :], in0=po[cc][:], in1=rden[:], op=ALU.mult),
                             reads=[po_b[cc], rden_b], pwrites=[oln_b])
                    for cc in range(2):
                        P.op("pe", lambda e, cc=cc: e.matmul(
                            pm[0][:], lhsT=wuvb[:, h, cc, :], rhs=oln[:, cc, :], start=(cc == 0), stop=(cc == 1)),
                            reads=[wuvb_b, oln_b], pwrites=[pm_b[0]])
                    P.op("act", lambda e: e.activation(out=oT[:, h, :], in_=pm[0][:], func=AF.Copy),
                         reads=[pm_b[0]], pwrites=[oT_b])

            nun = len(units)
            stage_a(0)
            stage_b(0)
            for u in range(nun):
                if u + 1 < nun:
                    stage_a(u + 1)
                    stage_b(u + 1)
                stage_c(u)
            for dc in range(DC):
                w_, wb_ = wso.load(wo, 0, dc * 128)
                p_, pb_ = pm[nd % 2], pm_b[nd % 2]
                hr, hrb = hres[nd % 2], hres_b[nd % 2]
                residual_load(k, hr, hrb, dc, g)
                for c in range(DC):
                    P.op("pe", lambda e, p_=p_, w_=w_, c=c: e.matmul(
                        p_[:], lhsT=w_[:, c, :], rhs=oT[:, c, :], start=(c == 0), stop=(c == DC - 1)),
                        reads=[wb_, oT_b], pwrites=[pb_])
                P.op("dve", lambda e, p_=p_, hr=hr: e.tensor_tensor(out=hr[:], in0=p_[:], in1=hr[:], op=ALU.add),
                     reads=[pb_, hrb], writes=[hrb])
                residual_store(k, hr, hrb, dc, g)
                nd += 1
    P.barrier()


WEIGHT_SHAPES = {
    "a_w_in": [1, 2048, 848], "a_w_uq": [1, 512, 2048], "a_w_qidx": [1, 512, 1024],
    "a_w_uk": [1, 16, 128, 256], "a_w_uv": [1, 16, 256, 128], "a_w_o": [1, 2048, 2048],
    "b_w_in": [1, 2048, 8192], "b_w_o": [1, 2048, 2048],
    "c_w_group": [1, 4, 512, 512],
    "d_w_pw1": [1, 2048, 4096], "d_w_pw2": [1, 2048, 2048],
    "ffn_w_up": [4, 2048, 11264], "ffn_w_down": [4, 5632, 2048],
}

PLAN_FULL = ["in"] + sum([["norm_mix%d" % i, "mix%d" % i, "norm_ffn%d" % i, "ffn%d" % i] for i in range(DEPTH)], []) + ["out"]


def plan_weights(plan):
    ws = set()
    for p in plan:
        if p.startswith("ffn"):
            ws.update(["ffn_w_up", "ffn_w_down"])
        if p == "mix0":
            ws.update(["a_w_in", "a_w_uq", "a_w_qidx", "a_w_uk", "a_w_uv", "a_w_o"])
        if p == "mix1":
            ws.update(["b_w_in", "b_w_o"])
        if p == "mix2":
            ws.update(["c_w_group"])
        if p == "mix3":
            ws.update(["d_w_pw1", "d_w_pw2"])
    return sorted(ws)


def build_nc(plan=PLAN_FULL, raw_out=False):
    nc = bass.Bass("TRN2", target_bir_lowering=False)
    k = K()
    k.nc = nc
    k.raw_out = raw_out
    k.R = vec_registry()
    k.x = nc.dram_tensor("x", [S, D], F32, kind="ExternalInput").ap()
    k.out = nc.dram_tensor("out", [S, D], F32, kind="ExternalOutput").ap()
    vecs_d = nc.dram_tensor("vecs", [128, k.R.n], F32, kind="ExternalInput").ap()
    ident_d = nc.dram_tensor("ident", [128, 128], F32, kind="ExternalInput").ap()
    k.consts = {}
    for name, arr in make_consts().items():
        k.consts[name] = nc.dram_tensor("c_" + name, list(arr.shape), F32, kind="ExternalInput").ap()
    k.w = {}
    for name in plan_weights(plan):
        k.w[name] = nc.dram_tensor(name, WEIGHT_SHAPES[name], F32, kind="ExternalInput").ap()
    k.lay = {}
    if "mix0" in plan:
        for name, shp in DSA_LAYOUT_SHAPES.items():
            k.lay[name] = nc.dram_tensor(name, shp, F32, kind="ExternalInput").ap()
        k.cqT = nc.dram_tensor("s_cqT", [512, S], BF16).ap()
        k.ckvT = nc.dram_tensor("s_ckvT", [256, S], BF16).ap()
        k.ckv_tok = nc.dram_tensor("s_ckvtok", [S, 256], BF16).ap()
        k.kidxT = nc.dram_tensor("s_kidxT", [128, S], BF16).ap()
        k.widx = nc.dram_tensor("s_widx", [128, NSB, 16], F32).ap()
        k.maskT = nc.dram_tensor("s_maskT", [128, NSB, S], BF16).ap()
    k.gT = nc.dram_tensor("s_gT", [DFF, S], BF16).ap()
    k.hT = nc.dram_tensor("hT", [D, S], F32).ap()
    k.xnT = nc.dram_tensor("xnT", [D, S], BF16).ap()
    with ExitStack() as st:
        P = Prog(nc, st)
        k.P = P
        k.hT_b = P.bufs(NT)
        k.xnT_b = P.bufs(NT)
        k.out_b = P.buf()
        k.dsa_b = P.buf()
        k.gT_b = P.buf()
        k.mask_b = P.buf()
        k.vecs = st.enter_context(nc.sbuf_tensor("vecs_t", [128, k.R.n], F32))
        k.vecs_b = P.buf()
        k.ident = st.enter_context(nc.sbuf_tensor("ident_t", [128, 128], F32))
        k.ident_b = P.buf()
        k.ones_bf = st.enter_context(nc.sbuf_tensor("ones_bf", [128, 128], BF16))
        k.ones_b = P.buf()
        k.eps_t = st.enter_context(nc.sbuf_tensor("eps_t", [128, 1], F32))
        k.eps_b = P.buf()
        P.dma("sp", lambda e: e.dma_start(out=k.vecs[:], in_=vecs_d), writes=[k.vecs_b])
        P.dma("sp", lambda e: e.dma_start(out=k.ident[:], in_=ident_d), writes=[k.ident_b])
        P.op("pool", lambda e: e.memset(k.ones_bf[:], 1.0), writes=[k.ones_b])
        P.op("pool", lambda e: e.memset(k.eps_t[:], EPS), writes=[k.eps_b])
        k.nc = NCProxy(nc)
        for p in plan:
            k.nc.tag += 1
            if p == "in":
                phase_in(k)
            elif p == "out":
                phase_out(k)
            elif p.startswith("norm_"):
                phase_norm(k, p)
            elif p.startswith("ffn"):
                phase_ffn(k, int(p[3:]))
            elif p == "mix0":
                phase_dsa_a(k)
                phase_dsa_b(k)
                phase_dsa_c(k)
            elif p == "mix1":
                phase_mix_hgrn(k, 1)
            elif p == "mix2":
                phase_mix_pool(k)
            elif p == "mix3":
                phase_mix_conf(k)
            else:
                raise NotImplementedError(p)
        P.barrier()
        P.emit()
    return nc


def make_consts():
    c = {}
    invc = np.zeros((128, 64), np.float32)
    for g, w in enumerate((2, 4, 8, 16)):
        for t in range(16):
            invc[:, g * 16 + t] = 1.0 / min(t + 1, w)
    c["pool_invc"] = invc
    blk = np.zeros((128, 128), np.float32)
    blk[0:64, 0:64] = np.triu(np.ones((64, 64), np.float32))
    blk[64:128, 64:128] = np.triu(np.ones((64, 64), np.float32))
    c["hg_mask"] = np.ascontiguousarray(np.tile(blk, (1, TT // 128)))
    hmk = np.zeros((128, 2), np.float32)
    hmk[0:64, 0] = 1.0
    hmk[64:128, 1] = 1.0
    c["half_mask"] = hmk
    cm = np.zeros((128, 128), np.float32)
    cm[np.triu_indices(128, 1)] = -1.0e30
    c["dsa_cm"] = cm
    return c


def make_in_maps(inp, plan, n_cores=8, xs=None):
    vecs = pack_vecs(inp)
    consts = make_consts()
    ident = np.eye(128, dtype=np.float32)
    wnames = plan_weights(plan)
    lay = dsa_layout_inputs(inp) if "mix0" in plan else {}
    maps = []
    for c in range(n_cores):
        m = {"x": np.ascontiguousarray(inp["x"][c] if xs is None else xs[c]), "vecs": vecs, "ident": ident}
        for w in wnames:
            m[w] = np.ascontiguousarray(inp[w], dtype=np.float32)
        for cn, arr in consts.items():
            m["c_" + cn] = arr
        m.update(lay)
        maps.append(m)
    return maps


def kernel(**inputs):
    inp = {k_: np.asarray(v) for k_, v in inputs.items()}
    nc = build_nc(PLAN_FULL)
    maps = make_in_maps(inp, PLAN_FULL, 8)
    res = run_bass_kernel_spmd(nc, maps, core_ids=list(range(8)))
    return np.stack([np.asarray(r["out"]) for r in res.results], axis=0).astype(np.float32)
```
